# Optimizing a Trainium2 kernel written in Bass

```python
import math
import jax, jax.numpy as jnp
from jax import lax
import numpy as np

D_MODEL = 1024
BATCH = 4
SEQ = 4096
DEPTH = 1

FOURIER_WIDTH = D_MODEL // 2
FOURIER_GROUPS = 4
FOURIER_GROUP_CH = FOURIER_WIDTH // FOURIER_GROUPS
SSM_WIDTH = D_MODEL // 2
SSM_GROUP_CH = 16
SSM_GROUPS = SSM_WIDTH // SSM_GROUP_CH
SSM_STATE = 64
DT_MIN = 1e-3
DT_MAX = 1e-1
N_BRANCHES = 2
IN_WIDTH = FOURIER_WIDTH + SSM_WIDTH + N_BRANCHES * D_MODEL
MOE_GROUPS = 8
EXPERTS_PER_GROUP = 8
N_EXPERTS = MOE_GROUPS * EXPERTS_PER_GROUP
MOE_TOP_K = 2
D_EXPERT = D_MODEL // 2
MOE_BLOCK = 128
RMS_EPS = 1e-6

kernel_name = "hybrid_fnet_s5_hiermoe_encoder_block"


def _rmsnorm(x, g):
    xf = x.astype(jnp.float32)
    inv = lax.rsqrt(jnp.mean(xf * xf, axis=-1, keepdims=True) + RMS_EPS)
    return (xf * inv * g.astype(jnp.float32)).astype(x.dtype)


def _fourier_mix(u):
    b, l, _ = u.shape
    ug = u.astype(jnp.float32).reshape(b, l, FOURIER_GROUPS, FOURIER_GROUP_CH)
    f = jnp.fft.fft2(ug, axes=(1, 3), norm='ortho').real
    return f.reshape(b, l, FOURIER_WIDTH).astype(u.dtype)


def _scan_combine(e1, e2):
    a1, b1 = e1
    a2, b2 = e2
    return a1 * a2, a2 * b1 + b2


def _ssm_direction(ug, a_re, a_im, log_dt, b_re, b_im, c_re, c_im, reverse):
    lam = lax.complex(a_re.astype(jnp.float32), a_im.astype(jnp.float32))
    dt = jnp.exp(log_dt.astype(jnp.float32))[:, None]
    a_bar = jnp.exp(lam * dt)
    b_mat = lax.complex(b_re.astype(jnp.float32), b_im.astype(jnp.float32))
    b_bar = ((a_bar - 1.0) / lam)[..., None] * b_mat
    bu = jnp.einsum('blgc,gnc->blgn', ug.astype(jnp.complex64), b_bar)
    a = jnp.broadcast_to(a_bar, bu.shape)
    _, h = lax.associative_scan(_scan_combine, (a, bu), axis=1, reverse=reverse)
    c_mat = lax.complex(c_re.astype(jnp.float32), c_im.astype(jnp.float32))
    return jnp.einsum('blgn,gcn->blgc', h, c_mat).real


def _ssm_mix(u, a_re, a_im, log_dt, b_re, b_im, c_re, c_im, d_skip, w_glu):
    b, l, _ = u.shape
    uf = u.astype(jnp.float32)
    ug = uf.reshape(b, l, SSM_GROUPS, SSM_GROUP_CH)
    y = (_ssm_direction(ug, a_re[0], a_im[0], log_dt[0], b_re[0], b_im[0], c_re[0], c_im[0], False)
         + _ssm_direction(ug, a_re[1], a_im[1], log_dt[1], b_re[1], b_im[1], c_re[1], c_im[1], True))
    y = y.reshape(b, l, SSM_WIDTH) + d_skip.astype(jnp.float32) * uf
    y = jax.nn.gelu(y)
    y = y * jax.nn.sigmoid(y @ w_glu.astype(jnp.float32))
    return y.astype(u.dtype)


def _hier_moe(h, wg, bg, we, be, w_gate, w_up, w_down):
    bsz, l, d = h.shape
    t = bsz * l
    ht = h.reshape(t, d)
    g_prob = jax.nn.softmax((ht @ wg).astype(jnp.float32) + bg.astype(jnp.float32), axis=-1)
    p_g, g_idx = lax.top_k(g_prob, 1)
    e_logits = ((ht @ we).astype(jnp.float32) + be.astype(jnp.float32)).reshape(t, MOE_GROUPS, EXPERTS_PER_GROUP)
    e_sel = jnp.take_along_axis(e_logits, g_idx[:, :, None], axis=1)[:, 0]
    top_l, top_i = lax.top_k(e_sel, MOE_TOP_K)
    gate_w = p_g * jax.nn.softmax(top_l, axis=-1)
    expert = g_idx * EXPERTS_PER_GROUP + top_i

    n_assign = t * MOE_TOP_K
    e_flat = expert.reshape(n_assign)
    w_flat = gate_w.reshape(n_assign)
    tok_flat = jnp.repeat(jnp.arange(t, dtype=jnp.int32), MOE_TOP_K)
    order = jnp.argsort(e_flat)
    e_s = e_flat[order]
    tok_s = tok_flat[order]
    w_s = w_flat[order]
    counts = jnp.bincount(e_flat, length=N_EXPERTS)
    starts = jnp.cumsum(counts) - counts
    padded = ((counts + MOE_BLOCK - 1) // MOE_BLOCK) * MOE_BLOCK
    pstarts = jnp.cumsum(padded) - padded
    pends = pstarts + padded
    dest = pstarts[e_s] + jnp.arange(n_assign, dtype=jnp.int32) - starts[e_s]
    n_blocks = -(-n_assign // MOE_BLOCK) + N_EXPERTS
    rows = n_blocks * MOE_BLOCK
    x_pad = jnp.zeros((rows, d), h.dtype).at[dest].set(ht[tok_s])
    block_start = jnp.arange(n_blocks, dtype=jnp.int32) * MOE_BLOCK
    block_e = jnp.minimum(jnp.searchsorted(pends, block_start, side='right'), N_EXPERTS - 1)

    def expert_block(args):
        xb, e = args
        a = jax.nn.silu(xb @ w_gate[e]) * (xb @ w_up[e])
        return a @ w_down[e]

    y_pad = lax.map(expert_block, (x_pad.reshape(n_blocks, MOE_BLOCK, d), block_e)).reshape(rows, d)
    y = y_pad[dest] * w_s[:, None].astype(h.dtype)
    out = jax.ops.segment_sum(y, tok_s, num_segments=t)
    return out.reshape(bsz, l, d)


def setup_inputs(seed: int = 0) -> dict:
    key = jax.random.key(seed)
    ks = jax.random.split(key, 24)
    f32 = jnp.float32

    def nrm(k, shape, scale):
        return jax.random.normal(k, shape, f32) * scale

    S, F, D = SSM_WIDTH, FOURIER_WIDTH, D_MODEL
    G, N, C = SSM_GROUPS, SSM_STATE, SSM_GROUP_CH
    n_idx = jnp.arange(N, dtype=f32)
    return {
        'x': nrm(ks[0], (BATCH, SEQ, D), 1.0),
        'mix_norm_g': 1.0 + nrm(ks[1], (DEPTH, D), 0.02),
        'w_in': nrm(ks[2], (DEPTH, D, IN_WIDTH), D ** -0.5),
        'w_fourier_out': nrm(ks[3], (DEPTH, F, D), F ** -0.5),
        'ssm_A_re': -0.5 + nrm(ks[4], (DEPTH, 2, G, N), 0.01),
        'ssm_A_im': math.pi * n_idx + nrm(ks[5], (DEPTH, 2, G, N), 0.01),
        'ssm_log_dt': jax.random.uniform(ks[6], (DEPTH, 2, G), f32, math.log(DT_MIN), math.log(DT_MAX)),
        'ssm_B_re': nrm(ks[7], (DEPTH, 2, G, N, C), (2 * C) ** -0.5),
        'ssm_B_im': nrm(ks[8], (DEPTH, 2, G, N, C), (2 * C) ** -0.5),
        'ssm_C_re': nrm(ks[9], (DEPTH, 2, G, C, N), N ** -0.5),
        'ssm_C_im': nrm(ks[10], (DEPTH, 2, G, C, N), N ** -0.5),
        'ssm_D': nrm(ks[11], (DEPTH, S), 1.0),
        'ssm_w_glu': nrm(ks[12], (DEPTH, S, S), S ** -0.5),
        'w_ssm_out': nrm(ks[13], (DEPTH, S, D), S ** -0.5),
        'w_out': nrm(ks[14], (DEPTH, D, D), D ** -0.5),
        'ffn_norm_g': 1.0 + nrm(ks[15], (DEPTH, D), 0.02),
        'router_group_w': nrm(ks[16], (DEPTH, D, MOE_GROUPS), D ** -0.5),
        'router_group_b': nrm(ks[17], (DEPTH, MOE_GROUPS), 0.01),
        'router_expert_w': nrm(ks[18], (DEPTH, D, N_EXPERTS), D ** -0.5),
        'router_expert_b': nrm(ks[19], (DEPTH, N_EXPERTS), 0.01),
        'expert_w_gate': nrm(ks[20], (DEPTH, N_EXPERTS, D, D_EXPERT), D ** -0.5),
        'expert_w_up': nrm(ks[21], (DEPTH, N_EXPERTS, D, D_EXPERT), D ** -0.5),
        'expert_w_down': nrm(ks[22], (DEPTH, N_EXPERTS, D_EXPERT, D), D_EXPERT ** -0.5),
        'final_norm_g': 1.0 + nrm(ks[23], (D,), 0.02),
    }


def reference(x, mix_norm_g, w_in, w_fourier_out, ssm_A_re, ssm_A_im, ssm_log_dt,
              ssm_B_re, ssm_B_im, ssm_C_re, ssm_C_im, ssm_D, ssm_w_glu, w_ssm_out,
              w_out, ffn_norm_g, router_group_w, router_group_b, router_expert_w,
              router_expert_b, expert_w_gate, expert_w_up, expert_w_down, final_norm_g):
    for i in range(DEPTH):
        h = _rmsnorm(x, mix_norm_g[i])
        z = h @ w_in[i]
        u_f = z[..., :FOURIER_WIDTH]
        u_s = z[..., FOURIER_WIDTH:FOURIER_WIDTH + SSM_WIDTH]
        gates = jax.nn.sigmoid(z[..., FOURIER_WIDTH + SSM_WIDTH:].astype(jnp.float32)).astype(x.dtype)
        y_f = _fourier_mix(u_f) @ w_fourier_out[i]
        y_s = _ssm_mix(u_s, ssm_A_re[i], ssm_A_im[i], ssm_log_dt[i], ssm_B_re[i], ssm_B_im[i],
                       ssm_C_re[i], ssm_C_im[i], ssm_D[i], ssm_w_glu[i]) @ w_ssm_out[i]
        merged = gates[..., :D_MODEL] * y_f + gates[..., D_MODEL:] * y_s
        x = x + merged @ w_out[i]
        hn = _rmsnorm(x, ffn_norm_g[i])
        x = x + _hier_moe(hn, router_group_w[i], router_group_b[i], router_expert_w[i],
                          router_expert_b[i], expert_w_gate[i], expert_w_up[i], expert_w_down[i])
    return _rmsnorm(x, final_norm_g)
```

```python
import math
from contextlib import ExitStack

import ml_dtypes
import numpy as np

import concourse.bass as bass
import concourse.mybir as mybir
from concourse.bass_utils import run_bass_kernel_spmd

F32 = mybir.dt.float32
BF16 = mybir.dt.bfloat16
I32 = mybir.dt.int32
U8 = mybir.dt.uint8
ALU = mybir.AluOpType
AF = mybir.ActivationFunctionType
AX = mybir.AxisListType

ENGS = ["sync", "scalar", "vector", "gpsimd", "tensor"]
TWO_PI = 2.0 * math.pi
PI_SAFE = 3.14159
CAP = 128
NEXP = 64
STAGE = 2
DEBUG = False
MOE_LIMIT = 64


class Tok:
    __slots__ = ("kind", "eng", "n")

    def __init__(self, kind, eng, n):
        self.kind = kind
        self.eng = eng
        self.n = n


class Buf:
    def __init__(self, name=""):
        self.name = name
        self.w = []
        self.r = []


class Sched:
    def __init__(self, nc, stack):
        self.nc = nc
        self.stack = stack
        self.ops = {e: [] for e in ENGS}
        self.cnt = {e: 0 for e in ENGS}
        self.sem = {e: stack.enter_context(nc.semaphore("c_" + e)) for e in ENGS}
        self.waited = {e: {} for e in ENGS}
        self.dsem = {}
        self.dcnt = {}

    def _waits(self, eng, deps):
        waits = []
        for d in deps:
            if d is None:
                continue
            if d.kind == "eng":
                if d.eng == eng and eng == "tensor":
                    continue
                key = d.eng
                sem = self.sem[d.eng]
            else:
                key = "d_" + d.eng
                sem = self.dsem[d.eng]
            if self.waited[eng].get(key, 0) >= d.n:
                continue
            self.waited[eng][key] = d.n
            waits.append((sem, d.n))
        return waits

    @staticmethod
    def _deps(reads, writes, extra):
        deps = list(extra)
        for b in reads:
            deps += b.w
        for b in writes:
            deps += b.w
            deps += b.r
        return deps

    @staticmethod
    def _note(tok, reads, writes):
        for b in reads:
            b.r = [t for t in b.r if not (t.kind == tok.kind and t.eng == tok.eng)] + [tok]
        for b in writes:
            b.w = [tok]
            b.r = []

    def op(self, eng, build, reads=(), writes=(), extra=()):
        waits = self._waits(eng, self._deps(reads, writes, extra))
        self.cnt[eng] += 1
        n = self.cnt[eng]
        sem = self.sem[eng]

        def fn(h):
            for (s, v) in waits:
                h.wait_ge(s, v)
            build(h).then_inc(sem, 1)

        self.ops[eng].append(fn)
        tok = Tok("eng", eng, n)
        self._note(tok, reads, writes)
        return tok

    def dma(self, queue, semname, build, reads=(), writes=(), extra=()):
        if semname not in self.dsem:
            self.dsem[semname] = self.stack.enter_context(self.nc.semaphore("d_" + semname))
            self.dcnt[semname] = 0
        waits = self._waits(queue, self._deps(reads, writes, extra))
        self.dcnt[semname] += 16
        n = self.dcnt[semname]
        sem = self.dsem[semname]

        def fn(h):
            for (s, v) in waits:
                h.wait_ge(s, v)
            build(h).then_inc(sem, 16)

        self.ops[queue].append(fn)
        tok = Tok("dma", semname, n)
        self._note(tok, reads, writes)
        return tok

    def all_toks(self):
        t = [Tok("eng", e, self.cnt[e]) for e in ENGS if self.cnt[e] > 0]
        t += [Tok("dma", k, v) for k, v in self.dcnt.items() if v > 0]
        return t

    def barrier(self):
        toks = self.all_toks()
        for e in ENGS:
            waits = self._waits(e, toks)

            def fn(h, waits=waits):
                for (s, v) in waits:
                    h.wait_ge(s, v)

            self.ops[e].append(fn)

    def run(self):
        with self.nc.Block() as block:
            for e in ENGS:
                ops = self.ops[e]

                def body(h, ops=ops):
                    for fn in ops:
                        fn(h)

                getattr(block, e)(body)


class Arena:
    def __init__(self, ap_u8, size):
        self.ap = ap_u8
        self.size = size
        self.top = 0

    def alloc(self, free_shape, dt):
        isz = {F32: 4, I32: 4, BF16: 2}[dt]
        n = 1
        for s in free_shape:
            n *= s
        nb = n * isz
        off = self.top
        self.top += (nb + 63) // 64 * 64
        assert self.top <= self.size, ("SBUF arena overflow", self.top, self.size)
        v = self.ap[:, off:off + nb].bitcast(dt)
        if len(free_shape) > 1:
            names = ["a%d" % i for i in range(len(free_shape))]
            kw = {names[i]: free_shape[i] for i in range(1, len(free_shape))}
            v = v.rearrange("p (%s) -> p %s" % (" ".join(names), " ".join(names)), **kw)
        return v

    def mark(self):
        return self.top

    def release(self, m):
        self.top = m


def flat(ap):
    nd = len(ap.shape)
    if nd == 2:
        return ap
    names = ["a%d" % i for i in range(nd - 1)]
    return ap.rearrange("p %s -> p (%s)" % (" ".join(names), " ".join(names)))


C16 = dict(ident=0, triL=128, triU=256, selL=384, selF=512, CC=640, SS=768, n=896)
C32 = dict(ident=0, triS=128, ones=256, mask0=384, mask1=512, kf=640, kb=641, ev=642, ecap=659, iota=723, evr=851, n=868)


def build_program():
    nc = bass.Bass("TRN2", target_bir_lowering=False)

    def din(name, shape, dt=F32):
        return nc.dram_tensor(name, list(shape), dt, kind="ExternalInput").ap()

    x_d = din("x", [4096, 1024])
    g1_d = din("mix_norm_g", [1024])
    w_in_d = din("w_in", [1024, 3072])
    wfo_d = din("w_fourier_out", [512, 1024])
    sAre_d = din("sA_re", [2, 32, 64])
    sAim_d = din("sA_im", [2, 32, 64])
    sldt_d = din("s_ldt", [2, 32])
    sBre_d = din("sB_re", [2, 32, 64, 16])
    sBim_d = din("sB_im", [2, 32, 64, 16])
    sCre_d = din("sC_re", [2, 32, 16, 64])
    sCim_d = din("sC_im", [2, 32, 16, 64])
    sD_d = din("ssm_D", [512])
    wglu_d = din("ssm_w_glu", [512, 512])
    wso_d = din("w_ssm_out", [512, 1024])
    wout_d = din("w_out", [1024, 1024])
    g2_d = din("ffn_norm_g", [1024])
    wr_d = din("w_router", [1024, 72])
    br_d = din("b_router", [72])
    if STAGE >= 2:
        ewg_d = din("expert_w_gate", [64, 1024, 512])
        ewu_d = din("expert_w_up", [64, 1024, 512])
        ewd_d = din("expert_w_down", [64, 512, 1024])
    gf_d = din("final_norm_g", [1024])
    tc_d = din("tab_c", [4096, 2048], BF16)
    ts_d = din("tab_s", [4096, 2048], BF16)
    c16_d = din("cst16", [128, C16["n"]], BF16)
    c32_d = din("cst32", [128, C32["n"]])
    out_d = nc.dram_tensor("out", [2048, 1024], F32, kind="ExternalOutput").ap()
    xpad_d = nc.dram_tensor("xpad", [NEXP * CAP, 1024], BF16, kind="Internal").ap()
    ypad_d = nc.dram_tensor("ypad", [NEXP * CAP, 1024], F32, kind="Internal").ap()
    x2_d = nc.dram_tensor("x2s", [2048, 1024], F32, kind="Internal").ap()
    dbg_outs = {}

    with ExitStack() as st:
        S = Sched(nc, st)
        ARENA_BYTES = 206 * 1024
        arena_t = st.enter_context(nc.sbuf_tensor("arena", [128, ARENA_BYTES], U8))
        AR = Arena(arena_t[:, :], ARENA_BYTES)
        PSF, PSB, PB = [], [], []
        for i in range(8):
            pt = st.enter_context(nc.psum_tensor("ps%d" % i, [128, 512], F32))
            PSF.append(pt[:, :])
            PSB.append(pt[:, :].bitcast(BF16))
            PB.append(Buf("ps%d" % i))

        def vop(fn, r=(), w=(), x=()):
            return S.op("vector", fn, reads=r, writes=w, extra=x)

        def aop(fn, r=(), w=(), x=()):
            return S.op("scalar", fn, reads=r, writes=w, extra=x)

        def gop(fn, r=(), w=(), x=()):
            return S.op("gpsimd", fn, reads=r, writes=w, extra=x)

        def pop(fn, r=(), w=(), x=()):
            return S.op("tensor", fn, reads=r, writes=w, extra=x)

        _dq = [0]
        _breg = []

        def breg(h):
            if not _breg:
                _breg.append(h.to_reg(NEXP * CAP - 1))
            return _breg[0]

        def dma(q, fn, r=(), w=(), x=(), sem=None):
            if sem is None:
                _dq[0] += 1
                sem = "q%d" % (_dq[0] % 8)
            return S.dma(q, sem, fn, reads=r, writes=w, extra=x)

        def dbg(name, ap, shape):
            if not DEBUG:
                return
            d = nc.dram_tensor("dbg_" + name, list(shape), ap.dtype, kind="ExternalOutput").ap()
            dbg_outs[name] = d
            b = Buf()
            S.barrier()
            dma("sync", lambda h: h.dma_start(out=d, in_=ap), w=[b], sem="dbg")
            S.barrier()

        c16 = AR.alloc([C16["n"]], BF16)
        c32 = AR.alloc([C32["n"]], F32)
        Bc16, Bc32 = Buf(), Buf()
        dma("sync", lambda h: h.dma_start(out=c16, in_=c16_d), w=[Bc16])
        dma("sync", lambda h: h.dma_start(out=c32, in_=c32_d), w=[Bc32])

        def k16(name):
            o = C16[name]
            return c16[:, o:o + 128]

        def k32(name, n=128):
            o = C32[name]
            return c32[:, o:o + n]

        gw = AR.alloc([16, 2], F32)
        idx = AR.alloc([16, 2], I32)
        Bgw, Bidx = Buf(), Buf()
        persist0 = AR.top
        Min = AR.alloc([2, 32, 128], BF16)
        Q16 = AR.alloc([2, 32, 128], BF16)
        Mi = AR.alloc([32, 128], BF16)
        BMin, BQ16, BMi = Buf(), Buf(), Buf()
        ft_off = AR.top
        FT = AR.alloc([4, 2048], BF16)
        yT = AR.alloc([4, 2048], BF16)
        xs_off = AR.top
        Xs = AR.alloc([32, 512], BF16)
        uf_off = AR.top
        uf_tm = AR.alloc([32, 512], BF16)
        persist_mark = AR.mark()

        AR.release(ft_off)
        are = AR.alloc([64], F32)
        aim = AR.alloc([64], F32)
        dtn = AR.alloc([64], F32)
        Bre = AR.alloc([64, 16], F32)
        Bim = AR.alloc([64, 16], F32)
        Cre = AR.alloc([64, 16], F32)
        Cim = AR.alloc([64, 16], F32)
        Cld = AR.alloc([8, 2, 64], F32)
        Dp = AR.alloc([32], F32)
        Bp = Buf("params")
        for hf in range(2):
            ps_ = slice(hf * 64, hf * 64 + 64)
            dma("sync", lambda h, ps_=ps_: h.dma_start(out=are[ps_, :], in_=sAre_d.rearrange("d g n -> n (d g)"), allow_slow_non_contiguous=True), w=[Bp])
            dma("sync", lambda h, ps_=ps_: h.dma_start(out=aim[ps_, :], in_=sAim_d.rearrange("d g n -> n (d g)"), allow_slow_non_contiguous=True), w=[Bp])
            dma("sync", lambda h, ps_=ps_: h.dma_start(out=Bre[ps_, :, :], in_=sBre_d.rearrange("d g n c -> n (d g) c")), w=[Bp])
            dma("sync", lambda h, ps_=ps_: h.dma_start(out=Bim[ps_, :, :], in_=sBim_d.rearrange("d g n c -> n (d g) c")), w=[Bp])
        dma("sync", lambda h: h.dma_start(out=dtn, in_=sldt_d.rearrange("d g -> (d g)").partition_broadcast(128)), w=[Bp])
        for j in range(8):
            dma("sync", lambda h, j=j: h.dma_start(out=Dp[j * 16:(j + 1) * 16, :], in_=sD_d.rearrange("(g c) -> c g", c=16), allow_slow_non_contiguous=True), w=[Bp])
        Cld2 = AR.alloc([8, 2, 64], F32)
        for (src, cl) in ((sCre_d, Cld), (sCim_d, Cld2)):
            for dup in range(2):
                dma("sync", lambda h, src=src, dup=dup, cl=cl: h.dma_start(out=cl[:, :, dup, :], in_=src.rearrange("d g c n -> (d g c) n").rearrange("(t p) n -> p t n", p=128)), w=[Bp])
        S.barrier()
        for (cl, dst) in ((Cld, Cre), (Cld2, Cim)):
            for half in range(2):
                bk = half
                for t4 in range(4):
                    t = half * 4 + t4
                    pop(lambda h, t=t, t4=t4, bk=bk, cl=cl: h.transpose(out=PSF[bk][:, t4 * 128:(t4 + 1) * 128], in_=flat(cl[:, t, :, :]), identity=k32("ident")), r=[Bp, Bc32], w=[PB[bk]])
                vop(lambda h, dst=dst, half=half, bk=bk: h.tensor_copy(out=flat(dst)[:, half * 512:(half + 1) * 512], in_=PSF[bk]), r=[PB[bk]], w=[Bp])

        ev = k32("ev", 17)
        evr = k32("evr", 17)
        t0 = AR.alloc([32, 8, 16], F32)
        t1 = AR.alloc([32, 8, 16], F32)
        t0f, t1f = flat(t0), flat(t1)
        tA = t0f[:, 0:1088].rearrange("p (a b) -> p a b", b=64)
        tB = t0f[:, 1088:2176].rearrange("p (a b) -> p a b", b=64)
        tI = t0f[:, 2176:3264].bitcast(I32).rearrange("p (a b) -> p a b", b=64)
        Emag = t1f[:, 0:1088].rearrange("p (a b) -> p a b", b=64)
        Er = AR.alloc([17, 64], F32)
        Ei = AR.alloc([17, 64], F32)
        ErR = AR.alloc([17, 64], F32)
        EiR = AR.alloc([17, 64], F32)
        sm = [AR.alloc([64], F32) for _ in range(8)]
        BE = Buf("E")

        def sincos(xin, sin_out, cos_out, ti, tf, bufs):
            vop(lambda h: h.tensor_scalar(out=ti, in0=xin, scalar1=1.0 / TWO_PI, scalar2=None, op0=ALU.mult), r=bufs, w=bufs)
            vop(lambda h: h.tensor_copy(out=tf, in_=ti), r=bufs, w=bufs)
            vop(lambda h: h.scalar_tensor_tensor(out=tf, in0=tf, scalar=-TWO_PI, in1=xin, op0=ALU.mult, op1=ALU.add), r=bufs, w=bufs)
            vop(lambda h: h.tensor_scalar(out=tf, in0=tf, scalar1=PI_SAFE, scalar2=-PI_SAFE, op0=ALU.min, op1=ALU.max), r=bufs, w=bufs)
            aop(lambda h: h.activation(out=sin_out, in_=tf, func=AF.Sin), r=bufs, w=bufs)
            vop(lambda h: h.tensor_scalar(out=cos_out, in0=tf, scalar1=math.pi / 2, scalar2=-TWO_PI, op0=ALU.is_gt, op1=ALU.mult), r=bufs, w=bufs)
            vop(lambda h: h.scalar_tensor_tensor(out=cos_out, in0=tf, scalar=math.pi / 2, in1=cos_out, op0=ALU.add, op1=ALU.add), r=bufs, w=bufs)
            vop(lambda h: h.tensor_scalar(out=cos_out, in0=cos_out, scalar1=PI_SAFE, scalar2=-PI_SAFE, op0=ALU.min, op1=ALU.max), r=bufs, w=bufs)
            aop(lambda h: h.activation(out=cos_out, in_=cos_out, func=AF.Sin), r=bufs, w=bufs)

        PB0 = [Bp, BE, Bc32]
        aop(lambda h: h.activation(out=dtn, in_=dtn, func=AF.Exp), r=PB0, w=PB0)
        ar_, th_ = sm[0], sm[1]
        vop(lambda h: h.tensor_tensor(out=ar_, in0=are, in1=dtn, op=ALU.mult), r=PB0, w=PB0)
        vop(lambda h: h.tensor_tensor(out=th_, in0=aim, in1=dtn, op=ALU.mult), r=PB0, w=PB0)

        def etable(evc, Er_, Ei_):
            vop(lambda h: h.tensor_tensor(out=tA, in0=ar_.unsqueeze(1).to_broadcast([128, 17, 64]), in1=evc.unsqueeze(2).to_broadcast([128, 17, 64]), op=ALU.mult), r=PB0, w=PB0)
            aop(lambda h: h.activation(out=flat(Emag), in_=flat(tA), func=AF.Exp), r=PB0, w=PB0)
            vop(lambda h: h.tensor_tensor(out=tA, in0=th_.unsqueeze(1).to_broadcast([128, 17, 64]), in1=evc.unsqueeze(2).to_broadcast([128, 17, 64]), op=ALU.mult), r=PB0, w=PB0)
            sincos(flat(tA), flat(Ei_), flat(Er_), flat(tI), flat(tB), PB0)
            vop(lambda h: h.tensor_tensor(out=flat(Er_), in0=flat(Er_), in1=flat(Emag), op=ALU.mult), r=PB0, w=PB0)
            vop(lambda h: h.tensor_tensor(out=flat(Ei_), in0=flat(Ei_), in1=flat(Emag), op=ALU.mult), r=PB0, w=PB0)

        etable(ev, Er, Ei)
        etable(evr, ErR, EiR)
        am1, nr, ni, den, fr, fi = sm[2], sm[3], sm[4], sm[5], sm[6], sm[7]
        ar1, ai1 = Er[:, 9, :], Ei[:, 9, :]
        vop(lambda h: h.tensor_scalar(out=am1, in0=ar1, scalar1=-1.0, scalar2=None, op0=ALU.add), r=PB0, w=PB0)
        vop(lambda h: h.tensor_tensor(out=nr, in0=am1, in1=are, op=ALU.mult), r=PB0, w=PB0)
        vop(lambda h: h.tensor_tensor(out=den, in0=ai1, in1=aim, op=ALU.mult), r=PB0, w=PB0)
        vop(lambda h: h.tensor_tensor(out=nr, in0=nr, in1=den, op=ALU.add), r=PB0, w=PB0)
        vop(lambda h: h.tensor_tensor(out=ni, in0=ai1, in1=are, op=ALU.mult), r=PB0, w=PB0)
        vop(lambda h: h.tensor_tensor(out=den, in0=am1, in1=aim, op=ALU.mult), r=PB0, w=PB0)
        vop(lambda h: h.tensor_tensor(out=ni, in0=ni, in1=den, op=ALU.subtract), r=PB0, w=PB0)
        vop(lambda h: h.tensor_tensor(out=den, in0=are, in1=are, op=ALU.mult), r=PB0, w=PB0)
        vop(lambda h: h.tensor_tensor(out=am1, in0=aim, in1=aim, op=ALU.mult), r=PB0, w=PB0)
        vop(lambda h: h.tensor_tensor(out=den, in0=den, in1=am1, op=ALU.add), r=PB0, w=PB0)
        vop(lambda h: h.reciprocal(out=den, in_=den), r=PB0, w=PB0)
        vop(lambda h: h.tensor_tensor(out=fr, in0=nr, in1=den, op=ALU.mult), r=PB0, w=PB0)
        vop(lambda h: h.tensor_tensor(out=fi, in0=ni, in1=den, op=ALU.mult), r=PB0, w=PB0)
        bbr = AR.alloc([64, 16], F32)
        bbi = AR.alloc([64, 16], F32)
        tb1 = AR.alloc([64, 16], F32)

        def bc16(a):
            return a.unsqueeze(2).to_broadcast([128, a.shape[1], 16])

        vop(lambda h: h.tensor_tensor(out=bbr, in0=Bre, in1=bc16(fr), op=ALU.mult), r=PB0, w=PB0)
        vop(lambda h: h.tensor_tensor(out=tb1, in0=Bim, in1=bc16(fi), op=ALU.mult), r=PB0, w=PB0)
        vop(lambda h: h.tensor_tensor(out=bbr, in0=bbr, in1=tb1, op=ALU.subtract), r=PB0, w=PB0)
        vop(lambda h: h.tensor_tensor(out=bbi, in0=Bim, in1=bc16(fr), op=ALU.mult), r=PB0, w=PB0)
        vop(lambda h: h.tensor_tensor(out=tb1, in0=Bre, in1=bc16(fi), op=ALU.mult), r=PB0, w=PB0)
        vop(lambda h: h.tensor_tensor(out=bbi, in0=bbi, in1=tb1, op=ALU.add), r=PB0, w=PB0)

        PinN = AR.alloc([2, 32, 8, 16], BF16)
        P16 = AR.alloc([2, 32, 8, 16], BF16)
        BPin, BPm = Buf(), Buf()
        Bt_lo, Bt_hi = Buf(), Buf()
        Q16v = Q16.rearrange("p d g (i c) -> p d g i c", c=16)

        def cmul_batch(out_ap, outbuf, Ar, Ai, d, Tr_, Ti_, i0, mode):
            def vv(T, lo, hi):
                return T[lo:hi, i0:i0 + 8, d * 32:(d + 1) * 32].rearrange("p j g -> p g j").unsqueeze(3).to_broadcast([hi - lo, 32, 8, 16])

            def aa(A, lo, hi):
                return A[lo:hi, d * 32:(d + 1) * 32, :].unsqueeze(2).to_broadcast([hi - lo, 32, 8, 16])

            rr = PB0
            vop(lambda h: h.tensor_tensor(out=t0[0:64], in0=aa(Ar, 0, 64), in1=vv(Tr_, 0, 64), op=ALU.mult), r=rr, w=[Bt_lo])
            vop(lambda h: h.tensor_tensor(out=t1[0:64], in0=aa(Ai, 0, 64), in1=vv(Ti_, 0, 64), op=ALU.mult), r=rr, w=[Bt_lo])
            vop(lambda h: h.tensor_tensor(out=out_ap[0:64], in0=t0[0:64], in1=t1[0:64], op=ALU.subtract), r=[Bt_lo], w=[outbuf])
            gop(lambda h: h.tensor_tensor(out=t0[64:128], in0=aa(Ar, 64, 128), in1=vv(Ti_, 64, 128), op=ALU.mult), r=rr, w=[Bt_hi])
            gop(lambda h: h.tensor_tensor(out=t1[64:128], in0=aa(Ai, 64, 128), in1=vv(Tr_, 64, 128), op=ALU.mult), r=rr, w=[Bt_hi])
            if mode == "P":
                gop(lambda h: h.tensor_tensor(out=out_ap[64:128], in0=t0[64:128], in1=t1[64:128], op=ALU.add), r=[Bt_hi], w=[outbuf])
            else:
                gop(lambda h: h.tensor_tensor(out=t0[64:128], in0=t0[64:128], in1=t1[64:128], op=ALU.add), r=[Bt_hi], w=[Bt_hi])
                gop(lambda h: h.tensor_scalar(out=out_ap[64:128], in0=t0[64:128], scalar1=-1.0, scalar2=None, op0=ALU.mult), r=[Bt_hi], w=[outbuf])

        cmul_batch(PinN[:, 0], BPin, bbr, bbi, 0, ErR, EiR, 1, "P")
        cmul_batch(P16[:, 0], BPm, bbr, bbi, 0, ErR, EiR, 9, "P")
        cmul_batch(Q16v[:, 0], BQ16, Cre, Cim, 0, Er, Ei, 9, "Q")
        cmul_batch(PinN[:, 1], BPin, bbr, bbi, 1, Er, Ei, 8, "P")
        cmul_batch(P16[:, 1], BPm, bbr, bbi, 1, Er, Ei, 0, "P")
        cmul_batch(Q16v[:, 1], BQ16, Cre, Cim, 1, ErR, EiR, 0, "Q")
        for d in range(2):
            for g8 in range(4):
                bk = 2 + (g8 % 2)
                for gi in range(8):
                    g = g8 * 8 + gi
                    pop(lambda h, d=d, g=g, gi=gi, bk=bk: h.transpose(out=PSB[bk][:, gi * 128:(gi + 1) * 128], in_=flat(PinN[:, d, g, :, :]), identity=k16("ident")), r=[BPin, Bc16], w=[PB[bk]])
                aop(lambda h, d=d, g8=g8, bk=bk: h.activation(out=flat(Min[:, d, g8 * 8:(g8 + 1) * 8, :]), in_=PSB[bk], func=AF.Copy), r=[PB[bk]], w=[BMin])
        mt0 = AR.alloc([4, 128], F32)
        mt1 = AR.alloc([4, 128], F32)
        Bmt = Buf()
        for g4 in range(8):
            for d in range(2):
                bk = 4 + d
                for gi in range(4):
                    g = g4 * 4 + gi
                    pop(lambda h, d=d, g=g, gi=gi, bk=bk: h.matmul(PSF[bk][:, gi * 128:(gi + 1) * 128], lhsT=flat(P16[:, d, g, :, :]), rhs=Q16[:, d, g, :], start=True, stop=True), r=[BPm, BQ16], w=[PB[bk]])
            vop(lambda h: h.tensor_tensor(out=mt0, in0=PSF[4].rearrange("p (a b) -> p a b", b=128), in1=k32("mask0").unsqueeze(1).to_broadcast([128, 4, 128]), op=ALU.mult), r=[PB[4], Bc32], w=[Bmt])
            vop(lambda h: h.tensor_tensor(out=mt1, in0=PSF[5].rearrange("p (a b) -> p a b", b=128), in1=k32("mask1").unsqueeze(1).to_broadcast([128, 4, 128]), op=ALU.mult), r=[PB[5], Bc32], w=[Bmt])
            vop(lambda h: h.tensor_tensor(out=mt0, in0=mt0, in1=mt1, op=ALU.add), r=[Bmt], w=[Bmt])
            for gi in range(4):
                g = g4 * 4 + gi
                vop(lambda h, g=g, gi=gi: h.scalar_tensor_tensor(out=Mi[:, g, :], in0=k32("ident"), scalar=Dp[:, g:g + 1], in1=mt0[:, gi, :], op0=ALU.mult, op1=ALU.add), r=[Bmt, Bp, Bc32], w=[BMi])
        S.barrier()
        AR.release(persist_mark)

        Buf_uf, BX = Buf(), Buf()
        p1_mark = AR.mark()
        Wfs = AR.alloc([8, 1024], BF16)
        BWfs = Buf()
        dma("gpsimd", lambda h: h.dma_start(out=Wfs, in_=w_in_d[:, 0:1024].rearrange("(kc p) n -> p kc n", p=128)), w=[BWfs], sem="wfs")
        gbc = AR.alloc([1024], F32)
        Bg = Buf()
        dma("sync", lambda h: h.dma_start(out=gbc, in_=g1_d.partition_broadcast(128)), w=[Bg], sem="g1a")
        hT = AR.alloc([8, 1024], BF16)
        BhT = Buf()
        Zt = AR.alloc([32, 8, 16], BF16)
        BZ = Buf()
        NXB = 3
        xt = [AR.alloc([1024], F32) for _ in range(NXB)]
        Bxt = [Buf() for _ in range(NXB)]
        xn = [AR.alloc([1024], BF16) for _ in range(2)]
        Bxn = [Buf() for _ in range(2)]
        junk = AR.alloc([1024], BF16)
        Bjunk = Buf()
        stat = AR.alloc([64], F32)
        Bstat = Buf()

        def norm_tile(xsrc_ap, xt_ap, bxt, xn_ap, bxn, col, gb, bg, qsem):
            dma("sync", lambda h: h.dma_start(out=xt_ap, in_=xsrc_ap), w=[bxt], sem=qsem)
            aop(lambda h: h.activation(out=junk, in_=xt_ap, func=AF.Square, accum_out=stat[:, col:col + 1]), r=[bxt], w=[Bjunk, Bstat])
            aop(lambda h: h.activation(out=stat[:, col + 1:col + 2], in_=stat[:, col:col + 1], func=AF.Sqrt, scale=1.0 / 1024, bias=1e-6), r=[Bstat], w=[Bstat])
            vop(lambda h: h.reciprocal(out=stat[:, col + 2:col + 3], in_=stat[:, col + 1:col + 2]), r=[Bstat], w=[Bstat])
            vop(lambda h: h.scalar_tensor_tensor(out=xn_ap, in0=xt_ap, scalar=stat[:, col + 2:col + 3], in1=gb, op0=ALU.mult, op1=ALU.mult), r=[bxt, Bstat, bg], w=[bxn])

        ti_ = 0
        for b in range(4):
            for t in range(8):
                tg = b * 8 + t
                xi = ti_ % NXB
                ni_ = ti_ % 2
                col = (ti_ % 8) * 4
                ti_ += 1
                norm_tile(x_d[tg * 128:(tg + 1) * 128, :], xt[xi], Bxt[xi], xn[ni_], Bxn[ni_], col, gbc, Bg, "x%d" % xi)
                bk = ni_
                for kc in range(8):
                    pop(lambda h, kc=kc, bk=bk, ni_=ni_: h.transpose(out=PSB[bk][:, kc * 128:(kc + 1) * 128], in_=xn[ni_][:, kc * 128:(kc + 1) * 128], identity=k16("ident")), r=[Bxn[ni_], Bc16], w=[PB[bk]])
                aop(lambda h, t=t, bk=bk: h.activation(out=hT[:, :, t * 128:(t + 1) * 128], in_=PSB[bk].rearrange("p (a b) -> p a b", b=128), func=AF.Copy), r=[PB[bk]], w=[BhT])
            for t in range(8):
                tg = b * 8 + t
                bk = 2 + (t % 2)
                for kc in range(8):
                    pop(lambda h, kc=kc, t=t, bk=bk: h.matmul(PSF[bk], lhsT=hT[:, kc, t * 128:(t + 1) * 128], rhs=Wfs[:, kc, 0:512], start=(kc == 0), stop=(kc == 7)), r=[BhT, BWfs], w=[PB[bk]])
                vop(lambda h, tg=tg, bk=bk: h.tensor_copy(out=uf_tm[:, tg, :], in_=PSF[bk]), r=[PB[bk]], w=[Buf_uf])
            for j in range(8):
                bk = 4 + (j % 2)
                for kc in range(8):
                    pop(lambda h, kc=kc, j=j, bk=bk: h.matmul(PSF[bk], lhsT=hT[:, kc, j:1024:8], rhs=Wfs[:, kc, 512:1024], start=(kc == 0), stop=(kc == 7)), r=[BhT, BWfs], w=[PB[bk]])
                aop(lambda h, j=j, bk=bk: h.activation(out=Zt[:, :, j, :], in_=PSF[bk].rearrange("p (g c) -> p g c", c=16), func=AF.Copy), r=[PB[bk]], w=[BZ])
            for g8 in range(4):
                bk = 6 + (g8 % 2)
                for gi in range(8):
                    g = g8 * 8 + gi
                    pop(lambda h, g=g, gi=gi, bk=bk: h.transpose(out=PSB[bk][:, gi * 128:(gi + 1) * 128], in_=flat(Zt[:, g, :, :]), identity=k16("ident")), r=[BZ, Bc16], w=[PB[bk]])
                vop(lambda h, g8=g8, b=b, bk=bk: h.tensor_copy(out=Xs[:, g8 * 8:(g8 + 1) * 8, b * 128:(b + 1) * 128], in_=PSB[bk].rearrange("p (a b) -> p a b", b=128)), r=[PB[bk]], w=[BX])
        S.barrier()
        dbg("wfs", Wfs, [128, 8, 1024])
        dbg("ht", hT, [128, 8, 1024])
        dbg("xn", xn[1], [128, 1024])
        dbg("stat", stat, [128, 64])
        dbg("zt", Zt, [128, 32, 8, 16])
        dbg("min", Min, [128, 2, 32, 128])
        dbg("q16", Q16, [128, 2, 32, 128])
        dbg("mi", Mi, [128, 32, 128])
        AR.release(p1_mark)
        dbg("uf", uf_tm, [128, 32, 512])
        dbg("xs", Xs, [128, 32, 512])
        if STAGE == 0:
            S.barrier()
            S.run()
            return nc, dbg_outs

        BFT = Buf()
        p2_mark = AR.mark()
        NTB = 4
        tct = [AR.alloc([256], BF16) for _ in range(NTB)]
        tst = [AR.alloc([256], BF16) for _ in range(NTB)]
        Btc = [Buf() for _ in range(NTB)]
        Bts = [Buf() for _ in range(NTB)]
        UrT = AR.alloc([4, 256], BF16)
        UiT = AR.alloc([4, 256], BF16)
        BUU = Buf()
        it_ = 0
        for kb in range(8):
            for lt in range(32):
                bi = it_ % NTB
                it_ += 1
                dma("sync", lambda h, bi=bi, lt=lt, kb=kb: h.dma_start(out=tct[bi], in_=tc_d[lt * 128:(lt + 1) * 128, kb * 256:(kb + 1) * 256]), w=[Btc[bi]], sem="tc%d" % bi)
                dma("scalar", lambda h, bi=bi, lt=lt, kb=kb: h.dma_start(out=tst[bi], in_=ts_d[lt * 128:(lt + 1) * 128, kb * 256:(kb + 1) * 256]), w=[Bts[bi]], sem="ts%d" % bi)
                for g in range(4):
                    pop(lambda h, g=g, lt=lt, bi=bi: h.matmul(PSF[g][:, 0:256], lhsT=uf_tm[:, lt, g * 128:(g + 1) * 128], rhs=tct[bi], start=(lt == 0), stop=(lt == 31)), r=[Buf_uf, Btc[bi]], w=[PB[g]])
                    pop(lambda h, g=g, lt=lt, bi=bi: h.matmul(PSF[4 + g][:, 0:256], lhsT=uf_tm[:, lt, g * 128:(g + 1) * 128], rhs=tst[bi], start=(lt == 0), stop=(lt == 31)), r=[Buf_uf, Bts[bi]], w=[PB[4 + g]])
            for g in range(4):
                vop(lambda h, g=g: h.tensor_copy(out=UrT[:, g, :], in_=PSF[g][:, 0:256]), r=[PB[g]], w=[BUU])
                aop(lambda h, g=g: h.activation(out=UiT[:, g, :], in_=PSF[4 + g][:, 0:256], func=AF.Copy), r=[PB[4 + g]], w=[BUU])
            for g in range(4):
                pop(lambda h, g=g: h.matmul(PSF[g][:, 256:512], lhsT=k16("CC"), rhs=UrT[:, g, :], start=True, stop=False), r=[BUU, Bc16], w=[PB[g]])
                pop(lambda h, g=g: h.matmul(PSF[g][:, 256:512], lhsT=k16("SS"), rhs=UiT[:, g, :], start=False, stop=True), r=[BUU, Bc16], w=[PB[g]])
                vop(lambda h, g=g, kb=kb: h.tensor_copy(out=FT[:, g, kb * 256:(kb + 1) * 256], in_=PSF[g][:, 256:512]), r=[PB[g]], w=[BFT])
        S.barrier()
        AR.release(p2_mark)
        dbg("ft", FT, [128, 4, 2048])

        ByT = Buf()
        AR.release(uf_off)
        p3_mark = AR.mark()
        Wglu = AR.alloc([4, 512], BF16)
        BWglu = Buf()
        dma("gpsimd", lambda h: h.dma_start(out=Wglu, in_=wglu_d.rearrange("(q p) n -> p q n", p=128)), w=[BWglu], sem="wglu")
        pAre = AR.alloc([16, 64], F32)
        pAim = AR.alloc([16, 64], F32)
        pdt = AR.alloc([16], F32)
        Trp = AR.alloc([16, 64], F32)
        Tip = AR.alloc([16, 64], F32)
        Trm = AR.alloc([16, 64], F32)
        Tim = AR.alloc([16, 64], F32)
        tX = AR.alloc([16, 64], F32)
        tY = AR.alloc([16, 64], F32)
        tZi = AR.alloc([16, 64], I32)
        BT = Buf("tables")
        Hc = [AR.alloc([16, 2, 64], BF16) for _ in range(2)]
        BHc = [Buf(), Buf()]
        HTd = [AR.alloc([16, 257], BF16) for _ in range(2)]
        BHT = [Buf(), Buf()]
        Vt = [AR.alloc([4, 2, 64], BF16) for _ in range(2)]
        BV = [Buf(), Buf()]
        m1 = [AR.alloc([4, 2, 64], F32) for _ in range(2)]
        m2 = [AR.alloc([4, 2, 64], F32) for _ in range(2)]
        Bm = [Buf(), Buf()]
        Yg = AR.alloc([8, 256], BF16)
        BYg = Buf()
        sg = AR.alloc([4, 512], BF16)
        Bsg = Buf()

        def cplx_mul(src4, bsrc, Tr, Ti, k, out_re, out_im, bout):
            a, bb = m1[k], m2[k]
            Trb = Tr.unsqueeze(2).to_broadcast([128, 4, 2, 64])
            vop(lambda h: h.tensor_tensor(out=a, in0=src4, in1=Trb, op=ALU.mult), r=[BT, bsrc], w=[Bm[k]])
            vop(lambda h: h.tensor_tensor(out=bb[:, :, 0, :], in0=src4[:, :, 1, :], in1=Ti, op=ALU.mult), r=[BT, bsrc], w=[Bm[k]])
            vop(lambda h: h.tensor_tensor(out=bb[:, :, 1, :], in0=src4[:, :, 0, :], in1=Ti, op=ALU.mult), r=[BT, bsrc], w=[Bm[k]])
            gop(lambda h: h.tensor_tensor(out=out_re, in0=a[:, :, 0, :], in1=bb[:, :, 0, :], op=ALU.subtract), r=[Bm[k]], w=[bout])
            gop(lambda h: h.tensor_tensor(out=out_im, in0=a[:, :, 1, :], in1=bb[:, :, 1, :], op=ALU.add), r=[Bm[k]], w=[bout])

        for half in range(2):
            g_lo = half * 16
            for d in range(2):
                kcol = c32[:, C32["kf"] + d:C32["kf"] + d + 1]
                dma("sync", lambda h, d=d, g_lo=g_lo: h.dma_start(out=flat(pAre), in_=sAre_d[d, g_lo:g_lo + 16, :].rearrange("g n -> (g n)").partition_broadcast(128)), w=[BT], sem="pa")
                dma("sync", lambda h, d=d, g_lo=g_lo: h.dma_start(out=flat(pAim), in_=sAim_d[d, g_lo:g_lo + 16, :].rearrange("g n -> (g n)").partition_broadcast(128)), w=[BT], sem="pb")
                dma("sync", lambda h, d=d, g_lo=g_lo: h.dma_start(out=pdt, in_=sldt_d[d, g_lo:g_lo + 16].partition_broadcast(128)), w=[BT], sem="pc")
                TB = [BT, Bc32]
                xt3 = [Tok("dma", "pa", S.dcnt["pa"]), Tok("dma", "pb", S.dcnt["pb"]), Tok("dma", "pc", S.dcnt["pc"])]
                aop(lambda h: h.activation(out=pdt, in_=pdt, func=AF.Exp, scale=1.0), r=TB, w=TB, x=xt3)
                dtb = pdt.unsqueeze(2).to_broadcast([128, 16, 64])
                vop(lambda h, dtb=dtb: h.scalar_tensor_tensor(out=pAre, in0=pAre, scalar=8.0, in1=dtb, op0=ALU.mult, op1=ALU.mult), r=TB, w=TB, x=xt3)
                vop(lambda h, dtb=dtb: h.scalar_tensor_tensor(out=pAim, in0=pAim, scalar=8.0, in1=dtb, op0=ALU.mult, op1=ALU.mult), r=TB, w=TB)
                vop(lambda h: h.tensor_scalar(out=flat(tZi), in0=flat(pAim), scalar1=1.0 / TWO_PI, scalar2=None, op0=ALU.mult), r=TB, w=TB)
                vop(lambda h: h.tensor_copy(out=flat(tX), in_=flat(tZi)), r=TB, w=TB)
                vop(lambda h: h.scalar_tensor_tensor(out=flat(pAim), in0=flat(tX), scalar=-TWO_PI, in1=flat(pAim), op0=ALU.mult, op1=ALU.add), r=TB, w=TB)
                vop(lambda h, kcol=kcol: h.tensor_scalar(out=flat(tY), in0=flat(pAim), scalar1=kcol, scalar2=None, op0=ALU.mult), r=TB, w=TB)
                sincos(flat(tY), flat(Tip), flat(Trp), flat(tZi), flat(tX), TB)
                vop(lambda h, kcol=kcol: h.tensor_scalar(out=flat(tY), in0=flat(pAre), scalar1=kcol, scalar2=None, op0=ALU.mult), r=TB, w=TB)
                aop(lambda h: h.activation(out=flat(tX), in_=flat(tY), func=AF.Exp), r=TB, w=TB)
                aop(lambda h: h.activation(out=flat(tY), in_=flat(tY), func=AF.Exp, scale=-1.0), r=TB, w=TB)
                vop(lambda h: h.tensor_tensor(out=flat(Trm), in0=flat(Trp), in1=flat(tY), op=ALU.mult), r=TB, w=TB)
                vop(lambda h: h.scalar_tensor_tensor(out=flat(Tim), in0=flat(Tip), scalar=-1.0, in1=flat(tY), op0=ALU.mult, op1=ALU.mult), r=TB, w=TB)
                vop(lambda h: h.tensor_tensor(out=flat(Trp), in0=flat(Trp), in1=flat(tX), op=ALU.mult), r=TB, w=TB)
                vop(lambda h: h.tensor_tensor(out=flat(Tip), in0=flat(Tip), in1=flat(tX), op=ALU.mult), r=TB, w=TB)
                blocks = [0, 1] if d == 0 else [3, 2, 1, 0]
                tri = k16("triL") if d == 0 else k16("triU")
                sel = k16("selL") if d == 0 else k16("selF")
                if d == 0:
                    vop(lambda h: h.memset(HTd[0][:, :, 0:1], 0.0), r=[], w=[BHT[0]])
                for bi_, blk in enumerate(blocks):
                    cur, prv = bi_ % 2, (bi_ + 1) % 2
                    first = (bi_ == 0)
                    for c4 in range(4):
                        gl = c4 * 4
                        k = c4 % 2
                        bS, bW, bTp = (0, 1, 2) if k == 0 else (3, 4, 5)
                        for gi in range(4):
                            g = g_lo + gl + gi
                            pop(lambda h, g=g, gi=gi, blk=blk, d=d, bS=bS: h.matmul(PSF[bS][:, gi * 128:(gi + 1) * 128], lhsT=Xs[:, g, blk * 128:(blk + 1) * 128], rhs=Min[:, d, g, :], start=True, stop=True), r=[BX, BMin], w=[PB[bS]])
                        srcS = PSF[bS].rearrange("p (g h n) -> p g h n", h=2, n=64)
                        cplx_mul(srcS, PB[bS], Trm[:, gl:gl + 4, :], Tim[:, gl:gl + 4, :], k, Vt[k][:, :, 0, :], Vt[k][:, :, 1, :], BV[k])
                        pop(lambda h, k=k, bW=bW, first=first, tri=tri: h.matmul(PSF[bW], lhsT=tri, rhs=flat(Vt[k]), start=True, stop=first), r=[BV[k], Bc16], w=[PB[bW]])
                        if not first:
                            pop(lambda h, bW=bW, prv=prv, gl=gl, sel=sel: h.matmul(PSF[bW], lhsT=sel, rhs=flat(Hc[prv][:, gl:gl + 4, :, :]), start=False, stop=True), r=[BHc[prv], Bc16], w=[PB[bW]])
                        srcW = PSF[bW].rearrange("p (g h n) -> p g h n", h=2, n=64)
                        cplx_mul(srcW, PB[bW], Trp[:, gl:gl + 4, :], Tip[:, gl:gl + 4, :], k, Hc[cur][:, gl:gl + 4, 0, :], Hc[cur][:, gl:gl + 4, 1, :], BHc[cur])
                        if blk <= 2:
                            for gi in range(4):
                                pop(lambda h, gi=gi, gl=gl, cur=cur, bTp=bTp: h.transpose(out=PSB[bTp][:, gi * 128:(gi + 1) * 128], in_=flat(Hc[cur][:, gl + gi, :, :]), identity=k16("ident")), r=[BHc[cur], Bc16], w=[PB[bTp]])
                            srcT = PSB[bTp][:, 0:512].rearrange("p (a b) -> p a b", b=128)
                            if blk < 2:
                                co = blk * 128 + (1 if d == 0 else 0)
                                aop(lambda h, gl=gl, co=co, srcT=srcT, d=d: h.activation(out=HTd[d][:, gl:gl + 4, co:co + 128], in_=srcT, func=AF.Copy), r=[PB[bTp]], w=[BHT[d]])
                            else:
                                aop(lambda h, gl=gl, srcT=srcT, d=d: h.activation(out=HTd[d][:, gl:gl + 4, 256:257], in_=srcT[:, :, 0:1], func=AF.Copy), r=[PB[bTp]], w=[BHT[d]])
            for blk in range(2):
                for c4 in range(4):
                    gl = c4 * 4
                    bk = 6 + (c4 % 2)
                    for gi in range(4):
                        g = g_lo + gl + gi
                        o = gi * 128
                        pop(lambda h, g=g, o=o, blk=blk, bk=bk: h.matmul(PSF[bk][:, o:o + 128], lhsT=Xs[:, g, blk * 128:(blk + 1) * 128], rhs=Mi[:, g, :], start=True, stop=False), r=[BX, BMi], w=[PB[bk]])
                        pop(lambda h, g=g, o=o, blk=blk, bk=bk, gl=gl, gi=gi: h.matmul(PSF[bk][:, o:o + 128], lhsT=HTd[0][:, gl + gi, blk * 128:blk * 128 + 128], rhs=Q16[:, 0, g, :], start=False, stop=False), r=[BHT[0], BQ16], w=[PB[bk]])
                        pop(lambda h, g=g, o=o, blk=blk, bk=bk, gl=gl, gi=gi: h.matmul(PSF[bk][:, o:o + 128], lhsT=HTd[1][:, gl + gi, blk * 128 + 1:blk * 128 + 129], rhs=Q16[:, 1, g, :], start=False, stop=True), r=[BHT[1], BQ16], w=[PB[bk]])
                    aop(lambda h, bk=bk, gl=gl: h.activation(out=Yg[:, :, gl * 16:(gl + 4) * 16].rearrange("p i (g c) -> p g i c", c=16), in_=PSF[bk].rearrange("p (g i c) -> p g i c", i=8, c=16), func=AF.Gelu), r=[PB[bk]], w=[BYg])
                for qq in range(2):
                    q = 2 * half + qq
                    bk = 2 if qq == 0 else 5
                    for i in range(8):
                        pop(lambda h, i=i, qq=qq, bk=bk: h.transpose(out=PSB[bk][:, i * 128:(i + 1) * 128], in_=Yg[:, i, qq * 128:(qq + 1) * 128], identity=k16("ident")), r=[BYg, Bc16], w=[PB[bk]])
                    vop(lambda h, q=q, blk=blk, bk=bk: h.tensor_copy(out=yT[:, q, blk * 1024:(blk + 1) * 1024].rearrange("p (k i) -> p i k", i=8), in_=PSB[bk].rearrange("p (i k) -> p i k", k=128)), r=[PB[bk]], w=[ByT])
        dbg("yt", yT, [128, 4, 2048])
        for tb in range(4):
            for co in range(4):
                for q in range(4):
                    pop(lambda h, co=co, q=q, tb=tb: h.matmul(PSF[co], lhsT=Wglu[:, q, co * 128:(co + 1) * 128], rhs=yT[:, q, tb * 512:(tb + 1) * 512], start=(q == 0), stop=(q == 3)), r=[ByT, BWglu], w=[PB[co]])
                aop(lambda h, co=co: h.activation(out=sg[:, co, :], in_=PSF[co], func=AF.Sigmoid), r=[PB[co]], w=[Bsg])
            vop(lambda h, tb=tb: h.tensor_tensor(out=yT[:, :, tb * 512:(tb + 1) * 512], in0=yT[:, :, tb * 512:(tb + 1) * 512], in1=sg, op=ALU.mult), r=[Bsg, ByT], w=[ByT])
        S.barrier()
        AR.release(p3_mark)
        dbg("y2t", yT, [128, 4, 2048])

        A4 = Arena(arena_t[:, persist0:ft_off], ft_off - persist0)
        A5 = Arena(arena_t[:, xs_off:ARENA_BYTES], ARENA_BYTES - xs_off)
        Wg_ = A4.alloc([8, 2048], BF16)
        Wout = A5.alloc([8, 1024], BF16)
        Wfo = A4.alloc([4, 1024], BF16)
        Wso = A5.alloc([4, 1024], BF16)
        BW4 = [Buf() for _ in range(4)]
        dma("gpsimd", lambda h: h.dma_start(out=Wfo, in_=wfo_d.rearrange("(q p) n -> p q n", p=128)), w=[BW4[0]], sem="w4a")
        dma("gpsimd", lambda h: h.dma_start(out=Wso, in_=wso_d.rearrange("(q p) n -> p q n", p=128)), w=[BW4[1]], sem="w4b")
        dma("gpsimd", lambda h: h.dma_start(out=Wg_, in_=w_in_d[:, 1024:3072].rearrange("(kc p) n -> p kc n", p=128)), w=[BW4[2]], sem="w4c")
        dma("gpsimd", lambda h: h.dma_start(out=Wout, in_=wout_d.rearrange("(kc p) n -> p kc n", p=128)), w=[BW4[3]], sem="w4d")
        hT4 = A5.alloc([8, 512], BF16)
        BhT4 = Buf()
        xq = [A5.alloc([1024], F32) for _ in range(4)]
        Bxq = [Buf() for _ in range(4)]
        xn4 = [A5.alloc([1024], BF16) for _ in range(2)]
        Bxn4 = [Buf() for _ in range(2)]
        mg = A5.alloc([8, 512], BF16)
        Bmg = Buf()
        g1bc = A5.alloc([1024], F32)
        g2bc = A5.alloc([1024], F32)
        Bgg = Buf()
        dma("sync", lambda h: h.dma_start(out=g1bc, in_=g1_d.partition_broadcast(128)), w=[Bgg], sem="g1l")
        Bgg2 = Buf()
        dma("sync", lambda h: h.dma_start(out=g2bc, in_=g2_d.partition_broadcast(128)), w=[Bgg2], sem="g2l")
        junk4 = A5.alloc([1024], BF16)
        stat4 = A5.alloc([64], F32)
        s12 = [[A5.alloc([512], BF16) for _ in range(2)] for _ in range(2)]
        t12 = [[A5.alloc([512], BF16) for _ in range(2)] for _ in range(2)]
        Bs12 = [Buf(), Buf()]
        x2t = [A5.alloc([1024], F32) for _ in range(2)]
        Bx2 = [Buf(), Buf()]
        hn32 = [A5.alloc([1024], F32) for _ in range(2)]
        Bhn32 = [Buf(), Buf()]
        hn16 = [A5.alloc([1024], BF16) for _ in range(2)]
        Bhn16 = [Buf(), Buf()]
        hnT = A5.alloc([8, 128], F32)
        BhnT = Buf()
        Wr = A5.alloc([8, 72], F32)
        BWr = Buf()
        dma("sync", lambda h: h.dma_start(out=Wr, in_=wr_d.rearrange("(kc p) n -> p kc n", p=128)), w=[BWr], sem="wrl")
        brb = A5.alloc([72], F32)
        Bbr = Buf()
        dma("sync", lambda h: h.dma_start(out=brb, in_=br_d.partition_broadcast(128)), w=[Bbr], sem="brl")
        Lg = A5.alloc([72], F32)
        rs = A5.alloc([512], F32)
        Brs = Buf()
        Cacc = A5.alloc([64], F32)
        BCacc = Buf()
        vop(lambda h: h.memset(Cacc, 0.0), w=[BCacc])
        Bxpad = Buf()
        Bx2d = Buf()

        def norm4(xsrc_ap, xt_ap, bxt, xn_ap, bxn, col, gb, bg, qsem):
            dma("sync", lambda h: h.dma_start(out=xt_ap, in_=xsrc_ap), w=[bxt], sem=qsem)
            aop(lambda h: h.activation(out=junk4, in_=xt_ap, func=AF.Square, accum_out=stat4[:, col:col + 1]), r=[bxt], w=[Bjunk, Bstat])
            aop(lambda h: h.activation(out=stat4[:, col + 1:col + 2], in_=stat4[:, col:col + 1], func=AF.Sqrt, scale=1.0 / 1024, bias=1e-6), r=[Bstat], w=[Bstat])
            vop(lambda h: h.reciprocal(out=stat4[:, col + 2:col + 3], in_=stat4[:, col + 1:col + 2]), r=[Bstat], w=[Bstat])
            vop(lambda h: h.scalar_tensor_tensor(out=xn_ap, in0=xt_ap, scalar=stat4[:, col + 2:col + 3], in1=gb, op0=ALU.mult, op1=ALU.mult), r=[bxt, Bstat, bg], w=[bxn])

        tcount = 0
        for tb in range(4):
            for t in range(4):
                tg = tb * 4 + t
                ni_ = t % 2
                col = (tg % 8) * 4
                norm4(x_d[tg * 128:(tg + 1) * 128, :], xq[t], Bxq[t], xn4[ni_], Bxn4[ni_], col, g1bc, Bgg, "xq%d" % t)
                bk = ni_
                for kc in range(8):
                    pop(lambda h, kc=kc, bk=bk, ni_=ni_: h.transpose(out=PSB[bk][:, kc * 128:(kc + 1) * 128], in_=xn4[ni_][:, kc * 128:(kc + 1) * 128], identity=k16("ident")), r=[Bxn4[ni_], Bc16], w=[PB[bk]])
                aop(lambda h, t=t, bk=bk: h.activation(out=hT4[:, :, t * 128:(t + 1) * 128], in_=PSB[bk].rearrange("p (a b) -> p a b", b=128), func=AF.Copy), r=[PB[bk]], w=[BhT4])
            tsl = slice(tb * 512, (tb + 1) * 512)
            for dc in range(8):
                pz = dc % 2
                bA, bB, bC, bD = (0, 1, 2, 3) if pz == 0 else (4, 5, 6, 7)
                dsl = slice(dc * 128, (dc + 1) * 128)
                dsl2 = slice(1024 + dc * 128, 1024 + (dc + 1) * 128)
                for q in range(4):
                    pop(lambda h, q=q, bA=bA, dsl=dsl, tsl=tsl: h.matmul(PSF[bA], lhsT=Wfo[:, q, dsl], rhs=FT[:, q, tsl], start=(q == 0), stop=(q == 3)), r=[BW4[0], BFT], w=[PB[bA]])
                for q in range(4):
                    pop(lambda h, q=q, bB=bB, dsl=dsl, tsl=tsl: h.matmul(PSF[bB], lhsT=Wso[:, q, dsl], rhs=yT[:, q, tsl], start=(q == 0), stop=(q == 3)), r=[BW4[1], ByT], w=[PB[bB]])
                for kc in range(8):
                    pop(lambda h, kc=kc, bC=bC, dsl=dsl: h.matmul(PSF[bC], lhsT=Wg_[:, kc, dsl], rhs=hT4[:, kc, :], start=(kc == 0), stop=(kc == 7)), r=[BW4[2], BhT4], w=[PB[bC]])
                for kc in range(8):
                    pop(lambda h, kc=kc, bD=bD, dsl2=dsl2: h.matmul(PSF[bD], lhsT=Wg_[:, kc, dsl2], rhs=hT4[:, kc, :], start=(kc == 0), stop=(kc == 7)), r=[BW4[2], BhT4], w=[PB[bD]])
                aop(lambda h, pz=pz, bC=bC: h.activation(out=s12[pz][0], in_=PSF[bC], func=AF.Sigmoid), r=[PB[bC]], w=[Bs12[pz]])
                aop(lambda h, pz=pz, bD=bD: h.activation(out=s12[pz][1], in_=PSF[bD], func=AF.Sigmoid), r=[PB[bD]], w=[Bs12[pz]])
                vop(lambda h, pz=pz, bA=bA: h.tensor_tensor(out=t12[pz][0], in0=PSF[bA], in1=s12[pz][0], op=ALU.mult), r=[PB[bA], Bs12[pz]], w=[Bs12[pz]])
                vop(lambda h, pz=pz, bB=bB: h.tensor_tensor(out=t12[pz][1], in0=PSF[bB], in1=s12[pz][1], op=ALU.mult), r=[PB[bB], Bs12[pz]], w=[Bs12[pz]])
                gop(lambda h, pz=pz, dc=dc: h.tensor_tensor(out=mg[:, dc, :], in0=t12[pz][0], in1=t12[pz][1], op=ALU.add), r=[Bs12[pz]], w=[Bmg])
            for t in range(4):
                tg = tb * 4 + t
                u = tg % 2
                bk0, bk1 = (0, 1) if u == 0 else (2, 3)
                for hh, bk in ((0, bk0), (1, bk1)):
                    for dc in range(8):
                        pop(lambda h, dc=dc, hh=hh, bk=bk, t=t: h.matmul(PSF[bk], lhsT=mg[:, dc, t * 128:(t + 1) * 128], rhs=Wout[:, dc, hh * 512:(hh + 1) * 512], start=(dc == 0), stop=(dc == 7)), r=[Bmg, BW4[3]], w=[PB[bk]])
                for hh, bk in ((0, bk0), (1, bk1)):
                    vop(lambda h, hh=hh, bk=bk, t=t, u=u: h.tensor_tensor(out=x2t[u][:, hh * 512:(hh + 1) * 512], in0=PSF[bk], in1=xq[t][:, hh * 512:(hh + 1) * 512], op=ALU.add), r=[PB[bk], Bxq[t]], w=[Bx2[u]])
                if STAGE == 1:
                    dma("sync", lambda h, tg=tg, u=u: h.dma_start(out=out_d[tg * 128:(tg + 1) * 128, :], in_=x2t[u]), r=[Bx2[u]], sem="os%d" % u)
                    continue
                dma("sync", lambda h, tg=tg, u=u: h.dma_start(out=x2_d[tg * 128:(tg + 1) * 128, :], in_=x2t[u]), r=[Bx2[u]], w=[Bx2d], sem="x2s%d" % u)
                col = 32 + (tg % 4) * 4
                aop(lambda h, u=u, col=col: h.activation(out=junk4, in_=x2t[u], func=AF.Square, accum_out=stat4[:, col:col + 1]), r=[Bx2[u]], w=[Bjunk, Bstat])
                aop(lambda h, col=col: h.activation(out=stat4[:, col + 1:col + 2], in_=stat4[:, col:col + 1], func=AF.Sqrt, scale=1.0 / 1024, bias=1e-6), r=[Bstat], w=[Bstat])
                vop(lambda h, col=col: h.reciprocal(out=stat4[:, col + 2:col + 3], in_=stat4[:, col + 1:col + 2]), r=[Bstat], w=[Bstat])
                vop(lambda h, u=u, col=col: h.scalar_tensor_tensor(out=hn32[u], in0=x2t[u], scalar=stat4[:, col + 2:col + 3], in1=g2bc, op0=ALU.mult, op1=ALU.mult), r=[Bx2[u], Bstat, Bgg2], w=[Bhn32[u]])
                gop(lambda h, u=u: h.tensor_copy(out=hn16[u], in_=hn32[u]), r=[Bhn32[u]], w=[Bhn16[u]])
                for kc in range(8):
                    bk = 4 + kc // 4
                    o = (kc % 4) * 128
                    pop(lambda h, kc=kc, bk=bk, o=o, u=u: h.transpose(out=PSF[bk][:, o:o + 128], in_=hn32[u][:, kc * 128:(kc + 1) * 128], identity=k32("ident")), r=[Bhn32[u], Bc32], w=[PB[bk]])
                aop(lambda h: h.activation(out=flat(hnT[:, 0:4, :]), in_=PSF[4], func=AF.Copy), r=[PB[4]], w=[BhnT])
                vop(lambda h: h.tensor_copy(out=flat(hnT[:, 4:8, :]), in_=PSF[5]), r=[PB[5]], w=[BhnT])
                for kc in range(8):
                    pop(lambda h, kc=kc: h.matmul(PSF[6][:, 0:72], lhsT=hnT[:, kc, :], rhs=Wr[:, kc, :], start=(kc == 0), stop=(kc == 7)), r=[BhnT, BWr], w=[PB[6]])
                R = [Brs]
                L8, L64 = Lg[:, 0:8], Lg[:, 8:72]
                m8, ohg, negm, ex, sumg, pg = rs[:, 0:8], rs[:, 8:16], rs[:, 16:17], rs[:, 24:32], rs[:, 17:18], rs[:, 18:19]
                tmp64, esel, m8e = rs[:, 64:128], rs[:, 32:40], rs[:, 40:48]
                dv, w1 = rs[:, 19:20], rs[:, 20:21]
                mk1, mk2, msk, slot = rs[:, 128:192], rs[:, 192:256], rs[:, 256:320], rs[:, 320:384]
                idf = rs[:, 48:50]
                vop(lambda h: h.tensor_tensor(out=Lg, in0=PSF[6][:, 0:72], in1=brb, op=ALU.add), r=[PB[6], Bbr], w=R)
                vop(lambda h: h.max(out=m8, in_=L8), r=R, w=R)
                vop(lambda h: h.tensor_scalar(out=ohg, in0=L8, scalar1=m8[:, 0:1], scalar2=None, op0=ALU.is_equal), r=R, w=R)
                vop(lambda h: h.tensor_scalar(out=negm, in0=m8[:, 0:1], scalar1=-1.0, scalar2=None, op0=ALU.mult), r=R, w=R)
                aop(lambda h: h.activation(out=ex, in_=L8, func=AF.Exp, bias=negm, scale=1.0, accum_out=sumg), r=R, w=R)
                vop(lambda h: h.reciprocal(out=pg, in_=sumg), r=R, w=R)
                vop(lambda h: h.tensor_tensor(out=tmp64.rearrange("p (g j) -> p g j", j=8), in0=L64.rearrange("p (g j) -> p g j", j=8), in1=ohg.unsqueeze(2).to_broadcast([128, 8, 8]), op=ALU.mult), r=R, w=R)
                vop(lambda h: h.tensor_reduce(out=esel, in_=tmp64.rearrange("p (g j) -> p j g", j=8), axis=AX.X, op=ALU.add), r=R, w=R)
                vop(lambda h: h.max(out=m8e, in_=esel), r=R, w=R)
                vop(lambda h: h.tensor_tensor(out=dv, in0=m8e[:, 1:2], in1=m8e[:, 0:1], op=ALU.subtract), r=R, w=R)
                aop(lambda h: h.activation(out=w1, in_=dv, func=AF.Sigmoid, scale=-1.0), r=R, w=R)
                vop(lambda h, tg=tg: h.tensor_tensor(out=gw[:, tg, 0:1], in0=pg, in1=w1, op=ALU.mult), r=R, w=[Bgw])
                vop(lambda h, tg=tg: h.tensor_tensor(out=gw[:, tg, 1:2], in0=pg, in1=gw[:, tg, 0:1], op=ALU.subtract), r=R + [Bgw], w=[Bgw])
                ohb = ohg.unsqueeze(2).to_broadcast([128, 8, 8])
                vop(lambda h: h.tensor_scalar(out=mk1, in0=L64, scalar1=m8e[:, 0:1], scalar2=None, op0=ALU.is_equal), r=R, w=R)
                vop(lambda h, ohb=ohb: h.tensor_tensor(out=mk1.rearrange("p (g j) -> p g j", j=8), in0=mk1.rearrange("p (g j) -> p g j", j=8), in1=ohb, op=ALU.mult), r=R, w=R)
                vop(lambda h: h.tensor_scalar(out=mk2, in0=L64, scalar1=m8e[:, 1:2], scalar2=None, op0=ALU.is_equal), r=R, w=R)
                vop(lambda h, ohb=ohb: h.tensor_tensor(out=mk2.rearrange("p (g j) -> p g j", j=8), in0=mk2.rearrange("p (g j) -> p g j", j=8), in1=ohb, op=ALU.mult), r=R, w=R)
                vop(lambda h: h.tensor_tensor(out=msk, in0=mk1, in1=mk2, op=ALU.add), r=R, w=R)
                pop(lambda h: h.matmul(PSF[7][:, 0:64], lhsT=k32("triS"), rhs=msk, start=True, stop=False), r=R + [Bc32], w=[PB[7]])
                pop(lambda h: h.matmul(PSF[7][:, 0:64], lhsT=k32("ones"), rhs=Cacc, start=False, stop=True), r=[BCacc, Bc32], w=[PB[7]])
                vop(lambda h: h.tensor_tensor(out=slot, in0=PSF[7][:, 0:64], in1=k32("ecap", 64), op=ALU.add), r=[PB[7], Bc32], w=R)
                vop(lambda h: h.tensor_tensor(out=Cacc, in0=Cacc, in1=msk, op=ALU.add), r=R + [BCacc], w=[BCacc])
                vop(lambda h: h.tensor_tensor(out=mk1, in0=mk1, in1=slot, op=ALU.mult), r=R, w=R)
                vop(lambda h: h.reduce_sum(out=idf[:, 0:1], in_=mk1, axis=AX.X), r=R, w=R)
                vop(lambda h: h.tensor_tensor(out=mk2, in0=mk2, in1=slot, op=ALU.mult), r=R, w=R)
                vop(lambda h: h.reduce_sum(out=idf[:, 1:2], in_=mk2, axis=AX.X), r=R, w=R)
                vop(lambda h, tg=tg: h.tensor_copy(out=idx[:, tg, :], in_=idf), r=R, w=[Bidx])
                for kk in range(2):
                    dma("gpsimd", lambda h, tg=tg, kk=kk, u=u: h.indirect_dma_start(out=xpad_d, out_offset=bass.IndirectOffsetOnAxis(ap=idx[:, tg, kk:kk + 1], axis=0), in_=hn16[u], in_offset=None, bounds_check=breg(h), oob_is_err=False), r=[Bhn16[u], Bidx], w=[Bxpad], sem="scat")
        S.barrier()
        if STAGE >= 2:
            A6 = Arena(arena_t[:, persist0:ARENA_BYTES], ARENA_BYTES - persist0)
            NWB = 2
            Wge = [A6.alloc([8, 512], BF16) for _ in range(NWB)]
            Wue = [A6.alloc([8, 512], BF16) for _ in range(NWB)]
            Wde = [A6.alloc([4, 1024], BF16) for _ in range(NWB)]
            Sge = [A6.alloc([8, 512], F32) for _ in range(NWB)]
            Sue = [A6.alloc([8, 512], F32) for _ in range(NWB)]
            Sde = [A6.alloc([4, 1024], F32) for _ in range(NWB)]
            BSge = [Buf() for _ in range(NWB)]
            BSue = [Buf() for _ in range(NWB)]
            BSde = [Buf() for _ in range(NWB)]
            BWge = [Buf() for _ in range(NWB)]
            BWue = [Buf() for _ in range(NWB)]
            BWde = [Buf() for _ in range(NWB)]
            Xe = [A6.alloc([1024], BF16) for _ in range(2)]
            BXe = [Buf(), Buf()]
            XeT = [A6.alloc([8, 128], BF16) for _ in range(2)]
            BXeT = [Buf(), Buf()]
            Gs = [A6.alloc([512], BF16) for _ in range(2)]
            BGs = [Buf(), Buf()]
            Aa = [A6.alloc([512], BF16) for _ in range(2)]
            BAa = [Buf(), Buf()]
            AT = [A6.alloc([4, 128], BF16) for _ in range(2)]
            BAT = [Buf(), Buf()]
            Ye = [A6.alloc([1024], F32) for _ in range(2)]
            BYe = [Buf(), Buf()]
            Bypad = Buf()
            def issue_loads(e):
                wb = e % NWB
                dma("sync", lambda h, e=e, wb=wb: h.dma_start(out=Sge[wb], in_=ewg_d[e].rearrange("(kc p) n -> p kc n", p=128)), w=[BSge[wb]], sem="wg%d" % wb)
                dma("scalar", lambda h, e=e, wb=wb: h.dma_start(out=Sue[wb], in_=ewu_d[e].rearrange("(kc p) n -> p kc n", p=128)), w=[BSue[wb]], sem="wu%d" % wb)
                dma("sync", lambda h, e=e, wb=wb: h.dma_start(out=Sde[wb], in_=ewd_d[e].rearrange("(hc p) n -> p hc n", p=128)), w=[BSde[wb]], sem="wd%d" % wb)

            for e in range(min(NWB, MOE_LIMIT)):
                issue_loads(e)
            for e in range(MOE_LIMIT):
                wb = e % NWB
                u = e % 2
                aop(lambda h, wb=wb: h.activation(out=flat(Wge[wb]), in_=flat(Sge[wb]), func=AF.Copy), r=[BSge[wb]], w=[BWge[wb]])
                gop(lambda h, wb=wb: h.tensor_copy(out=flat(Wue[wb]), in_=flat(Sue[wb])), r=[BSue[wb]], w=[BWue[wb]])
                vop(lambda h, wb=wb: h.tensor_copy(out=flat(Wde[wb]), in_=flat(Sde[wb])), r=[BSde[wb]], w=[BWde[wb]])
                if e + NWB < MOE_LIMIT:
                    issue_loads(e + NWB)
                dma("sync", lambda h, e=e, u=u: h.dma_start(out=Xe[u], in_=xpad_d[e * CAP:(e + 1) * CAP, :]), r=[Bxpad], w=[BXe[u]], sem="xe%d" % u)
                bkT = 0 if u == 0 else 4
                for kc in range(8):
                    pop(lambda h, kc=kc, u=u, bkT=bkT: h.transpose(out=PSB[bkT][:, kc * 128:(kc + 1) * 128], in_=Xe[u][:, kc * 128:(kc + 1) * 128], identity=k16("ident")), r=[BXe[u], Bc16], w=[PB[bkT]])
                vop(lambda h, u=u, bkT=bkT: h.tensor_copy(out=flat(XeT[u]), in_=PSB[bkT]), r=[PB[bkT]], w=[BXeT[u]])
                bG, bU = (1, 2) if u == 0 else (5, 6)
                for kc in range(8):
                    pop(lambda h, kc=kc, u=u, wb=wb, bG=bG: h.matmul(PSF[bG], lhsT=XeT[u][:, kc, :], rhs=Wge[wb][:, kc, :], start=(kc == 0), stop=(kc == 7)), r=[BXeT[u], BWge[wb]], w=[PB[bG]])
                for kc in range(8):
                    pop(lambda h, kc=kc, u=u, wb=wb, bU=bU: h.matmul(PSF[bU], lhsT=XeT[u][:, kc, :], rhs=Wue[wb][:, kc, :], start=(kc == 0), stop=(kc == 7)), r=[BXeT[u], BWue[wb]], w=[PB[bU]])
                aop(lambda h, u=u, bG=bG: h.activation(out=Gs[u], in_=PSF[bG], func=AF.Silu), r=[PB[bG]], w=[BGs[u]])
                vop(lambda h, u=u, bU=bU: h.tensor_tensor(out=Aa[u], in0=PSF[bU], in1=Gs[u], op=ALU.mult), r=[PB[bU], BGs[u]], w=[BAa[u]])
                bA = 3 if u == 0 else 7
                for hc in range(4):
                    pop(lambda h, hc=hc, u=u, bA=bA: h.transpose(out=PSB[bA][:, hc * 128:(hc + 1) * 128], in_=Aa[u][:, hc * 128:(hc + 1) * 128], identity=k16("ident")), r=[BAa[u], Bc16], w=[PB[bA]])
                aop(lambda h, u=u, bA=bA: h.activation(out=flat(AT[u]), in_=PSB[bA][:, 0:512], func=AF.Copy), r=[PB[bA]], w=[BAT[u]])
                for hh, bk in ((0, bG), (1, bU)):
                    for hc in range(4):
                        pop(lambda h, hc=hc, hh=hh, bk=bk, u=u, wb=wb: h.matmul(PSF[bk], lhsT=AT[u][:, hc, :], rhs=Wde[wb][:, hc, hh * 512:(hh + 1) * 512], start=(hc == 0), stop=(hc == 3)), r=[BAT[u], BWde[wb]], w=[PB[bk]])
                vop(lambda h, u=u, bG=bG: h.tensor_copy(out=Ye[u][:, 0:512], in_=PSF[bG]), r=[PB[bG]], w=[BYe[u]])
                aop(lambda h, u=u, bU=bU: h.activation(out=Ye[u][:, 512:1024], in_=PSF[bU], func=AF.Copy), r=[PB[bU]], w=[BYe[u]])
                dma("sync", lambda h, e=e, u=u: h.dma_start(out=ypad_d[e * CAP:(e + 1) * CAP, :], in_=Ye[u]), r=[BYe[u]], w=[Bypad], sem="ys%d" % u)
            S.barrier()
            A6.release(0)
            gfbc = A6.alloc([1024], F32)
            Bgf = Buf()
            dma("sync", lambda h: h.dma_start(out=gfbc, in_=gf_d.partition_broadcast(128)), w=[Bgf], sem="gfl")
            y1 = [A6.alloc([1024], F32) for _ in range(2)]
            y2 = [A6.alloc([1024], F32) for _ in range(2)]
            xx = [A6.alloc([1024], F32) for _ in range(2)]
            oo = [A6.alloc([1024], F32) for _ in range(2)]
            By1 = [Buf(), Buf()]
            By2 = [Buf(), Buf()]
            Bxx = [Buf(), Buf()]
            Boo = [Buf(), Buf()]
            junk6 = A6.alloc([1024], BF16)
            Bj6 = Buf()
            st6 = A6.alloc([64], F32)
            Bst6 = Buf()
            for tg in range(16):
                u = tg % 2
                dma("gpsimd", lambda h, tg=tg, u=u: h.indirect_dma_start(out=y1[u], out_offset=None, in_=ypad_d, in_offset=bass.IndirectOffsetOnAxis(ap=idx[:, tg, 0:1], axis=0), bounds_check=breg(h), oob_is_err=False), r=[Bypad, Bidx], w=[By1[u]], sem="ga%d" % u)
                dma("gpsimd", lambda h, tg=tg, u=u: h.indirect_dma_start(out=y2[u], out_offset=None, in_=ypad_d, in_offset=bass.IndirectOffsetOnAxis(ap=idx[:, tg, 1:2], axis=0), bounds_check=breg(h), oob_is_err=False), r=[Bypad, Bidx], w=[By2[u]], sem="gb%d" % u)
                dma("sync", lambda h, tg=tg, u=u: h.dma_start(out=xx[u], in_=x2_d[tg * 128:(tg + 1) * 128, :]), r=[Bx2d], w=[Bxx[u]], sem="xl%d" % u)
                vop(lambda h, tg=tg, u=u: h.scalar_tensor_tensor(out=xx[u], in0=y1[u], scalar=gw[:, tg, 0:1], in1=xx[u], op0=ALU.mult, op1=ALU.add), r=[By1[u], Bgw, Bxx[u]], w=[Bxx[u]])
                vop(lambda h, tg=tg, u=u: h.scalar_tensor_tensor(out=xx[u], in0=y2[u], scalar=gw[:, tg, 1:2], in1=xx[u], op0=ALU.mult, op1=ALU.add), r=[By2[u], Bgw, Bxx[u]], w=[Bxx[u]])
                col = (tg % 8) * 4
                aop(lambda h, u=u, col=col: h.activation(out=junk6, in_=xx[u], func=AF.Square, accum_out=st6[:, col:col + 1]), r=[Bxx[u]], w=[Bj6, Bst6])
                aop(lambda h, col=col: h.activation(out=st6[:, col + 1:col + 2], in_=st6[:, col:col + 1], func=AF.Sqrt, scale=1.0 / 1024, bias=1e-6), r=[Bst6], w=[Bst6])
                vop(lambda h, col=col: h.reciprocal(out=st6[:, col + 2:col + 3], in_=st6[:, col + 1:col + 2]), r=[Bst6], w=[Bst6])
                vop(lambda h, u=u, col=col: h.scalar_tensor_tensor(out=oo[u], in0=xx[u], scalar=st6[:, col + 2:col + 3], in1=gfbc, op0=ALU.mult, op1=ALU.mult), r=[Bxx[u], Bst6, Bgf], w=[Boo[u]])
                dma("sync", lambda h, tg=tg, u=u: h.dma_start(out=out_d[tg * 128:(tg + 1) * 128, :], in_=oo[u]), r=[Boo[u]], sem="os%d" % u)
        S.barrier()
        S.run()
    return nc, dbg_outs


def _consts():
    bf = ml_dtypes.bfloat16
    p = np.arange(128)
    c16 = np.zeros((128, C16["n"]), np.float32)
    c16[:, C16["ident"]:C16["ident"] + 128] = np.eye(128)
    c16[:, C16["triL"]:C16["triL"] + 128] = (p[:, None] <= p[None, :])
    c16[:, C16["triU"]:C16["triU"] + 128] = (p[:, None] >= p[None, :])
    c16[127, C16["selL"]:C16["selL"] + 128] = 1.0
    c16[0, C16["selF"]:C16["selF"] + 128] = 1.0
    ang = 2.0 * np.pi * ((p[:, None] * p[None, :]) % 128) / 128.0
    c16[:, C16["CC"]:C16["CC"] + 128] = np.cos(ang)
    c16[:, C16["SS"]:C16["SS"] + 128] = np.sin(ang)
    c32 = np.zeros((128, C32["n"]), np.float32)
    c32[:, C32["ident"]:C32["ident"] + 128] = np.eye(128)
    c32[:, C32["triS"]:C32["triS"] + 128] = (p[:, None] < p[None, :])
    c32[:, C32["ones"]:C32["ones"] + 128] = 1.0
    jj = p // 16
    c32[:, C32["mask0"]:C32["mask0"] + 128] = (jj[None, :] >= jj[:, None])
    c32[:, C32["mask1"]:C32["mask1"] + 128] = (jj[:, None] >= jj[None, :])
    c32[:, C32["kf"]] = p + 1
    c32[:, C32["kb"]] = 128 - p
    c32[:, C32["ev"]:C32["ev"] + 17] = np.arange(-8, 9)[None, :]
    c32[:, C32["evr"]:C32["evr"] + 17] = np.arange(8, -9, -1)[None, :]
    c32[:, C32["ecap"]:C32["ecap"] + 64] = (np.arange(64) * CAP)[None, :]
    c32[:, C32["iota"]:C32["iota"] + 128] = p[None, :]
    return c16.astype(bf), c32


def _dft_tables(hf):
    bf = ml_dtypes.bfloat16
    L = 4096
    base = np.arange(L, dtype=np.float64) * (2.0 * np.pi / L)
    sc = 1.0 / math.sqrt(L * 128.0)
    cosb = (np.cos(base) * sc).astype(np.float32)
    sinb = (-np.sin(base) * sc).astype(np.float32)
    t = np.arange(L, dtype=np.int64)
    m = np.arange(L // 2, dtype=np.int64)
    l = t if hf == 0 else (L - 1 - t)
    k = m if hf == 0 else (L - 1 - m)
    prod = (l[:, None] * k[None, :]) % L
    return cosb[prod].astype(bf), sinb[prod].astype(bf)


_CACHE = {}


def kernel(**inp):
    f32 = np.float32
    x = np.asarray(inp["x"], f32)
    if "nc" not in _CACHE:
        _CACHE["nc"] = build_program()
        _CACHE["c"] = _consts()
        _CACHE["tab"] = [_dft_tables(0), _dft_tables(1)]
    nc, dbg_outs = _CACHE["nc"]
    c16, c32 = _CACHE["c"]
    shared = {
        "mix_norm_g": np.ascontiguousarray(inp["mix_norm_g"][0], f32),
        "w_in": np.ascontiguousarray(inp["w_in"][0], f32),
        "w_fourier_out": np.ascontiguousarray(inp["w_fourier_out"][0], f32),
        "ssm_D": np.ascontiguousarray(inp["ssm_D"][0], f32),
        "ssm_w_glu": np.ascontiguousarray(inp["ssm_w_glu"][0], f32),
        "w_ssm_out": np.ascontiguousarray(inp["w_ssm_out"][0], f32),
        "w_out": np.ascontiguousarray(inp["w_out"][0], f32),
        "ffn_norm_g": np.ascontiguousarray(inp["ffn_norm_g"][0], f32),
        "w_router": np.ascontiguousarray(np.concatenate([inp["router_group_w"][0], inp["router_expert_w"][0]], axis=1), f32),
        "b_router": np.ascontiguousarray(np.concatenate([inp["router_group_b"][0], inp["router_expert_b"][0]], axis=0), f32),
        "final_norm_g": np.ascontiguousarray(inp["final_norm_g"], f32),
        "cst16": c16,
        "cst32": c32,
    }
    if STAGE >= 2:
        shared["expert_w_gate"] = np.ascontiguousarray(inp["expert_w_gate"][0], f32)
        shared["expert_w_up"] = np.ascontiguousarray(inp["expert_w_up"][0], f32)
        shared["expert_w_down"] = np.ascontiguousarray(inp["expert_w_down"][0], f32)
    in_maps = []
    for c in range(8):
        b, hf = c // 2, c % 2
        dsel = [0, 1] if hf == 0 else [1, 0]
        xl = x[b] if hf == 0 else x[b][::-1]
        m = dict(shared)
        m["x"] = np.ascontiguousarray(xl, f32)
        m["sA_re"] = np.ascontiguousarray(inp["ssm_A_re"][0][dsel], f32)
        m["sA_im"] = np.ascontiguousarray(inp["ssm_A_im"][0][dsel], f32)
        m["s_ldt"] = np.ascontiguousarray(inp["ssm_log_dt"][0][dsel], f32)
        m["sB_re"] = np.ascontiguousarray(inp["ssm_B_re"][0][dsel], f32)
        m["sB_im"] = np.ascontiguousarray(inp["ssm_B_im"][0][dsel], f32)
        m["sC_re"] = np.ascontiguousarray(inp["ssm_C_re"][0][dsel], f32)
        m["sC_im"] = np.ascontiguousarray(inp["ssm_C_im"][0][dsel], f32)
        m["tab_c"], m["tab_s"] = _CACHE["tab"][hf]
        in_maps.append(m)
    res = run_bass_kernel_spmd(nc, in_maps, core_ids=list(range(8)))
    out = np.empty((4, 4096, 1024), f32)
    for c in range(8):
        b, hf = c // 2, c % 2
        o = np.asarray(res.results[c]["out"], f32)
        if hf == 0:
            out[b, 0:2048] = o
        else:
            out[b, 2048:4096] = o[::-1]
    _CACHE["last"] = res
    return out
```

```python
import math
from contextlib import ExitStack

import ml_dtypes
import numpy as np

import concourse.bass as bass
import concourse.mybir as mybir
from concourse.bass_utils import run_bass_kernel_spmd

F32 = mybir.dt.float32
BF16 = mybir.dt.bfloat16
I32 = mybir.dt.int32
U8 = mybir.dt.uint8
ALU = mybir.AluOpType
AF = mybir.ActivationFunctionType
AX = mybir.AxisListType

ENGS = ["sync", "scalar", "vector", "gpsimd", "tensor"]
TWO_PI = 2.0 * math.pi
PI_SAFE = 3.14159
CAP = 128
NEXP = 64
STAGE = 2
DEBUG = False
MOE_LIMIT = 64


class Tok:
    __slots__ = ("kind", "eng", "n")

    def __init__(self, kind, eng, n):
        self.kind = kind
        self.eng = eng
        self.n = n


class Buf:
    def __init__(self, name=""):
        self.name = name
        self.w = []
        self.r = []


class Sched:
    def __init__(self, nc, stack):
        self.nc = nc
        self.stack = stack
        self.ops = {e: [] for e in ENGS}
        self.cnt = {e: 0 for e in ENGS}
        self.sem = {e: stack.enter_context(nc.semaphore("c_" + e)) for e in ENGS}
        self.waited = {e: {} for e in ENGS}
        self.dsem = {}
        self.dcnt = {}

    def _waits(self, eng, deps):
        waits = []
        for d in deps:
            if d is None:
                continue
            if d.kind == "eng":
                if d.eng == eng and eng == "tensor":
                    continue
                key = d.eng
                sem = self.sem[d.eng]
            else:
                key = "d_" + d.eng
                sem = self.dsem[d.eng]
            if self.waited[eng].get(key, 0) >= d.n:
                continue
            self.waited[eng][key] = d.n
            waits.append((sem, d.n))
        return waits

    @staticmethod
    def _deps(reads, writes, extra):
        deps = list(extra)
        for b in reads:
            deps += b.w
        for b in writes:
            deps += b.w
            deps += b.r
        return deps

    @staticmethod
    def _note(tok, reads, writes):
        for b in reads:
            b.r = [t for t in b.r if not (t.kind == tok.kind and t.eng == tok.eng)] + [tok]
        for b in writes:
            b.w = [tok]
            b.r = []

    def op(self, eng, build, reads=(), writes=(), extra=()):
        waits = self._waits(eng, self._deps(reads, writes, extra))
        self.cnt[eng] += 1
        n = self.cnt[eng]
        sem = self.sem[eng]

        def fn(h):
            for (s, v) in waits:
                h.wait_ge(s, v)
            build(h).then_inc(sem, 1)

        self.ops[eng].append(fn)
        tok = Tok("eng", eng, n)
        self._note(tok, reads, writes)
        return tok

    def dma(self, queue, semname, build, reads=(), writes=(), extra=()):
        if semname not in self.dsem:
            self.dsem[semname] = self.stack.enter_context(self.nc.semaphore("d_" + semname))
            self.dcnt[semname] = 0
        waits = self._waits(queue, self._deps(reads, writes, extra))
        self.dcnt[semname] += 16
        n = self.dcnt[semname]
        sem = self.dsem[semname]

        def fn(h):
            for (s, v) in waits:
                h.wait_ge(s, v)
            build(h).then_inc(sem, 16)

        self.ops[queue].append(fn)
        tok = Tok("dma", semname, n)
        self._note(tok, reads, writes)
        return tok

    def all_toks(self):
        t = [Tok("eng", e, self.cnt[e]) for e in ENGS if self.cnt[e] > 0]
        t += [Tok("dma", k, v) for k, v in self.dcnt.items() if v > 0]
        return t

    def barrier(self):
        toks = self.all_toks()
        for e in ENGS:
            waits = self._waits(e, toks)

            def fn(h, waits=waits):
                for (s, v) in waits:
                    h.wait_ge(s, v)

            self.ops[e].append(fn)

    def run(self):
        with self.nc.Block() as block:
            for e in ENGS:
                ops = self.ops[e]

                def body(h, ops=ops):
                    for fn in ops:
                        fn(h)

                getattr(block, e)(body)


class Arena:
    def __init__(self, ap_u8, size):
        self.ap = ap_u8
        self.size = size
        self.top = 0

    def alloc(self, free_shape, dt):
        isz = {F32: 4, I32: 4, BF16: 2}[dt]
        n = 1
        for s in free_shape:
            n *= s
        nb = n * isz
        off = self.top
        self.top += (nb + 63) // 64 * 64
        assert self.top <= self.size, ("SBUF arena overflow", self.top, self.size)
        v = self.ap[:, off:off + nb].bitcast(dt)
        if len(free_shape) > 1:
            names = ["a%d" % i for i in range(len(free_shape))]
            kw = {names[i]: free_shape[i] for i in range(1, len(free_shape))}
            v = v.rearrange("p (%s) -> p %s" % (" ".join(names), " ".join(names)), **kw)
        return v

    def mark(self):
        return self.top

    def release(self, m):
        self.top = m


def flat(ap):
    nd = len(ap.shape)
    if nd == 2:
        return ap
    names = ["a%d" % i for i in range(nd - 1)]
    return ap.rearrange("p %s -> p (%s)" % (" ".join(names), " ".join(names)))


C16 = dict(ident=0, triL=128, triU=256, selL=384, selF=512, CC=640, SS=768, n=896)
C32 = dict(ident=0, triS=128, ones=256, mask0=384, mask1=512, kf=640, kb=641, ev=642, ecap=659, iota=723, evr=851, n=868)


def build_program():
    nc = bass.Bass("TRN2", target_bir_lowering=False)

    def din(name, shape, dt=F32):
        return nc.dram_tensor(name, list(shape), dt, kind="ExternalInput").ap()

    x_d = din("x", [4096, 1024])
    g1_d = din("mix_norm_g", [1024])
    w_in_d = din("w_in", [1024, 3072])
    wfo_d = din("w_fourier_out", [512, 1024])
    sAre_d = din("sA_re", [2, 32, 64])
    sAim_d = din("sA_im", [2, 32, 64])
    sldt_d = din("s_ldt", [2, 32])
    sBre_d = din("sB_re", [2, 32, 64, 16])
    sBim_d = din("sB_im", [2, 32, 64, 16])
    sCre_d = din("sC_re", [2, 32, 16, 64])
    sCim_d = din("sC_im", [2, 32, 16, 64])
    sD_d = din("ssm_D", [512])
    wglu_d = din("ssm_w_glu", [512, 512])
    wso_d = din("w_ssm_out", [512, 1024])
    wout_d = din("w_out", [1024, 1024])
    g2_d = din("ffn_norm_g", [1024])
    wr_d = din("w_router", [1024, 72])
    br_d = din("b_router", [72])
    if STAGE >= 2:
        ewg_d = din("expert_w_gate", [64, 1024, 512])
        ewu_d = din("expert_w_up", [64, 1024, 512])
        ewd_d = din("expert_w_down", [64, 512, 1024])
    gf_d = din("final_norm_g", [1024])
    tc_d = din("tab_c", [8, 128, 32, 256], BF16)
    ts_d = din("tab_s", [8, 128, 32, 256], BF16)
    c16_d = din("cst16", [128, C16["n"]], BF16)
    c32_d = din("cst32", [128, C32["n"]])
    out_d = nc.dram_tensor("out", [2048, 1024], F32, kind="ExternalOutput").ap()
    xpad_d = nc.dram_tensor("xpad", [NEXP * CAP, 1024], BF16, kind="Internal").ap()
    ypad_d = nc.dram_tensor("ypad", [NEXP * CAP, 1024], F32, kind="Internal").ap()
    x2_d = nc.dram_tensor("x2s", [2048, 1024], F32, kind="Internal").ap()
    dbg_outs = {}

    with ExitStack() as st:
        S = Sched(nc, st)
        ARENA_BYTES = 206 * 1024
        arena_t = st.enter_context(nc.sbuf_tensor("arena", [128, ARENA_BYTES], U8))
        AR = Arena(arena_t[:, :], ARENA_BYTES)
        PSF, PSB, PB = [], [], []
        for i in range(8):
            pt = st.enter_context(nc.psum_tensor("ps%d" % i, [128, 512], F32))
            PSF.append(pt[:, :])
            PSB.append(pt[:, :].bitcast(BF16))
            PB.append(Buf("ps%d" % i))

        def vop(fn, r=(), w=(), x=()):
            return S.op("vector", fn, reads=r, writes=w, extra=x)

        def aop(fn, r=(), w=(), x=()):
            return S.op("scalar", fn, reads=r, writes=w, extra=x)

        def gop(fn, r=(), w=(), x=()):
            return S.op("gpsimd", fn, reads=r, writes=w, extra=x)

        def pop(fn, r=(), w=(), x=()):
            return S.op("tensor", fn, reads=r, writes=w, extra=x)

        _dq = [0]
        _breg = []

        def breg(h):
            if not _breg:
                _breg.append(h.to_reg(NEXP * CAP - 1))
            return _breg[0]

        def dma(q, fn, r=(), w=(), x=(), sem=None):
            if sem is None:
                _dq[0] += 1
                sem = "q%d" % (_dq[0] % 8)
            return S.dma(q, sem, fn, reads=r, writes=w, extra=x)

        def dbg(name, ap, shape):
            if not DEBUG:
                return
            d = nc.dram_tensor("dbg_" + name, list(shape), ap.dtype, kind="ExternalOutput").ap()
            dbg_outs[name] = d
            b = Buf()
            S.barrier()
            dma("sync", lambda h: h.dma_start(out=d, in_=ap), w=[b], sem="dbg")
            S.barrier()

        c16 = AR.alloc([C16["n"]], BF16)
        c32 = AR.alloc([C32["n"]], F32)
        Bc16, Bc32 = Buf(), Buf()
        dma("sync", lambda h: h.dma_start(out=c16, in_=c16_d), w=[Bc16])
        dma("sync", lambda h: h.dma_start(out=c32, in_=c32_d), w=[Bc32])

        def k16(name):
            o = C16[name]
            return c16[:, o:o + 128]

        def k32(name, n=128):
            o = C32[name]
            return c32[:, o:o + n]

        gw = AR.alloc([16, 2], F32)
        idx = AR.alloc([16, 2], I32)
        Bgw, Bidx = Buf(), Buf()
        persist0 = AR.top
        Min = AR.alloc([2, 32, 128], BF16)
        Q16 = AR.alloc([2, 32, 128], BF16)
        Mi = AR.alloc([32, 128], BF16)
        BMin, BQ16, BMi = Buf(), Buf(), Buf()
        ft_off = AR.top
        FT = AR.alloc([4, 2048], BF16)
        yT = AR.alloc([4, 2048], BF16)
        xs_off = AR.top
        Xs = AR.alloc([32, 512], BF16)
        uf_off = AR.top
        uf_tm = AR.alloc([32, 512], BF16)
        persist_mark = AR.mark()

        AR.release(ft_off)
        are = AR.alloc([64], F32)
        aim = AR.alloc([64], F32)
        dtn = AR.alloc([64], F32)
        Bre = AR.alloc([64, 16], F32)
        Bim = AR.alloc([64, 16], F32)
        Cre = AR.alloc([64, 16], F32)
        Cim = AR.alloc([64, 16], F32)
        Cld = AR.alloc([8, 2, 64], F32)
        Dp = AR.alloc([32], F32)
        Bp = Buf("params")
        for hf in range(2):
            ps_ = slice(hf * 64, hf * 64 + 64)
            dma("sync", lambda h, ps_=ps_: h.dma_start(out=are[ps_, :], in_=sAre_d.rearrange("d g n -> n (d g)"), allow_slow_non_contiguous=True), w=[Bp])
            dma("sync", lambda h, ps_=ps_: h.dma_start(out=aim[ps_, :], in_=sAim_d.rearrange("d g n -> n (d g)"), allow_slow_non_contiguous=True), w=[Bp])
            dma("sync", lambda h, ps_=ps_: h.dma_start(out=Bre[ps_, :, :], in_=sBre_d.rearrange("d g n c -> n (d g) c")), w=[Bp])
            dma("sync", lambda h, ps_=ps_: h.dma_start(out=Bim[ps_, :, :], in_=sBim_d.rearrange("d g n c -> n (d g) c")), w=[Bp])
        dma("sync", lambda h: h.dma_start(out=dtn, in_=sldt_d.rearrange("d g -> (d g)").partition_broadcast(128)), w=[Bp])
        for j in range(8):
            dma("sync", lambda h, j=j: h.dma_start(out=Dp[j * 16:(j + 1) * 16, :], in_=sD_d.rearrange("(g c) -> c g", c=16), allow_slow_non_contiguous=True), w=[Bp])
        Cld2 = AR.alloc([8, 2, 64], F32)
        for (src, cl) in ((sCre_d, Cld), (sCim_d, Cld2)):
            for dup in range(2):
                dma("sync", lambda h, src=src, dup=dup, cl=cl: h.dma_start(out=cl[:, :, dup, :], in_=src.rearrange("d g c n -> (d g c) n").rearrange("(t p) n -> p t n", p=128)), w=[Bp])
        S.barrier()
        for (cl, dst) in ((Cld, Cre), (Cld2, Cim)):
            for half in range(2):
                bk = half
                for t4 in range(4):
                    t = half * 4 + t4
                    pop(lambda h, t=t, t4=t4, bk=bk, cl=cl: h.transpose(out=PSF[bk][:, t4 * 128:(t4 + 1) * 128], in_=flat(cl[:, t, :, :]), identity=k32("ident")), r=[Bp, Bc32], w=[PB[bk]])
                vop(lambda h, dst=dst, half=half, bk=bk: h.tensor_copy(out=flat(dst)[:, half * 512:(half + 1) * 512], in_=PSF[bk]), r=[PB[bk]], w=[Bp])

        ev = k32("ev", 17)
        evr = k32("evr", 17)
        t0 = AR.alloc([32, 8, 16], F32)
        t1 = AR.alloc([32, 8, 16], F32)
        t0f, t1f = flat(t0), flat(t1)
        tA = t0f[:, 0:1088].rearrange("p (a b) -> p a b", b=64)
        tB = t0f[:, 1088:2176].rearrange("p (a b) -> p a b", b=64)
        tI = t0f[:, 2176:3264].bitcast(I32).rearrange("p (a b) -> p a b", b=64)
        Emag = t1f[:, 0:1088].rearrange("p (a b) -> p a b", b=64)
        Er = AR.alloc([17, 64], F32)
        Ei = AR.alloc([17, 64], F32)
        ErR = AR.alloc([17, 64], F32)
        EiR = AR.alloc([17, 64], F32)
        sm = [AR.alloc([64], F32) for _ in range(8)]
        BE = Buf("E")

        def sincos(xin, sin_out, cos_out, ti, tf, bufs):
            vop(lambda h: h.tensor_scalar(out=ti, in0=xin, scalar1=1.0 / TWO_PI, scalar2=None, op0=ALU.mult), r=bufs, w=bufs)
            vop(lambda h: h.tensor_copy(out=tf, in_=ti), r=bufs, w=bufs)
            vop(lambda h: h.scalar_tensor_tensor(out=tf, in0=tf, scalar=-TWO_PI, in1=xin, op0=ALU.mult, op1=ALU.add), r=bufs, w=bufs)
            vop(lambda h: h.tensor_scalar(out=tf, in0=tf, scalar1=PI_SAFE, scalar2=-PI_SAFE, op0=ALU.min, op1=ALU.max), r=bufs, w=bufs)
            aop(lambda h: h.activation(out=sin_out, in_=tf, func=AF.Sin), r=bufs, w=bufs)
            vop(lambda h: h.tensor_scalar(out=cos_out, in0=tf, scalar1=math.pi / 2, scalar2=-TWO_PI, op0=ALU.is_gt, op1=ALU.mult), r=bufs, w=bufs)
            vop(lambda h: h.scalar_tensor_tensor(out=cos_out, in0=tf, scalar=math.pi / 2, in1=cos_out, op0=ALU.add, op1=ALU.add), r=bufs, w=bufs)
            vop(lambda h: h.tensor_scalar(out=cos_out, in0=cos_out, scalar1=PI_SAFE, scalar2=-PI_SAFE, op0=ALU.min, op1=ALU.max), r=bufs, w=bufs)
            aop(lambda h: h.activation(out=cos_out, in_=cos_out, func=AF.Sin), r=bufs, w=bufs)

        PB0 = [Bp, BE, Bc32]
        aop(lambda h: h.activation(out=dtn, in_=dtn, func=AF.Exp), r=PB0, w=PB0)
        ar_, th_ = sm[0], sm[1]
        vop(lambda h: h.tensor_tensor(out=ar_, in0=are, in1=dtn, op=ALU.mult), r=PB0, w=PB0)
        vop(lambda h: h.tensor_tensor(out=th_, in0=aim, in1=dtn, op=ALU.mult), r=PB0, w=PB0)

        def etable(evc, Er_, Ei_):
            vop(lambda h: h.tensor_tensor(out=tA, in0=ar_.unsqueeze(1).to_broadcast([128, 17, 64]), in1=evc.unsqueeze(2).to_broadcast([128, 17, 64]), op=ALU.mult), r=PB0, w=PB0)
            aop(lambda h: h.activation(out=flat(Emag), in_=flat(tA), func=AF.Exp), r=PB0, w=PB0)
            vop(lambda h: h.tensor_tensor(out=tA, in0=th_.unsqueeze(1).to_broadcast([128, 17, 64]), in1=evc.unsqueeze(2).to_broadcast([128, 17, 64]), op=ALU.mult), r=PB0, w=PB0)
            sincos(flat(tA), flat(Ei_), flat(Er_), flat(tI), flat(tB), PB0)
            vop(lambda h: h.tensor_tensor(out=flat(Er_), in0=flat(Er_), in1=flat(Emag), op=ALU.mult), r=PB0, w=PB0)
            vop(lambda h: h.tensor_tensor(out=flat(Ei_), in0=flat(Ei_), in1=flat(Emag), op=ALU.mult), r=PB0, w=PB0)

        etable(ev, Er, Ei)
        etable(evr, ErR, EiR)
        am1, nr, ni, den, fr, fi = sm[2], sm[3], sm[4], sm[5], sm[6], sm[7]
        ar1, ai1 = Er[:, 9, :], Ei[:, 9, :]
        vop(lambda h: h.tensor_scalar(out=am1, in0=ar1, scalar1=-1.0, scalar2=None, op0=ALU.add), r=PB0, w=PB0)
        vop(lambda h: h.tensor_tensor(out=nr, in0=am1, in1=are, op=ALU.mult), r=PB0, w=PB0)
        vop(lambda h: h.tensor_tensor(out=den, in0=ai1, in1=aim, op=ALU.mult), r=PB0, w=PB0)
        vop(lambda h: h.tensor_tensor(out=nr, in0=nr, in1=den, op=ALU.add), r=PB0, w=PB0)
        vop(lambda h: h.tensor_tensor(out=ni, in0=ai1, in1=are, op=ALU.mult), r=PB0, w=PB0)
        vop(lambda h: h.tensor_tensor(out=den, in0=am1, in1=aim, op=ALU.mult), r=PB0, w=PB0)
        vop(lambda h: h.tensor_tensor(out=ni, in0=ni, in1=den, op=ALU.subtract), r=PB0, w=PB0)
        vop(lambda h: h.tensor_tensor(out=den, in0=are, in1=are, op=ALU.mult), r=PB0, w=PB0)
        vop(lambda h: h.tensor_tensor(out=am1, in0=aim, in1=aim, op=ALU.mult), r=PB0, w=PB0)
        vop(lambda h: h.tensor_tensor(out=den, in0=den, in1=am1, op=ALU.add), r=PB0, w=PB0)
        vop(lambda h: h.reciprocal(out=den, in_=den), r=PB0, w=PB0)
        vop(lambda h: h.tensor_tensor(out=fr, in0=nr, in1=den, op=ALU.mult), r=PB0, w=PB0)
        vop(lambda h: h.tensor_tensor(out=fi, in0=ni, in1=den, op=ALU.mult), r=PB0, w=PB0)
        bbr = AR.alloc([64, 16], F32)
        bbi = AR.alloc([64, 16], F32)
        tb1 = AR.alloc([64, 16], F32)

        def bc16(a):
            return a.unsqueeze(2).to_broadcast([128, a.shape[1], 16])

        vop(lambda h: h.tensor_tensor(out=bbr, in0=Bre, in1=bc16(fr), op=ALU.mult), r=PB0, w=PB0)
        vop(lambda h: h.tensor_tensor(out=tb1, in0=Bim, in1=bc16(fi), op=ALU.mult), r=PB0, w=PB0)
        vop(lambda h: h.tensor_tensor(out=bbr, in0=bbr, in1=tb1, op=ALU.subtract), r=PB0, w=PB0)
        vop(lambda h: h.tensor_tensor(out=bbi, in0=Bim, in1=bc16(fr), op=ALU.mult), r=PB0, w=PB0)
        vop(lambda h: h.tensor_tensor(out=tb1, in0=Bre, in1=bc16(fi), op=ALU.mult), r=PB0, w=PB0)
        vop(lambda h: h.tensor_tensor(out=bbi, in0=bbi, in1=tb1, op=ALU.add), r=PB0, w=PB0)

        PinN = AR.alloc([2, 32, 8, 16], BF16)
        P16 = AR.alloc([2, 32, 8, 16], BF16)
        BPin, BPm = Buf(), Buf()
        Bt_lo, Bt_hi = Buf(), Buf()
        Q16v = Q16.rearrange("p d g (i c) -> p d g i c", c=16)

        def cmul_batch(out_ap, outbuf, Ar, Ai, d, Tr_, Ti_, i0, mode):
            def vv(T, lo, hi):
                return T[lo:hi, i0:i0 + 8, d * 32:(d + 1) * 32].rearrange("p j g -> p g j").unsqueeze(3).to_broadcast([hi - lo, 32, 8, 16])

            def aa(A, lo, hi):
                return A[lo:hi, d * 32:(d + 1) * 32, :].unsqueeze(2).to_broadcast([hi - lo, 32, 8, 16])

            rr = PB0
            vop(lambda h: h.tensor_tensor(out=t0[0:64], in0=aa(Ar, 0, 64), in1=vv(Tr_, 0, 64), op=ALU.mult), r=rr, w=[Bt_lo])
            vop(lambda h: h.tensor_tensor(out=t1[0:64], in0=aa(Ai, 0, 64), in1=vv(Ti_, 0, 64), op=ALU.mult), r=rr, w=[Bt_lo])
            vop(lambda h: h.tensor_tensor(out=out_ap[0:64], in0=t0[0:64], in1=t1[0:64], op=ALU.subtract), r=[Bt_lo], w=[outbuf])
            gop(lambda h: h.tensor_tensor(out=t0[64:128], in0=aa(Ar, 64, 128), in1=vv(Ti_, 64, 128), op=ALU.mult), r=rr, w=[Bt_hi])
            gop(lambda h: h.tensor_tensor(out=t1[64:128], in0=aa(Ai, 64, 128), in1=vv(Tr_, 64, 128), op=ALU.mult), r=rr, w=[Bt_hi])
            if mode == "P":
                gop(lambda h: h.tensor_tensor(out=out_ap[64:128], in0=t0[64:128], in1=t1[64:128], op=ALU.add), r=[Bt_hi], w=[outbuf])
            else:
                gop(lambda h: h.tensor_tensor(out=t0[64:128], in0=t0[64:128], in1=t1[64:128], op=ALU.add), r=[Bt_hi], w=[Bt_hi])
                gop(lambda h: h.tensor_scalar(out=out_ap[64:128], in0=t0[64:128], scalar1=-1.0, scalar2=None, op0=ALU.mult), r=[Bt_hi], w=[outbuf])

        cmul_batch(PinN[:, 0], BPin, bbr, bbi, 0, ErR, EiR, 1, "P")
        cmul_batch(P16[:, 0], BPm, bbr, bbi, 0, ErR, EiR, 9, "P")
        cmul_batch(Q16v[:, 0], BQ16, Cre, Cim, 0, Er, Ei, 9, "Q")
        cmul_batch(PinN[:, 1], BPin, bbr, bbi, 1, Er, Ei, 8, "P")
        cmul_batch(P16[:, 1], BPm, bbr, bbi, 1, Er, Ei, 0, "P")
        cmul_batch(Q16v[:, 1], BQ16, Cre, Cim, 1, ErR, EiR, 0, "Q")
        for d in range(2):
            for g8 in range(4):
                bk = 2 + (g8 % 2)
                for gi in range(8):
                    g = g8 * 8 + gi
                    pop(lambda h, d=d, g=g, gi=gi, bk=bk: h.transpose(out=PSB[bk][:, gi * 128:(gi + 1) * 128], in_=flat(PinN[:, d, g, :, :]), identity=k16("ident")), r=[BPin, Bc16], w=[PB[bk]])
                aop(lambda h, d=d, g8=g8, bk=bk: h.activation(out=flat(Min[:, d, g8 * 8:(g8 + 1) * 8, :]), in_=PSB[bk], func=AF.Copy), r=[PB[bk]], w=[BMin])
        mt0 = AR.alloc([4, 128], F32)
        mt1 = AR.alloc([4, 128], F32)
        Bmt = Buf()
        for g4 in range(8):
            for d in range(2):
                bk = 4 + d
                for gi in range(4):
                    g = g4 * 4 + gi
                    pop(lambda h, d=d, g=g, gi=gi, bk=bk: h.matmul(PSF[bk][:, gi * 128:(gi + 1) * 128], lhsT=flat(P16[:, d, g, :, :]), rhs=Q16[:, d, g, :], start=True, stop=True), r=[BPm, BQ16], w=[PB[bk]])
            vop(lambda h: h.tensor_tensor(out=mt0, in0=PSF[4].rearrange("p (a b) -> p a b", b=128), in1=k32("mask0").unsqueeze(1).to_broadcast([128, 4, 128]), op=ALU.mult), r=[PB[4], Bc32], w=[Bmt])
            vop(lambda h: h.tensor_tensor(out=mt1, in0=PSF[5].rearrange("p (a b) -> p a b", b=128), in1=k32("mask1").unsqueeze(1).to_broadcast([128, 4, 128]), op=ALU.mult), r=[PB[5], Bc32], w=[Bmt])
            vop(lambda h: h.tensor_tensor(out=mt0, in0=mt0, in1=mt1, op=ALU.add), r=[Bmt], w=[Bmt])
            for gi in range(4):
                g = g4 * 4 + gi
                vop(lambda h, g=g, gi=gi: h.scalar_tensor_tensor(out=Mi[:, g, :], in0=k32("ident"), scalar=Dp[:, g:g + 1], in1=mt0[:, gi, :], op0=ALU.mult, op1=ALU.add), r=[Bmt, Bp, Bc32], w=[BMi])
        S.barrier()
        AR.release(persist_mark)

        Buf_uf, BX = Buf(), Buf()
        p1_mark = AR.mark()
        Wfs = AR.alloc([8, 1024], BF16)
        BWfs = Buf()
        dma("gpsimd", lambda h: h.dma_start(out=Wfs, in_=w_in_d[:, 0:1024].rearrange("(kc p) n -> p kc n", p=128)), w=[BWfs], sem="wfs")
        gbc = AR.alloc([1024], F32)
        Bg = Buf()
        dma("sync", lambda h: h.dma_start(out=gbc, in_=g1_d.partition_broadcast(128)), w=[Bg], sem="g1a")
        hT = AR.alloc([8, 1024], BF16)
        BhT = Buf()
        Zt = AR.alloc([32, 8, 16], BF16)
        BZ = Buf()
        NXB = 3
        xt = [AR.alloc([1024], F32) for _ in range(NXB)]
        Bxt = [Buf() for _ in range(NXB)]
        xn = [AR.alloc([1024], BF16) for _ in range(2)]
        Bxn = [Buf() for _ in range(2)]
        junk = AR.alloc([1024], BF16)
        Bjunk = Buf()
        stat = AR.alloc([64], F32)
        Bstat = Buf()

        def norm_tile(xsrc_ap, xt_ap, bxt, xn_ap, bxn, col, gb, bg, qsem):
            dma("sync", lambda h: h.dma_start(out=xt_ap, in_=xsrc_ap), w=[bxt], sem=qsem)
            aop(lambda h: h.activation(out=junk, in_=xt_ap, func=AF.Square, accum_out=stat[:, col:col + 1]), r=[bxt], w=[Bjunk, Bstat])
            aop(lambda h: h.activation(out=stat[:, col + 1:col + 2], in_=stat[:, col:col + 1], func=AF.Sqrt, scale=1.0 / 1024, bias=1e-6), r=[Bstat], w=[Bstat])
            vop(lambda h: h.reciprocal(out=stat[:, col + 2:col + 3], in_=stat[:, col + 1:col + 2]), r=[Bstat], w=[Bstat])
            vop(lambda h: h.scalar_tensor_tensor(out=xn_ap, in0=xt_ap, scalar=stat[:, col + 2:col + 3], in1=gb, op0=ALU.mult, op1=ALU.mult), r=[bxt, Bstat, bg], w=[bxn])

        ti_ = 0
        for b in range(4):
            for t in range(8):
                tg = b * 8 + t
                xi = ti_ % NXB
                ni_ = ti_ % 2
                col = (ti_ % 8) * 4
                ti_ += 1
                norm_tile(x_d[tg * 128:(tg + 1) * 128, :], xt[xi], Bxt[xi], xn[ni_], Bxn[ni_], col, gbc, Bg, "x%d" % xi)
                bk = ni_
                for kc in range(8):
                    pop(lambda h, kc=kc, bk=bk, ni_=ni_: h.transpose(out=PSB[bk][:, kc * 128:(kc + 1) * 128], in_=xn[ni_][:, kc * 128:(kc + 1) * 128], identity=k16("ident")), r=[Bxn[ni_], Bc16], w=[PB[bk]])
                aop(lambda h, t=t, bk=bk: h.activation(out=hT[:, :, t * 128:(t + 1) * 128], in_=PSB[bk].rearrange("p (a b) -> p a b", b=128), func=AF.Copy), r=[PB[bk]], w=[BhT])
            for t in range(8):
                tg = b * 8 + t
                bk = 2 + (t % 2)
                for kc in range(8):
                    pop(lambda h, kc=kc, t=t, bk=bk: h.matmul(PSF[bk], lhsT=hT[:, kc, t * 128:(t + 1) * 128], rhs=Wfs[:, kc, 0:512], start=(kc == 0), stop=(kc == 7)), r=[BhT, BWfs], w=[PB[bk]])
                vop(lambda h, tg=tg, bk=bk: h.tensor_copy(out=uf_tm[:, tg, :], in_=PSF[bk]), r=[PB[bk]], w=[Buf_uf])
            for j in range(8):
                bk = 4 + (j % 2)
                for kc in range(8):
                    pop(lambda h, kc=kc, j=j, bk=bk: h.matmul(PSF[bk], lhsT=hT[:, kc, j:1024:8], rhs=Wfs[:, kc, 512:1024], start=(kc == 0), stop=(kc == 7)), r=[BhT, BWfs], w=[PB[bk]])
                aop(lambda h, j=j, bk=bk: h.activation(out=Zt[:, :, j, :], in_=PSF[bk].rearrange("p (g c) -> p g c", c=16), func=AF.Copy), r=[PB[bk]], w=[BZ])
            for g8 in range(4):
                bk = 6 + (g8 % 2)
                for gi in range(8):
                    g = g8 * 8 + gi
                    pop(lambda h, g=g, gi=gi, bk=bk: h.transpose(out=PSB[bk][:, gi * 128:(gi + 1) * 128], in_=flat(Zt[:, g, :, :]), identity=k16("ident")), r=[BZ, Bc16], w=[PB[bk]])
                vop(lambda h, g8=g8, b=b, bk=bk: h.tensor_copy(out=Xs[:, g8 * 8:(g8 + 1) * 8, b * 128:(b + 1) * 128], in_=PSB[bk].rearrange("p (a b) -> p a b", b=128)), r=[PB[bk]], w=[BX])
        S.barrier()
        dbg("wfs", Wfs, [128, 8, 1024])
        dbg("ht", hT, [128, 8, 1024])
        dbg("xn", xn[1], [128, 1024])
        dbg("stat", stat, [128, 64])
        dbg("zt", Zt, [128, 32, 8, 16])
        dbg("min", Min, [128, 2, 32, 128])
        dbg("q16", Q16, [128, 2, 32, 128])
        dbg("mi", Mi, [128, 32, 128])
        AR.release(p1_mark)
        dbg("uf", uf_tm, [128, 32, 512])
        dbg("xs", Xs, [128, 32, 512])
        if STAGE == 0:
            S.barrier()
            S.run()
            return nc, dbg_outs

        BFT = Buf()
        p2_mark = AR.mark()
        NTB = 3
        tct = [AR.alloc([16, 256], BF16) for _ in range(NTB)]
        tst = [AR.alloc([16, 256], BF16) for _ in range(NTB)]
        Btc = [Buf() for _ in range(NTB)]
        Bts = [Buf() for _ in range(NTB)]
        UrT = AR.alloc([4, 256], BF16)
        UiT = AR.alloc([4, 256], BF16)
        BUU = Buf()
        it_ = 0
        for kb in range(8):
            for lt in range(32):
                l16 = lt % 16
                if l16 == 0:
                    bi = it_ % NTB
                    it_ += 1
                    hh_ = lt // 16
                    dma("sync", lambda h, bi=bi, hh_=hh_, kb=kb: h.dma_start(out=tct[bi], in_=tc_d[kb, :, hh_ * 16:(hh_ + 1) * 16, :]), w=[Btc[bi]], sem="tc%d" % bi)
                    dma("scalar", lambda h, bi=bi, hh_=hh_, kb=kb: h.dma_start(out=tst[bi], in_=ts_d[kb, :, hh_ * 16:(hh_ + 1) * 16, :]), w=[Bts[bi]], sem="ts%d" % bi)
                for g in range(4):
                    pop(lambda h, g=g, lt=lt, bi=bi, l16=l16: h.matmul(PSF[g][:, 0:256], lhsT=uf_tm[:, lt, g * 128:(g + 1) * 128], rhs=tct[bi][:, l16, :], start=(lt == 0), stop=(lt == 31)), r=[Buf_uf, Btc[bi]], w=[PB[g]])
                    pop(lambda h, g=g, lt=lt, bi=bi, l16=l16: h.matmul(PSF[4 + g][:, 0:256], lhsT=uf_tm[:, lt, g * 128:(g + 1) * 128], rhs=tst[bi][:, l16, :], start=(lt == 0), stop=(lt == 31)), r=[Buf_uf, Bts[bi]], w=[PB[4 + g]])
            for g in range(4):
                vop(lambda h, g=g: h.tensor_copy(out=UrT[:, g, :], in_=PSF[g][:, 0:256]), r=[PB[g]], w=[BUU])
                aop(lambda h, g=g: h.activation(out=UiT[:, g, :], in_=PSF[4 + g][:, 0:256], func=AF.Copy), r=[PB[4 + g]], w=[BUU])
            for g in range(4):
                pop(lambda h, g=g: h.matmul(PSF[g][:, 256:512], lhsT=k16("CC"), rhs=UrT[:, g, :], start=True, stop=False), r=[BUU, Bc16], w=[PB[g]])
                pop(lambda h, g=g: h.matmul(PSF[g][:, 256:512], lhsT=k16("SS"), rhs=UiT[:, g, :], start=False, stop=True), r=[BUU, Bc16], w=[PB[g]])
                vop(lambda h, g=g, kb=kb: h.tensor_copy(out=FT[:, g, kb * 256:(kb + 1) * 256], in_=PSF[g][:, 256:512]), r=[PB[g]], w=[BFT])
        S.barrier()
        AR.release(p2_mark)
        dbg("ft", FT, [128, 4, 2048])

        ByT = Buf()
        AR.release(uf_off)
        p3_mark = AR.mark()
        Wglu = AR.alloc([4, 512], BF16)
        BWglu = Buf()
        dma("gpsimd", lambda h: h.dma_start(out=Wglu, in_=wglu_d.rearrange("(q p) n -> p q n", p=128)), w=[BWglu], sem="wglu")
        pAre = AR.alloc([16, 64], F32)
        pAim = AR.alloc([16, 64], F32)
        pdt = AR.alloc([16], F32)
        Trp = AR.alloc([16, 64], F32)
        Tip = AR.alloc([16, 64], F32)
        Trm = AR.alloc([16, 64], F32)
        Tim = AR.alloc([16, 64], F32)
        tX = AR.alloc([16, 64], F32)
        tY = AR.alloc([16, 64], F32)
        tZi = AR.alloc([16, 64], I32)
        BT = Buf("tables")
        Hc = [AR.alloc([16, 2, 64], BF16) for _ in range(2)]
        BHc = [Buf(), Buf()]
        HTd = [AR.alloc([16, 257], BF16) for _ in range(2)]
        BHT = [Buf(), Buf()]
        Vt = [AR.alloc([4, 2, 64], BF16) for _ in range(2)]
        BV = [Buf(), Buf()]
        m1 = [AR.alloc([4, 2, 64], F32) for _ in range(2)]
        m2 = [AR.alloc([4, 2, 64], F32) for _ in range(2)]
        Bm = [Buf(), Buf()]
        Yg = AR.alloc([8, 256], BF16)
        BYg = Buf()
        sg = AR.alloc([4, 512], BF16)
        Bsg = Buf()

        def cplx_mul(src4, bsrc, Tr, Ti, k, out_re, out_im, bout):
            a, bb = m1[k], m2[k]
            Trb = Tr.unsqueeze(2).to_broadcast([128, 4, 2, 64])
            vop(lambda h: h.tensor_tensor(out=a, in0=src4, in1=Trb, op=ALU.mult), r=[BT, bsrc], w=[Bm[k]])
            vop(lambda h: h.tensor_tensor(out=bb[:, :, 0, :], in0=src4[:, :, 1, :], in1=Ti, op=ALU.mult), r=[BT, bsrc], w=[Bm[k]])
            vop(lambda h: h.tensor_tensor(out=bb[:, :, 1, :], in0=src4[:, :, 0, :], in1=Ti, op=ALU.mult), r=[BT, bsrc], w=[Bm[k]])
            gop(lambda h: h.tensor_tensor(out=out_re, in0=a[:, :, 0, :], in1=bb[:, :, 0, :], op=ALU.subtract), r=[Bm[k]], w=[bout])
            gop(lambda h: h.tensor_tensor(out=out_im, in0=a[:, :, 1, :], in1=bb[:, :, 1, :], op=ALU.add), r=[Bm[k]], w=[bout])

        for half in range(2):
            g_lo = half * 16
            for d in range(2):
                kcol = c32[:, C32["kf"] + d:C32["kf"] + d + 1]
                dma("sync", lambda h, d=d, g_lo=g_lo: h.dma_start(out=flat(pAre), in_=sAre_d[d, g_lo:g_lo + 16, :].rearrange("g n -> (g n)").partition_broadcast(128)), w=[BT], sem="pa")
                dma("sync", lambda h, d=d, g_lo=g_lo: h.dma_start(out=flat(pAim), in_=sAim_d[d, g_lo:g_lo + 16, :].rearrange("g n -> (g n)").partition_broadcast(128)), w=[BT], sem="pb")
                dma("sync", lambda h, d=d, g_lo=g_lo: h.dma_start(out=pdt, in_=sldt_d[d, g_lo:g_lo + 16].partition_broadcast(128)), w=[BT], sem="pc")
                TB = [BT, Bc32]
                xt3 = [Tok("dma", "pa", S.dcnt["pa"]), Tok("dma", "pb", S.dcnt["pb"]), Tok("dma", "pc", S.dcnt["pc"])]
                aop(lambda h: h.activation(out=pdt, in_=pdt, func=AF.Exp, scale=1.0), r=TB, w=TB, x=xt3)
                dtb = pdt.unsqueeze(2).to_broadcast([128, 16, 64])
                vop(lambda h, dtb=dtb: h.scalar_tensor_tensor(out=pAre, in0=pAre, scalar=8.0, in1=dtb, op0=ALU.mult, op1=ALU.mult), r=TB, w=TB, x=xt3)
                vop(lambda h, dtb=dtb: h.scalar_tensor_tensor(out=pAim, in0=pAim, scalar=8.0, in1=dtb, op0=ALU.mult, op1=ALU.mult), r=TB, w=TB)
                vop(lambda h: h.tensor_scalar(out=flat(tZi), in0=flat(pAim), scalar1=1.0 / TWO_PI, scalar2=None, op0=ALU.mult), r=TB, w=TB)
                vop(lambda h: h.tensor_copy(out=flat(tX), in_=flat(tZi)), r=TB, w=TB)
                vop(lambda h: h.scalar_tensor_tensor(out=flat(pAim), in0=flat(tX), scalar=-TWO_PI, in1=flat(pAim), op0=ALU.mult, op1=ALU.add), r=TB, w=TB)
                vop(lambda h, kcol=kcol: h.tensor_scalar(out=flat(tY), in0=flat(pAim), scalar1=kcol, scalar2=None, op0=ALU.mult), r=TB, w=TB)
                sincos(flat(tY), flat(Tip), flat(Trp), flat(tZi), flat(tX), TB)
                vop(lambda h, kcol=kcol: h.tensor_scalar(out=flat(tY), in0=flat(pAre), scalar1=kcol, scalar2=None, op0=ALU.mult), r=TB, w=TB)
                aop(lambda h: h.activation(out=flat(tX), in_=flat(tY), func=AF.Exp), r=TB, w=TB)
                aop(lambda h: h.activation(out=flat(tY), in_=flat(tY), func=AF.Exp, scale=-1.0), r=TB, w=TB)
                vop(lambda h: h.tensor_tensor(out=flat(Trm), in0=flat(Trp), in1=flat(tY), op=ALU.mult), r=TB, w=TB)
                vop(lambda h: h.scalar_tensor_tensor(out=flat(Tim), in0=flat(Tip), scalar=-1.0, in1=flat(tY), op0=ALU.mult, op1=ALU.mult), r=TB, w=TB)
                vop(lambda h: h.tensor_tensor(out=flat(Trp), in0=flat(Trp), in1=flat(tX), op=ALU.mult), r=TB, w=TB)
                vop(lambda h: h.tensor_tensor(out=flat(Tip), in0=flat(Tip), in1=flat(tX), op=ALU.mult), r=TB, w=TB)
                blocks = [0, 1] if d == 0 else [3, 2, 1, 0]
                tri = k16("triL") if d == 0 else k16("triU")
                sel = k16("selL") if d == 0 else k16("selF")
                if d == 0:
                    vop(lambda h: h.memset(HTd[0][:, :, 0:1], 0.0), r=[], w=[BHT[0]])
                for bi_, blk in enumerate(blocks):
                    cur, prv = bi_ % 2, (bi_ + 1) % 2
                    first = (bi_ == 0)
                    for c4 in range(4):
                        gl = c4 * 4
                        k = c4 % 2
                        bS, bW, bTp = (0, 1, 2) if k == 0 else (3, 4, 5)
                        for gi in range(4):
                            g = g_lo + gl + gi
                            pop(lambda h, g=g, gi=gi, blk=blk, d=d, bS=bS: h.matmul(PSF[bS][:, gi * 128:(gi + 1) * 128], lhsT=Xs[:, g, blk * 128:(blk + 1) * 128], rhs=Min[:, d, g, :], start=True, stop=True), r=[BX, BMin], w=[PB[bS]])
                        srcS = PSF[bS].rearrange("p (g h n) -> p g h n", h=2, n=64)
                        cplx_mul(srcS, PB[bS], Trm[:, gl:gl + 4, :], Tim[:, gl:gl + 4, :], k, Vt[k][:, :, 0, :], Vt[k][:, :, 1, :], BV[k])
                        pop(lambda h, k=k, bW=bW, first=first, tri=tri: h.matmul(PSF[bW], lhsT=tri, rhs=flat(Vt[k]), start=True, stop=first), r=[BV[k], Bc16], w=[PB[bW]])
                        if not first:
                            pop(lambda h, bW=bW, prv=prv, gl=gl, sel=sel: h.matmul(PSF[bW], lhsT=sel, rhs=flat(Hc[prv][:, gl:gl + 4, :, :]), start=False, stop=True), r=[BHc[prv], Bc16], w=[PB[bW]])
                        srcW = PSF[bW].rearrange("p (g h n) -> p g h n", h=2, n=64)
                        cplx_mul(srcW, PB[bW], Trp[:, gl:gl + 4, :], Tip[:, gl:gl + 4, :], k, Hc[cur][:, gl:gl + 4, 0, :], Hc[cur][:, gl:gl + 4, 1, :], BHc[cur])
                        if blk <= 2:
                            for gi in range(4):
                                pop(lambda h, gi=gi, gl=gl, cur=cur, bTp=bTp: h.transpose(out=PSB[bTp][:, gi * 128:(gi + 1) * 128], in_=flat(Hc[cur][:, gl + gi, :, :]), identity=k16("ident")), r=[BHc[cur], Bc16], w=[PB[bTp]])
                            srcT = PSB[bTp][:, 0:512].rearrange("p (a b) -> p a b", b=128)
                            if blk < 2:
                                co = blk * 128 + (1 if d == 0 else 0)
                                aop(lambda h, gl=gl, co=co, srcT=srcT, d=d: h.activation(out=HTd[d][:, gl:gl + 4, co:co + 128], in_=srcT, func=AF.Copy), r=[PB[bTp]], w=[BHT[d]])
                            else:
                                aop(lambda h, gl=gl, srcT=srcT, d=d: h.activation(out=HTd[d][:, gl:gl + 4, 256:257], in_=srcT[:, :, 0:1], func=AF.Copy), r=[PB[bTp]], w=[BHT[d]])
            for blk in range(2):
                for c4 in range(4):
                    gl = c4 * 4
                    bk = 6 + (c4 % 2)
                    for gi in range(4):
                        g = g_lo + gl + gi
                        o = gi * 128
                        pop(lambda h, g=g, o=o, blk=blk, bk=bk: h.matmul(PSF[bk][:, o:o + 128], lhsT=Xs[:, g, blk * 128:(blk + 1) * 128], rhs=Mi[:, g, :], start=True, stop=False), r=[BX, BMi], w=[PB[bk]])
                        pop(lambda h, g=g, o=o, blk=blk, bk=bk, gl=gl, gi=gi: h.matmul(PSF[bk][:, o:o + 128], lhsT=HTd[0][:, gl + gi, blk * 128:blk * 128 + 128], rhs=Q16[:, 0, g, :], start=False, stop=False), r=[BHT[0], BQ16], w=[PB[bk]])
                        pop(lambda h, g=g, o=o, blk=blk, bk=bk, gl=gl, gi=gi: h.matmul(PSF[bk][:, o:o + 128], lhsT=HTd[1][:, gl + gi, blk * 128 + 1:blk * 128 + 129], rhs=Q16[:, 1, g, :], start=False, stop=True), r=[BHT[1], BQ16], w=[PB[bk]])
                    aop(lambda h, bk=bk, gl=gl: h.activation(out=Yg[:, :, gl * 16:(gl + 4) * 16].rearrange("p i (g c) -> p g i c", c=16), in_=PSF[bk].rearrange("p (g i c) -> p g i c", i=8, c=16), func=AF.Gelu), r=[PB[bk]], w=[BYg])
                for qq in range(2):
                    q = 2 * half + qq
                    bk = 2 if qq == 0 else 5
                    for i in range(8):
                        pop(lambda h, i=i, qq=qq, bk=bk: h.transpose(out=PSB[bk][:, i * 128:(i + 1) * 128], in_=Yg[:, i, qq * 128:(qq + 1) * 128], identity=k16("ident")), r=[BYg, Bc16], w=[PB[bk]])
                    vop(lambda h, q=q, blk=blk, bk=bk: h.tensor_copy(out=yT[:, q, blk * 1024:(blk + 1) * 1024].rearrange("p (k i) -> p i k", i=8), in_=PSB[bk].rearrange("p (i k) -> p i k", k=128)), r=[PB[bk]], w=[ByT])
        dbg("yt", yT, [128, 4, 2048])
        for tb in range(4):
            for co in range(4):
                for q in range(4):
                    pop(lambda h, co=co, q=q, tb=tb: h.matmul(PSF[co], lhsT=Wglu[:, q, co * 128:(co + 1) * 128], rhs=yT[:, q, tb * 512:(tb + 1) * 512], start=(q == 0), stop=(q == 3)), r=[ByT, BWglu], w=[PB[co]])
                aop(lambda h, co=co: h.activation(out=sg[:, co, :], in_=PSF[co], func=AF.Sigmoid), r=[PB[co]], w=[Bsg])
            vop(lambda h, tb=tb: h.tensor_tensor(out=yT[:, :, tb * 512:(tb + 1) * 512], in0=yT[:, :, tb * 512:(tb + 1) * 512], in1=sg, op=ALU.mult), r=[Bsg, ByT], w=[ByT])
        S.barrier()
        AR.release(p3_mark)
        dbg("y2t", yT, [128, 4, 2048])

        A4 = Arena(arena_t[:, persist0:ft_off], ft_off - persist0)
        A5 = Arena(arena_t[:, xs_off:ARENA_BYTES], ARENA_BYTES - xs_off)
        Wg_ = A4.alloc([8, 2048], BF16)
        Wout = A5.alloc([8, 1024], BF16)
        Wfo = A4.alloc([4, 1024], BF16)
        Wso = A5.alloc([4, 1024], BF16)
        BW4 = [Buf() for _ in range(4)]
        dma("gpsimd", lambda h: h.dma_start(out=Wfo, in_=wfo_d.rearrange("(q p) n -> p q n", p=128)), w=[BW4[0]], sem="w4a")
        dma("gpsimd", lambda h: h.dma_start(out=Wso, in_=wso_d.rearrange("(q p) n -> p q n", p=128)), w=[BW4[1]], sem="w4b")
        dma("gpsimd", lambda h: h.dma_start(out=Wg_, in_=w_in_d[:, 1024:3072].rearrange("(kc p) n -> p kc n", p=128)), w=[BW4[2]], sem="w4c")
        dma("gpsimd", lambda h: h.dma_start(out=Wout, in_=wout_d.rearrange("(kc p) n -> p kc n", p=128)), w=[BW4[3]], sem="w4d")
        hT4 = A5.alloc([8, 512], BF16)
        BhT4 = Buf()
        xq = [A5.alloc([1024], F32) for _ in range(4)]
        Bxq = [Buf() for _ in range(4)]
        xn4 = [A5.alloc([1024], BF16) for _ in range(2)]
        Bxn4 = [Buf() for _ in range(2)]
        mg = A5.alloc([8, 512], BF16)
        Bmg = Buf()
        g1bc = A5.alloc([1024], F32)
        g2bc = A5.alloc([1024], F32)
        Bgg = Buf()
        dma("sync", lambda h: h.dma_start(out=g1bc, in_=g1_d.partition_broadcast(128)), w=[Bgg], sem="g1l")
        Bgg2 = Buf()
        dma("sync", lambda h: h.dma_start(out=g2bc, in_=g2_d.partition_broadcast(128)), w=[Bgg2], sem="g2l")
        junk4 = A5.alloc([1024], BF16)
        stat4 = A5.alloc([64], F32)
        s12 = [[A5.alloc([512], BF16) for _ in range(2)] for _ in range(2)]
        t12 = [[A5.alloc([512], BF16) for _ in range(2)] for _ in range(2)]
        Bs12 = [Buf(), Buf()]
        x2t = [A5.alloc([1024], F32) for _ in range(2)]
        Bx2 = [Buf(), Buf()]
        hn32 = [A5.alloc([1024], F32) for _ in range(2)]
        Bhn32 = [Buf(), Buf()]
        hn16 = [A5.alloc([1024], BF16) for _ in range(2)]
        Bhn16 = [Buf(), Buf()]
        hnT = A5.alloc([8, 128], F32)
        BhnT = Buf()
        Wr = A5.alloc([8, 72], F32)
        BWr = Buf()
        dma("sync", lambda h: h.dma_start(out=Wr, in_=wr_d.rearrange("(kc p) n -> p kc n", p=128)), w=[BWr], sem="wrl")
        brb = A5.alloc([72], F32)
        Bbr = Buf()
        dma("sync", lambda h: h.dma_start(out=brb, in_=br_d.partition_broadcast(128)), w=[Bbr], sem="brl")
        Lg = A5.alloc([72], F32)
        rs = A5.alloc([512], F32)
        Brs = Buf()
        Cacc = A5.alloc([64], F32)
        BCacc = Buf()
        vop(lambda h: h.memset(Cacc, 0.0), w=[BCacc])
        Bxpad = Buf()
        Bx2d = Buf()

        def norm4(xsrc_ap, xt_ap, bxt, xn_ap, bxn, col, gb, bg, qsem):
            dma("sync", lambda h: h.dma_start(out=xt_ap, in_=xsrc_ap), w=[bxt], sem=qsem)
            aop(lambda h: h.activation(out=junk4, in_=xt_ap, func=AF.Square, accum_out=stat4[:, col:col + 1]), r=[bxt], w=[Bjunk, Bstat])
            aop(lambda h: h.activation(out=stat4[:, col + 1:col + 2], in_=stat4[:, col:col + 1], func=AF.Sqrt, scale=1.0 / 1024, bias=1e-6), r=[Bstat], w=[Bstat])
            vop(lambda h: h.reciprocal(out=stat4[:, col + 2:col + 3], in_=stat4[:, col + 1:col + 2]), r=[Bstat], w=[Bstat])
            vop(lambda h: h.scalar_tensor_tensor(out=xn_ap, in0=xt_ap, scalar=stat4[:, col + 2:col + 3], in1=gb, op0=ALU.mult, op1=ALU.mult), r=[bxt, Bstat, bg], w=[bxn])

        tcount = 0
        for tb in range(4):
            for t in range(4):
                tg = tb * 4 + t
                ni_ = t % 2
                col = (tg % 8) * 4
                norm4(x_d[tg * 128:(tg + 1) * 128, :], xq[t], Bxq[t], xn4[ni_], Bxn4[ni_], col, g1bc, Bgg, "xq%d" % t)
                bk = ni_
                for kc in range(8):
                    pop(lambda h, kc=kc, bk=bk, ni_=ni_: h.transpose(out=PSB[bk][:, kc * 128:(kc + 1) * 128], in_=xn4[ni_][:, kc * 128:(kc + 1) * 128], identity=k16("ident")), r=[Bxn4[ni_], Bc16], w=[PB[bk]])
                aop(lambda h, t=t, bk=bk: h.activation(out=hT4[:, :, t * 128:(t + 1) * 128], in_=PSB[bk].rearrange("p (a b) -> p a b", b=128), func=AF.Copy), r=[PB[bk]], w=[BhT4])
            tsl = slice(tb * 512, (tb + 1) * 512)
            for dc in range(8):
                pz = dc % 2
                bA, bB, bC, bD = (0, 1, 2, 3) if pz == 0 else (4, 5, 6, 7)
                dsl = slice(dc * 128, (dc + 1) * 128)
                dsl2 = slice(1024 + dc * 128, 1024 + (dc + 1) * 128)
                for q in range(4):
                    pop(lambda h, q=q, bA=bA, dsl=dsl, tsl=tsl: h.matmul(PSF[bA], lhsT=Wfo[:, q, dsl], rhs=FT[:, q, tsl], start=(q == 0), stop=(q == 3)), r=[BW4[0], BFT], w=[PB[bA]])
                for q in range(4):
                    pop(lambda h, q=q, bB=bB, dsl=dsl, tsl=tsl: h.matmul(PSF[bB], lhsT=Wso[:, q, dsl], rhs=yT[:, q, tsl], start=(q == 0), stop=(q == 3)), r=[BW4[1], ByT], w=[PB[bB]])
                for kc in range(8):
                    pop(lambda h, kc=kc, bC=bC, dsl=dsl: h.matmul(PSF[bC], lhsT=Wg_[:, kc, dsl], rhs=hT4[:, kc, :], start=(kc == 0), stop=(kc == 7)), r=[BW4[2], BhT4], w=[PB[bC]])
                for kc in range(8):
                    pop(lambda h, kc=kc, bD=bD, dsl2=dsl2: h.matmul(PSF[bD], lhsT=Wg_[:, kc, dsl2], rhs=hT4[:, kc, :], start=(kc == 0), stop=(kc == 7)), r=[BW4[2], BhT4], w=[PB[bD]])
                aop(lambda h, pz=pz, bC=bC: h.activation(out=s12[pz][0], in_=PSF[bC], func=AF.Sigmoid), r=[PB[bC]], w=[Bs12[pz]])
                aop(lambda h, pz=pz, bD=bD: h.activation(out=s12[pz][1], in_=PSF[bD], func=AF.Sigmoid), r=[PB[bD]], w=[Bs12[pz]])
                vop(lambda h, pz=pz, bA=bA: h.tensor_tensor(out=t12[pz][0], in0=PSF[bA], in1=s12[pz][0], op=ALU.mult), r=[PB[bA], Bs12[pz]], w=[Bs12[pz]])
                vop(lambda h, pz=pz, bB=bB: h.tensor_tensor(out=t12[pz][1], in0=PSF[bB], in1=s12[pz][1], op=ALU.mult), r=[PB[bB], Bs12[pz]], w=[Bs12[pz]])
                gop(lambda h, pz=pz, dc=dc: h.tensor_tensor(out=mg[:, dc, :], in0=t12[pz][0], in1=t12[pz][1], op=ALU.add), r=[Bs12[pz]], w=[Bmg])
            for t in range(4):
                tg = tb * 4 + t
                u = tg % 2
                bk0, bk1 = (0, 1) if u == 0 else (2, 3)
                for hh, bk in ((0, bk0), (1, bk1)):
                    for dc in range(8):
                        pop(lambda h, dc=dc, hh=hh, bk=bk, t=t: h.matmul(PSF[bk], lhsT=mg[:, dc, t * 128:(t + 1) * 128], rhs=Wout[:, dc, hh * 512:(hh + 1) * 512], start=(dc == 0), stop=(dc == 7)), r=[Bmg, BW4[3]], w=[PB[bk]])
                for hh, bk in ((0, bk0), (1, bk1)):
                    vop(lambda h, hh=hh, bk=bk, t=t, u=u: h.tensor_tensor(out=x2t[u][:, hh * 512:(hh + 1) * 512], in0=PSF[bk], in1=xq[t][:, hh * 512:(hh + 1) * 512], op=ALU.add), r=[PB[bk], Bxq[t]], w=[Bx2[u]])
                if STAGE == 1:
                    dma("sync", lambda h, tg=tg, u=u: h.dma_start(out=out_d[tg * 128:(tg + 1) * 128, :], in_=x2t[u]), r=[Bx2[u]], sem="os%d" % u)
                    continue
                dma("sync", lambda h, tg=tg, u=u: h.dma_start(out=x2_d[tg * 128:(tg + 1) * 128, :], in_=x2t[u]), r=[Bx2[u]], w=[Bx2d], sem="x2s%d" % u)
                col = 32 + (tg % 4) * 4
                aop(lambda h, u=u, col=col: h.activation(out=junk4, in_=x2t[u], func=AF.Square, accum_out=stat4[:, col:col + 1]), r=[Bx2[u]], w=[Bjunk, Bstat])
                aop(lambda h, col=col: h.activation(out=stat4[:, col + 1:col + 2], in_=stat4[:, col:col + 1], func=AF.Sqrt, scale=1.0 / 1024, bias=1e-6), r=[Bstat], w=[Bstat])
                vop(lambda h, col=col: h.reciprocal(out=stat4[:, col + 2:col + 3], in_=stat4[:, col + 1:col + 2]), r=[Bstat], w=[Bstat])
                vop(lambda h, u=u, col=col: h.scalar_tensor_tensor(out=hn32[u], in0=x2t[u], scalar=stat4[:, col + 2:col + 3], in1=g2bc, op0=ALU.mult, op1=ALU.mult), r=[Bx2[u], Bstat, Bgg2], w=[Bhn32[u]])
                gop(lambda h, u=u: h.tensor_copy(out=hn16[u], in_=hn32[u]), r=[Bhn32[u]], w=[Bhn16[u]])
                for kc in range(8):
                    bk = 4 + kc // 4
                    o = (kc % 4) * 128
                    pop(lambda h, kc=kc, bk=bk, o=o, u=u: h.transpose(out=PSF[bk][:, o:o + 128], in_=hn32[u][:, kc * 128:(kc + 1) * 128], identity=k32("ident")), r=[Bhn32[u], Bc32], w=[PB[bk]])
                aop(lambda h: h.activation(out=flat(hnT[:, 0:4, :]), in_=PSF[4], func=AF.Copy), r=[PB[4]], w=[BhnT])
                vop(lambda h: h.tensor_copy(out=flat(hnT[:, 4:8, :]), in_=PSF[5]), r=[PB[5]], w=[BhnT])
                for kc in range(8):
                    pop(lambda h, kc=kc: h.matmul(PSF[6][:, 0:72], lhsT=hnT[:, kc, :], rhs=Wr[:, kc, :], start=(kc == 0), stop=(kc == 7)), r=[BhnT, BWr], w=[PB[6]])
                R = [Brs]
                L8, L64 = Lg[:, 0:8], Lg[:, 8:72]
                m8, ohg, negm, ex, sumg, pg = rs[:, 0:8], rs[:, 8:16], rs[:, 16:17], rs[:, 24:32], rs[:, 17:18], rs[:, 18:19]
                tmp64, esel, m8e = rs[:, 64:128], rs[:, 32:40], rs[:, 40:48]
                dv, w1 = rs[:, 19:20], rs[:, 20:21]
                mk1, mk2, msk, slot = rs[:, 128:192], rs[:, 192:256], rs[:, 256:320], rs[:, 320:384]
                idf = rs[:, 48:50]
                vop(lambda h: h.tensor_tensor(out=Lg, in0=PSF[6][:, 0:72], in1=brb, op=ALU.add), r=[PB[6], Bbr], w=R)
                vop(lambda h: h.max(out=m8, in_=L8), r=R, w=R)
                vop(lambda h: h.tensor_scalar(out=ohg, in0=L8, scalar1=m8[:, 0:1], scalar2=None, op0=ALU.is_equal), r=R, w=R)
                vop(lambda h: h.tensor_scalar(out=negm, in0=m8[:, 0:1], scalar1=-1.0, scalar2=None, op0=ALU.mult), r=R, w=R)
                aop(lambda h: h.activation(out=ex, in_=L8, func=AF.Exp, bias=negm, scale=1.0, accum_out=sumg), r=R, w=R)
                vop(lambda h: h.reciprocal(out=pg, in_=sumg), r=R, w=R)
                vop(lambda h: h.tensor_tensor(out=tmp64.rearrange("p (g j) -> p g j", j=8), in0=L64.rearrange("p (g j) -> p g j", j=8), in1=ohg.unsqueeze(2).to_broadcast([128, 8, 8]), op=ALU.mult), r=R, w=R)
                vop(lambda h: h.tensor_reduce(out=esel, in_=tmp64.rearrange("p (g j) -> p j g", j=8), axis=AX.X, op=ALU.add), r=R, w=R)
                vop(lambda h: h.max(out=m8e, in_=esel), r=R, w=R)
                vop(lambda h: h.tensor_tensor(out=dv, in0=m8e[:, 1:2], in1=m8e[:, 0:1], op=ALU.subtract), r=R, w=R)
                aop(lambda h: h.activation(out=w1, in_=dv, func=AF.Sigmoid, scale=-1.0), r=R, w=R)
                vop(lambda h, tg=tg: h.tensor_tensor(out=gw[:, tg, 0:1], in0=pg, in1=w1, op=ALU.mult), r=R, w=[Bgw])
                vop(lambda h, tg=tg: h.tensor_tensor(out=gw[:, tg, 1:2], in0=pg, in1=gw[:, tg, 0:1], op=ALU.subtract), r=R + [Bgw], w=[Bgw])
                ohb = ohg.unsqueeze(2).to_broadcast([128, 8, 8])
                vop(lambda h: h.tensor_scalar(out=mk1, in0=L64, scalar1=m8e[:, 0:1], scalar2=None, op0=ALU.is_equal), r=R, w=R)
                vop(lambda h, ohb=ohb: h.tensor_tensor(out=mk1.rearrange("p (g j) -> p g j", j=8), in0=mk1.rearrange("p (g j) -> p g j", j=8), in1=ohb, op=ALU.mult), r=R, w=R)
                vop(lambda h: h.tensor_scalar(out=mk2, in0=L64, scalar1=m8e[:, 1:2], scalar2=None, op0=ALU.is_equal), r=R, w=R)
                vop(lambda h, ohb=ohb: h.tensor_tensor(out=mk2.rearrange("p (g j) -> p g j", j=8), in0=mk2.rearrange("p (g j) -> p g j", j=8), in1=ohb, op=ALU.mult), r=R, w=R)
                vop(lambda h: h.tensor_tensor(out=msk, in0=mk1, in1=mk2, op=ALU.add), r=R, w=R)
                pop(lambda h: h.matmul(PSF[7][:, 0:64], lhsT=k32("triS"), rhs=msk, start=True, stop=False), r=R + [Bc32], w=[PB[7]])
                pop(lambda h: h.matmul(PSF[7][:, 0:64], lhsT=k32("ones"), rhs=Cacc, start=False, stop=True), r=[BCacc, Bc32], w=[PB[7]])
                vop(lambda h: h.tensor_tensor(out=slot, in0=PSF[7][:, 0:64], in1=k32("ecap", 64), op=ALU.add), r=[PB[7], Bc32], w=R)
                vop(lambda h: h.tensor_tensor(out=Cacc, in0=Cacc, in1=msk, op=ALU.add), r=R + [BCacc], w=[BCacc])
                vop(lambda h: h.tensor_tensor(out=mk1, in0=mk1, in1=slot, op=ALU.mult), r=R, w=R)
                vop(lambda h: h.reduce_sum(out=idf[:, 0:1], in_=mk1, axis=AX.X), r=R, w=R)
                vop(lambda h: h.tensor_tensor(out=mk2, in0=mk2, in1=slot, op=ALU.mult), r=R, w=R)
                vop(lambda h: h.reduce_sum(out=idf[:, 1:2], in_=mk2, axis=AX.X), r=R, w=R)
                vop(lambda h, tg=tg: h.tensor_copy(out=idx[:, tg, :], in_=idf), r=R, w=[Bidx])
                for kk in range(2):
                    dma("gpsimd", lambda h, tg=tg, kk=kk, u=u: h.indirect_dma_start(out=xpad_d, out_offset=bass.IndirectOffsetOnAxis(ap=idx[:, tg, kk:kk + 1], axis=0), in_=hn16[u], in_offset=None, bounds_check=breg(h), oob_is_err=False), r=[Bhn16[u], Bidx], w=[Bxpad], sem="scat")
        S.barrier()
        if STAGE >= 2:
            A6 = Arena(arena_t[:, persist0:ARENA_BYTES], ARENA_BYTES - persist0)
            NWB = 2
            Wge = [A6.alloc([8, 512], BF16) for _ in range(NWB)]
            Wue = [A6.alloc([8, 512], BF16) for _ in range(NWB)]
            Wde = [A6.alloc([4, 1024], BF16) for _ in range(NWB)]
            Sge = [A6.alloc([8, 512], F32) for _ in range(NWB)]
            Sue = [A6.alloc([8, 512], F32) for _ in range(NWB)]
            Sde = [A6.alloc([4, 1024], F32) for _ in range(NWB)]
            BSge = [Buf() for _ in range(NWB)]
            BSue = [Buf() for _ in range(NWB)]
            BSde = [Buf() for _ in range(NWB)]
            BWge = [Buf() for _ in range(NWB)]
            BWue = [Buf() for _ in range(NWB)]
            BWde = [Buf() for _ in range(NWB)]
            Xe = [A6.alloc([1024], BF16) for _ in range(2)]
            BXe = [Buf(), Buf()]
            XeT = [A6.alloc([8, 128], BF16) for _ in range(2)]
            BXeT = [Buf(), Buf()]
            Gs = [A6.alloc([512], BF16) for _ in range(2)]
            BGs = [Buf(), Buf()]
            Aa = [A6.alloc([512], BF16) for _ in range(2)]
            BAa = [Buf(), Buf()]
            AT = [A6.alloc([4, 128], BF16) for _ in range(2)]
            BAT = [Buf(), Buf()]
            Ye = [A6.alloc([1024], F32) for _ in range(2)]
            BYe = [Buf(), Buf()]
            Bypad = Buf()
            def issue_loads(e):
                wb = e % NWB
                dma("sync", lambda h, e=e, wb=wb: h.dma_start(out=Sge[wb], in_=ewg_d[e].rearrange("(p kc) n -> p kc n", kc=8)), w=[BSge[wb]], sem="wg%d" % wb)
                dma("scalar", lambda h, e=e, wb=wb: h.dma_start(out=Sue[wb], in_=ewu_d[e].rearrange("(p kc) n -> p kc n", kc=8)), w=[BSue[wb]], sem="wu%d" % wb)
                dma("sync", lambda h, e=e, wb=wb: h.dma_start(out=Sde[wb], in_=ewd_d[e].rearrange("(p hc) n -> p hc n", hc=4)), w=[BSde[wb]], sem="wd%d" % wb)

            for e in range(min(NWB, MOE_LIMIT)):
                issue_loads(e)
            for e in range(MOE_LIMIT):
                wb = e % NWB
                u = e % 2
                aop(lambda h, wb=wb: h.activation(out=flat(Wge[wb]), in_=flat(Sge[wb]), func=AF.Copy), r=[BSge[wb]], w=[BWge[wb]])
                gop(lambda h, wb=wb: h.tensor_copy(out=flat(Wue[wb]), in_=flat(Sue[wb])), r=[BSue[wb]], w=[BWue[wb]])
                aop(lambda h, wb=wb: h.activation(out=flat(Wde[wb]), in_=flat(Sde[wb]), func=AF.Copy), r=[BSde[wb]], w=[BWde[wb]])
                if e + NWB < MOE_LIMIT:
                    issue_loads(e + NWB)
                dma("sync", lambda h, e=e, u=u: h.dma_start(out=Xe[u], in_=xpad_d[e * CAP:(e + 1) * CAP, :]), r=[Bxpad], w=[BXe[u]], sem="xe%d" % u)
                bkT = 0 if u == 0 else 4
                for kc in range(8):
                    pop(lambda h, kc=kc, u=u, bkT=bkT: h.transpose(out=PSB[bkT][:, kc * 128:(kc + 1) * 128], in_=Xe[u][:, kc:1024:8], identity=k16("ident")), r=[BXe[u], Bc16], w=[PB[bkT]])
                vop(lambda h, u=u, bkT=bkT: h.tensor_copy(out=flat(XeT[u]), in_=PSB[bkT]), r=[PB[bkT]], w=[BXeT[u]])
                bG, bU = (1, 2) if u == 0 else (5, 6)
                for kc in range(8):
                    pop(lambda h, kc=kc, u=u, wb=wb, bG=bG: h.matmul(PSF[bG], lhsT=XeT[u][:, kc, :], rhs=Wge[wb][:, kc, :], start=(kc == 0), stop=(kc == 7)), r=[BXeT[u], BWge[wb]], w=[PB[bG]])
                for kc in range(8):
                    pop(lambda h, kc=kc, u=u, wb=wb, bU=bU: h.matmul(PSF[bU], lhsT=XeT[u][:, kc, :], rhs=Wue[wb][:, kc, :], start=(kc == 0), stop=(kc == 7)), r=[BXeT[u], BWue[wb]], w=[PB[bU]])
                aop(lambda h, u=u, bG=bG: h.activation(out=Gs[u], in_=PSF[bG], func=AF.Silu), r=[PB[bG]], w=[BGs[u]])
                vop(lambda h, u=u, bU=bU: h.tensor_tensor(out=Aa[u], in0=PSF[bU], in1=Gs[u], op=ALU.mult), r=[PB[bU], BGs[u]], w=[BAa[u]])
                bA = 3 if u == 0 else 7
                for hc in range(4):
                    pop(lambda h, hc=hc, u=u, bA=bA: h.transpose(out=PSB[bA][:, hc * 128:(hc + 1) * 128], in_=Aa[u][:, hc:512:4], identity=k16("ident")), r=[BAa[u], Bc16], w=[PB[bA]])
                aop(lambda h, u=u, bA=bA: h.activation(out=flat(AT[u]), in_=PSB[bA][:, 0:512], func=AF.Copy), r=[PB[bA]], w=[BAT[u]])
                for hh, bk in ((0, bG), (1, bU)):
                    for hc in range(4):
                        pop(lambda h, hc=hc, hh=hh, bk=bk, u=u, wb=wb: h.matmul(PSF[bk], lhsT=AT[u][:, hc, :], rhs=Wde[wb][:, hc, hh * 512:(hh + 1) * 512], start=(hc == 0), stop=(hc == 3)), r=[BAT[u], BWde[wb]], w=[PB[bk]])
                vop(lambda h, u=u, bG=bG: h.tensor_copy(out=Ye[u][:, 0:512], in_=PSF[bG]), r=[PB[bG]], w=[BYe[u]])
                aop(lambda h, u=u, bU=bU: h.activation(out=Ye[u][:, 512:1024], in_=PSF[bU], func=AF.Copy), r=[PB[bU]], w=[BYe[u]])
                dma("sync", lambda h, e=e, u=u: h.dma_start(out=ypad_d[e * CAP:(e + 1) * CAP, :], in_=Ye[u]), r=[BYe[u]], w=[Bypad], sem="ys%d" % u)
            S.barrier()
            A6.release(0)
            gfbc = A6.alloc([1024], F32)
            Bgf = Buf()
            dma("sync", lambda h: h.dma_start(out=gfbc, in_=gf_d.partition_broadcast(128)), w=[Bgf], sem="gfl")
            y1 = [A6.alloc([1024], F32) for _ in range(2)]
            y2 = [A6.alloc([1024], F32) for _ in range(2)]
            xx = [A6.alloc([1024], F32) for _ in range(2)]
            oo = [A6.alloc([1024], F32) for _ in range(2)]
            By1 = [Buf(), Buf()]
            By2 = [Buf(), Buf()]
            Bxx = [Buf(), Buf()]
            Boo = [Buf(), Buf()]
            junk6 = A6.alloc([1024], BF16)
            Bj6 = Buf()
            st6 = A6.alloc([64], F32)
            Bst6 = Buf()
            for tg in range(16):
                u = tg % 2
                dma("gpsimd", lambda h, tg=tg, u=u: h.indirect_dma_start(out=y1[u], out_offset=None, in_=ypad_d, in_offset=bass.IndirectOffsetOnAxis(ap=idx[:, tg, 0:1], axis=0), bounds_check=breg(h), oob_is_err=False), r=[Bypad, Bidx], w=[By1[u]], sem="ga%d" % u)
                dma("gpsimd", lambda h, tg=tg, u=u: h.indirect_dma_start(out=y2[u], out_offset=None, in_=ypad_d, in_offset=bass.IndirectOffsetOnAxis(ap=idx[:, tg, 1:2], axis=0), bounds_check=breg(h), oob_is_err=False), r=[Bypad, Bidx], w=[By2[u]], sem="gb%d" % u)
                dma("sync", lambda h, tg=tg, u=u: h.dma_start(out=xx[u], in_=x2_d[tg * 128:(tg + 1) * 128, :]), r=[Bx2d], w=[Bxx[u]], sem="xl%d" % u)
                vop(lambda h, tg=tg, u=u: h.scalar_tensor_tensor(out=xx[u], in0=y1[u], scalar=gw[:, tg, 0:1], in1=xx[u], op0=ALU.mult, op1=ALU.add), r=[By1[u], Bgw, Bxx[u]], w=[Bxx[u]])
                vop(lambda h, tg=tg, u=u: h.scalar_tensor_tensor(out=xx[u], in0=y2[u], scalar=gw[:, tg, 1:2], in1=xx[u], op0=ALU.mult, op1=ALU.add), r=[By2[u], Bgw, Bxx[u]], w=[Bxx[u]])
                col = (tg % 8) * 4
                aop(lambda h, u=u, col=col: h.activation(out=junk6, in_=xx[u], func=AF.Square, accum_out=st6[:, col:col + 1]), r=[Bxx[u]], w=[Bj6, Bst6])
                aop(lambda h, col=col: h.activation(out=st6[:, col + 1:col + 2], in_=st6[:, col:col + 1], func=AF.Sqrt, scale=1.0 / 1024, bias=1e-6), r=[Bst6], w=[Bst6])
                vop(lambda h, col=col: h.reciprocal(out=st6[:, col + 2:col + 3], in_=st6[:, col + 1:col + 2]), r=[Bst6], w=[Bst6])
                vop(lambda h, u=u, col=col: h.scalar_tensor_tensor(out=oo[u], in0=xx[u], scalar=st6[:, col + 2:col + 3], in1=gfbc, op0=ALU.mult, op1=ALU.mult), r=[Bxx[u], Bst6, Bgf], w=[Boo[u]])
                dma("sync", lambda h, tg=tg, u=u: h.dma_start(out=out_d[tg * 128:(tg + 1) * 128, :], in_=oo[u]), r=[Boo[u]], sem="os%d" % u)
        S.barrier()
        S.run()
    return nc, dbg_outs


def _consts():
    bf = ml_dtypes.bfloat16
    p = np.arange(128)
    c16 = np.zeros((128, C16["n"]), np.float32)
    c16[:, C16["ident"]:C16["ident"] + 128] = np.eye(128)
    c16[:, C16["triL"]:C16["triL"] + 128] = (p[:, None] <= p[None, :])
    c16[:, C16["triU"]:C16["triU"] + 128] = (p[:, None] >= p[None, :])
    c16[127, C16["selL"]:C16["selL"] + 128] = 1.0
    c16[0, C16["selF"]:C16["selF"] + 128] = 1.0
    ang = 2.0 * np.pi * ((p[:, None] * p[None, :]) % 128) / 128.0
    c16[:, C16["CC"]:C16["CC"] + 128] = np.cos(ang)
    c16[:, C16["SS"]:C16["SS"] + 128] = np.sin(ang)
    c32 = np.zeros((128, C32["n"]), np.float32)
    c32[:, C32["ident"]:C32["ident"] + 128] = np.eye(128)
    c32[:, C32["triS"]:C32["triS"] + 128] = (p[:, None] < p[None, :])
    c32[:, C32["ones"]:C32["ones"] + 128] = 1.0
    jj = p // 16
    c32[:, C32["mask0"]:C32["mask0"] + 128] = (jj[None, :] >= jj[:, None])
    c32[:, C32["mask1"]:C32["mask1"] + 128] = (jj[:, None] >= jj[None, :])
    c32[:, C32["kf"]] = p + 1
    c32[:, C32["kb"]] = 128 - p
    c32[:, C32["ev"]:C32["ev"] + 17] = np.arange(-8, 9)[None, :]
    c32[:, C32["evr"]:C32["evr"] + 17] = np.arange(8, -9, -1)[None, :]
    c32[:, C32["ecap"]:C32["ecap"] + 64] = (np.arange(64) * CAP)[None, :]
    c32[:, C32["iota"]:C32["iota"] + 128] = p[None, :]
    return c16.astype(bf), c32


def _dft_tables(hf):
    bf = ml_dtypes.bfloat16
    L = 4096
    base = np.arange(L, dtype=np.float64) * (2.0 * np.pi / L)
    sc = 1.0 / math.sqrt(L * 128.0)
    cosb = (np.cos(base) * sc).astype(np.float32)
    sinb = (-np.sin(base) * sc).astype(np.float32)
    t = np.arange(L, dtype=np.int64)
    m = np.arange(L // 2, dtype=np.int64)
    l = t if hf == 0 else (L - 1 - t)
    k = m if hf == 0 else (L - 1 - m)
    prod = (l[:, None] * k[None, :]) % L

    def lay(t):
        return np.ascontiguousarray(t.reshape(32, 128, 8, 256).transpose(2, 1, 0, 3))

    return lay(cosb[prod].astype(bf)), lay(sinb[prod].astype(bf))


_CACHE = {}


def kernel(**inp):
    f32 = np.float32
    x = np.asarray(inp["x"], f32)
    if "nc" not in _CACHE:
        _CACHE["nc"] = build_program()
        _CACHE["c"] = _consts()
        _CACHE["tab"] = [_dft_tables(0), _dft_tables(1)]
    nc, dbg_outs = _CACHE["nc"]
    c16, c32 = _CACHE["c"]
    shared = {
        "mix_norm_g": np.ascontiguousarray(inp["mix_norm_g"][0], f32),
        "w_in": np.ascontiguousarray(inp["w_in"][0], f32),
        "w_fourier_out": np.ascontiguousarray(inp["w_fourier_out"][0], f32),
        "ssm_D": np.ascontiguousarray(inp["ssm_D"][0], f32),
        "ssm_w_glu": np.ascontiguousarray(inp["ssm_w_glu"][0], f32),
        "w_ssm_out": np.ascontiguousarray(inp["w_ssm_out"][0], f32),
        "w_out": np.ascontiguousarray(inp["w_out"][0], f32),
        "ffn_norm_g": np.ascontiguousarray(inp["ffn_norm_g"][0], f32),
        "w_router": np.ascontiguousarray(np.concatenate([inp["router_group_w"][0], inp["router_expert_w"][0]], axis=1), f32),
        "b_router": np.ascontiguousarray(np.concatenate([inp["router_group_b"][0], inp["router_expert_b"][0]], axis=0), f32),
        "final_norm_g": np.ascontiguousarray(inp["final_norm_g"], f32),
        "cst16": c16,
        "cst32": c32,
    }
    if STAGE >= 2:
        shared["expert_w_gate"] = np.ascontiguousarray(inp["expert_w_gate"][0], f32)
        shared["expert_w_up"] = np.ascontiguousarray(inp["expert_w_up"][0], f32)
        shared["expert_w_down"] = np.ascontiguousarray(inp["expert_w_down"][0], f32)
    in_maps = []
    for c in range(8):
        b, hf = c // 2, c % 2
        dsel = [0, 1] if hf == 0 else [1, 0]
        xl = x[b] if hf == 0 else x[b][::-1]
        m = dict(shared)
        m["x"] = np.ascontiguousarray(xl, f32)
        m["sA_re"] = np.ascontiguousarray(inp["ssm_A_re"][0][dsel], f32)
        m["sA_im"] = np.ascontiguousarray(inp["ssm_A_im"][0][dsel], f32)
        m["s_ldt"] = np.ascontiguousarray(inp["ssm_log_dt"][0][dsel], f32)
        m["sB_re"] = np.ascontiguousarray(inp["ssm_B_re"][0][dsel], f32)
        m["sB_im"] = np.ascontiguousarray(inp["ssm_B_im"][0][dsel], f32)
        m["sC_re"] = np.ascontiguousarray(inp["ssm_C_re"][0][dsel], f32)
        m["sC_im"] = np.ascontiguousarray(inp["ssm_C_im"][0][dsel], f32)
        m["tab_c"], m["tab_s"] = _CACHE["tab"][hf]
        in_maps.append(m)
    res = run_bass_kernel_spmd(nc, in_maps, core_ids=list(range(8)))
    out = np.empty((4, 4096, 1024), f32)
    for c in range(8):
        b, hf = c // 2, c % 2
        o = np.asarray(res.results[c]["out"], f32)
        if hf == 0:
            out[b, 0:2048] = o
        else:
            out[b, 2048:4096] = o[::-1]
    _CACHE["last"] = res
    return out
```

```python
import math
from contextlib import ExitStack

import ml_dtypes
import numpy as np

import concourse.bass as bass
import concourse.mybir as mybir
from concourse.bass_utils import run_bass_kernel_spmd

F32 = mybir.dt.float32
BF16 = mybir.dt.bfloat16
I32 = mybir.dt.int32
U8 = mybir.dt.uint8
ALU = mybir.AluOpType
AF = mybir.ActivationFunctionType
AX = mybir.AxisListType

ENGS = ["sync", "scalar", "vector", "gpsimd", "tensor"]
TWO_PI = 2.0 * math.pi
PI_SAFE = 3.14159
CAP = 128
NEXP = 64
STAGE = 2
DEBUG = False
MOE_LIMIT = 64


class Tok:
    __slots__ = ("kind", "eng", "n")

    def __init__(self, kind, eng, n):
        self.kind = kind
        self.eng = eng
        self.n = n


class Buf:
    def __init__(self, name=""):
        self.name = name
        self.w = []
        self.r = []


class Sched:
    def __init__(self, nc, stack):
        self.nc = nc
        self.stack = stack
        self.ops = {e: [] for e in ENGS}
        self.cnt = {e: 0 for e in ENGS}
        self.sem = {e: stack.enter_context(nc.semaphore("c_" + e)) for e in ENGS}
        self.waited = {e: {} for e in ENGS}
        self.dsem = {}
        self.dcnt = {}

    def _waits(self, eng, deps):
        waits = []
        for d in deps:
            if d is None:
                continue
            if d.kind == "eng":
                if d.eng == eng and eng == "tensor":
                    continue
                key = d.eng
                sem = self.sem[d.eng]
            else:
                key = "d_" + d.eng
                sem = self.dsem[d.eng]
            if self.waited[eng].get(key, 0) >= d.n:
                continue
            self.waited[eng][key] = d.n
            waits.append((sem, d.n))
        return waits

    @staticmethod
    def _deps(reads, writes, extra):
        deps = list(extra)
        for b in reads:
            deps += b.w
        for b in writes:
            deps += b.w
            deps += b.r
        return deps

    @staticmethod
    def _note(tok, reads, writes):
        for b in reads:
            b.r = [t for t in b.r if not (t.kind == tok.kind and t.eng == tok.eng)] + [tok]
        for b in writes:
            b.w = [t for t in b.w if not (t.kind == tok.kind and t.eng == tok.eng)] + [tok]
            b.r = []

    def op(self, eng, build, reads=(), writes=(), extra=()):
        waits = self._waits(eng, self._deps(reads, writes, extra))
        self.cnt[eng] += 1
        n = self.cnt[eng]
        sem = self.sem[eng]

        def fn(h):
            for (s, v) in waits:
                h.wait_ge(s, v)
            build(h).then_inc(sem, 1)

        self.ops[eng].append(fn)
        tok = Tok("eng", eng, n)
        self._note(tok, reads, writes)
        return tok

    def dma(self, queue, semname, build, reads=(), writes=(), extra=()):
        if semname not in self.dsem:
            self.dsem[semname] = self.stack.enter_context(self.nc.semaphore("d_" + semname))
            self.dcnt[semname] = 0
        waits = self._waits(queue, self._deps(reads, writes, extra))
        self.dcnt[semname] += 16
        n = self.dcnt[semname]
        sem = self.dsem[semname]

        def fn(h):
            for (s, v) in waits:
                h.wait_ge(s, v)
            build(h).then_inc(sem, 16)

        self.ops[queue].append(fn)
        tok = Tok("dma", semname, n)
        self._note(tok, reads, writes)
        return tok

    def all_toks(self):
        t = [Tok("eng", e, self.cnt[e]) for e in ENGS if self.cnt[e] > 0]
        t += [Tok("dma", k, v) for k, v in self.dcnt.items() if v > 0]
        return t

    def barrier(self):
        toks = self.all_toks()
        for e in ENGS:
            waits = self._waits(e, toks)

            def fn(h, waits=waits):
                for (s, v) in waits:
                    h.wait_ge(s, v)

            self.ops[e].append(fn)

    def run(self):
        with self.nc.Block() as block:
            for e in ENGS:
                ops = self.ops[e]

                def body(h, ops=ops):
                    for fn in ops:
                        fn(h)

                getattr(block, e)(body)


class Arena:
    def __init__(self, ap_u8, size):
        self.ap = ap_u8
        self.size = size
        self.top = 0

    def alloc(self, free_shape, dt):
        isz = {F32: 4, I32: 4, BF16: 2}[dt]
        n = 1
        for s in free_shape:
            n *= s
        nb = n * isz
        off = self.top
        self.top += (nb + 63) // 64 * 64
        assert self.top <= self.size, ("SBUF arena overflow", self.top, self.size)
        v = self.ap[:, off:off + nb].bitcast(dt)
        if len(free_shape) > 1:
            names = ["a%d" % i for i in range(len(free_shape))]
            kw = {names[i]: free_shape[i] for i in range(1, len(free_shape))}
            v = v.rearrange("p (%s) -> p %s" % (" ".join(names), " ".join(names)), **kw)
        return v

    def mark(self):
        return self.top

    def release(self, m):
        self.top = m


def flat(ap):
    nd = len(ap.shape)
    if nd == 2:
        return ap
    names = ["a%d" % i for i in range(nd - 1)]
    return ap.rearrange("p %s -> p (%s)" % (" ".join(names), " ".join(names)))


C16 = dict(ident=0, triL=128, triU=256, selL=384, selF=512, CC=640, SS=768, n=896)
C32 = dict(ident=0, triS=128, ones=256, mask0=384, mask1=512, kf=640, kb=641, ev=642, ecap=659, iota=723, evr=851, n=868)


def build_program():
    nc = bass.Bass("TRN2", target_bir_lowering=False)

    def din(name, shape, dt=F32):
        return nc.dram_tensor(name, list(shape), dt, kind="ExternalInput").ap()

    x_d = din("x", [4096, 1024])
    g1_d = din("mix_norm_g", [1024])
    w_in_d = din("w_in", [1024, 3072])
    wfo_d = din("w_fourier_out", [512, 1024])
    sAre_d = din("sA_re", [2, 32, 64])
    sAim_d = din("sA_im", [2, 32, 64])
    sldt_d = din("s_ldt", [2, 32])
    sBre_d = din("sB_re", [2, 32, 64, 16])
    sBim_d = din("sB_im", [2, 32, 64, 16])
    sCre_d = din("sC_re", [2, 32, 16, 64])
    sCim_d = din("sC_im", [2, 32, 16, 64])
    sD_d = din("ssm_D", [512])
    wglu_d = din("ssm_w_glu", [512, 512])
    wso_d = din("w_ssm_out", [512, 1024])
    wout_d = din("w_out", [1024, 1024])
    g2_d = din("ffn_norm_g", [1024])
    wr_d = din("w_router", [1024, 72])
    br_d = din("b_router", [72])
    if STAGE >= 2:
        ewg_d = din("expert_w_gate", [64, 1024, 512])
        ewu_d = din("expert_w_up", [64, 1024, 512])
        ewd_d = din("expert_w_down", [64, 512, 1024])
    gf_d = din("final_norm_g", [1024])
    tc_d = din("tab_c", [8, 128, 32, 256], BF16)
    ts_d = din("tab_s", [8, 128, 32, 256], BF16)
    c16_d = din("cst16", [128, C16["n"]], BF16)
    c32_d = din("cst32", [128, C32["n"]])
    out_d = nc.dram_tensor("out", [2048, 1024], F32, kind="ExternalOutput").ap()
    xpad_d = nc.dram_tensor("xpad", [NEXP * CAP, 1024], BF16, kind="Internal").ap()
    ypad_d = nc.dram_tensor("ypad", [NEXP * CAP, 1024], F32, kind="Internal").ap()
    x2_d = nc.dram_tensor("x2s", [2048, 1024], F32, kind="Internal").ap()
    dbg_outs = {}

    with ExitStack() as st:
        S = Sched(nc, st)
        ARENA_BYTES = 206 * 1024
        arena_t = st.enter_context(nc.sbuf_tensor("arena", [128, ARENA_BYTES], U8))
        AR = Arena(arena_t[:, :], ARENA_BYTES)
        PSF, PSB, PB = [], [], []
        for i in range(8):
            pt = st.enter_context(nc.psum_tensor("ps%d" % i, [128, 512], F32))
            PSF.append(pt[:, :])
            PSB.append(pt[:, :].bitcast(BF16))
            PB.append(Buf("ps%d" % i))

        def vop(fn, r=(), w=(), x=()):
            return S.op("vector", fn, reads=r, writes=w, extra=x)

        def aop(fn, r=(), w=(), x=()):
            return S.op("scalar", fn, reads=r, writes=w, extra=x)

        def gop(fn, r=(), w=(), x=()):
            return S.op("gpsimd", fn, reads=r, writes=w, extra=x)

        def pop(fn, r=(), w=(), x=()):
            return S.op("tensor", fn, reads=r, writes=w, extra=x)

        _dq = [0]
        _breg = []

        def breg(h):
            if not _breg:
                _breg.append(h.to_reg(NEXP * CAP - 1))
            return _breg[0]

        def dma(q, fn, r=(), w=(), x=(), sem=None):
            if sem is None:
                _dq[0] += 1
                sem = "q%d" % (_dq[0] % 8)
            return S.dma(q, sem, fn, reads=r, writes=w, extra=x)

        def dbg(name, ap, shape):
            if not DEBUG:
                return
            d = nc.dram_tensor("dbg_" + name, list(shape), ap.dtype, kind="ExternalOutput").ap()
            dbg_outs[name] = d
            b = Buf()
            S.barrier()
            dma("sync", lambda h: h.dma_start(out=d, in_=ap), w=[b], sem="dbg")
            S.barrier()

        c16 = AR.alloc([C16["n"]], BF16)
        c32 = AR.alloc([C32["n"]], F32)
        Bc16, Bc32 = Buf(), Buf()
        dma("sync", lambda h: h.dma_start(out=c16, in_=c16_d), w=[Bc16])
        dma("sync", lambda h: h.dma_start(out=c32, in_=c32_d), w=[Bc32])

        def k16(name):
            o = C16[name]
            return c16[:, o:o + 128]

        def k32(name, n=128):
            o = C32[name]
            return c32[:, o:o + n]

        gw = AR.alloc([16, 2], F32)
        idx = AR.alloc([16, 2], I32)
        Bgw, Bidx = Buf(), Buf()
        persist0 = AR.top
        Min = AR.alloc([2, 32, 128], BF16)
        Q16 = AR.alloc([2, 32, 128], BF16)
        Mi = AR.alloc([32, 128], BF16)
        BMin, BQ16, BMi = Buf(), Buf(), Buf()
        ft_off = AR.top
        FT = AR.alloc([4, 2048], BF16)
        yT = AR.alloc([4, 2048], BF16)
        xs_off = AR.top
        Xs = AR.alloc([32, 512], BF16)
        uf_off = AR.top
        uf_tm = AR.alloc([32, 512], BF16)
        persist_mark = AR.mark()

        AR.release(ft_off)
        are = AR.alloc([64], F32)
        aim = AR.alloc([64], F32)
        dtn = AR.alloc([64], F32)
        Bre = AR.alloc([64, 16], F32)
        Bim = AR.alloc([64, 16], F32)
        Cre = AR.alloc([64, 16], F32)
        Cim = AR.alloc([64, 16], F32)
        Cld = AR.alloc([8, 2, 64], F32)
        Dp = AR.alloc([32], F32)
        Bp = Buf("params")
        for hf in range(2):
            ps_ = slice(hf * 64, hf * 64 + 64)
            dma("sync", lambda h, ps_=ps_: h.dma_start(out=are[ps_, :], in_=sAre_d.rearrange("d g n -> n (d g)"), allow_slow_non_contiguous=True), w=[Bp])
            dma("sync", lambda h, ps_=ps_: h.dma_start(out=aim[ps_, :], in_=sAim_d.rearrange("d g n -> n (d g)"), allow_slow_non_contiguous=True), w=[Bp])
            dma("sync", lambda h, ps_=ps_: h.dma_start(out=Bre[ps_, :, :], in_=sBre_d.rearrange("d g n c -> n (d g) c")), w=[Bp])
            dma("sync", lambda h, ps_=ps_: h.dma_start(out=Bim[ps_, :, :], in_=sBim_d.rearrange("d g n c -> n (d g) c")), w=[Bp])
        dma("sync", lambda h: h.dma_start(out=dtn, in_=sldt_d.rearrange("d g -> (d g)").partition_broadcast(128)), w=[Bp])
        for j in range(8):
            dma("sync", lambda h, j=j: h.dma_start(out=Dp[j * 16:(j + 1) * 16, :], in_=sD_d.rearrange("(g c) -> c g", c=16), allow_slow_non_contiguous=True), w=[Bp])
        Cld2 = AR.alloc([8, 2, 64], F32)
        for (src, cl) in ((sCre_d, Cld), (sCim_d, Cld2)):
            for dup in range(2):
                dma("sync", lambda h, src=src, dup=dup, cl=cl: h.dma_start(out=cl[:, :, dup, :], in_=src.rearrange("d g c n -> (d g c) n").rearrange("(t p) n -> p t n", p=128)), w=[Bp])
        S.barrier()
        for (cl, dst) in ((Cld, Cre), (Cld2, Cim)):
            for half in range(2):
                bk = half
                for t4 in range(4):
                    t = half * 4 + t4
                    pop(lambda h, t=t, t4=t4, bk=bk, cl=cl: h.transpose(out=PSF[bk][:, t4 * 128:(t4 + 1) * 128], in_=flat(cl[:, t, :, :]), identity=k32("ident")), r=[Bp, Bc32], w=[PB[bk]])
                vop(lambda h, dst=dst, half=half, bk=bk: h.tensor_copy(out=flat(dst)[:, half * 512:(half + 1) * 512], in_=PSF[bk]), r=[PB[bk]], w=[Bp])

        ev = k32("ev", 17)
        evr = k32("evr", 17)
        t0 = AR.alloc([32, 8, 16], F32)
        t1 = AR.alloc([32, 8, 16], F32)
        t0f, t1f = flat(t0), flat(t1)
        tA = t0f[:, 0:1088].rearrange("p (a b) -> p a b", b=64)
        tB = t0f[:, 1088:2176].rearrange("p (a b) -> p a b", b=64)
        tI = t0f[:, 2176:3264].bitcast(I32).rearrange("p (a b) -> p a b", b=64)
        Emag = t1f[:, 0:1088].rearrange("p (a b) -> p a b", b=64)
        Er = AR.alloc([17, 64], F32)
        Ei = AR.alloc([17, 64], F32)
        ErR = AR.alloc([17, 64], F32)
        EiR = AR.alloc([17, 64], F32)
        sm = [AR.alloc([64], F32) for _ in range(8)]
        BE = Buf("E")

        def sincos(xin, sin_out, cos_out, ti, tf, bufs):
            vop(lambda h: h.tensor_scalar(out=ti, in0=xin, scalar1=1.0 / TWO_PI, scalar2=None, op0=ALU.mult), r=bufs, w=bufs)
            vop(lambda h: h.tensor_copy(out=tf, in_=ti), r=bufs, w=bufs)
            vop(lambda h: h.scalar_tensor_tensor(out=tf, in0=tf, scalar=-TWO_PI, in1=xin, op0=ALU.mult, op1=ALU.add), r=bufs, w=bufs)
            vop(lambda h: h.tensor_scalar(out=tf, in0=tf, scalar1=PI_SAFE, scalar2=-PI_SAFE, op0=ALU.min, op1=ALU.max), r=bufs, w=bufs)
            aop(lambda h: h.activation(out=sin_out, in_=tf, func=AF.Sin), r=bufs, w=bufs)
            vop(lambda h: h.tensor_scalar(out=cos_out, in0=tf, scalar1=math.pi / 2, scalar2=-TWO_PI, op0=ALU.is_gt, op1=ALU.mult), r=bufs, w=bufs)
            vop(lambda h: h.scalar_tensor_tensor(out=cos_out, in0=tf, scalar=math.pi / 2, in1=cos_out, op0=ALU.add, op1=ALU.add), r=bufs, w=bufs)
            vop(lambda h: h.tensor_scalar(out=cos_out, in0=cos_out, scalar1=PI_SAFE, scalar2=-PI_SAFE, op0=ALU.min, op1=ALU.max), r=bufs, w=bufs)
            aop(lambda h: h.activation(out=cos_out, in_=cos_out, func=AF.Sin), r=bufs, w=bufs)

        PB0 = [Bp, BE, Bc32]
        aop(lambda h: h.activation(out=dtn, in_=dtn, func=AF.Exp), r=PB0, w=PB0)
        ar_, th_ = sm[0], sm[1]
        vop(lambda h: h.tensor_tensor(out=ar_, in0=are, in1=dtn, op=ALU.mult), r=PB0, w=PB0)
        vop(lambda h: h.tensor_tensor(out=th_, in0=aim, in1=dtn, op=ALU.mult), r=PB0, w=PB0)

        def etable(evc, Er_, Ei_):
            vop(lambda h: h.tensor_tensor(out=tA, in0=ar_.unsqueeze(1).to_broadcast([128, 17, 64]), in1=evc.unsqueeze(2).to_broadcast([128, 17, 64]), op=ALU.mult), r=PB0, w=PB0)
            aop(lambda h: h.activation(out=flat(Emag), in_=flat(tA), func=AF.Exp), r=PB0, w=PB0)
            vop(lambda h: h.tensor_tensor(out=tA, in0=th_.unsqueeze(1).to_broadcast([128, 17, 64]), in1=evc.unsqueeze(2).to_broadcast([128, 17, 64]), op=ALU.mult), r=PB0, w=PB0)
            sincos(flat(tA), flat(Ei_), flat(Er_), flat(tI), flat(tB), PB0)
            vop(lambda h: h.tensor_tensor(out=flat(Er_), in0=flat(Er_), in1=flat(Emag), op=ALU.mult), r=PB0, w=PB0)
            vop(lambda h: h.tensor_tensor(out=flat(Ei_), in0=flat(Ei_), in1=flat(Emag), op=ALU.mult), r=PB0, w=PB0)

        etable(ev, Er, Ei)
        etable(evr, ErR, EiR)
        am1, nr, ni, den, fr, fi = sm[2], sm[3], sm[4], sm[5], sm[6], sm[7]
        ar1, ai1 = Er[:, 9, :], Ei[:, 9, :]
        vop(lambda h: h.tensor_scalar(out=am1, in0=ar1, scalar1=-1.0, scalar2=None, op0=ALU.add), r=PB0, w=PB0)
        vop(lambda h: h.tensor_tensor(out=nr, in0=am1, in1=are, op=ALU.mult), r=PB0, w=PB0)
        vop(lambda h: h.tensor_tensor(out=den, in0=ai1, in1=aim, op=ALU.mult), r=PB0, w=PB0)
        vop(lambda h: h.tensor_tensor(out=nr, in0=nr, in1=den, op=ALU.add), r=PB0, w=PB0)
        vop(lambda h: h.tensor_tensor(out=ni, in0=ai1, in1=are, op=ALU.mult), r=PB0, w=PB0)
        vop(lambda h: h.tensor_tensor(out=den, in0=am1, in1=aim, op=ALU.mult), r=PB0, w=PB0)
        vop(lambda h: h.tensor_tensor(out=ni, in0=ni, in1=den, op=ALU.subtract), r=PB0, w=PB0)
        vop(lambda h: h.tensor_tensor(out=den, in0=are, in1=are, op=ALU.mult), r=PB0, w=PB0)
        vop(lambda h: h.tensor_tensor(out=am1, in0=aim, in1=aim, op=ALU.mult), r=PB0, w=PB0)
        vop(lambda h: h.tensor_tensor(out=den, in0=den, in1=am1, op=ALU.add), r=PB0, w=PB0)
        vop(lambda h: h.reciprocal(out=den, in_=den), r=PB0, w=PB0)
        vop(lambda h: h.tensor_tensor(out=fr, in0=nr, in1=den, op=ALU.mult), r=PB0, w=PB0)
        vop(lambda h: h.tensor_tensor(out=fi, in0=ni, in1=den, op=ALU.mult), r=PB0, w=PB0)
        bbr = AR.alloc([64, 16], F32)
        bbi = AR.alloc([64, 16], F32)
        tb1 = AR.alloc([64, 16], F32)

        def bc16(a):
            return a.unsqueeze(2).to_broadcast([128, a.shape[1], 16])

        vop(lambda h: h.tensor_tensor(out=bbr, in0=Bre, in1=bc16(fr), op=ALU.mult), r=PB0, w=PB0)
        vop(lambda h: h.tensor_tensor(out=tb1, in0=Bim, in1=bc16(fi), op=ALU.mult), r=PB0, w=PB0)
        vop(lambda h: h.tensor_tensor(out=bbr, in0=bbr, in1=tb1, op=ALU.subtract), r=PB0, w=PB0)
        vop(lambda h: h.tensor_tensor(out=bbi, in0=Bim, in1=bc16(fr), op=ALU.mult), r=PB0, w=PB0)
        vop(lambda h: h.tensor_tensor(out=tb1, in0=Bre, in1=bc16(fi), op=ALU.mult), r=PB0, w=PB0)
        vop(lambda h: h.tensor_tensor(out=bbi, in0=bbi, in1=tb1, op=ALU.add), r=PB0, w=PB0)

        PinN = AR.alloc([2, 32, 8, 16], BF16)
        P16 = AR.alloc([2, 32, 8, 16], BF16)
        BPin, BPm = Buf(), Buf()
        Bt_lo, Bt_hi = Buf(), Buf()
        Q16v = Q16.rearrange("p d g (i c) -> p d g i c", c=16)

        def cmul_batch(out_ap, outbuf, Ar, Ai, d, Tr_, Ti_, i0, mode):
            def vv(T, lo, hi):
                return T[lo:hi, i0:i0 + 8, d * 32:(d + 1) * 32].rearrange("p j g -> p g j").unsqueeze(3).to_broadcast([hi - lo, 32, 8, 16])

            def aa(A, lo, hi):
                return A[lo:hi, d * 32:(d + 1) * 32, :].unsqueeze(2).to_broadcast([hi - lo, 32, 8, 16])

            rr = PB0
            vop(lambda h: h.tensor_tensor(out=t0[0:64], in0=aa(Ar, 0, 64), in1=vv(Tr_, 0, 64), op=ALU.mult), r=rr, w=[Bt_lo])
            vop(lambda h: h.tensor_tensor(out=t1[0:64], in0=aa(Ai, 0, 64), in1=vv(Ti_, 0, 64), op=ALU.mult), r=rr, w=[Bt_lo])
            vop(lambda h: h.tensor_tensor(out=out_ap[0:64], in0=t0[0:64], in1=t1[0:64], op=ALU.subtract), r=[Bt_lo], w=[outbuf])
            gop(lambda h: h.tensor_tensor(out=t0[64:128], in0=aa(Ar, 64, 128), in1=vv(Ti_, 64, 128), op=ALU.mult), r=rr, w=[Bt_hi])
            gop(lambda h: h.tensor_tensor(out=t1[64:128], in0=aa(Ai, 64, 128), in1=vv(Tr_, 64, 128), op=ALU.mult), r=rr, w=[Bt_hi])
            if mode == "P":
                gop(lambda h: h.tensor_tensor(out=out_ap[64:128], in0=t0[64:128], in1=t1[64:128], op=ALU.add), r=[Bt_hi], w=[outbuf])
            else:
                gop(lambda h: h.tensor_tensor(out=t0[64:128], in0=t0[64:128], in1=t1[64:128], op=ALU.add), r=[Bt_hi], w=[Bt_hi])
                gop(lambda h: h.tensor_scalar(out=out_ap[64:128], in0=t0[64:128], scalar1=-1.0, scalar2=None, op0=ALU.mult), r=[Bt_hi], w=[outbuf])

        cmul_batch(PinN[:, 0], BPin, bbr, bbi, 0, ErR, EiR, 1, "P")
        cmul_batch(P16[:, 0], BPm, bbr, bbi, 0, ErR, EiR, 9, "P")
        cmul_batch(Q16v[:, 0], BQ16, Cre, Cim, 0, Er, Ei, 9, "Q")
        cmul_batch(PinN[:, 1], BPin, bbr, bbi, 1, Er, Ei, 8, "P")
        cmul_batch(P16[:, 1], BPm, bbr, bbi, 1, Er, Ei, 0, "P")
        cmul_batch(Q16v[:, 1], BQ16, Cre, Cim, 1, ErR, EiR, 0, "Q")
        for d in range(2):
            for g8 in range(4):
                bk = 2 + (g8 % 2)
                for gi in range(8):
                    g = g8 * 8 + gi
                    pop(lambda h, d=d, g=g, gi=gi, bk=bk: h.transpose(out=PSB[bk][:, gi * 128:(gi + 1) * 128], in_=flat(PinN[:, d, g, :, :]), identity=k16("ident")), r=[BPin, Bc16], w=[PB[bk]])
                aop(lambda h, d=d, g8=g8, bk=bk: h.activation(out=flat(Min[:, d, g8 * 8:(g8 + 1) * 8, :]), in_=PSB[bk], func=AF.Copy), r=[PB[bk]], w=[BMin])
        mt0 = AR.alloc([4, 128], F32)
        mt1 = AR.alloc([4, 128], F32)
        Bmt = Buf()
        for g4 in range(8):
            for d in range(2):
                bk = 4 + d
                for gi in range(4):
                    g = g4 * 4 + gi
                    pop(lambda h, d=d, g=g, gi=gi, bk=bk: h.matmul(PSF[bk][:, gi * 128:(gi + 1) * 128], lhsT=flat(P16[:, d, g, :, :]), rhs=Q16[:, d, g, :], start=True, stop=True), r=[BPm, BQ16], w=[PB[bk]])
            vop(lambda h: h.tensor_tensor(out=mt0, in0=PSF[4].rearrange("p (a b) -> p a b", b=128), in1=k32("mask0").unsqueeze(1).to_broadcast([128, 4, 128]), op=ALU.mult), r=[PB[4], Bc32], w=[Bmt])
            vop(lambda h: h.tensor_tensor(out=mt1, in0=PSF[5].rearrange("p (a b) -> p a b", b=128), in1=k32("mask1").unsqueeze(1).to_broadcast([128, 4, 128]), op=ALU.mult), r=[PB[5], Bc32], w=[Bmt])
            vop(lambda h: h.tensor_tensor(out=mt0, in0=mt0, in1=mt1, op=ALU.add), r=[Bmt], w=[Bmt])
            for gi in range(4):
                g = g4 * 4 + gi
                vop(lambda h, g=g, gi=gi: h.scalar_tensor_tensor(out=Mi[:, g, :], in0=k32("ident"), scalar=Dp[:, g:g + 1], in1=mt0[:, gi, :], op0=ALU.mult, op1=ALU.add), r=[Bmt, Bp, Bc32], w=[BMi])
        S.barrier()
        AR.release(persist_mark)

        Buf_uf, BX = Buf(), Buf()
        p1_mark = AR.mark()
        Wfs = AR.alloc([8, 1024], BF16)
        BWfs = Buf()
        dma("gpsimd", lambda h: h.dma_start(out=Wfs, in_=w_in_d[:, 0:1024].rearrange("(kc p) n -> p kc n", p=128)), w=[BWfs], sem="wfs")
        gbc = AR.alloc([1024], F32)
        Bg = Buf()
        dma("sync", lambda h: h.dma_start(out=gbc, in_=g1_d.partition_broadcast(128)), w=[Bg], sem="g1a")
        hT = AR.alloc([8, 1024], BF16)
        BhT = Buf()
        Zt = AR.alloc([32, 8, 16], BF16)
        BZ = Buf()
        NXB = 3
        xt = [AR.alloc([1024], F32) for _ in range(NXB)]
        Bxt = [Buf() for _ in range(NXB)]
        xn = [AR.alloc([1024], BF16) for _ in range(2)]
        Bxn = [Buf() for _ in range(2)]
        junk = AR.alloc([1024], BF16)
        Bjunk = Buf()
        stat = AR.alloc([64], F32)
        Bstat = Buf()

        def norm_tile(xsrc_ap, xt_ap, bxt, xn_ap, bxn, col, gb, bg, qsem):
            dma("sync", lambda h: h.dma_start(out=xt_ap, in_=xsrc_ap), w=[bxt], sem=qsem)
            aop(lambda h: h.activation(out=junk, in_=xt_ap, func=AF.Square, accum_out=stat[:, col:col + 1]), r=[bxt], w=[Bjunk, Bstat])
            aop(lambda h: h.activation(out=stat[:, col + 1:col + 2], in_=stat[:, col:col + 1], func=AF.Sqrt, scale=1.0 / 1024, bias=1e-6), r=[Bstat], w=[Bstat])
            vop(lambda h: h.reciprocal(out=stat[:, col + 2:col + 3], in_=stat[:, col + 1:col + 2]), r=[Bstat], w=[Bstat])
            vop(lambda h: h.scalar_tensor_tensor(out=xn_ap, in0=xt_ap, scalar=stat[:, col + 2:col + 3], in1=gb, op0=ALU.mult, op1=ALU.mult), r=[bxt, Bstat, bg], w=[bxn])

        ti_ = 0
        for b in range(4):
            for t in range(8):
                tg = b * 8 + t
                xi = ti_ % NXB
                ni_ = ti_ % 2
                col = (ti_ % 8) * 4
                ti_ += 1
                norm_tile(x_d[tg * 128:(tg + 1) * 128, :], xt[xi], Bxt[xi], xn[ni_], Bxn[ni_], col, gbc, Bg, "x%d" % xi)
                bk = ni_
                for kc in range(8):
                    pop(lambda h, kc=kc, bk=bk, ni_=ni_: h.transpose(out=PSB[bk][:, kc * 128:(kc + 1) * 128], in_=xn[ni_][:, kc * 128:(kc + 1) * 128], identity=k16("ident")), r=[Bxn[ni_], Bc16], w=[PB[bk]])
                aop(lambda h, t=t, bk=bk: h.activation(out=hT[:, :, t * 128:(t + 1) * 128], in_=PSB[bk].rearrange("p (a b) -> p a b", b=128), func=AF.Copy), r=[PB[bk]], w=[BhT])
            for t in range(8):
                tg = b * 8 + t
                bk = 2 + (t % 2)
                for kc in range(8):
                    pop(lambda h, kc=kc, t=t, bk=bk: h.matmul(PSF[bk], lhsT=hT[:, kc, t * 128:(t + 1) * 128], rhs=Wfs[:, kc, 0:512], start=(kc == 0), stop=(kc == 7)), r=[BhT, BWfs], w=[PB[bk]])
                vop(lambda h, tg=tg, bk=bk: h.tensor_copy(out=uf_tm[:, tg, :], in_=PSF[bk]), r=[PB[bk]], w=[Buf_uf])
            for j in range(8):
                bk = 4 + (j % 2)
                for kc in range(8):
                    pop(lambda h, kc=kc, j=j, bk=bk: h.matmul(PSF[bk], lhsT=hT[:, kc, j:1024:8], rhs=Wfs[:, kc, 512:1024], start=(kc == 0), stop=(kc == 7)), r=[BhT, BWfs], w=[PB[bk]])
                aop(lambda h, j=j, bk=bk: h.activation(out=Zt[:, :, j, :], in_=PSF[bk].rearrange("p (g c) -> p g c", c=16), func=AF.Copy), r=[PB[bk]], w=[BZ])
            for g8 in range(4):
                bk = 6 + (g8 % 2)
                for gi in range(8):
                    g = g8 * 8 + gi
                    pop(lambda h, g=g, gi=gi, bk=bk: h.transpose(out=PSB[bk][:, gi * 128:(gi + 1) * 128], in_=flat(Zt[:, g, :, :]), identity=k16("ident")), r=[BZ, Bc16], w=[PB[bk]])
                vop(lambda h, g8=g8, b=b, bk=bk: h.tensor_copy(out=Xs[:, g8 * 8:(g8 + 1) * 8, b * 128:(b + 1) * 128], in_=PSB[bk].rearrange("p (a b) -> p a b", b=128)), r=[PB[bk]], w=[BX])
        S.barrier()
        dbg("wfs", Wfs, [128, 8, 1024])
        dbg("ht", hT, [128, 8, 1024])
        dbg("xn", xn[1], [128, 1024])
        dbg("stat", stat, [128, 64])
        dbg("zt", Zt, [128, 32, 8, 16])
        dbg("min", Min, [128, 2, 32, 128])
        dbg("q16", Q16, [128, 2, 32, 128])
        dbg("mi", Mi, [128, 32, 128])
        AR.release(p1_mark)
        dbg("uf", uf_tm, [128, 32, 512])
        dbg("xs", Xs, [128, 32, 512])
        if STAGE == 0:
            S.barrier()
            S.run()
            return nc, dbg_outs

        BFT = Buf()
        p2_mark = AR.mark()
        NTB = 3
        tct = [AR.alloc([16, 256], BF16) for _ in range(NTB)]
        tst = [AR.alloc([16, 256], BF16) for _ in range(NTB)]
        Btc = [Buf() for _ in range(NTB)]
        Bts = [Buf() for _ in range(NTB)]
        UrT = AR.alloc([4, 256], BF16)
        UiT = AR.alloc([4, 256], BF16)
        BUU = Buf()
        it_ = 0
        for kb in range(8):
            for lt in range(32):
                l16 = lt % 16
                if l16 == 0:
                    bi = it_ % NTB
                    it_ += 1
                    hh_ = lt // 16
                    dma("sync", lambda h, bi=bi, hh_=hh_, kb=kb: h.dma_start(out=tct[bi], in_=tc_d[kb, :, hh_ * 16:(hh_ + 1) * 16, :]), w=[Btc[bi]], sem="tc%d" % bi)
                    dma("scalar", lambda h, bi=bi, hh_=hh_, kb=kb: h.dma_start(out=tst[bi], in_=ts_d[kb, :, hh_ * 16:(hh_ + 1) * 16, :]), w=[Bts[bi]], sem="ts%d" % bi)
                for g in range(4):
                    pop(lambda h, g=g, lt=lt, bi=bi, l16=l16: h.matmul(PSF[g][:, 0:256], lhsT=uf_tm[:, lt, g * 128:(g + 1) * 128], rhs=tct[bi][:, l16, :], start=(lt == 0), stop=(lt == 31)), r=[Buf_uf, Btc[bi]], w=[PB[g]])
                    pop(lambda h, g=g, lt=lt, bi=bi, l16=l16: h.matmul(PSF[4 + g][:, 0:256], lhsT=uf_tm[:, lt, g * 128:(g + 1) * 128], rhs=tst[bi][:, l16, :], start=(lt == 0), stop=(lt == 31)), r=[Buf_uf, Bts[bi]], w=[PB[4 + g]])
            for g in range(4):
                vop(lambda h, g=g: h.tensor_copy(out=UrT[:, g, :], in_=PSF[g][:, 0:256]), r=[PB[g]], w=[BUU])
                aop(lambda h, g=g: h.activation(out=UiT[:, g, :], in_=PSF[4 + g][:, 0:256], func=AF.Copy), r=[PB[4 + g]], w=[BUU])
            for g in range(4):
                pop(lambda h, g=g: h.matmul(PSF[g][:, 256:512], lhsT=k16("CC"), rhs=UrT[:, g, :], start=True, stop=False), r=[BUU, Bc16], w=[PB[g]])
                pop(lambda h, g=g: h.matmul(PSF[g][:, 256:512], lhsT=k16("SS"), rhs=UiT[:, g, :], start=False, stop=True), r=[BUU, Bc16], w=[PB[g]])
                vop(lambda h, g=g, kb=kb: h.tensor_copy(out=FT[:, g, kb * 256:(kb + 1) * 256], in_=PSF[g][:, 256:512]), r=[PB[g]], w=[BFT])
        S.barrier()
        AR.release(p2_mark)
        dbg("ft", FT, [128, 4, 2048])

        ByT = Buf()
        AR.release(uf_off)
        p3_mark = AR.mark()
        Wglu = AR.alloc([4, 512], BF16)
        BWglu = Buf()
        dma("gpsimd", lambda h: h.dma_start(out=Wglu, in_=wglu_d.rearrange("(q p) n -> p q n", p=128)), w=[BWglu], sem="wglu")
        pAre = AR.alloc([16, 64], F32)
        pAim = AR.alloc([16, 64], F32)
        pdt = AR.alloc([16], F32)
        Trp = AR.alloc([16, 64], F32)
        Tip = AR.alloc([16, 64], F32)
        Trm = AR.alloc([16, 64], F32)
        Tim = AR.alloc([16, 64], F32)
        tX = AR.alloc([16, 64], F32)
        tY = AR.alloc([16, 64], F32)
        tZi = AR.alloc([16, 64], I32)
        BT = Buf("tables")
        Hc = [AR.alloc([16, 2, 64], BF16) for _ in range(2)]
        BHc = [Buf(), Buf()]
        HTd = [AR.alloc([16, 257], BF16) for _ in range(2)]
        BHT = [Buf(), Buf()]
        Vt = [AR.alloc([4, 2, 64], BF16) for _ in range(2)]
        BV = [Buf(), Buf()]
        m1 = [AR.alloc([4, 2, 64], F32) for _ in range(2)]
        m2 = [AR.alloc([4, 2, 64], F32) for _ in range(2)]
        Bm = [Buf(), Buf()]
        Yg = AR.alloc([8, 256], BF16)
        BYg = Buf()
        sg = AR.alloc([4, 512], BF16)
        Bsg = Buf()

        def cplx_mul(src4, bsrc, Tr, Ti, k, out_re, out_im, bout):
            a, bb = m1[k], m2[k]
            Trb = Tr.unsqueeze(2).to_broadcast([128, 4, 2, 64])
            vop(lambda h: h.tensor_tensor(out=a, in0=src4, in1=Trb, op=ALU.mult), r=[BT, bsrc], w=[Bm[k]])
            vop(lambda h: h.tensor_tensor(out=bb[:, :, 0, :], in0=src4[:, :, 1, :], in1=Ti, op=ALU.mult), r=[BT, bsrc], w=[Bm[k]])
            vop(lambda h: h.tensor_tensor(out=bb[:, :, 1, :], in0=src4[:, :, 0, :], in1=Ti, op=ALU.mult), r=[BT, bsrc], w=[Bm[k]])
            gop(lambda h: h.tensor_tensor(out=out_re, in0=a[:, :, 0, :], in1=bb[:, :, 0, :], op=ALU.subtract), r=[Bm[k]], w=[bout])
            gop(lambda h: h.tensor_tensor(out=out_im, in0=a[:, :, 1, :], in1=bb[:, :, 1, :], op=ALU.add), r=[Bm[k]], w=[bout])

        for half in range(2):
            g_lo = half * 16
            for d in range(2):
                kcol = c32[:, C32["kf"] + d:C32["kf"] + d + 1]
                dma("sync", lambda h, d=d, g_lo=g_lo: h.dma_start(out=flat(pAre), in_=sAre_d[d, g_lo:g_lo + 16, :].rearrange("g n -> (g n)").partition_broadcast(128)), w=[BT], sem="pa")
                dma("sync", lambda h, d=d, g_lo=g_lo: h.dma_start(out=flat(pAim), in_=sAim_d[d, g_lo:g_lo + 16, :].rearrange("g n -> (g n)").partition_broadcast(128)), w=[BT], sem="pb")
                dma("sync", lambda h, d=d, g_lo=g_lo: h.dma_start(out=pdt, in_=sldt_d[d, g_lo:g_lo + 16].partition_broadcast(128)), w=[BT], sem="pc")
                TB = [BT, Bc32]
                xt3 = [Tok("dma", "pa", S.dcnt["pa"]), Tok("dma", "pb", S.dcnt["pb"]), Tok("dma", "pc", S.dcnt["pc"])]
                aop(lambda h: h.activation(out=pdt, in_=pdt, func=AF.Exp, scale=1.0), r=TB, w=TB, x=xt3)
                dtb = pdt.unsqueeze(2).to_broadcast([128, 16, 64])
                vop(lambda h, dtb=dtb: h.scalar_tensor_tensor(out=pAre, in0=pAre, scalar=8.0, in1=dtb, op0=ALU.mult, op1=ALU.mult), r=TB, w=TB, x=xt3)
                vop(lambda h, dtb=dtb: h.scalar_tensor_tensor(out=pAim, in0=pAim, scalar=8.0, in1=dtb, op0=ALU.mult, op1=ALU.mult), r=TB, w=TB)
                vop(lambda h: h.tensor_scalar(out=flat(tZi), in0=flat(pAim), scalar1=1.0 / TWO_PI, scalar2=None, op0=ALU.mult), r=TB, w=TB)
                vop(lambda h: h.tensor_copy(out=flat(tX), in_=flat(tZi)), r=TB, w=TB)
                vop(lambda h: h.scalar_tensor_tensor(out=flat(pAim), in0=flat(tX), scalar=-TWO_PI, in1=flat(pAim), op0=ALU.mult, op1=ALU.add), r=TB, w=TB)
                vop(lambda h, kcol=kcol: h.tensor_scalar(out=flat(tY), in0=flat(pAim), scalar1=kcol, scalar2=None, op0=ALU.mult), r=TB, w=TB)
                sincos(flat(tY), flat(Tip), flat(Trp), flat(tZi), flat(tX), TB)
                vop(lambda h, kcol=kcol: h.tensor_scalar(out=flat(tY), in0=flat(pAre), scalar1=kcol, scalar2=None, op0=ALU.mult), r=TB, w=TB)
                aop(lambda h: h.activation(out=flat(tX), in_=flat(tY), func=AF.Exp), r=TB, w=TB)
                aop(lambda h: h.activation(out=flat(tY), in_=flat(tY), func=AF.Exp, scale=-1.0), r=TB, w=TB)
                vop(lambda h: h.tensor_tensor(out=flat(Trm), in0=flat(Trp), in1=flat(tY), op=ALU.mult), r=TB, w=TB)
                vop(lambda h: h.scalar_tensor_tensor(out=flat(Tim), in0=flat(Tip), scalar=-1.0, in1=flat(tY), op0=ALU.mult, op1=ALU.mult), r=TB, w=TB)
                vop(lambda h: h.tensor_tensor(out=flat(Trp), in0=flat(Trp), in1=flat(tX), op=ALU.mult), r=TB, w=TB)
                vop(lambda h: h.tensor_tensor(out=flat(Tip), in0=flat(Tip), in1=flat(tX), op=ALU.mult), r=TB, w=TB)
                blocks = [0, 1] if d == 0 else [3, 2, 1, 0]
                tri = k16("triL") if d == 0 else k16("triU")
                sel = k16("selL") if d == 0 else k16("selF")
                if d == 0:
                    vop(lambda h: h.memset(HTd[0][:, :, 0:1], 0.0), r=[], w=[BHT[0]])
                for bi_, blk in enumerate(blocks):
                    cur, prv = bi_ % 2, (bi_ + 1) % 2
                    first = (bi_ == 0)
                    for c4 in range(4):
                        gl = c4 * 4
                        k = c4 % 2
                        bS, bW, bTp = (0, 1, 2) if k == 0 else (3, 4, 5)
                        for gi in range(4):
                            g = g_lo + gl + gi
                            pop(lambda h, g=g, gi=gi, blk=blk, d=d, bS=bS: h.matmul(PSF[bS][:, gi * 128:(gi + 1) * 128], lhsT=Xs[:, g, blk * 128:(blk + 1) * 128], rhs=Min[:, d, g, :], start=True, stop=True), r=[BX, BMin], w=[PB[bS]])
                        srcS = PSF[bS].rearrange("p (g h n) -> p g h n", h=2, n=64)
                        cplx_mul(srcS, PB[bS], Trm[:, gl:gl + 4, :], Tim[:, gl:gl + 4, :], k, Vt[k][:, :, 0, :], Vt[k][:, :, 1, :], BV[k])
                        pop(lambda h, k=k, bW=bW, first=first, tri=tri: h.matmul(PSF[bW], lhsT=tri, rhs=flat(Vt[k]), start=True, stop=first), r=[BV[k], Bc16], w=[PB[bW]])
                        if not first:
                            pop(lambda h, bW=bW, prv=prv, gl=gl, sel=sel: h.matmul(PSF[bW], lhsT=sel, rhs=flat(Hc[prv][:, gl:gl + 4, :, :]), start=False, stop=True), r=[BHc[prv], Bc16], w=[PB[bW]])
                        srcW = PSF[bW].rearrange("p (g h n) -> p g h n", h=2, n=64)
                        cplx_mul(srcW, PB[bW], Trp[:, gl:gl + 4, :], Tip[:, gl:gl + 4, :], k, Hc[cur][:, gl:gl + 4, 0, :], Hc[cur][:, gl:gl + 4, 1, :], BHc[cur])
                        if blk <= 2:
                            for gi in range(4):
                                pop(lambda h, gi=gi, gl=gl, cur=cur, bTp=bTp: h.transpose(out=PSB[bTp][:, gi * 128:(gi + 1) * 128], in_=flat(Hc[cur][:, gl + gi, :, :]), identity=k16("ident")), r=[BHc[cur], Bc16], w=[PB[bTp]])
                            srcT = PSB[bTp][:, 0:512].rearrange("p (a b) -> p a b", b=128)
                            if blk < 2:
                                co = blk * 128 + (1 if d == 0 else 0)
                                aop(lambda h, gl=gl, co=co, srcT=srcT, d=d: h.activation(out=HTd[d][:, gl:gl + 4, co:co + 128], in_=srcT, func=AF.Copy), r=[PB[bTp]], w=[BHT[d]])
                            else:
                                aop(lambda h, gl=gl, srcT=srcT, d=d: h.activation(out=HTd[d][:, gl:gl + 4, 256:257], in_=srcT[:, :, 0:1], func=AF.Copy), r=[PB[bTp]], w=[BHT[d]])
            for blk in range(2):
                for c4 in range(4):
                    gl = c4 * 4
                    bk = 6 + (c4 % 2)
                    for gi in range(4):
                        g = g_lo + gl + gi
                        o = gi * 128
                        pop(lambda h, g=g, o=o, blk=blk, bk=bk: h.matmul(PSF[bk][:, o:o + 128], lhsT=Xs[:, g, blk * 128:(blk + 1) * 128], rhs=Mi[:, g, :], start=True, stop=False), r=[BX, BMi], w=[PB[bk]])
                        pop(lambda h, g=g, o=o, blk=blk, bk=bk, gl=gl, gi=gi: h.matmul(PSF[bk][:, o:o + 128], lhsT=HTd[0][:, gl + gi, blk * 128:blk * 128 + 128], rhs=Q16[:, 0, g, :], start=False, stop=False), r=[BHT[0], BQ16], w=[PB[bk]])
                        pop(lambda h, g=g, o=o, blk=blk, bk=bk, gl=gl, gi=gi: h.matmul(PSF[bk][:, o:o + 128], lhsT=HTd[1][:, gl + gi, blk * 128 + 1:blk * 128 + 129], rhs=Q16[:, 1, g, :], start=False, stop=True), r=[BHT[1], BQ16], w=[PB[bk]])
                    aop(lambda h, bk=bk, gl=gl: h.activation(out=Yg[:, :, gl * 16:(gl + 4) * 16].rearrange("p i (g c) -> p g i c", c=16), in_=PSF[bk].rearrange("p (g i c) -> p g i c", i=8, c=16), func=AF.Gelu), r=[PB[bk]], w=[BYg])
                for qq in range(2):
                    q = 2 * half + qq
                    bk = 2 if qq == 0 else 5
                    for i in range(8):
                        pop(lambda h, i=i, qq=qq, bk=bk: h.transpose(out=PSB[bk][:, i * 128:(i + 1) * 128], in_=Yg[:, i, qq * 128:(qq + 1) * 128], identity=k16("ident")), r=[BYg, Bc16], w=[PB[bk]])
                    vop(lambda h, q=q, blk=blk, bk=bk: h.tensor_copy(out=yT[:, q, blk * 1024:(blk + 1) * 1024].rearrange("p (k i) -> p i k", i=8), in_=PSB[bk].rearrange("p (i k) -> p i k", k=128)), r=[PB[bk]], w=[ByT])
        dbg("yt", yT, [128, 4, 2048])
        for tb in range(4):
            for co in range(4):
                for q in range(4):
                    pop(lambda h, co=co, q=q, tb=tb: h.matmul(PSF[co], lhsT=Wglu[:, q, co * 128:(co + 1) * 128], rhs=yT[:, q, tb * 512:(tb + 1) * 512], start=(q == 0), stop=(q == 3)), r=[ByT, BWglu], w=[PB[co]])
                aop(lambda h, co=co: h.activation(out=sg[:, co, :], in_=PSF[co], func=AF.Sigmoid), r=[PB[co]], w=[Bsg])
            vop(lambda h, tb=tb: h.tensor_tensor(out=yT[:, :, tb * 512:(tb + 1) * 512], in0=yT[:, :, tb * 512:(tb + 1) * 512], in1=sg, op=ALU.mult), r=[Bsg, ByT], w=[ByT])
        S.barrier()
        AR.release(p3_mark)
        dbg("y2t", yT, [128, 4, 2048])

        A4 = Arena(arena_t[:, persist0:ft_off], ft_off - persist0)
        A5 = Arena(arena_t[:, xs_off:ARENA_BYTES], ARENA_BYTES - xs_off)
        Wg_ = A4.alloc([8, 2048], BF16)
        Wout = A5.alloc([8, 1024], BF16)
        Wfo = A4.alloc([4, 1024], BF16)
        Wso = A5.alloc([4, 1024], BF16)
        BW4 = [Buf() for _ in range(4)]
        dma("gpsimd", lambda h: h.dma_start(out=Wfo, in_=wfo_d.rearrange("(q p) n -> p q n", p=128)), w=[BW4[0]], sem="w4a")
        dma("gpsimd", lambda h: h.dma_start(out=Wso, in_=wso_d.rearrange("(q p) n -> p q n", p=128)), w=[BW4[1]], sem="w4b")
        dma("gpsimd", lambda h: h.dma_start(out=Wg_, in_=w_in_d[:, 1024:3072].rearrange("(kc p) n -> p kc n", p=128)), w=[BW4[2]], sem="w4c")
        dma("gpsimd", lambda h: h.dma_start(out=Wout, in_=wout_d.rearrange("(kc p) n -> p kc n", p=128)), w=[BW4[3]], sem="w4d")
        hT4 = A5.alloc([8, 512], BF16)
        BhT4 = Buf()
        xq = [A5.alloc([1024], F32) for _ in range(4)]
        Bxq = [Buf() for _ in range(4)]
        xn4 = [A5.alloc([1024], BF16) for _ in range(2)]
        Bxn4 = [Buf() for _ in range(2)]
        mg = A5.alloc([8, 512], BF16)
        Bmg = Buf()
        g1bc = A5.alloc([1024], F32)
        g2bc = A5.alloc([1024], F32)
        Bgg = Buf()
        dma("sync", lambda h: h.dma_start(out=g1bc, in_=g1_d.partition_broadcast(128)), w=[Bgg], sem="g1l")
        Bgg2 = Buf()
        dma("sync", lambda h: h.dma_start(out=g2bc, in_=g2_d.partition_broadcast(128)), w=[Bgg2], sem="g2l")
        junk4 = A5.alloc([1024], BF16)
        stat4 = A5.alloc([64], F32)
        s12 = [[A5.alloc([512], BF16) for _ in range(2)] for _ in range(2)]
        t12 = [[A5.alloc([512], BF16) for _ in range(2)] for _ in range(2)]
        Bs12 = [Buf(), Buf()]
        x2t = [A5.alloc([1024], F32) for _ in range(2)]
        Bx2 = [Buf(), Buf()]
        hn32 = [A5.alloc([1024], F32) for _ in range(2)]
        Bhn32 = [Buf(), Buf()]
        hn16 = [A5.alloc([1024], BF16) for _ in range(2)]
        Bhn16 = [Buf(), Buf()]
        hnT = A5.alloc([8, 128], F32)
        BhnT = Buf()
        Wr = A5.alloc([8, 72], F32)
        BWr = Buf()
        dma("sync", lambda h: h.dma_start(out=Wr, in_=wr_d.rearrange("(kc p) n -> p kc n", p=128)), w=[BWr], sem="wrl")
        brb = A5.alloc([72], F32)
        Bbr = Buf()
        dma("sync", lambda h: h.dma_start(out=brb, in_=br_d.partition_broadcast(128)), w=[Bbr], sem="brl")
        Lg = A5.alloc([72], F32)
        rs = A5.alloc([512], F32)
        Brs = Buf()
        Cacc = A5.alloc([64], F32)
        BCacc = Buf()
        vop(lambda h: h.memset(Cacc, 0.0), w=[BCacc])
        Bxpad = Buf()
        Bx2d = Buf()

        def norm4(xsrc_ap, xt_ap, bxt, xn_ap, bxn, col, gb, bg, qsem):
            dma("sync", lambda h: h.dma_start(out=xt_ap, in_=xsrc_ap), w=[bxt], sem=qsem)
            aop(lambda h: h.activation(out=junk4, in_=xt_ap, func=AF.Square, accum_out=stat4[:, col:col + 1]), r=[bxt], w=[Bjunk, Bstat])
            aop(lambda h: h.activation(out=stat4[:, col + 1:col + 2], in_=stat4[:, col:col + 1], func=AF.Sqrt, scale=1.0 / 1024, bias=1e-6), r=[Bstat], w=[Bstat])
            vop(lambda h: h.reciprocal(out=stat4[:, col + 2:col + 3], in_=stat4[:, col + 1:col + 2]), r=[Bstat], w=[Bstat])
            vop(lambda h: h.scalar_tensor_tensor(out=xn_ap, in0=xt_ap, scalar=stat4[:, col + 2:col + 3], in1=gb, op0=ALU.mult, op1=ALU.mult), r=[bxt, Bstat, bg], w=[bxn])

        tcount = 0
        for tb in range(4):
            for t in range(4):
                tg = tb * 4 + t
                ni_ = t % 2
                col = (tg % 8) * 4
                norm4(x_d[tg * 128:(tg + 1) * 128, :], xq[t], Bxq[t], xn4[ni_], Bxn4[ni_], col, g1bc, Bgg, "xq%d" % t)
                bk = ni_
                for kc in range(8):
                    pop(lambda h, kc=kc, bk=bk, ni_=ni_: h.transpose(out=PSB[bk][:, kc * 128:(kc + 1) * 128], in_=xn4[ni_][:, kc * 128:(kc + 1) * 128], identity=k16("ident")), r=[Bxn4[ni_], Bc16], w=[PB[bk]])
                aop(lambda h, t=t, bk=bk: h.activation(out=hT4[:, :, t * 128:(t + 1) * 128], in_=PSB[bk].rearrange("p (a b) -> p a b", b=128), func=AF.Copy), r=[PB[bk]], w=[BhT4])
            tsl = slice(tb * 512, (tb + 1) * 512)
            for dc in range(8):
                pz = dc % 2
                bA, bB, bC, bD = (0, 1, 2, 3) if pz == 0 else (4, 5, 6, 7)
                dsl = slice(dc * 128, (dc + 1) * 128)
                dsl2 = slice(1024 + dc * 128, 1024 + (dc + 1) * 128)
                for q in range(4):
                    pop(lambda h, q=q, bA=bA, dsl=dsl, tsl=tsl: h.matmul(PSF[bA], lhsT=Wfo[:, q, dsl], rhs=FT[:, q, tsl], start=(q == 0), stop=(q == 3)), r=[BW4[0], BFT], w=[PB[bA]])
                for q in range(4):
                    pop(lambda h, q=q, bB=bB, dsl=dsl, tsl=tsl: h.matmul(PSF[bB], lhsT=Wso[:, q, dsl], rhs=yT[:, q, tsl], start=(q == 0), stop=(q == 3)), r=[BW4[1], ByT], w=[PB[bB]])
                for kc in range(8):
                    pop(lambda h, kc=kc, bC=bC, dsl=dsl: h.matmul(PSF[bC], lhsT=Wg_[:, kc, dsl], rhs=hT4[:, kc, :], start=(kc == 0), stop=(kc == 7)), r=[BW4[2], BhT4], w=[PB[bC]])
                for kc in range(8):
                    pop(lambda h, kc=kc, bD=bD, dsl2=dsl2: h.matmul(PSF[bD], lhsT=Wg_[:, kc, dsl2], rhs=hT4[:, kc, :], start=(kc == 0), stop=(kc == 7)), r=[BW4[2], BhT4], w=[PB[bD]])
                aop(lambda h, pz=pz, bC=bC: h.activation(out=s12[pz][0], in_=PSF[bC], func=AF.Sigmoid), r=[PB[bC]], w=[Bs12[pz]])
                aop(lambda h, pz=pz, bD=bD: h.activation(out=s12[pz][1], in_=PSF[bD], func=AF.Sigmoid), r=[PB[bD]], w=[Bs12[pz]])
                vop(lambda h, pz=pz, bA=bA: h.tensor_tensor(out=t12[pz][0], in0=PSF[bA], in1=s12[pz][0], op=ALU.mult), r=[PB[bA], Bs12[pz]], w=[Bs12[pz]])
                vop(lambda h, pz=pz, bB=bB: h.tensor_tensor(out=t12[pz][1], in0=PSF[bB], in1=s12[pz][1], op=ALU.mult), r=[PB[bB], Bs12[pz]], w=[Bs12[pz]])
                gop(lambda h, pz=pz, dc=dc: h.tensor_tensor(out=mg[:, dc, :], in0=t12[pz][0], in1=t12[pz][1], op=ALU.add), r=[Bs12[pz]], w=[Bmg])
            for t in range(4):
                tg = tb * 4 + t
                u = tg % 2
                bk0, bk1 = (0, 1) if u == 0 else (2, 3)
                for hh, bk in ((0, bk0), (1, bk1)):
                    for dc in range(8):
                        pop(lambda h, dc=dc, hh=hh, bk=bk, t=t: h.matmul(PSF[bk], lhsT=mg[:, dc, t * 128:(t + 1) * 128], rhs=Wout[:, dc, hh * 512:(hh + 1) * 512], start=(dc == 0), stop=(dc == 7)), r=[Bmg, BW4[3]], w=[PB[bk]])
                for hh, bk in ((0, bk0), (1, bk1)):
                    vop(lambda h, hh=hh, bk=bk, t=t, u=u: h.tensor_tensor(out=x2t[u][:, hh * 512:(hh + 1) * 512], in0=PSF[bk], in1=xq[t][:, hh * 512:(hh + 1) * 512], op=ALU.add), r=[PB[bk], Bxq[t]], w=[Bx2[u]])
                if STAGE == 1:
                    dma("sync", lambda h, tg=tg, u=u: h.dma_start(out=out_d[tg * 128:(tg + 1) * 128, :], in_=x2t[u]), r=[Bx2[u]], sem="os%d" % u)
                    continue
                dma("sync", lambda h, tg=tg, u=u: h.dma_start(out=x2_d[tg * 128:(tg + 1) * 128, :], in_=x2t[u]), r=[Bx2[u]], w=[Bx2d], sem="x2s%d" % u)
                col = 32 + (tg % 4) * 4
                aop(lambda h, u=u, col=col: h.activation(out=junk4, in_=x2t[u], func=AF.Square, accum_out=stat4[:, col:col + 1]), r=[Bx2[u]], w=[Bjunk, Bstat])
                aop(lambda h, col=col: h.activation(out=stat4[:, col + 1:col + 2], in_=stat4[:, col:col + 1], func=AF.Sqrt, scale=1.0 / 1024, bias=1e-6), r=[Bstat], w=[Bstat])
                vop(lambda h, col=col: h.reciprocal(out=stat4[:, col + 2:col + 3], in_=stat4[:, col + 1:col + 2]), r=[Bstat], w=[Bstat])
                vop(lambda h, u=u, col=col: h.scalar_tensor_tensor(out=hn32[u], in0=x2t[u], scalar=stat4[:, col + 2:col + 3], in1=g2bc, op0=ALU.mult, op1=ALU.mult), r=[Bx2[u], Bstat, Bgg2], w=[Bhn32[u]])
                gop(lambda h, u=u: h.tensor_copy(out=hn16[u], in_=hn32[u]), r=[Bhn32[u]], w=[Bhn16[u]])
                for kc in range(8):
                    bk = 4 + kc // 4
                    o = (kc % 4) * 128
                    pop(lambda h, kc=kc, bk=bk, o=o, u=u: h.transpose(out=PSF[bk][:, o:o + 128], in_=hn32[u][:, kc * 128:(kc + 1) * 128], identity=k32("ident")), r=[Bhn32[u], Bc32], w=[PB[bk]])
                aop(lambda h: h.activation(out=flat(hnT[:, 0:4, :]), in_=PSF[4], func=AF.Copy), r=[PB[4]], w=[BhnT])
                vop(lambda h: h.tensor_copy(out=flat(hnT[:, 4:8, :]), in_=PSF[5]), r=[PB[5]], w=[BhnT])
                for kc in range(8):
                    pop(lambda h, kc=kc: h.matmul(PSF[6][:, 0:72], lhsT=hnT[:, kc, :], rhs=Wr[:, kc, :], start=(kc == 0), stop=(kc == 7)), r=[BhnT, BWr], w=[PB[6]])
                R = [Brs]
                L8, L64 = Lg[:, 0:8], Lg[:, 8:72]
                m8, ohg, negm, ex, sumg, pg = rs[:, 0:8], rs[:, 8:16], rs[:, 16:17], rs[:, 24:32], rs[:, 17:18], rs[:, 18:19]
                tmp64, esel, m8e = rs[:, 64:128], rs[:, 32:40], rs[:, 40:48]
                dv, w1 = rs[:, 19:20], rs[:, 20:21]
                mk1, mk2, msk, slot = rs[:, 128:192], rs[:, 192:256], rs[:, 256:320], rs[:, 320:384]
                idf = rs[:, 48:50]
                vop(lambda h: h.tensor_tensor(out=Lg, in0=PSF[6][:, 0:72], in1=brb, op=ALU.add), r=[PB[6], Bbr], w=R)
                vop(lambda h: h.max(out=m8, in_=L8), r=R, w=R)
                vop(lambda h: h.tensor_scalar(out=ohg, in0=L8, scalar1=m8[:, 0:1], scalar2=None, op0=ALU.is_equal), r=R, w=R)
                vop(lambda h: h.tensor_scalar(out=negm, in0=m8[:, 0:1], scalar1=-1.0, scalar2=None, op0=ALU.mult), r=R, w=R)
                aop(lambda h: h.activation(out=ex, in_=L8, func=AF.Exp, bias=negm, scale=1.0, accum_out=sumg), r=R, w=R)
                vop(lambda h: h.reciprocal(out=pg, in_=sumg), r=R, w=R)
                vop(lambda h: h.tensor_tensor(out=tmp64.rearrange("p (g j) -> p g j", j=8), in0=L64.rearrange("p (g j) -> p g j", j=8), in1=ohg.unsqueeze(2).to_broadcast([128, 8, 8]), op=ALU.mult), r=R, w=R)
                vop(lambda h: h.tensor_reduce(out=esel, in_=tmp64.rearrange("p (g j) -> p j g", j=8), axis=AX.X, op=ALU.add), r=R, w=R)
                vop(lambda h: h.max(out=m8e, in_=esel), r=R, w=R)
                vop(lambda h: h.tensor_tensor(out=dv, in0=m8e[:, 1:2], in1=m8e[:, 0:1], op=ALU.subtract), r=R, w=R)
                aop(lambda h: h.activation(out=w1, in_=dv, func=AF.Sigmoid, scale=-1.0), r=R, w=R)
                vop(lambda h, tg=tg: h.tensor_tensor(out=gw[:, tg, 0:1], in0=pg, in1=w1, op=ALU.mult), r=R, w=[Bgw])
                vop(lambda h, tg=tg: h.tensor_tensor(out=gw[:, tg, 1:2], in0=pg, in1=gw[:, tg, 0:1], op=ALU.subtract), r=R + [Bgw], w=[Bgw])
                ohb = ohg.unsqueeze(2).to_broadcast([128, 8, 8])
                vop(lambda h: h.tensor_scalar(out=mk1, in0=L64, scalar1=m8e[:, 0:1], scalar2=None, op0=ALU.is_equal), r=R, w=R)
                vop(lambda h, ohb=ohb: h.tensor_tensor(out=mk1.rearrange("p (g j) -> p g j", j=8), in0=mk1.rearrange("p (g j) -> p g j", j=8), in1=ohb, op=ALU.mult), r=R, w=R)
                vop(lambda h: h.tensor_scalar(out=mk2, in0=L64, scalar1=m8e[:, 1:2], scalar2=None, op0=ALU.is_equal), r=R, w=R)
                vop(lambda h, ohb=ohb: h.tensor_tensor(out=mk2.rearrange("p (g j) -> p g j", j=8), in0=mk2.rearrange("p (g j) -> p g j", j=8), in1=ohb, op=ALU.mult), r=R, w=R)
                vop(lambda h: h.tensor_tensor(out=msk, in0=mk1, in1=mk2, op=ALU.add), r=R, w=R)
                pop(lambda h: h.matmul(PSF[7][:, 0:64], lhsT=k32("triS"), rhs=msk, start=True, stop=False), r=R + [Bc32], w=[PB[7]])
                pop(lambda h: h.matmul(PSF[7][:, 0:64], lhsT=k32("ones"), rhs=Cacc, start=False, stop=True), r=[BCacc, Bc32], w=[PB[7]])
                vop(lambda h: h.tensor_tensor(out=slot, in0=PSF[7][:, 0:64], in1=k32("ecap", 64), op=ALU.add), r=[PB[7], Bc32], w=R)
                vop(lambda h: h.tensor_tensor(out=Cacc, in0=Cacc, in1=msk, op=ALU.add), r=R + [BCacc], w=[BCacc])
                vop(lambda h: h.tensor_tensor(out=mk1, in0=mk1, in1=slot, op=ALU.mult), r=R, w=R)
                vop(lambda h: h.reduce_sum(out=idf[:, 0:1], in_=mk1, axis=AX.X), r=R, w=R)
                vop(lambda h: h.tensor_tensor(out=mk2, in0=mk2, in1=slot, op=ALU.mult), r=R, w=R)
                vop(lambda h: h.reduce_sum(out=idf[:, 1:2], in_=mk2, axis=AX.X), r=R, w=R)
                vop(lambda h, tg=tg: h.tensor_copy(out=idx[:, tg, :], in_=idf), r=R, w=[Bidx])
                for kk in range(2):
                    dma("gpsimd", lambda h, tg=tg, kk=kk, u=u: h.indirect_dma_start(out=xpad_d, out_offset=bass.IndirectOffsetOnAxis(ap=idx[:, tg, kk:kk + 1], axis=0), in_=hn16[u], in_offset=None, bounds_check=breg(h), oob_is_err=False), r=[Bhn16[u], Bidx], w=[Bxpad], sem="scat")
        S.barrier()
        if STAGE >= 2:
            A6 = Arena(arena_t[:, persist0:ARENA_BYTES], ARENA_BYTES - persist0)
            NWB = 2
            Wge = [A6.alloc([8, 512], BF16) for _ in range(NWB)]
            Wue = [A6.alloc([8, 512], BF16) for _ in range(NWB)]
            Wde = [A6.alloc([4, 1024], BF16) for _ in range(NWB)]
            Sge = [A6.alloc([8, 512], F32) for _ in range(NWB)]
            Sue = [A6.alloc([8, 512], F32) for _ in range(NWB)]
            Sde = [A6.alloc([4, 1024], F32) for _ in range(NWB)]
            BSge = [Buf() for _ in range(NWB)]
            BSue = [Buf() for _ in range(NWB)]
            BSde = [Buf() for _ in range(NWB)]
            BWge = [Buf() for _ in range(NWB)]
            BWue = [Buf() for _ in range(NWB)]
            BWde = [Buf() for _ in range(NWB)]
            Xe = [A6.alloc([1024], BF16) for _ in range(3)]
            BXe = [Buf(), Buf(), Buf()]
            XeT = [A6.alloc([8, 128], BF16) for _ in range(2)]
            BXeT = [Buf(), Buf()]
            Gs = [A6.alloc([512], BF16) for _ in range(2)]
            BGs = [Buf(), Buf()]
            Aa = [A6.alloc([512], BF16) for _ in range(2)]
            BAa = [Buf(), Buf()]
            AT = [A6.alloc([4, 128], BF16) for _ in range(2)]
            BAT = [Buf(), Buf()]
            Ye = [A6.alloc([1024], F32) for _ in range(4)]
            BYe = [Buf() for _ in range(4)]
            Bypad = Buf()
            NXE = 3

            def issue_loads(e):
                wb = e % NWB
                dma("sync", lambda h, e=e, wb=wb: h.dma_start(out=Sge[wb], in_=ewg_d[e].rearrange("(p kc) n -> p kc n", kc=8)), w=[BSge[wb]], sem="wg%d" % wb)
                dma("sync", lambda h, e=e, wb=wb: h.dma_start(out=Sue[wb], in_=ewu_d[e].rearrange("(p kc) n -> p kc n", kc=8)), w=[BSue[wb]], sem="wu%d" % wb)
                dma("sync", lambda h, e=e, wb=wb: h.dma_start(out=Sde[wb], in_=ewd_d[e].rearrange("(p hc) n -> p hc n", hc=4)), w=[BSde[wb]], sem="wd%d" % wb)

            def issue_x(e):
                xb = e % NXE
                dma("scalar", lambda h, e=e, xb=xb: h.dma_start(out=Xe[xb], in_=xpad_d[e * CAP:(e + 1) * CAP, :]), r=[Bxpad], w=[BXe[xb]], sem="xe%d" % xb)

            issue_x(0)
            for e in range(min(NWB, MOE_LIMIT)):
                issue_loads(e)
            for e in range(MOE_LIMIT):
                wb = e % NWB
                u = e % 2
                xb = e % NXE
                if e + 1 < MOE_LIMIT:
                    issue_x(e + 1)
                aop(lambda h, wb=wb: h.activation(out=flat(Wge[wb]), in_=flat(Sge[wb]), func=AF.Copy), r=[BSge[wb]], w=[BWge[wb]])
                gop(lambda h, wb=wb: h.tensor_copy(out=flat(Wue[wb]), in_=flat(Sue[wb])), r=[BSue[wb]], w=[BWue[wb]])
                aop(lambda h, wb=wb: h.activation(out=flat(Wde[wb]), in_=flat(Sde[wb]), func=AF.Copy), r=[BSde[wb]], w=[BWde[wb]])
                if e + NWB < MOE_LIMIT:
                    issue_loads(e + NWB)
                bkT = 0 if u == 0 else 4
                for kc in range(8):
                    pop(lambda h, kc=kc, xb=xb, bkT=bkT: h.transpose(out=PSB[bkT][:, kc * 128:(kc + 1) * 128], in_=Xe[xb][:, kc:1024:8], identity=k16("ident")), r=[BXe[xb], Bc16], w=[PB[bkT]])
                vop(lambda h, u=u, bkT=bkT: h.tensor_copy(out=flat(XeT[u]), in_=PSB[bkT]), r=[PB[bkT]], w=[BXeT[u]])
                bG, bU = (1, 2) if u == 0 else (5, 6)
                for kc in range(8):
                    pop(lambda h, kc=kc, u=u, wb=wb, bG=bG: h.matmul(PSF[bG], lhsT=XeT[u][:, kc, :], rhs=Wge[wb][:, kc, :], start=(kc == 0), stop=(kc == 7)), r=[BXeT[u], BWge[wb]], w=[PB[bG]])
                for kc in range(8):
                    pop(lambda h, kc=kc, u=u, wb=wb, bU=bU: h.matmul(PSF[bU], lhsT=XeT[u][:, kc, :], rhs=Wue[wb][:, kc, :], start=(kc == 0), stop=(kc == 7)), r=[BXeT[u], BWue[wb]], w=[PB[bU]])
                aop(lambda h, u=u, bG=bG: h.activation(out=Gs[u], in_=PSF[bG], func=AF.Silu), r=[PB[bG]], w=[BGs[u]])
                vop(lambda h, u=u, bU=bU: h.tensor_tensor(out=Aa[u], in0=PSF[bU], in1=Gs[u], op=ALU.mult), r=[PB[bU], BGs[u]], w=[BAa[u]])
                bA = 3 if u == 0 else 7
                for hc in range(4):
                    pop(lambda h, hc=hc, u=u, bA=bA: h.transpose(out=PSB[bA][:, hc * 128:(hc + 1) * 128], in_=Aa[u][:, hc:512:4], identity=k16("ident")), r=[BAa[u], Bc16], w=[PB[bA]])
                aop(lambda h, u=u, bA=bA: h.activation(out=flat(AT[u]), in_=PSB[bA][:, 0:512], func=AF.Copy), r=[PB[bA]], w=[BAT[u]])
                for hh, bk in ((0, bG), (1, bU)):
                    for hc in range(4):
                        pop(lambda h, hc=hc, hh=hh, bk=bk, u=u, wb=wb: h.matmul(PSF[bk], lhsT=AT[u][:, hc, :], rhs=Wde[wb][:, hc, hh * 512:(hh + 1) * 512], start=(hc == 0), stop=(hc == 3)), r=[BAT[u], BWde[wb]], w=[PB[bk]])
                yb = e % 4
                vop(lambda h, yb=yb, bG=bG: h.tensor_copy(out=Ye[yb][:, 0:512], in_=PSF[bG]), r=[PB[bG]], w=[BYe[yb]])
                aop(lambda h, yb=yb, bU=bU: h.activation(out=Ye[yb][:, 512:1024], in_=PSF[bU], func=AF.Copy), r=[PB[bU]], w=[BYe[yb]])
                dma("scalar", lambda h, e=e, yb=yb: h.dma_start(out=ypad_d[e * CAP:(e + 1) * CAP, :], in_=Ye[yb]), r=[BYe[yb]], w=[Bypad], sem="ys%d" % yb)
            S.barrier()
            A6.release(0)
            gfbc = A6.alloc([1024], F32)
            Bgf = Buf()
            dma("sync", lambda h: h.dma_start(out=gfbc, in_=gf_d.partition_broadcast(128)), w=[Bgf], sem="gfl")
            y1 = [A6.alloc([1024], F32) for _ in range(2)]
            y2 = [A6.alloc([1024], F32) for _ in range(2)]
            xx = [A6.alloc([1024], F32) for _ in range(2)]
            oo = [A6.alloc([1024], F32) for _ in range(2)]
            By1 = [Buf(), Buf()]
            By2 = [Buf(), Buf()]
            Bxx = [Buf(), Buf()]
            Boo = [Buf(), Buf()]
            junk6 = A6.alloc([1024], BF16)
            Bj6 = Buf()
            st6 = A6.alloc([64], F32)
            Bst6 = Buf()
            for tg in range(16):
                u = tg % 2
                dma("gpsimd", lambda h, tg=tg, u=u: h.indirect_dma_start(out=y1[u], out_offset=None, in_=ypad_d, in_offset=bass.IndirectOffsetOnAxis(ap=idx[:, tg, 0:1], axis=0), bounds_check=breg(h), oob_is_err=False), r=[Bypad, Bidx], w=[By1[u]], sem="ga%d" % u)
                dma("gpsimd", lambda h, tg=tg, u=u: h.indirect_dma_start(out=y2[u], out_offset=None, in_=ypad_d, in_offset=bass.IndirectOffsetOnAxis(ap=idx[:, tg, 1:2], axis=0), bounds_check=breg(h), oob_is_err=False), r=[Bypad, Bidx], w=[By2[u]], sem="gb%d" % u)
                dma("sync", lambda h, tg=tg, u=u: h.dma_start(out=xx[u], in_=x2_d[tg * 128:(tg + 1) * 128, :]), r=[Bx2d], w=[Bxx[u]], sem="xl%d" % u)
                vop(lambda h, tg=tg, u=u: h.scalar_tensor_tensor(out=xx[u], in0=y1[u], scalar=gw[:, tg, 0:1], in1=xx[u], op0=ALU.mult, op1=ALU.add), r=[By1[u], Bgw, Bxx[u]], w=[Bxx[u]])
                vop(lambda h, tg=tg, u=u: h.scalar_tensor_tensor(out=xx[u], in0=y2[u], scalar=gw[:, tg, 1:2], in1=xx[u], op0=ALU.mult, op1=ALU.add), r=[By2[u], Bgw, Bxx[u]], w=[Bxx[u]])
                col = (tg % 8) * 4
                aop(lambda h, u=u, col=col: h.activation(out=junk6, in_=xx[u], func=AF.Square, accum_out=st6[:, col:col + 1]), r=[Bxx[u]], w=[Bj6, Bst6])
                aop(lambda h, col=col: h.activation(out=st6[:, col + 1:col + 2], in_=st6[:, col:col + 1], func=AF.Sqrt, scale=1.0 / 1024, bias=1e-6), r=[Bst6], w=[Bst6])
                vop(lambda h, col=col: h.reciprocal(out=st6[:, col + 2:col + 3], in_=st6[:, col + 1:col + 2]), r=[Bst6], w=[Bst6])
                vop(lambda h, u=u, col=col: h.scalar_tensor_tensor(out=oo[u], in0=xx[u], scalar=st6[:, col + 2:col + 3], in1=gfbc, op0=ALU.mult, op1=ALU.mult), r=[Bxx[u], Bst6, Bgf], w=[Boo[u]])
                dma("sync", lambda h, tg=tg, u=u: h.dma_start(out=out_d[tg * 128:(tg + 1) * 128, :], in_=oo[u]), r=[Boo[u]], sem="os%d" % u)
        S.barrier()
        S.run()
    return nc, dbg_outs


def _consts():
    bf = ml_dtypes.bfloat16
    p = np.arange(128)
    c16 = np.zeros((128, C16["n"]), np.float32)
    c16[:, C16["ident"]:C16["ident"] + 128] = np.eye(128)
    c16[:, C16["triL"]:C16["triL"] + 128] = (p[:, None] <= p[None, :])
    c16[:, C16["triU"]:C16["triU"] + 128] = (p[:, None] >= p[None, :])
    c16[127, C16["selL"]:C16["selL"] + 128] = 1.0
    c16[0, C16["selF"]:C16["selF"] + 128] = 1.0
    ang = 2.0 * np.pi * ((p[:, None] * p[None, :]) % 128) / 128.0
    c16[:, C16["CC"]:C16["CC"] + 128] = np.cos(ang)
    c16[:, C16["SS"]:C16["SS"] + 128] = np.sin(ang)
    c32 = np.zeros((128, C32["n"]), np.float32)
    c32[:, C32["ident"]:C32["ident"] + 128] = np.eye(128)
    c32[:, C32["triS"]:C32["triS"] + 128] = (p[:, None] < p[None, :])
    c32[:, C32["ones"]:C32["ones"] + 128] = 1.0
    jj = p // 16
    c32[:, C32["mask0"]:C32["mask0"] + 128] = (jj[None, :] >= jj[:, None])
    c32[:, C32["mask1"]:C32["mask1"] + 128] = (jj[:, None] >= jj[None, :])
    c32[:, C32["kf"]] = p + 1
    c32[:, C32["kb"]] = 128 - p
    c32[:, C32["ev"]:C32["ev"] + 17] = np.arange(-8, 9)[None, :]
    c32[:, C32["evr"]:C32["evr"] + 17] = np.arange(8, -9, -1)[None, :]
    c32[:, C32["ecap"]:C32["ecap"] + 64] = (np.arange(64) * CAP)[None, :]
    c32[:, C32["iota"]:C32["iota"] + 128] = p[None, :]
    return c16.astype(bf), c32


def _dft_tables(hf):
    bf = ml_dtypes.bfloat16
    L = 4096
    base = np.arange(L, dtype=np.float64) * (2.0 * np.pi / L)
    sc = 1.0 / math.sqrt(L * 128.0)
    cosb = (np.cos(base) * sc).astype(np.float32)
    sinb = (-np.sin(base) * sc).astype(np.float32)
    t = np.arange(L, dtype=np.int64)
    m = np.arange(L // 2, dtype=np.int64)
    l = t if hf == 0 else (L - 1 - t)
    k = m if hf == 0 else (L - 1 - m)
    prod = (l[:, None] * k[None, :]) % L

    def lay(t):
        return np.ascontiguousarray(t.reshape(32, 128, 8, 256).transpose(2, 1, 0, 3))

    return lay(cosb[prod].astype(bf)), lay(sinb[prod].astype(bf))


_CACHE = {}


def kernel(**inp):
    f32 = np.float32
    x = np.asarray(inp["x"], f32)
    if "nc" not in _CACHE:
        _CACHE["nc"] = build_program()
        _CACHE["c"] = _consts()
        _CACHE["tab"] = [_dft_tables(0), _dft_tables(1)]
    nc, dbg_outs = _CACHE["nc"]
    c16, c32 = _CACHE["c"]
    shared = {
        "mix_norm_g": np.ascontiguousarray(inp["mix_norm_g"][0], f32),
        "w_in": np.ascontiguousarray(inp["w_in"][0], f32),
        "w_fourier_out": np.ascontiguousarray(inp["w_fourier_out"][0], f32),
        "ssm_D": np.ascontiguousarray(inp["ssm_D"][0], f32),
        "ssm_w_glu": np.ascontiguousarray(inp["ssm_w_glu"][0], f32),
        "w_ssm_out": np.ascontiguousarray(inp["w_ssm_out"][0], f32),
        "w_out": np.ascontiguousarray(inp["w_out"][0], f32),
        "ffn_norm_g": np.ascontiguousarray(inp["ffn_norm_g"][0], f32),
        "w_router": np.ascontiguousarray(np.concatenate([inp["router_group_w"][0], inp["router_expert_w"][0]], axis=1), f32),
        "b_router": np.ascontiguousarray(np.concatenate([inp["router_group_b"][0], inp["router_expert_b"][0]], axis=0), f32),
        "final_norm_g": np.ascontiguousarray(inp["final_norm_g"], f32),
        "cst16": c16,
        "cst32": c32,
    }
    if STAGE >= 2:
        shared["expert_w_gate"] = np.ascontiguousarray(inp["expert_w_gate"][0], f32)
        shared["expert_w_up"] = np.ascontiguousarray(inp["expert_w_up"][0], f32)
        shared["expert_w_down"] = np.ascontiguousarray(inp["expert_w_down"][0], f32)
    in_maps = []
    for c in range(8):
        b, hf = c // 2, c % 2
        dsel = [0, 1] if hf == 0 else [1, 0]
        xl = x[b] if hf == 0 else x[b][::-1]
        m = dict(shared)
        m["x"] = np.ascontiguousarray(xl, f32)
        m["sA_re"] = np.ascontiguousarray(inp["ssm_A_re"][0][dsel], f32)
        m["sA_im"] = np.ascontiguousarray(inp["ssm_A_im"][0][dsel], f32)
        m["s_ldt"] = np.ascontiguousarray(inp["ssm_log_dt"][0][dsel], f32)
        m["sB_re"] = np.ascontiguousarray(inp["ssm_B_re"][0][dsel], f32)
        m["sB_im"] = np.ascontiguousarray(inp["ssm_B_im"][0][dsel], f32)
        m["sC_re"] = np.ascontiguousarray(inp["ssm_C_re"][0][dsel], f32)
        m["sC_im"] = np.ascontiguousarray(inp["ssm_C_im"][0][dsel], f32)
        m["tab_c"], m["tab_s"] = _CACHE["tab"][hf]
        in_maps.append(m)
    res = run_bass_kernel_spmd(nc, in_maps, core_ids=list(range(8)))
    out = np.empty((4, 4096, 1024), f32)
    for c in range(8):
        b, hf = c // 2, c % 2
        o = np.asarray(res.results[c]["out"], f32)
        if hf == 0:
            out[b, 0:2048] = o
        else:
            out[b, 2048:4096] = o[::-1]
    _CACHE["last"] = res
    return out
```

```python
import math
from contextlib import ExitStack

import ml_dtypes
import numpy as np

import concourse.bass as bass
import concourse.mybir as mybir
from concourse.bass_utils import run_bass_kernel_spmd

F32 = mybir.dt.float32
BF16 = mybir.dt.bfloat16
I32 = mybir.dt.int32
U8 = mybir.dt.uint8
ALU = mybir.AluOpType
AF = mybir.ActivationFunctionType
AX = mybir.AxisListType

ENGS = ["sync", "scalar", "vector", "gpsimd", "tensor"]
TWO_PI = 2.0 * math.pi
PI_SAFE = 3.14159
CAP = 128
NEXP = 64
STAGE = 2
DEBUG = False
MOE_LIMIT = 64


class Tok:
    __slots__ = ("kind", "eng", "n")

    def __init__(self, kind, eng, n):
        self.kind = kind
        self.eng = eng
        self.n = n


class Buf:
    def __init__(self, name=""):
        self.name = name
        self.w = []
        self.r = []


class Sched:
    def __init__(self, nc, stack):
        self.nc = nc
        self.stack = stack
        self.ops = {e: [] for e in ENGS}
        self.cnt = {e: 0 for e in ENGS}
        self.sem = {e: stack.enter_context(nc.semaphore("c_" + e)) for e in ENGS}
        self.waited = {e: {} for e in ENGS}
        self.dsem = {}
        self.dcnt = {}

    def _waits(self, eng, deps):
        waits = []
        for d in deps:
            if d is None:
                continue
            if d.kind == "eng":
                if d.eng == eng and eng == "tensor":
                    continue
                key = d.eng
                sem = self.sem[d.eng]
            else:
                key = "d_" + d.eng
                sem = self.dsem[d.eng]
            if self.waited[eng].get(key, 0) >= d.n:
                continue
            self.waited[eng][key] = d.n
            waits.append((sem, d.n))
        return waits

    @staticmethod
    def _deps(reads, writes, extra):
        deps = list(extra)
        for b in reads:
            deps += b.w
        for b in writes:
            deps += b.w
            deps += b.r
        return deps

    @staticmethod
    def _note(tok, reads, writes):
        for b in reads:
            b.r = [t for t in b.r if not (t.kind == tok.kind and t.eng == tok.eng)] + [tok]
        for b in writes:
            b.w = [t for t in b.w if not (t.kind == tok.kind and t.eng == tok.eng)] + [tok]
            b.r = []

    def op(self, eng, build, reads=(), writes=(), extra=()):
        waits = self._waits(eng, self._deps(reads, writes, extra))
        self.cnt[eng] += 1
        n = self.cnt[eng]
        sem = self.sem[eng]

        def fn(h):
            for (s, v) in waits:
                h.wait_ge(s, v)
            build(h).then_inc(sem, 1)

        self.ops[eng].append(fn)
        tok = Tok("eng", eng, n)
        self._note(tok, reads, writes)
        return tok

    def dma(self, queue, semname, build, reads=(), writes=(), extra=()):
        if semname not in self.dsem:
            self.dsem[semname] = self.stack.enter_context(self.nc.semaphore("d_" + semname))
            self.dcnt[semname] = 0
        waits = self._waits(queue, self._deps(reads, writes, extra))
        self.dcnt[semname] += 16
        n = self.dcnt[semname]
        sem = self.dsem[semname]

        def fn(h):
            for (s, v) in waits:
                h.wait_ge(s, v)
            build(h).then_inc(sem, 16)

        self.ops[queue].append(fn)
        tok = Tok("dma", semname, n)
        self._note(tok, reads, writes)
        return tok

    def all_toks(self):
        t = [Tok("eng", e, self.cnt[e]) for e in ENGS if self.cnt[e] > 0]
        t += [Tok("dma", k, v) for k, v in self.dcnt.items() if v > 0]
        return t

    def barrier(self):
        toks = self.all_toks()
        for e in ENGS:
            waits = self._waits(e, toks)

            def fn(h, waits=waits):
                for (s, v) in waits:
                    h.wait_ge(s, v)

            self.ops[e].append(fn)

    def run(self):
        with self.nc.Block() as block:
            for e in ENGS:
                ops = self.ops[e]

                def body(h, ops=ops):
                    for fn in ops:
                        fn(h)

                getattr(block, e)(body)


class Arena:
    def __init__(self, ap_u8, size):
        self.ap = ap_u8
        self.size = size
        self.top = 0

    def alloc(self, free_shape, dt):
        isz = {F32: 4, I32: 4, BF16: 2}[dt]
        n = 1
        for s in free_shape:
            n *= s
        nb = n * isz
        off = self.top
        self.top += (nb + 63) // 64 * 64
        assert self.top <= self.size, ("SBUF arena overflow", self.top, self.size)
        v = self.ap[:, off:off + nb].bitcast(dt)
        if len(free_shape) > 1:
            names = ["a%d" % i for i in range(len(free_shape))]
            kw = {names[i]: free_shape[i] for i in range(1, len(free_shape))}
            v = v.rearrange("p (%s) -> p %s" % (" ".join(names), " ".join(names)), **kw)
        return v

    def mark(self):
        return self.top

    def release(self, m):
        self.top = m


def flat(ap):
    nd = len(ap.shape)
    if nd == 2:
        return ap
    names = ["a%d" % i for i in range(nd - 1)]
    return ap.rearrange("p %s -> p (%s)" % (" ".join(names), " ".join(names)))


C16 = dict(ident=0, triL=128, triU=256, selL=384, selF=512, CC=640, SS=768, n=896)
C32 = dict(ident=0, triS=128, ones=256, mask0=384, mask1=512, kf=640, kb=641, ev=642, ecap=659, iota=723, evr=851, n=868)


def build_program():
    nc = bass.Bass("TRN2", target_bir_lowering=False)

    def din(name, shape, dt=F32):
        return nc.dram_tensor(name, list(shape), dt, kind="ExternalInput").ap()

    x_d = din("x", [4096, 1024])
    g1_d = din("mix_norm_g", [1024])
    w_in_d = din("w_in", [1024, 3072])
    wfo_d = din("w_fourier_out", [512, 1024])
    sAre_d = din("sA_re", [2, 32, 64])
    sAim_d = din("sA_im", [2, 32, 64])
    sldt_d = din("s_ldt", [2, 32])
    sBre_d = din("sB_re", [2, 32, 64, 16])
    sBim_d = din("sB_im", [2, 32, 64, 16])
    sCre_d = din("sC_re", [2, 32, 16, 64])
    sCim_d = din("sC_im", [2, 32, 16, 64])
    sD_d = din("ssm_D", [512])
    wglu_d = din("ssm_w_glu", [512, 512])
    wso_d = din("w_ssm_out", [512, 1024])
    wout_d = din("w_out", [1024, 1024])
    g2_d = din("ffn_norm_g", [1024])
    wr_d = din("w_router", [1024, 72])
    br_d = din("b_router", [72])
    if STAGE >= 2:
        ewg_d = din("expert_w_gate", [64, 1024, 512])
        ewu_d = din("expert_w_up", [64, 1024, 512])
        ewd_d = din("expert_w_down", [64, 512, 1024])
    gf_d = din("final_norm_g", [1024])
    tc_d = din("tab_c", [8, 128, 32, 256], BF16)
    ts_d = din("tab_s", [8, 128, 32, 256], BF16)
    c16_d = din("cst16", [128, C16["n"]], BF16)
    c32_d = din("cst32", [128, C32["n"]])
    out_d = nc.dram_tensor("out", [2048, 1024], F32, kind="ExternalOutput").ap()
    xpad_d = nc.dram_tensor("xpad", [NEXP * CAP, 1024], BF16, kind="Internal").ap()
    ypad_d = nc.dram_tensor("ypad", [NEXP * CAP, 1024], F32, kind="Internal").ap()
    x2_d = nc.dram_tensor("x2s", [2048, 1024], F32, kind="Internal").ap()
    dbg_outs = {}

    with ExitStack() as st:
        S = Sched(nc, st)
        ARENA_BYTES = 206 * 1024
        arena_t = st.enter_context(nc.sbuf_tensor("arena", [128, ARENA_BYTES], U8))
        AR = Arena(arena_t[:, :], ARENA_BYTES)
        PSF, PSB, PB = [], [], []
        for i in range(8):
            pt = st.enter_context(nc.psum_tensor("ps%d" % i, [128, 512], F32))
            PSF.append(pt[:, :])
            PSB.append(pt[:, :].bitcast(BF16))
            PB.append(Buf("ps%d" % i))

        def vop(fn, r=(), w=(), x=()):
            return S.op("vector", fn, reads=r, writes=w, extra=x)

        def aop(fn, r=(), w=(), x=()):
            return S.op("scalar", fn, reads=r, writes=w, extra=x)

        def gop(fn, r=(), w=(), x=()):
            return S.op("gpsimd", fn, reads=r, writes=w, extra=x)

        def pop(fn, r=(), w=(), x=()):
            return S.op("tensor", fn, reads=r, writes=w, extra=x)

        _dq = [0]
        _breg = []

        def breg(h):
            if not _breg:
                _breg.append(h.to_reg(NEXP * CAP - 1))
            return _breg[0]

        def dma(q, fn, r=(), w=(), x=(), sem=None):
            if sem is None:
                _dq[0] += 1
                sem = "q%d" % (_dq[0] % 8)
            return S.dma(q, sem, fn, reads=r, writes=w, extra=x)

        def dbg(name, ap, shape):
            if not DEBUG:
                return
            d = nc.dram_tensor("dbg_" + name, list(shape), ap.dtype, kind="ExternalOutput").ap()
            dbg_outs[name] = d
            b = Buf()
            S.barrier()
            dma("sync", lambda h: h.dma_start(out=d, in_=ap), w=[b], sem="dbg")
            S.barrier()

        c16 = AR.alloc([C16["n"]], BF16)
        c32 = AR.alloc([C32["n"]], F32)
        Bc16, Bc32 = Buf(), Buf()
        dma("sync", lambda h: h.dma_start(out=c16, in_=c16_d), w=[Bc16])
        dma("sync", lambda h: h.dma_start(out=c32, in_=c32_d), w=[Bc32])

        def k16(name):
            o = C16[name]
            return c16[:, o:o + 128]

        def k32(name, n=128):
            o = C32[name]
            return c32[:, o:o + n]

        gw = AR.alloc([16, 2], F32)
        idx = AR.alloc([16, 2], I32)
        Bgw, Bidx = Buf(), Buf()
        persist0 = AR.top
        Min = AR.alloc([2, 32, 128], BF16)
        Q16 = AR.alloc([2, 32, 128], BF16)
        Mi = AR.alloc([32, 128], BF16)
        BMin, BQ16, BMi = Buf(), Buf(), Buf()
        ft_off = AR.top
        FT = AR.alloc([4, 2048], BF16)
        yT = AR.alloc([4, 2048], BF16)
        xs_off = AR.top
        Xs = AR.alloc([32, 512], BF16)
        uf_off = AR.top
        uf_tm = AR.alloc([32, 512], BF16)
        persist_mark = AR.mark()

        AR.release(ft_off)
        are = AR.alloc([64], F32)
        aim = AR.alloc([64], F32)
        dtn = AR.alloc([64], F32)
        Bre = AR.alloc([64, 16], F32)
        Bim = AR.alloc([64, 16], F32)
        Cre = AR.alloc([64, 16], F32)
        Cim = AR.alloc([64, 16], F32)
        Cld = AR.alloc([8, 2, 64], F32)
        Dp = AR.alloc([32], F32)
        Bp = Buf("params")
        for hf in range(2):
            ps_ = slice(hf * 64, hf * 64 + 64)
            dma("sync", lambda h, ps_=ps_: h.dma_start(out=are[ps_, :], in_=sAre_d.rearrange("d g n -> n (d g)"), allow_slow_non_contiguous=True), w=[Bp])
            dma("sync", lambda h, ps_=ps_: h.dma_start(out=aim[ps_, :], in_=sAim_d.rearrange("d g n -> n (d g)"), allow_slow_non_contiguous=True), w=[Bp])
            dma("sync", lambda h, ps_=ps_: h.dma_start(out=Bre[ps_, :, :], in_=sBre_d.rearrange("d g n c -> n (d g) c")), w=[Bp])
            dma("sync", lambda h, ps_=ps_: h.dma_start(out=Bim[ps_, :, :], in_=sBim_d.rearrange("d g n c -> n (d g) c")), w=[Bp])
        dma("sync", lambda h: h.dma_start(out=dtn, in_=sldt_d.rearrange("d g -> (d g)").partition_broadcast(128)), w=[Bp])
        for j in range(8):
            dma("sync", lambda h, j=j: h.dma_start(out=Dp[j * 16:(j + 1) * 16, :], in_=sD_d.rearrange("(g c) -> c g", c=16), allow_slow_non_contiguous=True), w=[Bp])
        Cld2 = AR.alloc([8, 2, 64], F32)
        for (src, cl) in ((sCre_d, Cld), (sCim_d, Cld2)):
            for dup in range(2):
                dma("sync", lambda h, src=src, dup=dup, cl=cl: h.dma_start(out=cl[:, :, dup, :], in_=src.rearrange("d g c n -> (d g c) n").rearrange("(t p) n -> p t n", p=128)), w=[Bp])
        S.barrier()
        for (cl, dst) in ((Cld, Cre), (Cld2, Cim)):
            for half in range(2):
                bk = half
                for t4 in range(4):
                    t = half * 4 + t4
                    pop(lambda h, t=t, t4=t4, bk=bk, cl=cl: h.transpose(out=PSF[bk][:, t4 * 128:(t4 + 1) * 128], in_=flat(cl[:, t, :, :]), identity=k32("ident")), r=[Bp, Bc32], w=[PB[bk]])
                vop(lambda h, dst=dst, half=half, bk=bk: h.tensor_copy(out=flat(dst)[:, half * 512:(half + 1) * 512], in_=PSF[bk]), r=[PB[bk]], w=[Bp])

        ev = k32("ev", 17)
        evr = k32("evr", 17)
        t0 = AR.alloc([32, 8, 16], F32)
        t1 = AR.alloc([32, 8, 16], F32)
        t0f, t1f = flat(t0), flat(t1)
        tA = t0f[:, 0:1088].rearrange("p (a b) -> p a b", b=64)
        tB = t0f[:, 1088:2176].rearrange("p (a b) -> p a b", b=64)
        tI = t0f[:, 2176:3264].bitcast(I32).rearrange("p (a b) -> p a b", b=64)
        Emag = t1f[:, 0:1088].rearrange("p (a b) -> p a b", b=64)
        Er = AR.alloc([17, 64], F32)
        Ei = AR.alloc([17, 64], F32)
        ErR = AR.alloc([17, 64], F32)
        EiR = AR.alloc([17, 64], F32)
        sm = [AR.alloc([64], F32) for _ in range(8)]
        BE = Buf("E")

        def sincos(xin, sin_out, cos_out, ti, tf, bufs):
            vop(lambda h: h.tensor_scalar(out=ti, in0=xin, scalar1=1.0 / TWO_PI, scalar2=None, op0=ALU.mult), r=bufs, w=bufs)
            vop(lambda h: h.tensor_copy(out=tf, in_=ti), r=bufs, w=bufs)
            vop(lambda h: h.scalar_tensor_tensor(out=tf, in0=tf, scalar=-TWO_PI, in1=xin, op0=ALU.mult, op1=ALU.add), r=bufs, w=bufs)
            vop(lambda h: h.tensor_scalar(out=tf, in0=tf, scalar1=PI_SAFE, scalar2=-PI_SAFE, op0=ALU.min, op1=ALU.max), r=bufs, w=bufs)
            aop(lambda h: h.activation(out=sin_out, in_=tf, func=AF.Sin), r=bufs, w=bufs)
            vop(lambda h: h.tensor_scalar(out=cos_out, in0=tf, scalar1=math.pi / 2, scalar2=-TWO_PI, op0=ALU.is_gt, op1=ALU.mult), r=bufs, w=bufs)
            vop(lambda h: h.scalar_tensor_tensor(out=cos_out, in0=tf, scalar=math.pi / 2, in1=cos_out, op0=ALU.add, op1=ALU.add), r=bufs, w=bufs)
            vop(lambda h: h.tensor_scalar(out=cos_out, in0=cos_out, scalar1=PI_SAFE, scalar2=-PI_SAFE, op0=ALU.min, op1=ALU.max), r=bufs, w=bufs)
            aop(lambda h: h.activation(out=cos_out, in_=cos_out, func=AF.Sin), r=bufs, w=bufs)

        PB0 = [Bp, BE, Bc32]
        aop(lambda h: h.activation(out=dtn, in_=dtn, func=AF.Exp), r=PB0, w=PB0)
        ar_, th_ = sm[0], sm[1]
        vop(lambda h: h.tensor_tensor(out=ar_, in0=are, in1=dtn, op=ALU.mult), r=PB0, w=PB0)
        vop(lambda h: h.tensor_tensor(out=th_, in0=aim, in1=dtn, op=ALU.mult), r=PB0, w=PB0)

        def etable(evc, Er_, Ei_):
            vop(lambda h: h.tensor_tensor(out=tA, in0=ar_.unsqueeze(1).to_broadcast([128, 17, 64]), in1=evc.unsqueeze(2).to_broadcast([128, 17, 64]), op=ALU.mult), r=PB0, w=PB0)
            aop(lambda h: h.activation(out=flat(Emag), in_=flat(tA), func=AF.Exp), r=PB0, w=PB0)
            vop(lambda h: h.tensor_tensor(out=tA, in0=th_.unsqueeze(1).to_broadcast([128, 17, 64]), in1=evc.unsqueeze(2).to_broadcast([128, 17, 64]), op=ALU.mult), r=PB0, w=PB0)
            sincos(flat(tA), flat(Ei_), flat(Er_), flat(tI), flat(tB), PB0)
            vop(lambda h: h.tensor_tensor(out=flat(Er_), in0=flat(Er_), in1=flat(Emag), op=ALU.mult), r=PB0, w=PB0)
            vop(lambda h: h.tensor_tensor(out=flat(Ei_), in0=flat(Ei_), in1=flat(Emag), op=ALU.mult), r=PB0, w=PB0)

        etable(ev, Er, Ei)
        etable(evr, ErR, EiR)
        am1, nr, ni, den, fr, fi = sm[2], sm[3], sm[4], sm[5], sm[6], sm[7]
        ar1, ai1 = Er[:, 9, :], Ei[:, 9, :]
        vop(lambda h: h.tensor_scalar(out=am1, in0=ar1, scalar1=-1.0, scalar2=None, op0=ALU.add), r=PB0, w=PB0)
        vop(lambda h: h.tensor_tensor(out=nr, in0=am1, in1=are, op=ALU.mult), r=PB0, w=PB0)
        vop(lambda h: h.tensor_tensor(out=den, in0=ai1, in1=aim, op=ALU.mult), r=PB0, w=PB0)
        vop(lambda h: h.tensor_tensor(out=nr, in0=nr, in1=den, op=ALU.add), r=PB0, w=PB0)
        vop(lambda h: h.tensor_tensor(out=ni, in0=ai1, in1=are, op=ALU.mult), r=PB0, w=PB0)
        vop(lambda h: h.tensor_tensor(out=den, in0=am1, in1=aim, op=ALU.mult), r=PB0, w=PB0)
        vop(lambda h: h.tensor_tensor(out=ni, in0=ni, in1=den, op=ALU.subtract), r=PB0, w=PB0)
        vop(lambda h: h.tensor_tensor(out=den, in0=are, in1=are, op=ALU.mult), r=PB0, w=PB0)
        vop(lambda h: h.tensor_tensor(out=am1, in0=aim, in1=aim, op=ALU.mult), r=PB0, w=PB0)
        vop(lambda h: h.tensor_tensor(out=den, in0=den, in1=am1, op=ALU.add), r=PB0, w=PB0)
        vop(lambda h: h.reciprocal(out=den, in_=den), r=PB0, w=PB0)
        vop(lambda h: h.tensor_tensor(out=fr, in0=nr, in1=den, op=ALU.mult), r=PB0, w=PB0)
        vop(lambda h: h.tensor_tensor(out=fi, in0=ni, in1=den, op=ALU.mult), r=PB0, w=PB0)
        bbr = AR.alloc([64, 16], F32)
        bbi = AR.alloc([64, 16], F32)
        tb1 = AR.alloc([64, 16], F32)

        def bc16(a):
            return a.unsqueeze(2).to_broadcast([128, a.shape[1], 16])

        vop(lambda h: h.tensor_tensor(out=bbr, in0=Bre, in1=bc16(fr), op=ALU.mult), r=PB0, w=PB0)
        vop(lambda h: h.tensor_tensor(out=tb1, in0=Bim, in1=bc16(fi), op=ALU.mult), r=PB0, w=PB0)
        vop(lambda h: h.tensor_tensor(out=bbr, in0=bbr, in1=tb1, op=ALU.subtract), r=PB0, w=PB0)
        vop(lambda h: h.tensor_tensor(out=bbi, in0=Bim, in1=bc16(fr), op=ALU.mult), r=PB0, w=PB0)
        vop(lambda h: h.tensor_tensor(out=tb1, in0=Bre, in1=bc16(fi), op=ALU.mult), r=PB0, w=PB0)
        vop(lambda h: h.tensor_tensor(out=bbi, in0=bbi, in1=tb1, op=ALU.add), r=PB0, w=PB0)

        PinN = AR.alloc([2, 32, 8, 16], BF16)
        P16 = AR.alloc([2, 32, 8, 16], BF16)
        BPin, BPm = Buf(), Buf()
        Bt_lo, Bt_hi = Buf(), Buf()
        Q16v = AR.alloc([2, 32, 8, 16], BF16)
        BQt = Buf()

        def cmul_batch(out_ap, outbuf, Ar, Ai, d, Tr_, Ti_, i0, mode):
            def vv(T, lo, hi):
                return T[lo:hi, i0:i0 + 8, d * 32:(d + 1) * 32].rearrange("p j g -> p g j").unsqueeze(3).to_broadcast([hi - lo, 32, 8, 16])

            def aa(A, lo, hi):
                return A[lo:hi, d * 32:(d + 1) * 32, :].unsqueeze(2).to_broadcast([hi - lo, 32, 8, 16])

            rr = PB0
            vop(lambda h: h.tensor_tensor(out=t0[0:64], in0=aa(Ar, 0, 64), in1=vv(Tr_, 0, 64), op=ALU.mult), r=rr, w=[Bt_lo])
            vop(lambda h: h.tensor_tensor(out=t1[0:64], in0=aa(Ai, 0, 64), in1=vv(Ti_, 0, 64), op=ALU.mult), r=rr, w=[Bt_lo])
            vop(lambda h: h.tensor_tensor(out=out_ap[0:64], in0=t0[0:64], in1=t1[0:64], op=ALU.subtract), r=[Bt_lo], w=[outbuf])
            gop(lambda h: h.tensor_tensor(out=t0[64:128], in0=aa(Ar, 64, 128), in1=vv(Ti_, 64, 128), op=ALU.mult), r=rr, w=[Bt_hi])
            gop(lambda h: h.tensor_tensor(out=t1[64:128], in0=aa(Ai, 64, 128), in1=vv(Tr_, 64, 128), op=ALU.mult), r=rr, w=[Bt_hi])
            if mode == "P":
                gop(lambda h: h.tensor_tensor(out=out_ap[64:128], in0=t0[64:128], in1=t1[64:128], op=ALU.add), r=[Bt_hi], w=[outbuf])
            else:
                gop(lambda h: h.tensor_tensor(out=t0[64:128], in0=t0[64:128], in1=t1[64:128], op=ALU.add), r=[Bt_hi], w=[Bt_hi])
                gop(lambda h: h.tensor_scalar(out=out_ap[64:128], in0=t0[64:128], scalar1=-1.0, scalar2=None, op0=ALU.mult), r=[Bt_hi], w=[outbuf])

        cmul_batch(PinN[:, 0], BPin, bbr, bbi, 0, ErR, EiR, 1, "P")
        cmul_batch(P16[:, 0], BPm, bbr, bbi, 0, ErR, EiR, 9, "P")
        cmul_batch(Q16v[:, 0], BQt, Cre, Cim, 0, Er, Ei, 9, "Q")
        cmul_batch(PinN[:, 1], BPin, bbr, bbi, 1, Er, Ei, 8, "P")
        cmul_batch(P16[:, 1], BPm, bbr, bbi, 1, Er, Ei, 0, "P")
        cmul_batch(Q16v[:, 1], BQt, Cre, Cim, 1, ErR, EiR, 0, "Q")
        aop(lambda h: h.activation(out=flat(Q16), in_=flat(Q16v), func=AF.Copy), r=[BQt], w=[BQ16])
        for d in range(2):
            for g8 in range(4):
                bk = 2 + (g8 % 2)
                for gi in range(8):
                    g = g8 * 8 + gi
                    pop(lambda h, d=d, g=g, gi=gi, bk=bk: h.transpose(out=PSB[bk][:, gi * 128:(gi + 1) * 128], in_=flat(PinN[:, d, g, :, :]), identity=k16("ident")), r=[BPin, Bc16], w=[PB[bk]])
                aop(lambda h, d=d, g8=g8, bk=bk: h.activation(out=flat(Min[:, d, g8 * 8:(g8 + 1) * 8, :]), in_=PSB[bk], func=AF.Copy), r=[PB[bk]], w=[BMin])
        mt0 = AR.alloc([4, 128], F32)
        mt1 = AR.alloc([4, 128], F32)
        Bmt = Buf()
        for g4 in range(8):
            for d in range(2):
                bk = 4 + d
                for gi in range(4):
                    g = g4 * 4 + gi
                    pop(lambda h, d=d, g=g, gi=gi, bk=bk: h.matmul(PSF[bk][:, gi * 128:(gi + 1) * 128], lhsT=flat(P16[:, d, g, :, :]), rhs=Q16[:, d, g, :], start=True, stop=True), r=[BPm, BQ16], w=[PB[bk]])
            vop(lambda h: h.tensor_tensor(out=mt0, in0=PSF[4].rearrange("p (a b) -> p a b", b=128), in1=k32("mask0").unsqueeze(1).to_broadcast([128, 4, 128]), op=ALU.mult), r=[PB[4], Bc32], w=[Bmt])
            vop(lambda h: h.tensor_tensor(out=mt1, in0=PSF[5].rearrange("p (a b) -> p a b", b=128), in1=k32("mask1").unsqueeze(1).to_broadcast([128, 4, 128]), op=ALU.mult), r=[PB[5], Bc32], w=[Bmt])
            vop(lambda h: h.tensor_tensor(out=mt0, in0=mt0, in1=mt1, op=ALU.add), r=[Bmt], w=[Bmt])
            for gi in range(4):
                g = g4 * 4 + gi
                vop(lambda h, g=g, gi=gi: h.scalar_tensor_tensor(out=Mi[:, g, :], in0=k32("ident"), scalar=Dp[:, g:g + 1], in1=mt0[:, gi, :], op0=ALU.mult, op1=ALU.add), r=[Bmt, Bp, Bc32], w=[BMi])
        S.barrier()
        AR.release(persist_mark)

        Buf_uf, BX = Buf(), Buf()
        p1_mark = AR.mark()
        Wfs = AR.alloc([8, 1024], BF16)
        BWfs = Buf()
        dma("gpsimd", lambda h: h.dma_start(out=Wfs, in_=w_in_d[:, 0:1024].rearrange("(kc p) n -> p kc n", p=128)), w=[BWfs], sem="wfs")
        gbc = AR.alloc([1024], F32)
        Bg = Buf()
        dma("sync", lambda h: h.dma_start(out=gbc, in_=g1_d.partition_broadcast(128)), w=[Bg], sem="g1a")
        hT = AR.alloc([8, 1024], BF16)
        BhT = Buf()
        Zt = AR.alloc([32, 8, 16], BF16)
        BZ = Buf()
        NXB = 3
        xt = [AR.alloc([1024], F32) for _ in range(NXB)]
        Bxt = [Buf() for _ in range(NXB)]
        xn = [AR.alloc([1024], BF16) for _ in range(2)]
        Bxn = [Buf() for _ in range(2)]
        junk = AR.alloc([1024], BF16)
        Bjunk = Buf()
        stat = AR.alloc([64], F32)
        Bstat = Buf()

        def norm_tile(xsrc_ap, xt_ap, bxt, xn_ap, bxn, col, gb, bg, qsem):
            dma("sync", lambda h: h.dma_start(out=xt_ap, in_=xsrc_ap), w=[bxt], sem=qsem)
            aop(lambda h: h.activation(out=junk, in_=xt_ap, func=AF.Square, accum_out=stat[:, col:col + 1]), r=[bxt], w=[Bjunk, Bstat])
            aop(lambda h: h.activation(out=stat[:, col + 1:col + 2], in_=stat[:, col:col + 1], func=AF.Sqrt, scale=1.0 / 1024, bias=1e-6), r=[Bstat], w=[Bstat])
            vop(lambda h: h.reciprocal(out=stat[:, col + 2:col + 3], in_=stat[:, col + 1:col + 2]), r=[Bstat], w=[Bstat])
            vop(lambda h: h.scalar_tensor_tensor(out=xn_ap, in0=xt_ap, scalar=stat[:, col + 2:col + 3], in1=gb, op0=ALU.mult, op1=ALU.mult), r=[bxt, Bstat, bg], w=[bxn])

        ti_ = 0
        for b in range(4):
            for t in range(8):
                tg = b * 8 + t
                xi = ti_ % NXB
                ni_ = ti_ % 2
                col = (ti_ % 8) * 4
                ti_ += 1
                norm_tile(x_d[tg * 128:(tg + 1) * 128, :], xt[xi], Bxt[xi], xn[ni_], Bxn[ni_], col, gbc, Bg, "x%d" % xi)
                bk = ni_
                for kc in range(8):
                    pop(lambda h, kc=kc, bk=bk, ni_=ni_: h.transpose(out=PSB[bk][:, kc * 128:(kc + 1) * 128], in_=xn[ni_][:, kc * 128:(kc + 1) * 128], identity=k16("ident")), r=[Bxn[ni_], Bc16], w=[PB[bk]])
                aop(lambda h, t=t, bk=bk: h.activation(out=hT[:, :, t * 128:(t + 1) * 128], in_=PSB[bk].rearrange("p (a b) -> p a b", b=128), func=AF.Copy), r=[PB[bk]], w=[BhT])
            for t in range(8):
                tg = b * 8 + t
                bk = 2 + (t % 2)
                for kc in range(8):
                    pop(lambda h, kc=kc, t=t, bk=bk: h.matmul(PSF[bk], lhsT=hT[:, kc, t * 128:(t + 1) * 128], rhs=Wfs[:, kc, 0:512], start=(kc == 0), stop=(kc == 7)), r=[BhT, BWfs], w=[PB[bk]])
                vop(lambda h, tg=tg, bk=bk: h.tensor_copy(out=uf_tm[:, tg, :], in_=PSF[bk]), r=[PB[bk]], w=[Buf_uf])
            for j in range(8):
                bk = 4 + (j % 2)
                for kc in range(8):
                    pop(lambda h, kc=kc, j=j, bk=bk: h.matmul(PSF[bk], lhsT=hT[:, kc, j:1024:8], rhs=Wfs[:, kc, 512:1024], start=(kc == 0), stop=(kc == 7)), r=[BhT, BWfs], w=[PB[bk]])
                aop(lambda h, j=j, bk=bk: h.activation(out=Zt[:, :, j, :], in_=PSF[bk].rearrange("p (g c) -> p g c", c=16), func=AF.Copy), r=[PB[bk]], w=[BZ])
            for g8 in range(4):
                bk = 6 + (g8 % 2)
                for gi in range(8):
                    g = g8 * 8 + gi
                    pop(lambda h, g=g, gi=gi, bk=bk: h.transpose(out=PSB[bk][:, gi * 128:(gi + 1) * 128], in_=flat(Zt[:, g, :, :]), identity=k16("ident")), r=[BZ, Bc16], w=[PB[bk]])
                vop(lambda h, g8=g8, b=b, bk=bk: h.tensor_copy(out=Xs[:, g8 * 8:(g8 + 1) * 8, b * 128:(b + 1) * 128], in_=PSB[bk].rearrange("p (a b) -> p a b", b=128)), r=[PB[bk]], w=[BX])
        S.barrier()
        dbg("wfs", Wfs, [128, 8, 1024])
        dbg("ht", hT, [128, 8, 1024])
        dbg("xn", xn[1], [128, 1024])
        dbg("stat", stat, [128, 64])
        dbg("zt", Zt, [128, 32, 8, 16])
        dbg("min", Min, [128, 2, 32, 128])
        dbg("q16", Q16, [128, 2, 32, 128])
        dbg("mi", Mi, [128, 32, 128])
        AR.release(p1_mark)
        dbg("uf", uf_tm, [128, 32, 512])
        dbg("xs", Xs, [128, 32, 512])
        if STAGE == 0:
            S.barrier()
            S.run()
            return nc, dbg_outs

        BFT = Buf()
        p2_mark = AR.mark()
        NTB = 3
        tct = [AR.alloc([16, 256], BF16) for _ in range(NTB)]
        tst = [AR.alloc([16, 256], BF16) for _ in range(NTB)]
        Btc = [Buf() for _ in range(NTB)]
        Bts = [Buf() for _ in range(NTB)]
        UrT = AR.alloc([4, 256], BF16)
        UiT = AR.alloc([4, 256], BF16)
        BUU = Buf()
        it_ = 0
        for kb in range(8):
            for lt in range(32):
                l16 = lt % 16
                if l16 == 0:
                    bi = it_ % NTB
                    it_ += 1
                    hh_ = lt // 16
                    dma("sync", lambda h, bi=bi, hh_=hh_, kb=kb: h.dma_start(out=tct[bi], in_=tc_d[kb, :, hh_ * 16:(hh_ + 1) * 16, :]), w=[Btc[bi]], sem="tc%d" % bi)
                    dma("scalar", lambda h, bi=bi, hh_=hh_, kb=kb: h.dma_start(out=tst[bi], in_=ts_d[kb, :, hh_ * 16:(hh_ + 1) * 16, :]), w=[Bts[bi]], sem="ts%d" % bi)
                for g in range(4):
                    pop(lambda h, g=g, lt=lt, bi=bi, l16=l16: h.matmul(PSF[g][:, 0:256], lhsT=uf_tm[:, lt, g * 128:(g + 1) * 128], rhs=tct[bi][:, l16, :], start=(lt == 0), stop=(lt == 31)), r=[Buf_uf, Btc[bi]], w=[PB[g]])
                    pop(lambda h, g=g, lt=lt, bi=bi, l16=l16: h.matmul(PSF[4 + g][:, 0:256], lhsT=uf_tm[:, lt, g * 128:(g + 1) * 128], rhs=tst[bi][:, l16, :], start=(lt == 0), stop=(lt == 31)), r=[Buf_uf, Bts[bi]], w=[PB[4 + g]])
            for g in range(4):
                vop(lambda h, g=g: h.tensor_copy(out=UrT[:, g, :], in_=PSF[g][:, 0:256]), r=[PB[g]], w=[BUU])
                aop(lambda h, g=g: h.activation(out=UiT[:, g, :], in_=PSF[4 + g][:, 0:256], func=AF.Copy), r=[PB[4 + g]], w=[BUU])
            for g in range(4):
                pop(lambda h, g=g: h.matmul(PSF[g][:, 256:512], lhsT=k16("CC"), rhs=UrT[:, g, :], start=True, stop=False), r=[BUU, Bc16], w=[PB[g]])
                pop(lambda h, g=g: h.matmul(PSF[g][:, 256:512], lhsT=k16("SS"), rhs=UiT[:, g, :], start=False, stop=True), r=[BUU, Bc16], w=[PB[g]])
                vop(lambda h, g=g, kb=kb: h.tensor_copy(out=FT[:, g, kb * 256:(kb + 1) * 256], in_=PSF[g][:, 256:512]), r=[PB[g]], w=[BFT])
        S.barrier()
        AR.release(p2_mark)
        dbg("ft", FT, [128, 4, 2048])

        ByT = Buf()
        AR.release(uf_off)
        p3_mark = AR.mark()
        Wglu = AR.alloc([4, 512], BF16)
        BWglu = Buf()
        dma("gpsimd", lambda h: h.dma_start(out=Wglu, in_=wglu_d.rearrange("(q p) n -> p q n", p=128)), w=[BWglu], sem="wglu")
        pAre = AR.alloc([16, 64], F32)
        pAim = AR.alloc([16, 64], F32)
        pdt = AR.alloc([16], F32)
        Trp = AR.alloc([16, 64], F32)
        Tip = AR.alloc([16, 64], F32)
        Trm = AR.alloc([16, 64], F32)
        Tim = AR.alloc([16, 64], F32)
        tX = AR.alloc([16, 64], F32)
        tY = AR.alloc([16, 64], F32)
        tZi = AR.alloc([16, 64], I32)
        BT = Buf("tables")
        Hc = [AR.alloc([16, 2, 64], BF16) for _ in range(2)]
        BHc = [Buf(), Buf()]
        HTd = [AR.alloc([16, 257], BF16) for _ in range(2)]
        BHT = [Buf(), Buf()]
        Vt = [AR.alloc([4, 2, 64], BF16) for _ in range(2)]
        BV = [Buf(), Buf()]
        m1 = [AR.alloc([4, 2, 64], F32) for _ in range(2)]
        m2 = [AR.alloc([4, 2, 64], F32) for _ in range(2)]
        Bm = [Buf(), Buf()]
        Yg = AR.alloc([8, 256], BF16)
        BYg = Buf()
        sg = AR.alloc([4, 512], BF16)
        Bsg = Buf()

        def cplx_mul(src4, bsrc, Tr, Ti, k, out_re, out_im, bout):
            a, bb = m1[k], m2[k]
            Trb = Tr.unsqueeze(2).to_broadcast([128, 4, 2, 64])
            vop(lambda h: h.tensor_tensor(out=a, in0=src4, in1=Trb, op=ALU.mult), r=[BT, bsrc], w=[Bm[k]])
            vop(lambda h: h.tensor_tensor(out=bb[:, :, 0, :], in0=src4[:, :, 1, :], in1=Ti, op=ALU.mult), r=[BT, bsrc], w=[Bm[k]])
            vop(lambda h: h.tensor_tensor(out=bb[:, :, 1, :], in0=src4[:, :, 0, :], in1=Ti, op=ALU.mult), r=[BT, bsrc], w=[Bm[k]])
            gop(lambda h: h.tensor_tensor(out=out_re, in0=a[:, :, 0, :], in1=bb[:, :, 0, :], op=ALU.subtract), r=[Bm[k]], w=[bout])
            gop(lambda h: h.tensor_tensor(out=out_im, in0=a[:, :, 1, :], in1=bb[:, :, 1, :], op=ALU.add), r=[Bm[k]], w=[bout])

        for half in range(2):
            g_lo = half * 16
            for d in range(2):
                kcol = c32[:, C32["kf"] + d:C32["kf"] + d + 1]
                dma("sync", lambda h, d=d, g_lo=g_lo: h.dma_start(out=flat(pAre), in_=sAre_d[d, g_lo:g_lo + 16, :].rearrange("g n -> (g n)").partition_broadcast(128)), w=[BT], sem="pa")
                dma("sync", lambda h, d=d, g_lo=g_lo: h.dma_start(out=flat(pAim), in_=sAim_d[d, g_lo:g_lo + 16, :].rearrange("g n -> (g n)").partition_broadcast(128)), w=[BT], sem="pb")
                dma("sync", lambda h, d=d, g_lo=g_lo: h.dma_start(out=pdt, in_=sldt_d[d, g_lo:g_lo + 16].partition_broadcast(128)), w=[BT], sem="pc")
                TB = [BT, Bc32]
                xt3 = [Tok("dma", "pa", S.dcnt["pa"]), Tok("dma", "pb", S.dcnt["pb"]), Tok("dma", "pc", S.dcnt["pc"])]
                aop(lambda h: h.activation(out=pdt, in_=pdt, func=AF.Exp, scale=1.0), r=TB, w=TB, x=xt3)
                dtb = pdt.unsqueeze(2).to_broadcast([128, 16, 64])
                vop(lambda h, dtb=dtb: h.scalar_tensor_tensor(out=pAre, in0=pAre, scalar=8.0, in1=dtb, op0=ALU.mult, op1=ALU.mult), r=TB, w=TB, x=xt3)
                vop(lambda h, dtb=dtb: h.scalar_tensor_tensor(out=pAim, in0=pAim, scalar=8.0, in1=dtb, op0=ALU.mult, op1=ALU.mult), r=TB, w=TB)
                vop(lambda h: h.tensor_scalar(out=flat(tZi), in0=flat(pAim), scalar1=1.0 / TWO_PI, scalar2=None, op0=ALU.mult), r=TB, w=TB)
                vop(lambda h: h.tensor_copy(out=flat(tX), in_=flat(tZi)), r=TB, w=TB)
                vop(lambda h: h.scalar_tensor_tensor(out=flat(pAim), in0=flat(tX), scalar=-TWO_PI, in1=flat(pAim), op0=ALU.mult, op1=ALU.add), r=TB, w=TB)
                vop(lambda h, kcol=kcol: h.tensor_scalar(out=flat(tY), in0=flat(pAim), scalar1=kcol, scalar2=None, op0=ALU.mult), r=TB, w=TB)
                sincos(flat(tY), flat(Tip), flat(Trp), flat(tZi), flat(tX), TB)
                vop(lambda h, kcol=kcol: h.tensor_scalar(out=flat(tY), in0=flat(pAre), scalar1=kcol, scalar2=None, op0=ALU.mult), r=TB, w=TB)
                aop(lambda h: h.activation(out=flat(tX), in_=flat(tY), func=AF.Exp), r=TB, w=TB)
                aop(lambda h: h.activation(out=flat(tY), in_=flat(tY), func=AF.Exp, scale=-1.0), r=TB, w=TB)
                vop(lambda h: h.tensor_tensor(out=flat(Trm), in0=flat(Trp), in1=flat(tY), op=ALU.mult), r=TB, w=TB)
                vop(lambda h: h.scalar_tensor_tensor(out=flat(Tim), in0=flat(Tip), scalar=-1.0, in1=flat(tY), op0=ALU.mult, op1=ALU.mult), r=TB, w=TB)
                vop(lambda h: h.tensor_tensor(out=flat(Trp), in0=flat(Trp), in1=flat(tX), op=ALU.mult), r=TB, w=TB)
                vop(lambda h: h.tensor_tensor(out=flat(Tip), in0=flat(Tip), in1=flat(tX), op=ALU.mult), r=TB, w=TB)
                blocks = [0, 1] if d == 0 else [3, 2, 1, 0]
                tri = k16("triL") if d == 0 else k16("triU")
                sel = k16("selL") if d == 0 else k16("selF")
                if d == 0:
                    vop(lambda h: h.memset(HTd[0][:, :, 0:1], 0.0), r=[], w=[BHT[0]])
                for bi_, blk in enumerate(blocks):
                    cur, prv = bi_ % 2, (bi_ + 1) % 2
                    first = (bi_ == 0)
                    for c4 in range(4):
                        gl = c4 * 4
                        k = c4 % 2
                        bS, bW, bTp = (0, 1, 2) if k == 0 else (3, 4, 5)
                        for gi in range(4):
                            g = g_lo + gl + gi
                            pop(lambda h, g=g, gi=gi, blk=blk, d=d, bS=bS: h.matmul(PSF[bS][:, gi * 128:(gi + 1) * 128], lhsT=Xs[:, g, blk * 128:(blk + 1) * 128], rhs=Min[:, d, g, :], start=True, stop=True), r=[BX, BMin], w=[PB[bS]])
                        srcS = PSF[bS].rearrange("p (g h n) -> p g h n", h=2, n=64)
                        cplx_mul(srcS, PB[bS], Trm[:, gl:gl + 4, :], Tim[:, gl:gl + 4, :], k, Vt[k][:, :, 0, :], Vt[k][:, :, 1, :], BV[k])
                        pop(lambda h, k=k, bW=bW, first=first, tri=tri: h.matmul(PSF[bW], lhsT=tri, rhs=flat(Vt[k]), start=True, stop=first), r=[BV[k], Bc16], w=[PB[bW]])
                        if not first:
                            pop(lambda h, bW=bW, prv=prv, gl=gl, sel=sel: h.matmul(PSF[bW], lhsT=sel, rhs=flat(Hc[prv][:, gl:gl + 4, :, :]), start=False, stop=True), r=[BHc[prv], Bc16], w=[PB[bW]])
                        srcW = PSF[bW].rearrange("p (g h n) -> p g h n", h=2, n=64)
                        cplx_mul(srcW, PB[bW], Trp[:, gl:gl + 4, :], Tip[:, gl:gl + 4, :], k, Hc[cur][:, gl:gl + 4, 0, :], Hc[cur][:, gl:gl + 4, 1, :], BHc[cur])
                        if blk <= 2:
                            for gi in range(4):
                                pop(lambda h, gi=gi, gl=gl, cur=cur, bTp=bTp: h.transpose(out=PSB[bTp][:, gi * 128:(gi + 1) * 128], in_=flat(Hc[cur][:, gl + gi, :, :]), identity=k16("ident")), r=[BHc[cur], Bc16], w=[PB[bTp]])
                            srcT = PSB[bTp][:, 0:512].rearrange("p (a b) -> p a b", b=128)
                            if blk < 2:
                                co = blk * 128 + (1 if d == 0 else 0)
                                aop(lambda h, gl=gl, co=co, srcT=srcT, d=d: h.activation(out=HTd[d][:, gl:gl + 4, co:co + 128], in_=srcT, func=AF.Copy), r=[PB[bTp]], w=[BHT[d]])
                            else:
                                aop(lambda h, gl=gl, srcT=srcT, d=d: h.activation(out=HTd[d][:, gl:gl + 4, 256:257], in_=srcT[:, :, 0:1], func=AF.Copy), r=[PB[bTp]], w=[BHT[d]])
            for blk in range(2):
                for c4 in range(4):
                    gl = c4 * 4
                    bk = 6 + (c4 % 2)
                    for gi in range(4):
                        g = g_lo + gl + gi
                        o = gi * 128
                        pop(lambda h, g=g, o=o, blk=blk, bk=bk: h.matmul(PSF[bk][:, o:o + 128], lhsT=Xs[:, g, blk * 128:(blk + 1) * 128], rhs=Mi[:, g, :], start=True, stop=False), r=[BX, BMi], w=[PB[bk]])
                        pop(lambda h, g=g, o=o, blk=blk, bk=bk, gl=gl, gi=gi: h.matmul(PSF[bk][:, o:o + 128], lhsT=HTd[0][:, gl + gi, blk * 128:blk * 128 + 128], rhs=Q16[:, 0, g, :], start=False, stop=False), r=[BHT[0], BQ16], w=[PB[bk]])
                        pop(lambda h, g=g, o=o, blk=blk, bk=bk, gl=gl, gi=gi: h.matmul(PSF[bk][:, o:o + 128], lhsT=HTd[1][:, gl + gi, blk * 128 + 1:blk * 128 + 129], rhs=Q16[:, 1, g, :], start=False, stop=True), r=[BHT[1], BQ16], w=[PB[bk]])
                    aop(lambda h, bk=bk, gl=gl: h.activation(out=Yg[:, :, gl * 16:(gl + 4) * 16].rearrange("p i (g c) -> p g i c", c=16), in_=PSF[bk].rearrange("p (g i c) -> p g i c", i=8, c=16), func=AF.Gelu), r=[PB[bk]], w=[BYg])
                for qq in range(2):
                    q = 2 * half + qq
                    bk = 2 if qq == 0 else 5
                    for i in range(8):
                        pop(lambda h, i=i, qq=qq, bk=bk: h.transpose(out=PSB[bk][:, i * 128:(i + 1) * 128], in_=Yg[:, i, qq * 128:(qq + 1) * 128], identity=k16("ident")), r=[BYg, Bc16], w=[PB[bk]])
                    vop(lambda h, q=q, blk=blk, bk=bk: h.tensor_copy(out=yT[:, q, blk * 1024:(blk + 1) * 1024].rearrange("p (k i) -> p i k", i=8), in_=PSB[bk].rearrange("p (i k) -> p i k", k=128)), r=[PB[bk]], w=[ByT])
        dbg("yt", yT, [128, 4, 2048])
        for tb in range(4):
            for co in range(4):
                for q in range(4):
                    pop(lambda h, co=co, q=q, tb=tb: h.matmul(PSF[co], lhsT=Wglu[:, q, co * 128:(co + 1) * 128], rhs=yT[:, q, tb * 512:(tb + 1) * 512], start=(q == 0), stop=(q == 3)), r=[ByT, BWglu], w=[PB[co]])
                aop(lambda h, co=co: h.activation(out=sg[:, co, :], in_=PSF[co], func=AF.Sigmoid), r=[PB[co]], w=[Bsg])
            vop(lambda h, tb=tb: h.tensor_tensor(out=yT[:, :, tb * 512:(tb + 1) * 512], in0=yT[:, :, tb * 512:(tb + 1) * 512], in1=sg, op=ALU.mult), r=[Bsg, ByT], w=[ByT])
        S.barrier()
        AR.release(p3_mark)
        dbg("y2t", yT, [128, 4, 2048])

        A4 = Arena(arena_t[:, persist0:ft_off], ft_off - persist0)
        A5 = Arena(arena_t[:, xs_off:ARENA_BYTES], ARENA_BYTES - xs_off)
        Wg_ = A4.alloc([8, 2048], BF16)
        Wout = A5.alloc([8, 1024], BF16)
        Wfo = A4.alloc([4, 1024], BF16)
        Wso = A5.alloc([4, 1024], BF16)
        BW4 = [Buf() for _ in range(4)]
        dma("gpsimd", lambda h: h.dma_start(out=Wfo, in_=wfo_d.rearrange("(q p) n -> p q n", p=128)), w=[BW4[0]], sem="w4a")
        dma("gpsimd", lambda h: h.dma_start(out=Wso, in_=wso_d.rearrange("(q p) n -> p q n", p=128)), w=[BW4[1]], sem="w4b")
        dma("gpsimd", lambda h: h.dma_start(out=Wg_, in_=w_in_d[:, 1024:3072].rearrange("(kc p) n -> p kc n", p=128)), w=[BW4[2]], sem="w4c")
        dma("gpsimd", lambda h: h.dma_start(out=Wout, in_=wout_d.rearrange("(kc p) n -> p kc n", p=128)), w=[BW4[3]], sem="w4d")
        hT4 = A5.alloc([8, 512], BF16)
        BhT4 = Buf()
        xq = [A5.alloc([1024], F32) for _ in range(4)]
        Bxq = [Buf() for _ in range(4)]
        xn4 = [A5.alloc([1024], BF16) for _ in range(2)]
        Bxn4 = [Buf() for _ in range(2)]
        mg = A5.alloc([8, 512], BF16)
        Bmg = Buf()
        g1bc = A5.alloc([1024], F32)
        g2bc = A5.alloc([1024], F32)
        Bgg = Buf()
        dma("sync", lambda h: h.dma_start(out=g1bc, in_=g1_d.partition_broadcast(128)), w=[Bgg], sem="g1l")
        Bgg2 = Buf()
        dma("sync", lambda h: h.dma_start(out=g2bc, in_=g2_d.partition_broadcast(128)), w=[Bgg2], sem="g2l")
        junk4 = A5.alloc([1024], BF16)
        stat4 = A5.alloc([64], F32)
        s12 = [[A5.alloc([512], BF16) for _ in range(2)] for _ in range(2)]
        t12 = [[A5.alloc([512], BF16) for _ in range(2)] for _ in range(2)]
        Bs12 = [Buf(), Buf()]
        x2t = [A5.alloc([1024], F32) for _ in range(2)]
        Bx2 = [Buf(), Buf()]
        hn32 = [A5.alloc([1024], F32) for _ in range(2)]
        Bhn32 = [Buf(), Buf()]
        hn16 = [A5.alloc([1024], BF16) for _ in range(2)]
        Bhn16 = [Buf(), Buf()]
        hnT = A5.alloc([8, 128], F32)
        BhnT = Buf()
        Wr = A5.alloc([8, 72], F32)
        BWr = Buf()
        dma("sync", lambda h: h.dma_start(out=Wr, in_=wr_d.rearrange("(kc p) n -> p kc n", p=128)), w=[BWr], sem="wrl")
        brb = A5.alloc([72], F32)
        Bbr = Buf()
        dma("sync", lambda h: h.dma_start(out=brb, in_=br_d.partition_broadcast(128)), w=[Bbr], sem="brl")
        Lg = A5.alloc([72], F32)
        rs = A5.alloc([512], F32)
        Brs = Buf()
        Cacc = A5.alloc([64], F32)
        BCacc = Buf()
        vop(lambda h: h.memset(Cacc, 0.0), w=[BCacc])
        Bxpad = Buf()
        Bx2d = Buf()

        def norm4(xsrc_ap, xt_ap, bxt, xn_ap, bxn, col, gb, bg, qsem):
            dma("sync", lambda h: h.dma_start(out=xt_ap, in_=xsrc_ap), w=[bxt], sem=qsem)
            aop(lambda h: h.activation(out=junk4, in_=xt_ap, func=AF.Square, accum_out=stat4[:, col:col + 1]), r=[bxt], w=[Bjunk, Bstat])
            aop(lambda h: h.activation(out=stat4[:, col + 1:col + 2], in_=stat4[:, col:col + 1], func=AF.Sqrt, scale=1.0 / 1024, bias=1e-6), r=[Bstat], w=[Bstat])
            vop(lambda h: h.reciprocal(out=stat4[:, col + 2:col + 3], in_=stat4[:, col + 1:col + 2]), r=[Bstat], w=[Bstat])
            vop(lambda h: h.scalar_tensor_tensor(out=xn_ap, in0=xt_ap, scalar=stat4[:, col + 2:col + 3], in1=gb, op0=ALU.mult, op1=ALU.mult), r=[bxt, Bstat, bg], w=[bxn])

        tcount = 0
        for tb in range(4):
            for t in range(4):
                tg = tb * 4 + t
                ni_ = t % 2
                col = (tg % 8) * 4
                norm4(x_d[tg * 128:(tg + 1) * 128, :], xq[t], Bxq[t], xn4[ni_], Bxn4[ni_], col, g1bc, Bgg, "xq%d" % t)
                bk = ni_
                for kc in range(8):
                    pop(lambda h, kc=kc, bk=bk, ni_=ni_: h.transpose(out=PSB[bk][:, kc * 128:(kc + 1) * 128], in_=xn4[ni_][:, kc * 128:(kc + 1) * 128], identity=k16("ident")), r=[Bxn4[ni_], Bc16], w=[PB[bk]])
                aop(lambda h, t=t, bk=bk: h.activation(out=hT4[:, :, t * 128:(t + 1) * 128], in_=PSB[bk].rearrange("p (a b) -> p a b", b=128), func=AF.Copy), r=[PB[bk]], w=[BhT4])
            tsl = slice(tb * 512, (tb + 1) * 512)
            for dc in range(8):
                pz = dc % 2
                bA, bB, bC, bD = (0, 1, 2, 3) if pz == 0 else (4, 5, 6, 7)
                dsl = slice(dc * 128, (dc + 1) * 128)
                dsl2 = slice(1024 + dc * 128, 1024 + (dc + 1) * 128)
                for q in range(4):
                    pop(lambda h, q=q, bA=bA, dsl=dsl, tsl=tsl: h.matmul(PSF[bA], lhsT=Wfo[:, q, dsl], rhs=FT[:, q, tsl], start=(q == 0), stop=(q == 3)), r=[BW4[0], BFT], w=[PB[bA]])
                for q in range(4):
                    pop(lambda h, q=q, bB=bB, dsl=dsl, tsl=tsl: h.matmul(PSF[bB], lhsT=Wso[:, q, dsl], rhs=yT[:, q, tsl], start=(q == 0), stop=(q == 3)), r=[BW4[1], ByT], w=[PB[bB]])
                for kc in range(8):
                    pop(lambda h, kc=kc, bC=bC, dsl=dsl: h.matmul(PSF[bC], lhsT=Wg_[:, kc, dsl], rhs=hT4[:, kc, :], start=(kc == 0), stop=(kc == 7)), r=[BW4[2], BhT4], w=[PB[bC]])
                for kc in range(8):
                    pop(lambda h, kc=kc, bD=bD, dsl2=dsl2: h.matmul(PSF[bD], lhsT=Wg_[:, kc, dsl2], rhs=hT4[:, kc, :], start=(kc == 0), stop=(kc == 7)), r=[BW4[2], BhT4], w=[PB[bD]])
                aop(lambda h, pz=pz, bC=bC: h.activation(out=s12[pz][0], in_=PSF[bC], func=AF.Sigmoid), r=[PB[bC]], w=[Bs12[pz]])
                aop(lambda h, pz=pz, bD=bD: h.activation(out=s12[pz][1], in_=PSF[bD], func=AF.Sigmoid), r=[PB[bD]], w=[Bs12[pz]])
                vop(lambda h, pz=pz, bA=bA: h.tensor_tensor(out=t12[pz][0], in0=PSF[bA], in1=s12[pz][0], op=ALU.mult), r=[PB[bA], Bs12[pz]], w=[Bs12[pz]])
                vop(lambda h, pz=pz, bB=bB: h.tensor_tensor(out=t12[pz][1], in0=PSF[bB], in1=s12[pz][1], op=ALU.mult), r=[PB[bB], Bs12[pz]], w=[Bs12[pz]])
                gop(lambda h, pz=pz, dc=dc: h.tensor_tensor(out=mg[:, dc, :], in0=t12[pz][0], in1=t12[pz][1], op=ALU.add), r=[Bs12[pz]], w=[Bmg])
            for t in range(4):
                tg = tb * 4 + t
                u = tg % 2
                bk0, bk1 = (0, 1) if u == 0 else (2, 3)
                for hh, bk in ((0, bk0), (1, bk1)):
                    for dc in range(8):
                        pop(lambda h, dc=dc, hh=hh, bk=bk, t=t: h.matmul(PSF[bk], lhsT=mg[:, dc, t * 128:(t + 1) * 128], rhs=Wout[:, dc, hh * 512:(hh + 1) * 512], start=(dc == 0), stop=(dc == 7)), r=[Bmg, BW4[3]], w=[PB[bk]])
                for hh, bk in ((0, bk0), (1, bk1)):
                    vop(lambda h, hh=hh, bk=bk, t=t, u=u: h.tensor_tensor(out=x2t[u][:, hh * 512:(hh + 1) * 512], in0=PSF[bk], in1=xq[t][:, hh * 512:(hh + 1) * 512], op=ALU.add), r=[PB[bk], Bxq[t]], w=[Bx2[u]])
                if STAGE == 1:
                    dma("sync", lambda h, tg=tg, u=u: h.dma_start(out=out_d[tg * 128:(tg + 1) * 128, :], in_=x2t[u]), r=[Bx2[u]], sem="os%d" % u)
                    continue
                dma("sync", lambda h, tg=tg, u=u: h.dma_start(out=x2_d[tg * 128:(tg + 1) * 128, :], in_=x2t[u]), r=[Bx2[u]], w=[Bx2d], sem="x2s%d" % u)
                col = 32 + (tg % 4) * 4
                aop(lambda h, u=u, col=col: h.activation(out=junk4, in_=x2t[u], func=AF.Square, accum_out=stat4[:, col:col + 1]), r=[Bx2[u]], w=[Bjunk, Bstat])
                aop(lambda h, col=col: h.activation(out=stat4[:, col + 1:col + 2], in_=stat4[:, col:col + 1], func=AF.Sqrt, scale=1.0 / 1024, bias=1e-6), r=[Bstat], w=[Bstat])
                vop(lambda h, col=col: h.reciprocal(out=stat4[:, col + 2:col + 3], in_=stat4[:, col + 1:col + 2]), r=[Bstat], w=[Bstat])
                vop(lambda h, u=u, col=col: h.scalar_tensor_tensor(out=hn32[u], in0=x2t[u], scalar=stat4[:, col + 2:col + 3], in1=g2bc, op0=ALU.mult, op1=ALU.mult), r=[Bx2[u], Bstat, Bgg2], w=[Bhn32[u]])
                gop(lambda h, u=u: h.tensor_copy(out=hn16[u], in_=hn32[u]), r=[Bhn32[u]], w=[Bhn16[u]])
                for kc in range(8):
                    bk = 4 + kc // 4
                    o = (kc % 4) * 128
                    pop(lambda h, kc=kc, bk=bk, o=o, u=u: h.transpose(out=PSF[bk][:, o:o + 128], in_=hn32[u][:, kc * 128:(kc + 1) * 128], identity=k32("ident")), r=[Bhn32[u], Bc32], w=[PB[bk]])
                aop(lambda h: h.activation(out=flat(hnT[:, 0:4, :]), in_=PSF[4], func=AF.Copy), r=[PB[4]], w=[BhnT])
                vop(lambda h: h.tensor_copy(out=flat(hnT[:, 4:8, :]), in_=PSF[5]), r=[PB[5]], w=[BhnT])
                for kc in range(8):
                    pop(lambda h, kc=kc: h.matmul(PSF[6][:, 0:72], lhsT=hnT[:, kc, :], rhs=Wr[:, kc, :], start=(kc == 0), stop=(kc == 7)), r=[BhnT, BWr], w=[PB[6]])
                R = [Brs]
                L8, L64 = Lg[:, 0:8], Lg[:, 8:72]
                m8, ohg, negm, ex, sumg, pg = rs[:, 0:8], rs[:, 8:16], rs[:, 16:17], rs[:, 24:32], rs[:, 17:18], rs[:, 18:19]
                tmp64, esel, m8e = rs[:, 64:128], rs[:, 32:40], rs[:, 40:48]
                dv, w1 = rs[:, 19:20], rs[:, 20:21]
                mk1, mk2, msk, slot = rs[:, 128:192], rs[:, 192:256], rs[:, 256:320], rs[:, 320:384]
                idf = rs[:, 48:50]
                vop(lambda h: h.tensor_tensor(out=Lg, in0=PSF[6][:, 0:72], in1=brb, op=ALU.add), r=[PB[6], Bbr], w=R)
                vop(lambda h: h.max(out=m8, in_=L8), r=R, w=R)
                vop(lambda h: h.tensor_scalar(out=ohg, in0=L8, scalar1=m8[:, 0:1], scalar2=None, op0=ALU.is_equal), r=R, w=R)
                vop(lambda h: h.tensor_scalar(out=negm, in0=m8[:, 0:1], scalar1=-1.0, scalar2=None, op0=ALU.mult), r=R, w=R)
                aop(lambda h: h.activation(out=ex, in_=L8, func=AF.Exp, bias=negm, scale=1.0, accum_out=sumg), r=R, w=R)
                vop(lambda h: h.reciprocal(out=pg, in_=sumg), r=R, w=R)
                vop(lambda h: h.tensor_tensor(out=tmp64.rearrange("p (g j) -> p g j", j=8), in0=L64.rearrange("p (g j) -> p g j", j=8), in1=ohg.unsqueeze(2).to_broadcast([128, 8, 8]), op=ALU.mult), r=R, w=R)
                vop(lambda h: h.tensor_reduce(out=esel, in_=tmp64.rearrange("p (g j) -> p j g", j=8), axis=AX.X, op=ALU.add), r=R, w=R)
                vop(lambda h: h.max(out=m8e, in_=esel), r=R, w=R)
                vop(lambda h: h.tensor_tensor(out=dv, in0=m8e[:, 1:2], in1=m8e[:, 0:1], op=ALU.subtract), r=R, w=R)
                aop(lambda h: h.activation(out=w1, in_=dv, func=AF.Sigmoid, scale=-1.0), r=R, w=R)
                vop(lambda h, tg=tg: h.tensor_tensor(out=gw[:, tg, 0:1], in0=pg, in1=w1, op=ALU.mult), r=R, w=[Bgw])
                vop(lambda h, tg=tg: h.tensor_tensor(out=gw[:, tg, 1:2], in0=pg, in1=gw[:, tg, 0:1], op=ALU.subtract), r=R + [Bgw], w=[Bgw])
                ohb = ohg.unsqueeze(2).to_broadcast([128, 8, 8])
                vop(lambda h: h.tensor_scalar(out=mk1, in0=L64, scalar1=m8e[:, 0:1], scalar2=None, op0=ALU.is_equal), r=R, w=R)
                vop(lambda h, ohb=ohb: h.tensor_tensor(out=mk1.rearrange("p (g j) -> p g j", j=8), in0=mk1.rearrange("p (g j) -> p g j", j=8), in1=ohb, op=ALU.mult), r=R, w=R)
                vop(lambda h: h.tensor_scalar(out=mk2, in0=L64, scalar1=m8e[:, 1:2], scalar2=None, op0=ALU.is_equal), r=R, w=R)
                vop(lambda h, ohb=ohb: h.tensor_tensor(out=mk2.rearrange("p (g j) -> p g j", j=8), in0=mk2.rearrange("p (g j) -> p g j", j=8), in1=ohb, op=ALU.mult), r=R, w=R)
                vop(lambda h: h.tensor_tensor(out=msk, in0=mk1, in1=mk2, op=ALU.add), r=R, w=R)
                pop(lambda h: h.matmul(PSF[7][:, 0:64], lhsT=k32("triS"), rhs=msk, start=True, stop=False), r=R + [Bc32], w=[PB[7]])
                pop(lambda h: h.matmul(PSF[7][:, 0:64], lhsT=k32("ones"), rhs=Cacc, start=False, stop=True), r=[BCacc, Bc32], w=[PB[7]])
                vop(lambda h: h.tensor_tensor(out=slot, in0=PSF[7][:, 0:64], in1=k32("ecap", 64), op=ALU.add), r=[PB[7], Bc32], w=R)
                vop(lambda h: h.tensor_tensor(out=Cacc, in0=Cacc, in1=msk, op=ALU.add), r=R + [BCacc], w=[BCacc])
                vop(lambda h: h.tensor_tensor(out=mk1, in0=mk1, in1=slot, op=ALU.mult), r=R, w=R)
                vop(lambda h: h.reduce_sum(out=idf[:, 0:1], in_=mk1, axis=AX.X), r=R, w=R)
                vop(lambda h: h.tensor_tensor(out=mk2, in0=mk2, in1=slot, op=ALU.mult), r=R, w=R)
                vop(lambda h: h.reduce_sum(out=idf[:, 1:2], in_=mk2, axis=AX.X), r=R, w=R)
                vop(lambda h, tg=tg: h.tensor_copy(out=idx[:, tg, :], in_=idf), r=R, w=[Bidx])
                for kk in range(2):
                    dma("gpsimd", lambda h, tg=tg, kk=kk, u=u: h.indirect_dma_start(out=xpad_d, out_offset=bass.IndirectOffsetOnAxis(ap=idx[:, tg, kk:kk + 1], axis=0), in_=hn16[u], in_offset=None, bounds_check=breg(h), oob_is_err=False), r=[Bhn16[u], Bidx], w=[Bxpad], sem="scat")
        S.barrier()
        if STAGE >= 2:
            A6 = Arena(arena_t[:, persist0:ARENA_BYTES], ARENA_BYTES - persist0)
            NWB = 2
            Wge = [A6.alloc([8, 512], BF16) for _ in range(NWB)]
            Wue = [A6.alloc([8, 512], BF16) for _ in range(NWB)]
            Wde = [A6.alloc([4, 1024], BF16) for _ in range(NWB)]
            Sge = [A6.alloc([8, 512], F32) for _ in range(NWB)]
            Sue = [A6.alloc([8, 512], F32) for _ in range(NWB)]
            Sde = [A6.alloc([4, 1024], F32) for _ in range(NWB)]
            BSge = [Buf() for _ in range(NWB)]
            BSue = [Buf() for _ in range(NWB)]
            BSde = [Buf() for _ in range(NWB)]
            BWge = [Buf() for _ in range(NWB)]
            BWue = [Buf() for _ in range(NWB)]
            BWde = [Buf() for _ in range(NWB)]
            Xe = [A6.alloc([1024], BF16) for _ in range(3)]
            BXe = [Buf(), Buf(), Buf()]
            XeT = [A6.alloc([8, 128], BF16) for _ in range(2)]
            BXeT = [Buf(), Buf()]
            Gs = [A6.alloc([512], BF16) for _ in range(2)]
            BGs = [Buf(), Buf()]
            Aa = [A6.alloc([512], BF16) for _ in range(2)]
            BAa = [Buf(), Buf()]
            AT = [A6.alloc([4, 128], BF16) for _ in range(2)]
            BAT = [Buf(), Buf()]
            Ye = [A6.alloc([1024], F32) for _ in range(4)]
            BYe = [Buf() for _ in range(4)]
            Bypad = Buf()
            NXE = 3

            def issue_loads(e):
                wb = e % NWB
                dma("sync", lambda h, e=e, wb=wb: h.dma_start(out=Sge[wb], in_=ewg_d[e].rearrange("(p kc) n -> p kc n", kc=8)), w=[BSge[wb]], sem="wg%d" % wb)
                dma("sync", lambda h, e=e, wb=wb: h.dma_start(out=Sue[wb], in_=ewu_d[e].rearrange("(p kc) n -> p kc n", kc=8)), w=[BSue[wb]], sem="wu%d" % wb)
                dma("sync", lambda h, e=e, wb=wb: h.dma_start(out=Sde[wb], in_=ewd_d[e].rearrange("(p hc) n -> p hc n", hc=4)), w=[BSde[wb]], sem="wd%d" % wb)

            def issue_x(e):
                xb = e % NXE
                dma("scalar", lambda h, e=e, xb=xb: h.dma_start(out=Xe[xb], in_=xpad_d[e * CAP:(e + 1) * CAP, :]), r=[Bxpad], w=[BXe[xb]], sem="xe%d" % xb)

            issue_x(0)
            for e in range(min(NWB, MOE_LIMIT)):
                issue_loads(e)
            for e in range(MOE_LIMIT):
                wb = e % NWB
                u = e % 2
                xb = e % NXE
                if e + 1 < MOE_LIMIT:
                    issue_x(e + 1)
                aop(lambda h, wb=wb: h.activation(out=flat(Wge[wb]), in_=flat(Sge[wb]), func=AF.Copy), r=[BSge[wb]], w=[BWge[wb]])
                gop(lambda h, wb=wb: h.tensor_copy(out=flat(Wue[wb]), in_=flat(Sue[wb])), r=[BSue[wb]], w=[BWue[wb]])
                aop(lambda h, wb=wb: h.activation(out=flat(Wde[wb]), in_=flat(Sde[wb]), func=AF.Copy), r=[BSde[wb]], w=[BWde[wb]])
                if e + NWB < MOE_LIMIT:
                    issue_loads(e + NWB)
                bkT = 0 if u == 0 else 4
                for kc in range(8):
                    pop(lambda h, kc=kc, xb=xb, bkT=bkT: h.transpose(out=PSB[bkT][:, kc * 128:(kc + 1) * 128], in_=Xe[xb][:, kc:1024:8], identity=k16("ident")), r=[BXe[xb], Bc16], w=[PB[bkT]])
                vop(lambda h, u=u, bkT=bkT: h.tensor_copy(out=flat(XeT[u]), in_=PSB[bkT]), r=[PB[bkT]], w=[BXeT[u]])
                bG, bU = (1, 2) if u == 0 else (5, 6)
                for kc in range(8):
                    pop(lambda h, kc=kc, u=u, wb=wb, bG=bG: h.matmul(PSF[bG], lhsT=XeT[u][:, kc, :], rhs=Wge[wb][:, kc, :], start=(kc == 0), stop=(kc == 7)), r=[BXeT[u], BWge[wb]], w=[PB[bG]])
                for kc in range(8):
                    pop(lambda h, kc=kc, u=u, wb=wb, bU=bU: h.matmul(PSF[bU], lhsT=XeT[u][:, kc, :], rhs=Wue[wb][:, kc, :], start=(kc == 0), stop=(kc == 7)), r=[BXeT[u], BWue[wb]], w=[PB[bU]])
                aop(lambda h, u=u, bG=bG: h.activation(out=Gs[u], in_=PSF[bG], func=AF.Silu), r=[PB[bG]], w=[BGs[u]])
                vop(lambda h, u=u, bU=bU: h.tensor_tensor(out=Aa[u], in0=PSF[bU], in1=Gs[u], op=ALU.mult), r=[PB[bU], BGs[u]], w=[BAa[u]])
                bA = 3 if u == 0 else 7
                for hc in range(4):
                    pop(lambda h, hc=hc, u=u, bA=bA: h.transpose(out=PSB[bA][:, hc * 128:(hc + 1) * 128], in_=Aa[u][:, hc:512:4], identity=k16("ident")), r=[BAa[u], Bc16], w=[PB[bA]])
                aop(lambda h, u=u, bA=bA: h.activation(out=flat(AT[u]), in_=PSB[bA][:, 0:512], func=AF.Copy), r=[PB[bA]], w=[BAT[u]])
                for hh, bk in ((0, bG), (1, bU)):
                    for hc in range(4):
                        pop(lambda h, hc=hc, hh=hh, bk=bk, u=u, wb=wb: h.matmul(PSF[bk], lhsT=AT[u][:, hc, :], rhs=Wde[wb][:, hc, hh * 512:(hh + 1) * 512], start=(hc == 0), stop=(hc == 3)), r=[BAT[u], BWde[wb]], w=[PB[bk]])
                yb = e % 4
                vop(lambda h, yb=yb, bG=bG: h.tensor_copy(out=Ye[yb][:, 0:512], in_=PSF[bG]), r=[PB[bG]], w=[BYe[yb]])
                aop(lambda h, yb=yb, bU=bU: h.activation(out=Ye[yb][:, 512:1024], in_=PSF[bU], func=AF.Copy), r=[PB[bU]], w=[BYe[yb]])
                dma("scalar", lambda h, e=e, yb=yb: h.dma_start(out=ypad_d[e * CAP:(e + 1) * CAP, :], in_=Ye[yb]), r=[BYe[yb]], w=[Bypad], sem="ys%d" % yb)
            S.barrier()
            A6.release(0)
            gfbc = A6.alloc([1024], F32)
            Bgf = Buf()
            dma("sync", lambda h: h.dma_start(out=gfbc, in_=gf_d.partition_broadcast(128)), w=[Bgf], sem="gfl")
            NF = 4
            y1 = [A6.alloc([1024], F32) for _ in range(NF)]
            y2 = [A6.alloc([1024], F32) for _ in range(NF)]
            xx = [A6.alloc([1024], F32) for _ in range(NF)]
            oo = [A6.alloc([1024], F32) for _ in range(2)]
            By1 = [Buf() for _ in range(NF)]
            By2 = [Buf() for _ in range(NF)]
            Bxx = [Buf() for _ in range(NF)]
            Boo = [Buf(), Buf()]
            junk6 = A6.alloc([1024], BF16)
            Bj6 = Buf()
            st6 = A6.alloc([64], F32)
            Bst6 = Buf()

            def fetch6(tg):
                f = tg % NF
                dma("gpsimd", lambda h, tg=tg, f=f: h.indirect_dma_start(out=y1[f], out_offset=None, in_=ypad_d, in_offset=bass.IndirectOffsetOnAxis(ap=idx[:, tg, 0:1], axis=0), bounds_check=breg(h), oob_is_err=False), r=[Bypad, Bidx], w=[By1[f]], sem="ga%d" % f)
                dma("gpsimd", lambda h, tg=tg, f=f: h.indirect_dma_start(out=y2[f], out_offset=None, in_=ypad_d, in_offset=bass.IndirectOffsetOnAxis(ap=idx[:, tg, 1:2], axis=0), bounds_check=breg(h), oob_is_err=False), r=[Bypad, Bidx], w=[By2[f]], sem="gb%d" % f)
                dma("sync", lambda h, tg=tg, f=f: h.dma_start(out=xx[f], in_=x2_d[tg * 128:(tg + 1) * 128, :]), r=[Bx2d], w=[Bxx[f]], sem="xl%d" % f)

            for tg in range(NF):
                fetch6(tg)
            for tg in range(16):
                u = tg % 2
                f = tg % NF
                vop(lambda h, tg=tg, f=f: h.scalar_tensor_tensor(out=xx[f], in0=y1[f], scalar=gw[:, tg, 0:1], in1=xx[f], op0=ALU.mult, op1=ALU.add), r=[By1[f], Bgw, Bxx[f]], w=[Bxx[f]])
                vop(lambda h, tg=tg, f=f: h.scalar_tensor_tensor(out=xx[f], in0=y2[f], scalar=gw[:, tg, 1:2], in1=xx[f], op0=ALU.mult, op1=ALU.add), r=[By2[f], Bgw, Bxx[f]], w=[Bxx[f]])
                col = (tg % 8) * 4
                aop(lambda h, f=f, col=col: h.activation(out=junk6, in_=xx[f], func=AF.Square, accum_out=st6[:, col:col + 1]), r=[Bxx[f]], w=[Bj6, Bst6])
                aop(lambda h, col=col: h.activation(out=st6[:, col + 1:col + 2], in_=st6[:, col:col + 1], func=AF.Sqrt, scale=1.0 / 1024, bias=1e-6), r=[Bst6], w=[Bst6])
                vop(lambda h, col=col: h.reciprocal(out=st6[:, col + 2:col + 3], in_=st6[:, col + 1:col + 2]), r=[Bst6], w=[Bst6])
                vop(lambda h, u=u, f=f, col=col: h.scalar_tensor_tensor(out=oo[u], in0=xx[f], scalar=st6[:, col + 2:col + 3], in1=gfbc, op0=ALU.mult, op1=ALU.mult), r=[Bxx[f], Bst6, Bgf], w=[Boo[u]])
                dma("sync", lambda h, tg=tg, u=u: h.dma_start(out=out_d[tg * 128:(tg + 1) * 128, :], in_=oo[u]), r=[Boo[u]], sem="os%d" % u)
                if tg + NF < 16:
                    fetch6(tg + NF)
        S.barrier()
        S.run()
    return nc, dbg_outs


def _consts():
    bf = ml_dtypes.bfloat16
    p = np.arange(128)
    c16 = np.zeros((128, C16["n"]), np.float32)
    c16[:, C16["ident"]:C16["ident"] + 128] = np.eye(128)
    c16[:, C16["triL"]:C16["triL"] + 128] = (p[:, None] <= p[None, :])
    c16[:, C16["triU"]:C16["triU"] + 128] = (p[:, None] >= p[None, :])
    c16[127, C16["selL"]:C16["selL"] + 128] = 1.0
    c16[0, C16["selF"]:C16["selF"] + 128] = 1.0
    ang = 2.0 * np.pi * ((p[:, None] * p[None, :]) % 128) / 128.0
    c16[:, C16["CC"]:C16["CC"] + 128] = np.cos(ang)
    c16[:, C16["SS"]:C16["SS"] + 128] = np.sin(ang)
    c32 = np.zeros((128, C32["n"]), np.float32)
    c32[:, C32["ident"]:C32["ident"] + 128] = np.eye(128)
    c32[:, C32["triS"]:C32["triS"] + 128] = (p[:, None] < p[None, :])
    c32[:, C32["ones"]:C32["ones"] + 128] = 1.0
    jj = p // 16
    c32[:, C32["mask0"]:C32["mask0"] + 128] = (jj[None, :] >= jj[:, None])
    c32[:, C32["mask1"]:C32["mask1"] + 128] = (jj[:, None] >= jj[None, :])
    c32[:, C32["kf"]] = p + 1
    c32[:, C32["kb"]] = 128 - p
    c32[:, C32["ev"]:C32["ev"] + 17] = np.arange(-8, 9)[None, :]
    c32[:, C32["evr"]:C32["evr"] + 17] = np.arange(8, -9, -1)[None, :]
    c32[:, C32["ecap"]:C32["ecap"] + 64] = (np.arange(64) * CAP)[None, :]
    c32[:, C32["iota"]:C32["iota"] + 128] = p[None, :]
    return c16.astype(bf), c32


def _dft_tables(hf):
    bf = ml_dtypes.bfloat16
    L = 4096
    base = np.arange(L, dtype=np.float64) * (2.0 * np.pi / L)
    sc = 1.0 / math.sqrt(L * 128.0)
    cosb = (np.cos(base) * sc).astype(np.float32)
    sinb = (-np.sin(base) * sc).astype(np.float32)
    t = np.arange(L, dtype=np.int64)
    m = np.arange(L // 2, dtype=np.int64)
    l = t if hf == 0 else (L - 1 - t)
    k = m if hf == 0 else (L - 1 - m)
    prod = (l[:, None] * k[None, :]) % L

    def lay(t):
        return np.ascontiguousarray(t.reshape(32, 128, 8, 256).transpose(2, 1, 0, 3))

    return lay(cosb[prod].astype(bf)), lay(sinb[prod].astype(bf))


_CACHE = {}


def kernel(**inp):
    f32 = np.float32
    x = np.asarray(inp["x"], f32)
    if "nc" not in _CACHE:
        _CACHE["nc"] = build_program()
        _CACHE["c"] = _consts()
        _CACHE["tab"] = [_dft_tables(0), _dft_tables(1)]
    nc, dbg_outs = _CACHE["nc"]
    c16, c32 = _CACHE["c"]
    shared = {
        "mix_norm_g": np.ascontiguousarray(inp["mix_norm_g"][0], f32),
        "w_in": np.ascontiguousarray(inp["w_in"][0], f32),
        "w_fourier_out": np.ascontiguousarray(inp["w_fourier_out"][0], f32),
        "ssm_D": np.ascontiguousarray(inp["ssm_D"][0], f32),
        "ssm_w_glu": np.ascontiguousarray(inp["ssm_w_glu"][0], f32),
        "w_ssm_out": np.ascontiguousarray(inp["w_ssm_out"][0], f32),
        "w_out": np.ascontiguousarray(inp["w_out"][0], f32),
        "ffn_norm_g": np.ascontiguousarray(inp["ffn_norm_g"][0], f32),
        "w_router": np.ascontiguousarray(np.concatenate([inp["router_group_w"][0], inp["router_expert_w"][0]], axis=1), f32),
        "b_router": np.ascontiguousarray(np.concatenate([inp["router_group_b"][0], inp["router_expert_b"][0]], axis=0), f32),
        "final_norm_g": np.ascontiguousarray(inp["final_norm_g"], f32),
        "cst16": c16,
        "cst32": c32,
    }
    if STAGE >= 2:
        shared["expert_w_gate"] = np.ascontiguousarray(inp["expert_w_gate"][0], f32)
        shared["expert_w_up"] = np.ascontiguousarray(inp["expert_w_up"][0], f32)
        shared["expert_w_down"] = np.ascontiguousarray(inp["expert_w_down"][0], f32)
    in_maps = []
    for c in range(8):
        b, hf = c // 2, c % 2
        dsel = [0, 1] if hf == 0 else [1, 0]
        xl = x[b] if hf == 0 else x[b][::-1]
        m = dict(shared)
        m["x"] = np.ascontiguousarray(xl, f32)
        m["sA_re"] = np.ascontiguousarray(inp["ssm_A_re"][0][dsel], f32)
        m["sA_im"] = np.ascontiguousarray(inp["ssm_A_im"][0][dsel], f32)
        m["s_ldt"] = np.ascontiguousarray(inp["ssm_log_dt"][0][dsel], f32)
        m["sB_re"] = np.ascontiguousarray(inp["ssm_B_re"][0][dsel], f32)
        m["sB_im"] = np.ascontiguousarray(inp["ssm_B_im"][0][dsel], f32)
        m["sC_re"] = np.ascontiguousarray(inp["ssm_C_re"][0][dsel], f32)
        m["sC_im"] = np.ascontiguousarray(inp["ssm_C_im"][0][dsel], f32)
        m["tab_c"], m["tab_s"] = _CACHE["tab"][hf]
        in_maps.append(m)
    res = run_bass_kernel_spmd(nc, in_maps, core_ids=list(range(8)))
    out = np.empty((4, 4096, 1024), f32)
    for c in range(8):
        b, hf = c // 2, c % 2
        o = np.asarray(res.results[c]["out"], f32)
        if hf == 0:
            out[b, 0:2048] = o
        else:
            out[b, 2048:4096] = o[::-1]
    _CACHE["last"] = res
    return out
```

```python
import math
from contextlib import ExitStack

import ml_dtypes
import numpy as np

import concourse.bass as bass
import concourse.mybir as mybir
from concourse.bass_utils import run_bass_kernel_spmd

F32 = mybir.dt.float32
BF16 = mybir.dt.bfloat16
I32 = mybir.dt.int32
U8 = mybir.dt.uint8
ALU = mybir.AluOpType
AF = mybir.ActivationFunctionType
AX = mybir.AxisListType

ENGS = ["sync", "scalar", "vector", "gpsimd", "tensor"]
TWO_PI = 2.0 * math.pi
PI_SAFE = 3.14159
CAP = 128
NEXP = 64
STAGE = 2
DEBUG = False
MOE_LIMIT = 64


class Tok:
    __slots__ = ("kind", "eng", "n")

    def __init__(self, kind, eng, n):
        self.kind = kind
        self.eng = eng
        self.n = n


class Buf:
    def __init__(self, name=""):
        self.name = name
        self.w = []
        self.r = []


class Sched:
    def __init__(self, nc, stack):
        self.nc = nc
        self.stack = stack
        self.ops = {e: [] for e in ENGS}
        self.cnt = {e: 0 for e in ENGS}
        self.sem = {e: stack.enter_context(nc.semaphore("c_" + e)) for e in ENGS}
        self.waited = {e: {} for e in ENGS}
        self.dsem = {}
        self.dcnt = {}

    def _waits(self, eng, deps):
        waits = []
        for d in deps:
            if d is None:
                continue
            if d.kind == "eng":
                if d.eng == eng and eng == "tensor":
                    continue
                key = d.eng
                sem = self.sem[d.eng]
            else:
                key = "d_" + d.eng
                sem = self.dsem[d.eng]
            if self.waited[eng].get(key, 0) >= d.n:
                continue
            self.waited[eng][key] = d.n
            waits.append((sem, d.n))
        return waits

    @staticmethod
    def _deps(reads, writes, extra):
        deps = list(extra)
        for b in reads:
            deps += b.w
        for b in writes:
            deps += b.w
            deps += b.r
        return deps

    @staticmethod
    def _note(tok, reads, writes):
        for b in reads:
            b.r = [t for t in b.r if not (t.kind == tok.kind and t.eng == tok.eng)] + [tok]
        for b in writes:
            b.w = [t for t in b.w if not (t.kind == tok.kind and t.eng == tok.eng)] + [tok]
            b.r = []

    def op(self, eng, build, reads=(), writes=(), extra=()):
        waits = self._waits(eng, self._deps(reads, writes, extra))
        self.cnt[eng] += 1
        n = self.cnt[eng]
        sem = self.sem[eng]

        def fn(h):
            for (s, v) in waits:
                h.wait_ge(s, v)
            build(h).then_inc(sem, 1)

        self.ops[eng].append(fn)
        tok = Tok("eng", eng, n)
        self._note(tok, reads, writes)
        return tok

    def dma(self, queue, semname, build, reads=(), writes=(), extra=()):
        if semname not in self.dsem:
            self.dsem[semname] = self.stack.enter_context(self.nc.semaphore("d_" + semname))
            self.dcnt[semname] = 0
        waits = self._waits(queue, self._deps(reads, writes, extra))
        self.dcnt[semname] += 16
        n = self.dcnt[semname]
        sem = self.dsem[semname]

        def fn(h):
            for (s, v) in waits:
                h.wait_ge(s, v)
            build(h).then_inc(sem, 16)

        self.ops[queue].append(fn)
        tok = Tok("dma", semname, n)
        self._note(tok, reads, writes)
        return tok

    def all_toks(self):
        t = [Tok("eng", e, self.cnt[e]) for e in ENGS if self.cnt[e] > 0]
        t += [Tok("dma", k, v) for k, v in self.dcnt.items() if v > 0]
        return t

    def barrier(self):
        toks = self.all_toks()
        for e in ENGS:
            waits = self._waits(e, toks)

            def fn(h, waits=waits):
                for (s, v) in waits:
                    h.wait_ge(s, v)

            self.ops[e].append(fn)

    def run(self):
        with self.nc.Block() as block:
            for e in ENGS:
                ops = self.ops[e]

                def body(h, ops=ops):
                    for fn in ops:
                        fn(h)

                getattr(block, e)(body)


class Arena:
    def __init__(self, ap_u8, size):
        self.ap = ap_u8
        self.size = size
        self.top = 0

    def alloc(self, free_shape, dt):
        isz = {F32: 4, I32: 4, BF16: 2}[dt]
        n = 1
        for s in free_shape:
            n *= s
        nb = n * isz
        off = self.top
        self.top += (nb + 63) // 64 * 64
        assert self.top <= self.size, ("SBUF arena overflow", self.top, self.size)
        v = self.ap[:, off:off + nb].bitcast(dt)
        if len(free_shape) > 1:
            names = ["a%d" % i for i in range(len(free_shape))]
            kw = {names[i]: free_shape[i] for i in range(1, len(free_shape))}
            v = v.rearrange("p (%s) -> p %s" % (" ".join(names), " ".join(names)), **kw)
        return v

    def mark(self):
        return self.top

    def release(self, m):
        self.top = m


def flat(ap):
    nd = len(ap.shape)
    if nd == 2:
        return ap
    names = ["a%d" % i for i in range(nd - 1)]
    return ap.rearrange("p %s -> p (%s)" % (" ".join(names), " ".join(names)))


C16 = dict(ident=0, triL=128, triU=256, selL=384, selF=512, CC=640, SS=768, n=896)
C32 = dict(ident=0, triS=128, ones=256, mask0=384, mask1=512, kf=640, kb=641, ev=642, ecap=659, iota=723, evr=851, n=868)


def build_program():
    nc = bass.Bass("TRN2", target_bir_lowering=False)

    def din(name, shape, dt=F32):
        return nc.dram_tensor(name, list(shape), dt, kind="ExternalInput").ap()

    x_d = din("x", [4096, 1024])
    g1_d = din("mix_norm_g", [1024])
    w_in_d = din("w_in", [1024, 3072])
    wfo_d = din("w_fourier_out", [512, 1024])
    sAre_d = din("sA_re", [2, 32, 64])
    sAim_d = din("sA_im", [2, 32, 64])
    sldt_d = din("s_ldt", [2, 32])
    sBre_d = din("sB_re", [2, 32, 64, 16])
    sBim_d = din("sB_im", [2, 32, 64, 16])
    sCre_d = din("sC_re", [2, 32, 16, 64])
    sCim_d = din("sC_im", [2, 32, 16, 64])
    sD_d = din("ssm_D", [512])
    wglu_d = din("ssm_w_glu", [512, 512])
    wso_d = din("w_ssm_out", [512, 1024])
    wout_d = din("w_out", [1024, 1024])
    g2_d = din("ffn_norm_g", [1024])
    wr_d = din("w_router", [1024, 72])
    br_d = din("b_router", [72])
    if STAGE >= 2:
        ewg_d = din("expert_w_gate", [64, 1024, 512])
        ewu_d = din("expert_w_up", [64, 1024, 512])
        ewd_d = din("expert_w_down", [64, 512, 1024])
    gf_d = din("final_norm_g", [1024])
    tc_d = din("tab_c", [8, 128, 32, 256], BF16)
    ts_d = din("tab_s", [8, 128, 32, 256], BF16)
    c16_d = din("cst16", [128, C16["n"]], BF16)
    c32_d = din("cst32", [128, C32["n"]])
    out_d = nc.dram_tensor("out", [2048, 1024], F32, kind="ExternalOutput").ap()
    xpad_d = nc.dram_tensor("xpad", [NEXP * CAP, 1024], BF16, kind="Internal").ap()
    ypad_d = nc.dram_tensor("ypad", [NEXP * CAP, 1024], F32, kind="Internal").ap()
    x2_d = nc.dram_tensor("x2s", [2048, 1024], F32, kind="Internal").ap()
    dbg_outs = {}

    with ExitStack() as st:
        S = Sched(nc, st)
        ARENA_BYTES = 206 * 1024
        arena_t = st.enter_context(nc.sbuf_tensor("arena", [128, ARENA_BYTES], U8))
        AR = Arena(arena_t[:, :], ARENA_BYTES)
        PSF, PSB, PB = [], [], []
        for i in range(8):
            pt = st.enter_context(nc.psum_tensor("ps%d" % i, [128, 512], F32))
            PSF.append(pt[:, :])
            PSB.append(pt[:, :].bitcast(BF16))
            PB.append(Buf("ps%d" % i))

        def vop(fn, r=(), w=(), x=()):
            return S.op("vector", fn, reads=r, writes=w, extra=x)

        def aop(fn, r=(), w=(), x=()):
            return S.op("scalar", fn, reads=r, writes=w, extra=x)

        def gop(fn, r=(), w=(), x=()):
            return S.op("gpsimd", fn, reads=r, writes=w, extra=x)

        def pop(fn, r=(), w=(), x=()):
            return S.op("tensor", fn, reads=r, writes=w, extra=x)

        _dq = [0]
        _breg = []

        def breg(h):
            if not _breg:
                _breg.append(h.to_reg(NEXP * CAP - 1))
            return _breg[0]

        def dma(q, fn, r=(), w=(), x=(), sem=None):
            if sem is None:
                _dq[0] += 1
                sem = "q%d" % (_dq[0] % 8)
            return S.dma(q, sem, fn, reads=r, writes=w, extra=x)

        def dbg(name, ap, shape):
            if not DEBUG:
                return
            d = nc.dram_tensor("dbg_" + name, list(shape), ap.dtype, kind="ExternalOutput").ap()
            dbg_outs[name] = d
            b = Buf()
            S.barrier()
            dma("sync", lambda h: h.dma_start(out=d, in_=ap), w=[b], sem="dbg")
            S.barrier()

        c16 = AR.alloc([C16["n"]], BF16)
        c32 = AR.alloc([C32["n"]], F32)
        Bc16, Bc32 = Buf(), Buf()
        dma("sync", lambda h: h.dma_start(out=c16, in_=c16_d), w=[Bc16])
        dma("sync", lambda h: h.dma_start(out=c32, in_=c32_d), w=[Bc32])

        def k16(name):
            o = C16[name]
            return c16[:, o:o + 128]

        def k32(name, n=128):
            o = C32[name]
            return c32[:, o:o + n]

        gw = AR.alloc([16, 2], F32)
        idx = AR.alloc([16, 2], I32)
        Bgw, Bidx = Buf(), Buf()
        persist0 = AR.top
        Min = AR.alloc([2, 32, 128], BF16)
        Q16 = AR.alloc([2, 32, 128], BF16)
        Mi = AR.alloc([32, 128], BF16)
        BMin, BQ16, BMi = Buf(), Buf(), Buf()
        ft_off = AR.top
        FT = AR.alloc([4, 2048], BF16)
        yT = AR.alloc([4, 2048], BF16)
        xs_off = AR.top
        Xs = AR.alloc([32, 512], BF16)
        uf_off = AR.top
        uf_tm = AR.alloc([32, 512], BF16)
        persist_mark = AR.mark()

        AR.release(ft_off)
        are = AR.alloc([64], F32)
        aim = AR.alloc([64], F32)
        dtn = AR.alloc([64], F32)
        Bre = AR.alloc([64, 16], F32)
        Bim = AR.alloc([64, 16], F32)
        Cre = AR.alloc([64, 16], F32)
        Cim = AR.alloc([64, 16], F32)
        Cld = AR.alloc([8, 2, 64], F32)
        Dp = AR.alloc([32], F32)
        Bp = Buf("params")
        for hf in range(2):
            ps_ = slice(hf * 64, hf * 64 + 64)
            dma("sync", lambda h, ps_=ps_: h.dma_start(out=are[ps_, :], in_=sAre_d.rearrange("d g n -> n (d g)"), allow_slow_non_contiguous=True), w=[Bp])
            dma("sync", lambda h, ps_=ps_: h.dma_start(out=aim[ps_, :], in_=sAim_d.rearrange("d g n -> n (d g)"), allow_slow_non_contiguous=True), w=[Bp])
            dma("sync", lambda h, ps_=ps_: h.dma_start(out=Bre[ps_, :, :], in_=sBre_d.rearrange("d g n c -> n (d g) c")), w=[Bp])
            dma("sync", lambda h, ps_=ps_: h.dma_start(out=Bim[ps_, :, :], in_=sBim_d.rearrange("d g n c -> n (d g) c")), w=[Bp])
        dma("sync", lambda h: h.dma_start(out=dtn, in_=sldt_d.rearrange("d g -> (d g)").partition_broadcast(128)), w=[Bp])
        for j in range(8):
            dma("sync", lambda h, j=j: h.dma_start(out=Dp[j * 16:(j + 1) * 16, :], in_=sD_d.rearrange("(g c) -> c g", c=16), allow_slow_non_contiguous=True), w=[Bp])
        Cld2 = AR.alloc([8, 2, 64], F32)
        for (src, cl) in ((sCre_d, Cld), (sCim_d, Cld2)):
            for dup in range(2):
                dma("sync", lambda h, src=src, dup=dup, cl=cl: h.dma_start(out=cl[:, :, dup, :], in_=src.rearrange("d g c n -> (d g c) n").rearrange("(t p) n -> p t n", p=128)), w=[Bp])
        S.barrier()
        for (cl, dst) in ((Cld, Cre), (Cld2, Cim)):
            for half in range(2):
                bk = half
                for t4 in range(4):
                    t = half * 4 + t4
                    pop(lambda h, t=t, t4=t4, bk=bk, cl=cl: h.transpose(out=PSF[bk][:, t4 * 128:(t4 + 1) * 128], in_=flat(cl[:, t, :, :]), identity=k32("ident")), r=[Bp, Bc32], w=[PB[bk]])
                vop(lambda h, dst=dst, half=half, bk=bk: h.tensor_copy(out=flat(dst)[:, half * 512:(half + 1) * 512], in_=PSF[bk]), r=[PB[bk]], w=[Bp])

        gop(lambda h: h.tensor_scalar(out=flat(Cre)[64:128], in0=flat(Cre)[64:128], scalar1=-1.0, scalar2=None, op0=ALU.mult), r=[Bp], w=[Bp])
        gop(lambda h: h.tensor_scalar(out=flat(Cim)[64:128], in0=flat(Cim)[64:128], scalar1=-1.0, scalar2=None, op0=ALU.mult), r=[Bp], w=[Bp])
        ev = k32("ev", 17)
        evr = k32("evr", 17)
        t0 = AR.alloc([32, 8, 16], F32)
        t1 = AR.alloc([32, 8, 16], F32)
        t0f, t1f = flat(t0), flat(t1)
        tA = t0f[:, 0:1088].rearrange("p (a b) -> p a b", b=64)
        tB = t0f[:, 1088:2176].rearrange("p (a b) -> p a b", b=64)
        tI = t0f[:, 2176:3264].bitcast(I32).rearrange("p (a b) -> p a b", b=64)
        Emag = t1f[:, 0:1088].rearrange("p (a b) -> p a b", b=64)
        Er = AR.alloc([17, 64], F32)
        Ei = AR.alloc([17, 64], F32)
        ErR = AR.alloc([17, 64], F32)
        EiR = AR.alloc([17, 64], F32)
        sm = [AR.alloc([64], F32) for _ in range(8)]
        BE = Buf("E")

        def sincos(xin, sin_out, cos_out, ti, tf, bufs):
            vop(lambda h: h.tensor_scalar(out=ti, in0=xin, scalar1=1.0 / TWO_PI, scalar2=None, op0=ALU.mult), r=bufs, w=bufs)
            vop(lambda h: h.tensor_copy(out=tf, in_=ti), r=bufs, w=bufs)
            vop(lambda h: h.scalar_tensor_tensor(out=tf, in0=tf, scalar=-TWO_PI, in1=xin, op0=ALU.mult, op1=ALU.add), r=bufs, w=bufs)
            vop(lambda h: h.tensor_scalar(out=tf, in0=tf, scalar1=PI_SAFE, scalar2=-PI_SAFE, op0=ALU.min, op1=ALU.max), r=bufs, w=bufs)
            aop(lambda h: h.activation(out=sin_out, in_=tf, func=AF.Sin), r=bufs, w=bufs)
            vop(lambda h: h.tensor_scalar(out=cos_out, in0=tf, scalar1=math.pi / 2, scalar2=-TWO_PI, op0=ALU.is_gt, op1=ALU.mult), r=bufs, w=bufs)
            vop(lambda h: h.scalar_tensor_tensor(out=cos_out, in0=tf, scalar=math.pi / 2, in1=cos_out, op0=ALU.add, op1=ALU.add), r=bufs, w=bufs)
            vop(lambda h: h.tensor_scalar(out=cos_out, in0=cos_out, scalar1=PI_SAFE, scalar2=-PI_SAFE, op0=ALU.min, op1=ALU.max), r=bufs, w=bufs)
            aop(lambda h: h.activation(out=cos_out, in_=cos_out, func=AF.Sin), r=bufs, w=bufs)

        PB0 = [Bp, BE, Bc32]
        aop(lambda h: h.activation(out=dtn, in_=dtn, func=AF.Exp), r=PB0, w=PB0)
        ar_, th_ = sm[0], sm[1]
        vop(lambda h: h.tensor_tensor(out=ar_, in0=are, in1=dtn, op=ALU.mult), r=PB0, w=PB0)
        vop(lambda h: h.tensor_tensor(out=th_, in0=aim, in1=dtn, op=ALU.mult), r=PB0, w=PB0)

        def etable(evc, Er_, Ei_):
            vop(lambda h: h.tensor_tensor(out=tA, in0=ar_.unsqueeze(1).to_broadcast([128, 17, 64]), in1=evc.unsqueeze(2).to_broadcast([128, 17, 64]), op=ALU.mult), r=PB0, w=PB0)
            aop(lambda h: h.activation(out=flat(Emag), in_=flat(tA), func=AF.Exp), r=PB0, w=PB0)
            vop(lambda h: h.tensor_tensor(out=tA, in0=th_.unsqueeze(1).to_broadcast([128, 17, 64]), in1=evc.unsqueeze(2).to_broadcast([128, 17, 64]), op=ALU.mult), r=PB0, w=PB0)
            sincos(flat(tA), flat(Ei_), flat(Er_), flat(tI), flat(tB), PB0)
            vop(lambda h: h.tensor_tensor(out=flat(Er_), in0=flat(Er_), in1=flat(Emag), op=ALU.mult), r=PB0, w=PB0)
            vop(lambda h: h.tensor_tensor(out=flat(Ei_), in0=flat(Ei_), in1=flat(Emag), op=ALU.mult), r=PB0, w=PB0)

        etable(ev, Er, Ei)
        etable(evr, ErR, EiR)
        am1, nr, ni, den, fr, fi = sm[2], sm[3], sm[4], sm[5], sm[6], sm[7]
        ar1, ai1 = Er[:, 9, :], Ei[:, 9, :]
        vop(lambda h: h.tensor_scalar(out=am1, in0=ar1, scalar1=-1.0, scalar2=None, op0=ALU.add), r=PB0, w=PB0)
        vop(lambda h: h.tensor_tensor(out=nr, in0=am1, in1=are, op=ALU.mult), r=PB0, w=PB0)
        vop(lambda h: h.tensor_tensor(out=den, in0=ai1, in1=aim, op=ALU.mult), r=PB0, w=PB0)
        vop(lambda h: h.tensor_tensor(out=nr, in0=nr, in1=den, op=ALU.add), r=PB0, w=PB0)
        vop(lambda h: h.tensor_tensor(out=ni, in0=ai1, in1=are, op=ALU.mult), r=PB0, w=PB0)
        vop(lambda h: h.tensor_tensor(out=den, in0=am1, in1=aim, op=ALU.mult), r=PB0, w=PB0)
        vop(lambda h: h.tensor_tensor(out=ni, in0=ni, in1=den, op=ALU.subtract), r=PB0, w=PB0)
        vop(lambda h: h.tensor_tensor(out=den, in0=are, in1=are, op=ALU.mult), r=PB0, w=PB0)
        vop(lambda h: h.tensor_tensor(out=am1, in0=aim, in1=aim, op=ALU.mult), r=PB0, w=PB0)
        vop(lambda h: h.tensor_tensor(out=den, in0=den, in1=am1, op=ALU.add), r=PB0, w=PB0)
        vop(lambda h: h.reciprocal(out=den, in_=den), r=PB0, w=PB0)
        vop(lambda h: h.tensor_tensor(out=fr, in0=nr, in1=den, op=ALU.mult), r=PB0, w=PB0)
        vop(lambda h: h.tensor_tensor(out=fi, in0=ni, in1=den, op=ALU.mult), r=PB0, w=PB0)
        bbr = AR.alloc([64, 16], F32)
        bbi = AR.alloc([64, 16], F32)
        tb1 = AR.alloc([64, 16], F32)

        def bc16(a):
            return a.unsqueeze(2).to_broadcast([128, a.shape[1], 16])

        vop(lambda h: h.tensor_tensor(out=bbr, in0=Bre, in1=bc16(fr), op=ALU.mult), r=PB0, w=PB0)
        vop(lambda h: h.tensor_tensor(out=tb1, in0=Bim, in1=bc16(fi), op=ALU.mult), r=PB0, w=PB0)
        vop(lambda h: h.tensor_tensor(out=bbr, in0=bbr, in1=tb1, op=ALU.subtract), r=PB0, w=PB0)
        vop(lambda h: h.tensor_tensor(out=bbi, in0=Bim, in1=bc16(fr), op=ALU.mult), r=PB0, w=PB0)
        vop(lambda h: h.tensor_tensor(out=tb1, in0=Bre, in1=bc16(fi), op=ALU.mult), r=PB0, w=PB0)
        vop(lambda h: h.tensor_tensor(out=bbi, in0=bbi, in1=tb1, op=ALU.add), r=PB0, w=PB0)

        PinN = AR.alloc([2, 32, 8, 16], BF16)
        P16 = AR.alloc([2, 32, 8, 16], BF16)
        BPin, BPm = Buf(), Buf()
        Bt_lo, Bt_hi = Buf(), Buf()
        Q16v = AR.alloc([2, 32, 8, 16], BF16)
        BQt = Buf()

        def cmul_batch(out_ap, outbuf, Ar, Ai, d, Tr_, Ti_, i0, mode):
            def vv(T, lo, hi):
                return T[lo:hi, i0:i0 + 8, d * 32:(d + 1) * 32].rearrange("p j g -> p g j").unsqueeze(3).to_broadcast([hi - lo, 32, 8, 16])

            def aa(A, lo, hi):
                return A[lo:hi, d * 32:(d + 1) * 32, :].unsqueeze(2).to_broadcast([hi - lo, 32, 8, 16])

            rr = PB0
            vop(lambda h: h.tensor_tensor(out=t0[0:64], in0=aa(Ar, 0, 64), in1=vv(Tr_, 0, 64), op=ALU.mult), r=rr, w=[Bt_lo])
            vop(lambda h: h.tensor_tensor(out=t1[0:64], in0=aa(Ai, 0, 64), in1=vv(Ti_, 0, 64), op=ALU.mult), r=rr, w=[Bt_lo])
            vop(lambda h: h.tensor_tensor(out=out_ap[0:64], in0=t0[0:64], in1=t1[0:64], op=ALU.subtract), r=[Bt_lo], w=[outbuf])
            gop(lambda h: h.tensor_tensor(out=t0[64:128], in0=aa(Ar, 64, 128), in1=vv(Ti_, 64, 128), op=ALU.mult), r=rr, w=[Bt_hi])
            gop(lambda h: h.tensor_tensor(out=t1[64:128], in0=aa(Ai, 64, 128), in1=vv(Tr_, 64, 128), op=ALU.mult), r=rr, w=[Bt_hi])
            if mode == "P":
                gop(lambda h: h.tensor_tensor(out=out_ap[64:128], in0=t0[64:128], in1=t1[64:128], op=ALU.add), r=[Bt_hi], w=[outbuf])
            else:
                gop(lambda h: h.tensor_tensor(out=t0[64:128], in0=t0[64:128], in1=t1[64:128], op=ALU.add), r=[Bt_hi], w=[Bt_hi])
                gop(lambda h: h.tensor_scalar(out=out_ap[64:128], in0=t0[64:128], scalar1=-1.0, scalar2=None, op0=ALU.mult), r=[Bt_hi], w=[outbuf])

        cmul_batch(PinN[:, 0], BPin, bbr, bbi, 0, ErR, EiR, 1, "P")
        cmul_batch(P16[:, 0], BPm, bbr, bbi, 0, ErR, EiR, 9, "P")
        cmul_batch(Q16v[:, 0], BQt, Cre, Cim, 0, Er, Ei, 9, "P")
        cmul_batch(PinN[:, 1], BPin, bbr, bbi, 1, Er, Ei, 8, "P")
        cmul_batch(P16[:, 1], BPm, bbr, bbi, 1, Er, Ei, 0, "P")
        cmul_batch(Q16v[:, 1], BQt, Cre, Cim, 1, ErR, EiR, 0, "P")
        aop(lambda h: h.activation(out=flat(Q16), in_=flat(Q16v), func=AF.Copy), r=[BQt], w=[BQ16])
        for d in range(2):
            for g8 in range(4):
                bk = 2 + (g8 % 2)
                for gi in range(8):
                    g = g8 * 8 + gi
                    pop(lambda h, d=d, g=g, gi=gi, bk=bk: h.transpose(out=PSB[bk][:, gi * 128:(gi + 1) * 128], in_=flat(PinN[:, d, g, :, :]), identity=k16("ident")), r=[BPin, Bc16], w=[PB[bk]])
                aop(lambda h, d=d, g8=g8, bk=bk: h.activation(out=flat(Min[:, d, g8 * 8:(g8 + 1) * 8, :]), in_=PSB[bk], func=AF.Copy), r=[PB[bk]], w=[BMin])
        mt0 = AR.alloc([4, 128], F32)
        mt1 = AR.alloc([4, 128], F32)
        Bmt = Buf()
        for g4 in range(8):
            for d in range(2):
                bk = 4 + d
                for gi in range(4):
                    g = g4 * 4 + gi
                    pop(lambda h, d=d, g=g, gi=gi, bk=bk: h.matmul(PSF[bk][:, gi * 128:(gi + 1) * 128], lhsT=flat(P16[:, d, g, :, :]), rhs=Q16[:, d, g, :], start=True, stop=True), r=[BPm, BQ16], w=[PB[bk]])
            vop(lambda h: h.tensor_tensor(out=mt0, in0=PSF[4].rearrange("p (a b) -> p a b", b=128), in1=k32("mask0").unsqueeze(1).to_broadcast([128, 4, 128]), op=ALU.mult), r=[PB[4], Bc32], w=[Bmt])
            vop(lambda h: h.tensor_tensor(out=mt1, in0=PSF[5].rearrange("p (a b) -> p a b", b=128), in1=k32("mask1").unsqueeze(1).to_broadcast([128, 4, 128]), op=ALU.mult), r=[PB[5], Bc32], w=[Bmt])
            vop(lambda h: h.tensor_tensor(out=mt0, in0=mt0, in1=mt1, op=ALU.add), r=[Bmt], w=[Bmt])
            for gi in range(4):
                g = g4 * 4 + gi
                vop(lambda h, g=g, gi=gi: h.scalar_tensor_tensor(out=Mi[:, g, :], in0=k32("ident"), scalar=Dp[:, g:g + 1], in1=mt0[:, gi, :], op0=ALU.mult, op1=ALU.add), r=[Bmt, Bp, Bc32], w=[BMi])
        S.barrier()
        AR.release(persist_mark)

        Buf_uf, BX = Buf(), Buf()
        p1_mark = AR.mark()
        Wfs = AR.alloc([8, 1024], BF16)
        BWfs = Buf()
        dma("gpsimd", lambda h: h.dma_start(out=Wfs, in_=w_in_d[:, 0:1024].rearrange("(kc p) n -> p kc n", p=128)), w=[BWfs], sem="wfs")
        gbc = AR.alloc([1024], F32)
        Bg = Buf()
        dma("sync", lambda h: h.dma_start(out=gbc, in_=g1_d.partition_broadcast(128)), w=[Bg], sem="g1a")
        hT = AR.alloc([8, 1024], BF16)
        BhT = Buf()
        Zt = AR.alloc([32, 8, 16], BF16)
        BZ = Buf()
        NXB = 3
        xt = [AR.alloc([1024], F32) for _ in range(NXB)]
        Bxt = [Buf() for _ in range(NXB)]
        xn = [AR.alloc([1024], BF16) for _ in range(2)]
        Bxn = [Buf() for _ in range(2)]
        junk = AR.alloc([1024], BF16)
        Bjunk = Buf()
        stat = AR.alloc([64], F32)
        Bstat = Buf()

        def norm_tile(xsrc_ap, xt_ap, bxt, xn_ap, bxn, col, gb, bg, qsem):
            dma("sync", lambda h: h.dma_start(out=xt_ap, in_=xsrc_ap), w=[bxt], sem=qsem)
            aop(lambda h: h.activation(out=junk, in_=xt_ap, func=AF.Square, accum_out=stat[:, col:col + 1]), r=[bxt], w=[Bjunk, Bstat])
            aop(lambda h: h.activation(out=stat[:, col + 1:col + 2], in_=stat[:, col:col + 1], func=AF.Sqrt, scale=1.0 / 1024, bias=1e-6), r=[Bstat], w=[Bstat])
            vop(lambda h: h.reciprocal(out=stat[:, col + 2:col + 3], in_=stat[:, col + 1:col + 2]), r=[Bstat], w=[Bstat])
            vop(lambda h: h.scalar_tensor_tensor(out=xn_ap, in0=xt_ap, scalar=stat[:, col + 2:col + 3], in1=gb, op0=ALU.mult, op1=ALU.mult), r=[bxt, Bstat, bg], w=[bxn])

        ti_ = 0
        for b in range(4):
            for t in range(8):
                tg = b * 8 + t
                xi = ti_ % NXB
                ni_ = ti_ % 2
                col = (ti_ % 8) * 4
                ti_ += 1
                norm_tile(x_d[tg * 128:(tg + 1) * 128, :], xt[xi], Bxt[xi], xn[ni_], Bxn[ni_], col, gbc, Bg, "x%d" % xi)
                bk = ni_
                for kc in range(8):
                    pop(lambda h, kc=kc, bk=bk, ni_=ni_: h.transpose(out=PSB[bk][:, kc * 128:(kc + 1) * 128], in_=xn[ni_][:, kc * 128:(kc + 1) * 128], identity=k16("ident")), r=[Bxn[ni_], Bc16], w=[PB[bk]])
                aop(lambda h, t=t, bk=bk: h.activation(out=hT[:, :, t * 128:(t + 1) * 128], in_=PSB[bk].rearrange("p (a b) -> p a b", b=128), func=AF.Copy), r=[PB[bk]], w=[BhT])
            for t in range(8):
                tg = b * 8 + t
                bk = 2 + (t % 2)
                for kc in range(8):
                    pop(lambda h, kc=kc, t=t, bk=bk: h.matmul(PSF[bk], lhsT=hT[:, kc, t * 128:(t + 1) * 128], rhs=Wfs[:, kc, 0:512], start=(kc == 0), stop=(kc == 7)), r=[BhT, BWfs], w=[PB[bk]])
                vop(lambda h, tg=tg, bk=bk: h.tensor_copy(out=uf_tm[:, tg, :], in_=PSF[bk]), r=[PB[bk]], w=[Buf_uf])
            for j in range(8):
                bk = 4 + (j % 2)
                for kc in range(8):
                    pop(lambda h, kc=kc, j=j, bk=bk: h.matmul(PSF[bk], lhsT=hT[:, kc, j:1024:8], rhs=Wfs[:, kc, 512:1024], start=(kc == 0), stop=(kc == 7)), r=[BhT, BWfs], w=[PB[bk]])
                aop(lambda h, j=j, bk=bk: h.activation(out=Zt[:, :, j, :], in_=PSF[bk].rearrange("p (g c) -> p g c", c=16), func=AF.Copy), r=[PB[bk]], w=[BZ])
            for g8 in range(4):
                bk = 6 + (g8 % 2)
                for gi in range(8):
                    g = g8 * 8 + gi
                    pop(lambda h, g=g, gi=gi, bk=bk: h.transpose(out=PSB[bk][:, gi * 128:(gi + 1) * 128], in_=flat(Zt[:, g, :, :]), identity=k16("ident")), r=[BZ, Bc16], w=[PB[bk]])
                vop(lambda h, g8=g8, b=b, bk=bk: h.tensor_copy(out=Xs[:, g8 * 8:(g8 + 1) * 8, b * 128:(b + 1) * 128], in_=PSB[bk].rearrange("p (a b) -> p a b", b=128)), r=[PB[bk]], w=[BX])
        S.barrier()
        dbg("wfs", Wfs, [128, 8, 1024])
        dbg("ht", hT, [128, 8, 1024])
        dbg("xn", xn[1], [128, 1024])
        dbg("stat", stat, [128, 64])
        dbg("zt", Zt, [128, 32, 8, 16])
        dbg("min", Min, [128, 2, 32, 128])
        dbg("q16", Q16, [128, 2, 32, 128])
        dbg("mi", Mi, [128, 32, 128])
        AR.release(p1_mark)
        dbg("uf", uf_tm, [128, 32, 512])
        dbg("xs", Xs, [128, 32, 512])
        if STAGE == 0:
            S.barrier()
            S.run()
            return nc, dbg_outs

        BFT = Buf()
        p2_mark = AR.mark()
        NTB = 3
        tct = [AR.alloc([16, 256], BF16) for _ in range(NTB)]
        tst = [AR.alloc([16, 256], BF16) for _ in range(NTB)]
        Btc = [Buf() for _ in range(NTB)]
        Bts = [Buf() for _ in range(NTB)]
        UrT = AR.alloc([4, 256], BF16)
        UiT = AR.alloc([4, 256], BF16)
        BUU = Buf()
        it_ = 0
        for kb in range(8):
            for lt in range(32):
                l16 = lt % 16
                if l16 == 0:
                    bi = it_ % NTB
                    it_ += 1
                    hh_ = lt // 16
                    dma("sync", lambda h, bi=bi, hh_=hh_, kb=kb: h.dma_start(out=tct[bi], in_=tc_d[kb, :, hh_ * 16:(hh_ + 1) * 16, :]), w=[Btc[bi]], sem="tc%d" % bi)
                    dma("scalar", lambda h, bi=bi, hh_=hh_, kb=kb: h.dma_start(out=tst[bi], in_=ts_d[kb, :, hh_ * 16:(hh_ + 1) * 16, :]), w=[Bts[bi]], sem="ts%d" % bi)
                for g in range(4):
                    pop(lambda h, g=g, lt=lt, bi=bi, l16=l16: h.matmul(PSF[g][:, 0:256], lhsT=uf_tm[:, lt, g * 128:(g + 1) * 128], rhs=tct[bi][:, l16, :], start=(lt == 0), stop=(lt == 31)), r=[Buf_uf, Btc[bi]], w=[PB[g]])
                    pop(lambda h, g=g, lt=lt, bi=bi, l16=l16: h.matmul(PSF[4 + g][:, 0:256], lhsT=uf_tm[:, lt, g * 128:(g + 1) * 128], rhs=tst[bi][:, l16, :], start=(lt == 0), stop=(lt == 31)), r=[Buf_uf, Bts[bi]], w=[PB[4 + g]])
            for g in range(4):
                vop(lambda h, g=g: h.tensor_copy(out=UrT[:, g, :], in_=PSF[g][:, 0:256]), r=[PB[g]], w=[BUU])
                aop(lambda h, g=g: h.activation(out=UiT[:, g, :], in_=PSF[4 + g][:, 0:256], func=AF.Copy), r=[PB[4 + g]], w=[BUU])
            for g in range(4):
                pop(lambda h, g=g: h.matmul(PSF[g][:, 256:512], lhsT=k16("CC"), rhs=UrT[:, g, :], start=True, stop=False), r=[BUU, Bc16], w=[PB[g]])
                pop(lambda h, g=g: h.matmul(PSF[g][:, 256:512], lhsT=k16("SS"), rhs=UiT[:, g, :], start=False, stop=True), r=[BUU, Bc16], w=[PB[g]])
                vop(lambda h, g=g, kb=kb: h.tensor_copy(out=FT[:, g, kb * 256:(kb + 1) * 256], in_=PSF[g][:, 256:512]), r=[PB[g]], w=[BFT])
        S.barrier()
        AR.release(p2_mark)
        dbg("ft", FT, [128, 4, 2048])

        ByT = Buf()
        AR.release(uf_off)
        p3_mark = AR.mark()
        Wglu = AR.alloc([4, 512], BF16)
        BWglu = Buf()
        dma("gpsimd", lambda h: h.dma_start(out=Wglu, in_=wglu_d.rearrange("(q p) n -> p q n", p=128)), w=[BWglu], sem="wglu")
        pAre = AR.alloc([16, 64], F32)
        pAim = AR.alloc([16, 64], F32)
        pdt = AR.alloc([16], F32)
        Trp = AR.alloc([16, 64], F32)
        Tip = AR.alloc([16, 64], F32)
        Trm = AR.alloc([16, 64], F32)
        Tim = AR.alloc([16, 64], F32)
        tX = AR.alloc([16, 64], F32)
        tY = AR.alloc([16, 64], F32)
        tZi = AR.alloc([16, 64], I32)
        BT = Buf("tables")
        Hc = [AR.alloc([16, 2, 64], BF16) for _ in range(2)]
        BHc = [Buf(), Buf()]
        HTd = [AR.alloc([16, 257], BF16) for _ in range(2)]
        BHT = [Buf(), Buf()]
        Vt = [AR.alloc([4, 2, 64], BF16) for _ in range(2)]
        BV = [Buf(), Buf()]
        m1 = [AR.alloc([4, 2, 64], F32) for _ in range(2)]
        m2 = [AR.alloc([4, 2, 64], F32) for _ in range(2)]
        Bm = [Buf(), Buf()]
        Yg = AR.alloc([8, 256], BF16)
        BYg = Buf()
        sg = AR.alloc([4, 512], BF16)
        Bsg = Buf()

        def cplx_mul(src4, bsrc, Tr, Ti, k, out_re, out_im, bout):
            a, bb = m1[k], m2[k]
            Trb = Tr.unsqueeze(2).to_broadcast([128, 4, 2, 64])
            vop(lambda h: h.tensor_tensor(out=a, in0=src4, in1=Trb, op=ALU.mult), r=[BT, bsrc], w=[Bm[k]])
            vop(lambda h: h.tensor_tensor(out=bb[:, :, 0, :], in0=src4[:, :, 1, :], in1=Ti, op=ALU.mult), r=[BT, bsrc], w=[Bm[k]])
            vop(lambda h: h.tensor_tensor(out=bb[:, :, 1, :], in0=src4[:, :, 0, :], in1=Ti, op=ALU.mult), r=[BT, bsrc], w=[Bm[k]])
            gop(lambda h: h.tensor_tensor(out=out_re, in0=a[:, :, 0, :], in1=bb[:, :, 0, :], op=ALU.subtract), r=[Bm[k]], w=[bout])
            gop(lambda h: h.tensor_tensor(out=out_im, in0=a[:, :, 1, :], in1=bb[:, :, 1, :], op=ALU.add), r=[Bm[k]], w=[bout])

        for half in range(2):
            g_lo = half * 16
            for d in range(2):
                kcol = c32[:, C32["kf"] + d:C32["kf"] + d + 1]
                dma("sync", lambda h, d=d, g_lo=g_lo: h.dma_start(out=flat(pAre), in_=sAre_d[d, g_lo:g_lo + 16, :].rearrange("g n -> (g n)").partition_broadcast(128)), w=[BT], sem="pa")
                dma("sync", lambda h, d=d, g_lo=g_lo: h.dma_start(out=flat(pAim), in_=sAim_d[d, g_lo:g_lo + 16, :].rearrange("g n -> (g n)").partition_broadcast(128)), w=[BT], sem="pb")
                dma("sync", lambda h, d=d, g_lo=g_lo: h.dma_start(out=pdt, in_=sldt_d[d, g_lo:g_lo + 16].partition_broadcast(128)), w=[BT], sem="pc")
                TB = [BT, Bc32]
                xt3 = [Tok("dma", "pa", S.dcnt["pa"]), Tok("dma", "pb", S.dcnt["pb"]), Tok("dma", "pc", S.dcnt["pc"])]
                aop(lambda h: h.activation(out=pdt, in_=pdt, func=AF.Exp, scale=1.0), r=TB, w=TB, x=xt3)
                dtb = pdt.unsqueeze(2).to_broadcast([128, 16, 64])
                vop(lambda h, dtb=dtb: h.scalar_tensor_tensor(out=pAre, in0=pAre, scalar=8.0, in1=dtb, op0=ALU.mult, op1=ALU.mult), r=TB, w=TB, x=xt3)
                vop(lambda h, dtb=dtb: h.scalar_tensor_tensor(out=pAim, in0=pAim, scalar=8.0, in1=dtb, op0=ALU.mult, op1=ALU.mult), r=TB, w=TB)
                vop(lambda h: h.tensor_scalar(out=flat(tZi), in0=flat(pAim), scalar1=1.0 / TWO_PI, scalar2=None, op0=ALU.mult), r=TB, w=TB)
                vop(lambda h: h.tensor_copy(out=flat(tX), in_=flat(tZi)), r=TB, w=TB)
                vop(lambda h: h.scalar_tensor_tensor(out=flat(pAim), in0=flat(tX), scalar=-TWO_PI, in1=flat(pAim), op0=ALU.mult, op1=ALU.add), r=TB, w=TB)
                vop(lambda h, kcol=kcol: h.tensor_scalar(out=flat(tY), in0=flat(pAim), scalar1=kcol, scalar2=None, op0=ALU.mult), r=TB, w=TB)
                sincos(flat(tY), flat(Tip), flat(Trp), flat(tZi), flat(tX), TB)
                vop(lambda h, kcol=kcol: h.tensor_scalar(out=flat(tY), in0=flat(pAre), scalar1=kcol, scalar2=None, op0=ALU.mult), r=TB, w=TB)
                aop(lambda h: h.activation(out=flat(tX), in_=flat(tY), func=AF.Exp), r=TB, w=TB)
                aop(lambda h: h.activation(out=flat(tY), in_=flat(tY), func=AF.Exp, scale=-1.0), r=TB, w=TB)
                vop(lambda h: h.tensor_tensor(out=flat(Trm), in0=flat(Trp), in1=flat(tY), op=ALU.mult), r=TB, w=TB)
                vop(lambda h: h.scalar_tensor_tensor(out=flat(Tim), in0=flat(Tip), scalar=-1.0, in1=flat(tY), op0=ALU.mult, op1=ALU.mult), r=TB, w=TB)
                vop(lambda h: h.tensor_tensor(out=flat(Trp), in0=flat(Trp), in1=flat(tX), op=ALU.mult), r=TB, w=TB)
                vop(lambda h: h.tensor_tensor(out=flat(Tip), in0=flat(Tip), in1=flat(tX), op=ALU.mult), r=TB, w=TB)
                blocks = [0, 1] if d == 0 else [3, 2, 1, 0]
                tri = k16("triL") if d == 0 else k16("triU")
                sel = k16("selL") if d == 0 else k16("selF")
                if d == 0:
                    vop(lambda h: h.memset(HTd[0][:, :, 0:1], 0.0), r=[], w=[BHT[0]])
                for bi_, blk in enumerate(blocks):
                    cur, prv = bi_ % 2, (bi_ + 1) % 2
                    first = (bi_ == 0)
                    for c4 in range(4):
                        gl = c4 * 4
                        k = c4 % 2
                        bS, bW, bTp = (0, 1, 2) if k == 0 else (3, 4, 5)
                        for gi in range(4):
                            g = g_lo + gl + gi
                            pop(lambda h, g=g, gi=gi, blk=blk, d=d, bS=bS: h.matmul(PSF[bS][:, gi * 128:(gi + 1) * 128], lhsT=Xs[:, g, blk * 128:(blk + 1) * 128], rhs=Min[:, d, g, :], start=True, stop=True), r=[BX, BMin], w=[PB[bS]])
                        srcS = PSF[bS].rearrange("p (g h n) -> p g h n", h=2, n=64)
                        cplx_mul(srcS, PB[bS], Trm[:, gl:gl + 4, :], Tim[:, gl:gl + 4, :], k, Vt[k][:, :, 0, :], Vt[k][:, :, 1, :], BV[k])
                        pop(lambda h, k=k, bW=bW, first=first, tri=tri: h.matmul(PSF[bW], lhsT=tri, rhs=flat(Vt[k]), start=True, stop=first), r=[BV[k], Bc16], w=[PB[bW]])
                        if not first:
                            pop(lambda h, bW=bW, prv=prv, gl=gl, sel=sel: h.matmul(PSF[bW], lhsT=sel, rhs=flat(Hc[prv][:, gl:gl + 4, :, :]), start=False, stop=True), r=[BHc[prv], Bc16], w=[PB[bW]])
                        srcW = PSF[bW].rearrange("p (g h n) -> p g h n", h=2, n=64)
                        cplx_mul(srcW, PB[bW], Trp[:, gl:gl + 4, :], Tip[:, gl:gl + 4, :], k, Hc[cur][:, gl:gl + 4, 0, :], Hc[cur][:, gl:gl + 4, 1, :], BHc[cur])
                        if blk <= 2:
                            for gi in range(4):
                                pop(lambda h, gi=gi, gl=gl, cur=cur, bTp=bTp: h.transpose(out=PSB[bTp][:, gi * 128:(gi + 1) * 128], in_=flat(Hc[cur][:, gl + gi, :, :]), identity=k16("ident")), r=[BHc[cur], Bc16], w=[PB[bTp]])
                            srcT = PSB[bTp][:, 0:512].rearrange("p (a b) -> p a b", b=128)
                            if blk < 2:
                                co = blk * 128 + (1 if d == 0 else 0)
                                aop(lambda h, gl=gl, co=co, srcT=srcT, d=d: h.activation(out=HTd[d][:, gl:gl + 4, co:co + 128], in_=srcT, func=AF.Copy), r=[PB[bTp]], w=[BHT[d]])
                            else:
                                aop(lambda h, gl=gl, srcT=srcT, d=d: h.activation(out=HTd[d][:, gl:gl + 4, 256:257], in_=srcT[:, :, 0:1], func=AF.Copy), r=[PB[bTp]], w=[BHT[d]])
            for blk in range(2):
                for c4 in range(4):
                    gl = c4 * 4
                    bk = 6 + (c4 % 2)
                    for gi in range(4):
                        g = g_lo + gl + gi
                        o = gi * 128
                        pop(lambda h, g=g, o=o, blk=blk, bk=bk: h.matmul(PSF[bk][:, o:o + 128], lhsT=Xs[:, g, blk * 128:(blk + 1) * 128], rhs=Mi[:, g, :], start=True, stop=False), r=[BX, BMi], w=[PB[bk]])
                        pop(lambda h, g=g, o=o, blk=blk, bk=bk, gl=gl, gi=gi: h.matmul(PSF[bk][:, o:o + 128], lhsT=HTd[0][:, gl + gi, blk * 128:blk * 128 + 128], rhs=Q16[:, 0, g, :], start=False, stop=False), r=[BHT[0], BQ16], w=[PB[bk]])
                        pop(lambda h, g=g, o=o, blk=blk, bk=bk, gl=gl, gi=gi: h.matmul(PSF[bk][:, o:o + 128], lhsT=HTd[1][:, gl + gi, blk * 128 + 1:blk * 128 + 129], rhs=Q16[:, 1, g, :], start=False, stop=True), r=[BHT[1], BQ16], w=[PB[bk]])
                    aop(lambda h, bk=bk, gl=gl: h.activation(out=Yg[:, :, gl * 16:(gl + 4) * 16].rearrange("p i (g c) -> p g i c", c=16), in_=PSF[bk].rearrange("p (g i c) -> p g i c", i=8, c=16), func=AF.Gelu), r=[PB[bk]], w=[BYg])
                for qq in range(2):
                    q = 2 * half + qq
                    bk = 2 if qq == 0 else 5
                    for i in range(8):
                        pop(lambda h, i=i, qq=qq, bk=bk: h.transpose(out=PSB[bk][:, i * 128:(i + 1) * 128], in_=Yg[:, i, qq * 128:(qq + 1) * 128], identity=k16("ident")), r=[BYg, Bc16], w=[PB[bk]])
                    vop(lambda h, q=q, blk=blk, bk=bk: h.tensor_copy(out=yT[:, q, blk * 1024:(blk + 1) * 1024].rearrange("p (k i) -> p i k", i=8), in_=PSB[bk].rearrange("p (i k) -> p i k", k=128)), r=[PB[bk]], w=[ByT])
        dbg("yt", yT, [128, 4, 2048])
        for tb in range(4):
            for co in range(4):
                for q in range(4):
                    pop(lambda h, co=co, q=q, tb=tb: h.matmul(PSF[co], lhsT=Wglu[:, q, co * 128:(co + 1) * 128], rhs=yT[:, q, tb * 512:(tb + 1) * 512], start=(q == 0), stop=(q == 3)), r=[ByT, BWglu], w=[PB[co]])
                aop(lambda h, co=co: h.activation(out=sg[:, co, :], in_=PSF[co], func=AF.Sigmoid), r=[PB[co]], w=[Bsg])
            vop(lambda h, tb=tb: h.tensor_tensor(out=yT[:, :, tb * 512:(tb + 1) * 512], in0=yT[:, :, tb * 512:(tb + 1) * 512], in1=sg, op=ALU.mult), r=[Bsg, ByT], w=[ByT])
        S.barrier()
        AR.release(p3_mark)
        dbg("y2t", yT, [128, 4, 2048])

        A4 = Arena(arena_t[:, persist0:ft_off], ft_off - persist0)
        A5 = Arena(arena_t[:, xs_off:ARENA_BYTES], ARENA_BYTES - xs_off)
        Wg_ = A4.alloc([8, 2048], BF16)
        Wout = A5.alloc([8, 1024], BF16)
        Wfo = A4.alloc([4, 1024], BF16)
        Wso = A5.alloc([4, 1024], BF16)
        BW4 = [Buf() for _ in range(4)]
        dma("gpsimd", lambda h: h.dma_start(out=Wfo, in_=wfo_d.rearrange("(q p) n -> p q n", p=128)), w=[BW4[0]], sem="w4a")
        dma("gpsimd", lambda h: h.dma_start(out=Wso, in_=wso_d.rearrange("(q p) n -> p q n", p=128)), w=[BW4[1]], sem="w4b")
        dma("gpsimd", lambda h: h.dma_start(out=Wg_, in_=w_in_d[:, 1024:3072].rearrange("(kc p) n -> p kc n", p=128)), w=[BW4[2]], sem="w4c")
        dma("gpsimd", lambda h: h.dma_start(out=Wout, in_=wout_d.rearrange("(kc p) n -> p kc n", p=128)), w=[BW4[3]], sem="w4d")
        hT4 = A5.alloc([8, 512], BF16)
        BhT4 = Buf()
        xq = [A5.alloc([1024], F32) for _ in range(4)]
        Bxq = [Buf() for _ in range(4)]
        xn4 = [A5.alloc([1024], BF16) for _ in range(2)]
        Bxn4 = [Buf() for _ in range(2)]
        mg = A5.alloc([8, 512], BF16)
        Bmg = Buf()
        g1bc = A5.alloc([1024], F32)
        g2bc = A5.alloc([1024], F32)
        Bgg = Buf()
        dma("sync", lambda h: h.dma_start(out=g1bc, in_=g1_d.partition_broadcast(128)), w=[Bgg], sem="g1l")
        Bgg2 = Buf()
        dma("sync", lambda h: h.dma_start(out=g2bc, in_=g2_d.partition_broadcast(128)), w=[Bgg2], sem="g2l")
        junk4 = A5.alloc([1024], BF16)
        stat4 = A5.alloc([64], F32)
        s12 = [[A5.alloc([512], BF16) for _ in range(2)] for _ in range(2)]
        t12 = [[A5.alloc([512], BF16) for _ in range(2)] for _ in range(2)]
        Bs12 = [Buf(), Buf()]
        x2t = [A5.alloc([1024], F32) for _ in range(2)]
        Bx2 = [Buf(), Buf()]
        hn32 = [A5.alloc([1024], F32) for _ in range(2)]
        Bhn32 = [Buf(), Buf()]
        hn16 = [A5.alloc([1024], BF16) for _ in range(2)]
        Bhn16 = [Buf(), Buf()]
        hnT = A5.alloc([8, 128], F32)
        BhnT = Buf()
        Wr = A5.alloc([8, 72], F32)
        BWr = Buf()
        dma("sync", lambda h: h.dma_start(out=Wr, in_=wr_d.rearrange("(kc p) n -> p kc n", p=128)), w=[BWr], sem="wrl")
        brb = A5.alloc([72], F32)
        Bbr = Buf()
        dma("sync", lambda h: h.dma_start(out=brb, in_=br_d.partition_broadcast(128)), w=[Bbr], sem="brl")
        Lg = A5.alloc([72], F32)
        rs = A5.alloc([512], F32)
        Brs = Buf()
        Cacc = A5.alloc([64], F32)
        BCacc = Buf()
        vop(lambda h: h.memset(Cacc, 0.0), w=[BCacc])
        Bxpad = Buf()
        Bx2d = Buf()

        def norm4(xsrc_ap, xt_ap, bxt, xn_ap, bxn, col, gb, bg, qsem):
            dma("sync", lambda h: h.dma_start(out=xt_ap, in_=xsrc_ap), w=[bxt], sem=qsem)
            aop(lambda h: h.activation(out=junk4, in_=xt_ap, func=AF.Square, accum_out=stat4[:, col:col + 1]), r=[bxt], w=[Bjunk, Bstat])
            aop(lambda h: h.activation(out=stat4[:, col + 1:col + 2], in_=stat4[:, col:col + 1], func=AF.Sqrt, scale=1.0 / 1024, bias=1e-6), r=[Bstat], w=[Bstat])
            vop(lambda h: h.reciprocal(out=stat4[:, col + 2:col + 3], in_=stat4[:, col + 1:col + 2]), r=[Bstat], w=[Bstat])
            vop(lambda h: h.scalar_tensor_tensor(out=xn_ap, in0=xt_ap, scalar=stat4[:, col + 2:col + 3], in1=gb, op0=ALU.mult, op1=ALU.mult), r=[bxt, Bstat, bg], w=[bxn])

        tcount = 0
        for tb in range(4):
            for t in range(4):
                tg = tb * 4 + t
                ni_ = t % 2
                col = (tg % 8) * 4
                norm4(x_d[tg * 128:(tg + 1) * 128, :], xq[t], Bxq[t], xn4[ni_], Bxn4[ni_], col, g1bc, Bgg, "xq%d" % t)
                bk = ni_
                for kc in range(8):
                    pop(lambda h, kc=kc, bk=bk, ni_=ni_: h.transpose(out=PSB[bk][:, kc * 128:(kc + 1) * 128], in_=xn4[ni_][:, kc * 128:(kc + 1) * 128], identity=k16("ident")), r=[Bxn4[ni_], Bc16], w=[PB[bk]])
                aop(lambda h, t=t, bk=bk: h.activation(out=hT4[:, :, t * 128:(t + 1) * 128], in_=PSB[bk].rearrange("p (a b) -> p a b", b=128), func=AF.Copy), r=[PB[bk]], w=[BhT4])
            tsl = slice(tb * 512, (tb + 1) * 512)
            for dc in range(8):
                pz = dc % 2
                bA, bB, bC, bD = (0, 1, 2, 3) if pz == 0 else (4, 5, 6, 7)
                dsl = slice(dc * 128, (dc + 1) * 128)
                dsl2 = slice(1024 + dc * 128, 1024 + (dc + 1) * 128)
                for q in range(4):
                    pop(lambda h, q=q, bA=bA, dsl=dsl, tsl=tsl: h.matmul(PSF[bA], lhsT=Wfo[:, q, dsl], rhs=FT[:, q, tsl], start=(q == 0), stop=(q == 3)), r=[BW4[0], BFT], w=[PB[bA]])
                for q in range(4):
                    pop(lambda h, q=q, bB=bB, dsl=dsl, tsl=tsl: h.matmul(PSF[bB], lhsT=Wso[:, q, dsl], rhs=yT[:, q, tsl], start=(q == 0), stop=(q == 3)), r=[BW4[1], ByT], w=[PB[bB]])
                for kc in range(8):
                    pop(lambda h, kc=kc, bC=bC, dsl=dsl: h.matmul(PSF[bC], lhsT=Wg_[:, kc, dsl], rhs=hT4[:, kc, :], start=(kc == 0), stop=(kc == 7)), r=[BW4[2], BhT4], w=[PB[bC]])
                for kc in range(8):
                    pop(lambda h, kc=kc, bD=bD, dsl2=dsl2: h.matmul(PSF[bD], lhsT=Wg_[:, kc, dsl2], rhs=hT4[:, kc, :], start=(kc == 0), stop=(kc == 7)), r=[BW4[2], BhT4], w=[PB[bD]])
                aop(lambda h, pz=pz, bC=bC: h.activation(out=s12[pz][0], in_=PSF[bC], func=AF.Sigmoid), r=[PB[bC]], w=[Bs12[pz]])
                aop(lambda h, pz=pz, bD=bD: h.activation(out=s12[pz][1], in_=PSF[bD], func=AF.Sigmoid), r=[PB[bD]], w=[Bs12[pz]])
                vop(lambda h, pz=pz, bA=bA: h.tensor_tensor(out=t12[pz][0], in0=PSF[bA], in1=s12[pz][0], op=ALU.mult), r=[PB[bA], Bs12[pz]], w=[Bs12[pz]])
                vop(lambda h, pz=pz, bB=bB: h.tensor_tensor(out=t12[pz][1], in0=PSF[bB], in1=s12[pz][1], op=ALU.mult), r=[PB[bB], Bs12[pz]], w=[Bs12[pz]])
                gop(lambda h, pz=pz, dc=dc: h.tensor_tensor(out=mg[:, dc, :], in0=t12[pz][0], in1=t12[pz][1], op=ALU.add), r=[Bs12[pz]], w=[Bmg])
            for t in range(4):
                tg = tb * 4 + t
                u = tg % 2
                bk0, bk1 = (0, 1) if u == 0 else (2, 3)
                for hh, bk in ((0, bk0), (1, bk1)):
                    for dc in range(8):
                        pop(lambda h, dc=dc, hh=hh, bk=bk, t=t: h.matmul(PSF[bk], lhsT=mg[:, dc, t * 128:(t + 1) * 128], rhs=Wout[:, dc, hh * 512:(hh + 1) * 512], start=(dc == 0), stop=(dc == 7)), r=[Bmg, BW4[3]], w=[PB[bk]])
                for hh, bk in ((0, bk0), (1, bk1)):
                    vop(lambda h, hh=hh, bk=bk, t=t, u=u: h.tensor_tensor(out=x2t[u][:, hh * 512:(hh + 1) * 512], in0=PSF[bk], in1=xq[t][:, hh * 512:(hh + 1) * 512], op=ALU.add), r=[PB[bk], Bxq[t]], w=[Bx2[u]])
                if STAGE == 1:
                    dma("sync", lambda h, tg=tg, u=u: h.dma_start(out=out_d[tg * 128:(tg + 1) * 128, :], in_=x2t[u]), r=[Bx2[u]], sem="os%d" % u)
                    continue
                dma("sync", lambda h, tg=tg, u=u: h.dma_start(out=x2_d[tg * 128:(tg + 1) * 128, :], in_=x2t[u]), r=[Bx2[u]], w=[Bx2d], sem="x2s%d" % u)
                col = 32 + (tg % 4) * 4
                aop(lambda h, u=u, col=col: h.activation(out=junk4, in_=x2t[u], func=AF.Square, accum_out=stat4[:, col:col + 1]), r=[Bx2[u]], w=[Bjunk, Bstat])
                aop(lambda h, col=col: h.activation(out=stat4[:, col + 1:col + 2], in_=stat4[:, col:col + 1], func=AF.Sqrt, scale=1.0 / 1024, bias=1e-6), r=[Bstat], w=[Bstat])
                vop(lambda h, col=col: h.reciprocal(out=stat4[:, col + 2:col + 3], in_=stat4[:, col + 1:col + 2]), r=[Bstat], w=[Bstat])
                vop(lambda h, u=u, col=col: h.scalar_tensor_tensor(out=hn32[u], in0=x2t[u], scalar=stat4[:, col + 2:col + 3], in1=g2bc, op0=ALU.mult, op1=ALU.mult), r=[Bx2[u], Bstat, Bgg2], w=[Bhn32[u]])
                gop(lambda h, u=u: h.tensor_copy(out=hn16[u], in_=hn32[u]), r=[Bhn32[u]], w=[Bhn16[u]])
                for kc in range(8):
                    bk = 4 + kc // 4
                    o = (kc % 4) * 128
                    pop(lambda h, kc=kc, bk=bk, o=o, u=u: h.transpose(out=PSF[bk][:, o:o + 128], in_=hn32[u][:, kc * 128:(kc + 1) * 128], identity=k32("ident")), r=[Bhn32[u], Bc32], w=[PB[bk]])
                aop(lambda h: h.activation(out=flat(hnT[:, 0:4, :]), in_=PSF[4], func=AF.Copy), r=[PB[4]], w=[BhnT])
                vop(lambda h: h.tensor_copy(out=flat(hnT[:, 4:8, :]), in_=PSF[5]), r=[PB[5]], w=[BhnT])
                for kc in range(8):
                    pop(lambda h, kc=kc: h.matmul(PSF[6][:, 0:72], lhsT=hnT[:, kc, :], rhs=Wr[:, kc, :], start=(kc == 0), stop=(kc == 7)), r=[BhnT, BWr], w=[PB[6]])
                R = [Brs]
                L8, L64 = Lg[:, 0:8], Lg[:, 8:72]
                m8, ohg, negm, ex, sumg, pg = rs[:, 0:8], rs[:, 8:16], rs[:, 16:17], rs[:, 24:32], rs[:, 17:18], rs[:, 18:19]
                tmp64, esel, m8e = rs[:, 64:128], rs[:, 32:40], rs[:, 40:48]
                dv, w1 = rs[:, 19:20], rs[:, 20:21]
                mk1, mk2, msk, slot = rs[:, 128:192], rs[:, 192:256], rs[:, 256:320], rs[:, 320:384]
                idf = rs[:, 48:50]
                vop(lambda h: h.tensor_tensor(out=Lg, in0=PSF[6][:, 0:72], in1=brb, op=ALU.add), r=[PB[6], Bbr], w=R)
                vop(lambda h: h.max(out=m8, in_=L8), r=R, w=R)
                vop(lambda h: h.tensor_scalar(out=ohg, in0=L8, scalar1=m8[:, 0:1], scalar2=None, op0=ALU.is_equal), r=R, w=R)
                vop(lambda h: h.tensor_scalar(out=negm, in0=m8[:, 0:1], scalar1=-1.0, scalar2=None, op0=ALU.mult), r=R, w=R)
                aop(lambda h: h.activation(out=ex, in_=L8, func=AF.Exp, bias=negm, scale=1.0, accum_out=sumg), r=R, w=R)
                vop(lambda h: h.reciprocal(out=pg, in_=sumg), r=R, w=R)
                vop(lambda h: h.tensor_tensor(out=tmp64.rearrange("p (g j) -> p g j", j=8), in0=L64.rearrange("p (g j) -> p g j", j=8), in1=ohg.unsqueeze(2).to_broadcast([128, 8, 8]), op=ALU.mult), r=R, w=R)
                vop(lambda h: h.tensor_reduce(out=esel, in_=tmp64.rearrange("p (g j) -> p j g", j=8), axis=AX.X, op=ALU.add), r=R, w=R)
                vop(lambda h: h.max(out=m8e, in_=esel), r=R, w=R)
                vop(lambda h: h.tensor_tensor(out=dv, in0=m8e[:, 1:2], in1=m8e[:, 0:1], op=ALU.subtract), r=R, w=R)
                aop(lambda h: h.activation(out=w1, in_=dv, func=AF.Sigmoid, scale=-1.0), r=R, w=R)
                vop(lambda h, tg=tg: h.tensor_tensor(out=gw[:, tg, 0:1], in0=pg, in1=w1, op=ALU.mult), r=R, w=[Bgw])
                vop(lambda h, tg=tg: h.tensor_tensor(out=gw[:, tg, 1:2], in0=pg, in1=gw[:, tg, 0:1], op=ALU.subtract), r=R + [Bgw], w=[Bgw])
                ohb = ohg.unsqueeze(2).to_broadcast([128, 8, 8])
                vop(lambda h: h.tensor_scalar(out=mk1, in0=L64, scalar1=m8e[:, 0:1], scalar2=None, op0=ALU.is_equal), r=R, w=R)
                vop(lambda h, ohb=ohb: h.tensor_tensor(out=mk1.rearrange("p (g j) -> p g j", j=8), in0=mk1.rearrange("p (g j) -> p g j", j=8), in1=ohb, op=ALU.mult), r=R, w=R)
                vop(lambda h: h.tensor_scalar(out=mk2, in0=L64, scalar1=m8e[:, 1:2], scalar2=None, op0=ALU.is_equal), r=R, w=R)
                vop(lambda h, ohb=ohb: h.tensor_tensor(out=mk2.rearrange("p (g j) -> p g j", j=8), in0=mk2.rearrange("p (g j) -> p g j", j=8), in1=ohb, op=ALU.mult), r=R, w=R)
                vop(lambda h: h.tensor_tensor(out=msk, in0=mk1, in1=mk2, op=ALU.add), r=R, w=R)
                pop(lambda h: h.matmul(PSF[7][:, 0:64], lhsT=k32("triS"), rhs=msk, start=True, stop=False), r=R + [Bc32], w=[PB[7]])
                pop(lambda h: h.matmul(PSF[7][:, 0:64], lhsT=k32("ones"), rhs=Cacc, start=False, stop=True), r=[BCacc, Bc32], w=[PB[7]])
                vop(lambda h: h.tensor_tensor(out=slot, in0=PSF[7][:, 0:64], in1=k32("ecap", 64), op=ALU.add), r=[PB[7], Bc32], w=R)
                vop(lambda h: h.tensor_tensor(out=Cacc, in0=Cacc, in1=msk, op=ALU.add), r=R + [BCacc], w=[BCacc])
                vop(lambda h: h.tensor_tensor(out=mk1, in0=mk1, in1=slot, op=ALU.mult), r=R, w=R)
                vop(lambda h: h.reduce_sum(out=idf[:, 0:1], in_=mk1, axis=AX.X), r=R, w=R)
                vop(lambda h: h.tensor_tensor(out=mk2, in0=mk2, in1=slot, op=ALU.mult), r=R, w=R)
                vop(lambda h: h.reduce_sum(out=idf[:, 1:2], in_=mk2, axis=AX.X), r=R, w=R)
                vop(lambda h, tg=tg: h.tensor_copy(out=idx[:, tg, :], in_=idf), r=R, w=[Bidx])
                for kk in range(2):
                    dma("gpsimd", lambda h, tg=tg, kk=kk, u=u: h.indirect_dma_start(out=xpad_d, out_offset=bass.IndirectOffsetOnAxis(ap=idx[:, tg, kk:kk + 1], axis=0), in_=hn16[u], in_offset=None, bounds_check=breg(h), oob_is_err=False), r=[Bhn16[u], Bidx], w=[Bxpad], sem="scat")
        S.barrier()
        if STAGE >= 2:
            A6 = Arena(arena_t[:, persist0:ARENA_BYTES], ARENA_BYTES - persist0)
            NWB = 2
            Wge = [A6.alloc([8, 512], BF16) for _ in range(NWB)]
            Wue = [A6.alloc([8, 512], BF16) for _ in range(NWB)]
            Wde = [A6.alloc([4, 1024], BF16) for _ in range(NWB)]
            Sge = [A6.alloc([8, 512], F32) for _ in range(NWB)]
            Sue = [A6.alloc([8, 512], F32) for _ in range(NWB)]
            Sde = [A6.alloc([4, 1024], F32) for _ in range(NWB)]
            BSge = [Buf() for _ in range(NWB)]
            BSue = [Buf() for _ in range(NWB)]
            BSde = [Buf() for _ in range(NWB)]
            BWge = [Buf() for _ in range(NWB)]
            BWue = [Buf() for _ in range(NWB)]
            BWde = [Buf() for _ in range(NWB)]
            Xe = [A6.alloc([1024], BF16) for _ in range(3)]
            BXe = [Buf(), Buf(), Buf()]
            XeT = [A6.alloc([8, 128], BF16) for _ in range(2)]
            BXeT = [Buf(), Buf()]
            Gs = [A6.alloc([512], BF16) for _ in range(2)]
            BGs = [Buf(), Buf()]
            Aa = [A6.alloc([512], BF16) for _ in range(2)]
            BAa = [Buf(), Buf()]
            AT = [A6.alloc([4, 128], BF16) for _ in range(2)]
            BAT = [Buf(), Buf()]
            Ye = [A6.alloc([1024], F32) for _ in range(4)]
            BYe = [Buf() for _ in range(4)]
            Bypad = Buf()
            NXE = 3

            def issue_loads(e):
                wb = e % NWB
                dma("sync", lambda h, e=e, wb=wb: h.dma_start(out=Sge[wb], in_=ewg_d[e].rearrange("(p kc) n -> p kc n", kc=8)), w=[BSge[wb]], sem="wg%d" % wb)
                dma("sync", lambda h, e=e, wb=wb: h.dma_start(out=Sue[wb], in_=ewu_d[e].rearrange("(p kc) n -> p kc n", kc=8)), w=[BSue[wb]], sem="wu%d" % wb)
                dma("sync", lambda h, e=e, wb=wb: h.dma_start(out=Sde[wb], in_=ewd_d[e].rearrange("(p hc) n -> p hc n", hc=4)), w=[BSde[wb]], sem="wd%d" % wb)

            def issue_x(e):
                xb = e % NXE
                dma("scalar", lambda h, e=e, xb=xb: h.dma_start(out=Xe[xb], in_=xpad_d[e * CAP:(e + 1) * CAP, :]), r=[Bxpad], w=[BXe[xb]], sem="xe%d" % xb)

            issue_x(0)
            for e in range(min(NWB, MOE_LIMIT)):
                issue_loads(e)
            for e in range(MOE_LIMIT):
                wb = e % NWB
                u = e % 2
                xb = e % NXE
                if e + 1 < MOE_LIMIT:
                    issue_x(e + 1)
                aop(lambda h, wb=wb: h.activation(out=flat(Wge[wb]), in_=flat(Sge[wb]), func=AF.Copy), r=[BSge[wb]], w=[BWge[wb]])
                gop(lambda h, wb=wb: h.tensor_copy(out=flat(Wue[wb]), in_=flat(Sue[wb])), r=[BSue[wb]], w=[BWue[wb]])
                aop(lambda h, wb=wb: h.activation(out=flat(Wde[wb]), in_=flat(Sde[wb]), func=AF.Copy), r=[BSde[wb]], w=[BWde[wb]])
                if e + NWB < MOE_LIMIT:
                    issue_loads(e + NWB)
                bkT = 0 if u == 0 else 4
                for kc in range(8):
                    pop(lambda h, kc=kc, xb=xb, bkT=bkT: h.transpose(out=PSB[bkT][:, kc * 128:(kc + 1) * 128], in_=Xe[xb][:, kc:1024:8], identity=k16("ident")), r=[BXe[xb], Bc16], w=[PB[bkT]])
                vop(lambda h, u=u, bkT=bkT: h.tensor_copy(out=flat(XeT[u]), in_=PSB[bkT]), r=[PB[bkT]], w=[BXeT[u]])
                bG, bU = (1, 2) if u == 0 else (5, 6)
                for kc in range(8):
                    pop(lambda h, kc=kc, u=u, wb=wb, bG=bG: h.matmul(PSF[bG], lhsT=XeT[u][:, kc, :], rhs=Wge[wb][:, kc, :], start=(kc == 0), stop=(kc == 7)), r=[BXeT[u], BWge[wb]], w=[PB[bG]])
                for kc in range(8):
                    pop(lambda h, kc=kc, u=u, wb=wb, bU=bU: h.matmul(PSF[bU], lhsT=XeT[u][:, kc, :], rhs=Wue[wb][:, kc, :], start=(kc == 0), stop=(kc == 7)), r=[BXeT[u], BWue[wb]], w=[PB[bU]])
                aop(lambda h, u=u, bG=bG: h.activation(out=Gs[u], in_=PSF[bG], func=AF.Silu), r=[PB[bG]], w=[BGs[u]])
                vop(lambda h, u=u, bU=bU: h.tensor_tensor(out=Aa[u], in0=PSF[bU], in1=Gs[u], op=ALU.mult), r=[PB[bU], BGs[u]], w=[BAa[u]])
                bA = 3 if u == 0 else 7
                for hc in range(4):
                    pop(lambda h, hc=hc, u=u, bA=bA: h.transpose(out=PSB[bA][:, hc * 128:(hc + 1) * 128], in_=Aa[u][:, hc:512:4], identity=k16("ident")), r=[BAa[u], Bc16], w=[PB[bA]])
                aop(lambda h, u=u, bA=bA: h.activation(out=flat(AT[u]), in_=PSB[bA][:, 0:512], func=AF.Copy), r=[PB[bA]], w=[BAT[u]])
                for hh, bk in ((0, bG), (1, bU)):
                    for hc in range(4):
                        pop(lambda h, hc=hc, hh=hh, bk=bk, u=u, wb=wb: h.matmul(PSF[bk], lhsT=AT[u][:, hc, :], rhs=Wde[wb][:, hc, hh * 512:(hh + 1) * 512], start=(hc == 0), stop=(hc == 3)), r=[BAT[u], BWde[wb]], w=[PB[bk]])
                yb = e % 4
                vop(lambda h, yb=yb, bG=bG: h.tensor_copy(out=Ye[yb][:, 0:512], in_=PSF[bG]), r=[PB[bG]], w=[BYe[yb]])
                aop(lambda h, yb=yb, bU=bU: h.activation(out=Ye[yb][:, 512:1024], in_=PSF[bU], func=AF.Copy), r=[PB[bU]], w=[BYe[yb]])
                dma("scalar", lambda h, e=e, yb=yb: h.dma_start(out=ypad_d[e * CAP:(e + 1) * CAP, :], in_=Ye[yb]), r=[BYe[yb]], w=[Bypad], sem="ys%d" % yb)
            S.barrier()
            A6.release(0)
            gfbc = A6.alloc([1024], F32)
            Bgf = Buf()
            dma("sync", lambda h: h.dma_start(out=gfbc, in_=gf_d.partition_broadcast(128)), w=[Bgf], sem="gfl")
            NF = 4
            y1 = [A6.alloc([1024], F32) for _ in range(NF)]
            y2 = [A6.alloc([1024], F32) for _ in range(NF)]
            xx = [A6.alloc([1024], F32) for _ in range(NF)]
            oo = [A6.alloc([1024], F32) for _ in range(2)]
            By1 = [Buf() for _ in range(NF)]
            By2 = [Buf() for _ in range(NF)]
            Bxx = [Buf() for _ in range(NF)]
            Boo = [Buf(), Buf()]
            junk6 = A6.alloc([1024], BF16)
            Bj6 = Buf()
            st6 = A6.alloc([64], F32)
            Bst6 = Buf()

            def fetch6(tg):
                f = tg % NF
                dma("gpsimd", lambda h, tg=tg, f=f: h.indirect_dma_start(out=y1[f], out_offset=None, in_=ypad_d, in_offset=bass.IndirectOffsetOnAxis(ap=idx[:, tg, 0:1], axis=0), bounds_check=breg(h), oob_is_err=False), r=[Bypad, Bidx], w=[By1[f]], sem="ga%d" % f)
                dma("gpsimd", lambda h, tg=tg, f=f: h.indirect_dma_start(out=y2[f], out_offset=None, in_=ypad_d, in_offset=bass.IndirectOffsetOnAxis(ap=idx[:, tg, 1:2], axis=0), bounds_check=breg(h), oob_is_err=False), r=[Bypad, Bidx], w=[By2[f]], sem="gb%d" % f)
                dma("sync", lambda h, tg=tg, f=f: h.dma_start(out=xx[f], in_=x2_d[tg * 128:(tg + 1) * 128, :]), r=[Bx2d], w=[Bxx[f]], sem="xl%d" % f)

            for tg in range(NF):
                fetch6(tg)
            for tg in range(16):
                u = tg % 2
                f = tg % NF
                vop(lambda h, tg=tg, f=f: h.scalar_tensor_tensor(out=xx[f], in0=y1[f], scalar=gw[:, tg, 0:1], in1=xx[f], op0=ALU.mult, op1=ALU.add), r=[By1[f], Bgw, Bxx[f]], w=[Bxx[f]])
                vop(lambda h, tg=tg, f=f: h.scalar_tensor_tensor(out=xx[f], in0=y2[f], scalar=gw[:, tg, 1:2], in1=xx[f], op0=ALU.mult, op1=ALU.add), r=[By2[f], Bgw, Bxx[f]], w=[Bxx[f]])
                col = (tg % 8) * 4
                aop(lambda h, f=f, col=col: h.activation(out=junk6, in_=xx[f], func=AF.Square, accum_out=st6[:, col:col + 1]), r=[Bxx[f]], w=[Bj6, Bst6])
                aop(lambda h, col=col: h.activation(out=st6[:, col + 1:col + 2], in_=st6[:, col:col + 1], func=AF.Sqrt, scale=1.0 / 1024, bias=1e-6), r=[Bst6], w=[Bst6])
                vop(lambda h, col=col: h.reciprocal(out=st6[:, col + 2:col + 3], in_=st6[:, col + 1:col + 2]), r=[Bst6], w=[Bst6])
                vop(lambda h, u=u, f=f, col=col: h.scalar_tensor_tensor(out=oo[u], in0=xx[f], scalar=st6[:, col + 2:col + 3], in1=gfbc, op0=ALU.mult, op1=ALU.mult), r=[Bxx[f], Bst6, Bgf], w=[Boo[u]])
                dma("sync", lambda h, tg=tg, u=u: h.dma_start(out=out_d[tg * 128:(tg + 1) * 128, :], in_=oo[u]), r=[Boo[u]], sem="os%d" % u)
                if tg + NF < 16:
                    fetch6(tg + NF)
        S.barrier()
        S.run()
    return nc, dbg_outs


def _consts():
    bf = ml_dtypes.bfloat16
    p = np.arange(128)
    c16 = np.zeros((128, C16["n"]), np.float32)
    c16[:, C16["ident"]:C16["ident"] + 128] = np.eye(128)
    c16[:, C16["triL"]:C16["triL"] + 128] = (p[:, None] <= p[None, :])
    c16[:, C16["triU"]:C16["triU"] + 128] = (p[:, None] >= p[None, :])
    c16[127, C16["selL"]:C16["selL"] + 128] = 1.0
    c16[0, C16["selF"]:C16["selF"] + 128] = 1.0
    ang = 2.0 * np.pi * ((p[:, None] * p[None, :]) % 128) / 128.0
    c16[:, C16["CC"]:C16["CC"] + 128] = np.cos(ang)
    c16[:, C16["SS"]:C16["SS"] + 128] = np.sin(ang)
    c32 = np.zeros((128, C32["n"]), np.float32)
    c32[:, C32["ident"]:C32["ident"] + 128] = np.eye(128)
    c32[:, C32["triS"]:C32["triS"] + 128] = (p[:, None] < p[None, :])
    c32[:, C32["ones"]:C32["ones"] + 128] = 1.0
    jj = p // 16
    c32[:, C32["mask0"]:C32["mask0"] + 128] = (jj[None, :] >= jj[:, None])
    c32[:, C32["mask1"]:C32["mask1"] + 128] = (jj[:, None] >= jj[None, :])
    c32[:, C32["kf"]] = p + 1
    c32[:, C32["kb"]] = 128 - p
    c32[:, C32["ev"]:C32["ev"] + 17] = np.arange(-8, 9)[None, :]
    c32[:, C32["evr"]:C32["evr"] + 17] = np.arange(8, -9, -1)[None, :]
    c32[:, C32["ecap"]:C32["ecap"] + 64] = (np.arange(64) * CAP)[None, :]
    c32[:, C32["iota"]:C32["iota"] + 128] = p[None, :]
    return c16.astype(bf), c32


def _dft_tables(hf):
    bf = ml_dtypes.bfloat16
    L = 4096
    base = np.arange(L, dtype=np.float64) * (2.0 * np.pi / L)
    sc = 1.0 / math.sqrt(L * 128.0)
    cosb = (np.cos(base) * sc).astype(np.float32)
    sinb = (-np.sin(base) * sc).astype(np.float32)
    t = np.arange(L, dtype=np.int64)
    m = np.arange(L // 2, dtype=np.int64)
    l = t if hf == 0 else (L - 1 - t)
    k = m if hf == 0 else (L - 1 - m)
    prod = (l[:, None] * k[None, :]) % L

    def lay(t):
        return np.ascontiguousarray(t.reshape(32, 128, 8, 256).transpose(2, 1, 0, 3))

    return lay(cosb[prod].astype(bf)), lay(sinb[prod].astype(bf))


_CACHE = {}


def kernel(**inp):
    f32 = np.float32
    x = np.asarray(inp["x"], f32)
    if "nc" not in _CACHE:
        _CACHE["nc"] = build_program()
        _CACHE["c"] = _consts()
        _CACHE["tab"] = [_dft_tables(0), _dft_tables(1)]
    nc, dbg_outs = _CACHE["nc"]
    c16, c32 = _CACHE["c"]
    shared = {
        "mix_norm_g": np.ascontiguousarray(inp["mix_norm_g"][0], f32),
        "w_in": np.ascontiguousarray(inp["w_in"][0], f32),
        "w_fourier_out": np.ascontiguousarray(inp["w_fourier_out"][0], f32),
        "ssm_D": np.ascontiguousarray(inp["ssm_D"][0], f32),
        "ssm_w_glu": np.ascontiguousarray(inp["ssm_w_glu"][0], f32),
        "w_ssm_out": np.ascontiguousarray(inp["w_ssm_out"][0], f32),
        "w_out": np.ascontiguousarray(inp["w_out"][0], f32),
        "ffn_norm_g": np.ascontiguousarray(inp["ffn_norm_g"][0], f32),
        "w_router": np.ascontiguousarray(np.concatenate([inp["router_group_w"][0], inp["router_expert_w"][0]], axis=1), f32),
        "b_router": np.ascontiguousarray(np.concatenate([inp["router_group_b"][0], inp["router_expert_b"][0]], axis=0), f32),
        "final_norm_g": np.ascontiguousarray(inp["final_norm_g"], f32),
        "cst16": c16,
        "cst32": c32,
    }
    if STAGE >= 2:
        shared["expert_w_gate"] = np.ascontiguousarray(inp["expert_w_gate"][0], f32)
        shared["expert_w_up"] = np.ascontiguousarray(inp["expert_w_up"][0], f32)
        shared["expert_w_down"] = np.ascontiguousarray(inp["expert_w_down"][0], f32)
    in_maps = []
    for c in range(8):
        b, hf = c // 2, c % 2
        dsel = [0, 1] if hf == 0 else [1, 0]
        xl = x[b] if hf == 0 else x[b][::-1]
        m = dict(shared)
        m["x"] = np.ascontiguousarray(xl, f32)
        m["sA_re"] = np.ascontiguousarray(inp["ssm_A_re"][0][dsel], f32)
        m["sA_im"] = np.ascontiguousarray(inp["ssm_A_im"][0][dsel], f32)
        m["s_ldt"] = np.ascontiguousarray(inp["ssm_log_dt"][0][dsel], f32)
        m["sB_re"] = np.ascontiguousarray(inp["ssm_B_re"][0][dsel], f32)
        m["sB_im"] = np.ascontiguousarray(inp["ssm_B_im"][0][dsel], f32)
        m["sC_re"] = np.ascontiguousarray(inp["ssm_C_re"][0][dsel], f32)
        m["sC_im"] = np.ascontiguousarray(inp["ssm_C_im"][0][dsel], f32)
        m["tab_c"], m["tab_s"] = _CACHE["tab"][hf]
        in_maps.append(m)
    res = run_bass_kernel_spmd(nc, in_maps, core_ids=list(range(8)))
    out = np.empty((4, 4096, 1024), f32)
    for c in range(8):
        b, hf = c // 2, c % 2
        o = np.asarray(res.results[c]["out"], f32)
        if hf == 0:
            out[b, 0:2048] = o
        else:
            out[b, 2048:4096] = o[::-1]
    _CACHE["last"] = res
    return out
```
